# Optimizing a Trainium2 kernel written in Bass

```python
import jax, jax.numpy as jnp
from jax import lax
import numpy as np

D_MODEL = 1024
BATCH = 8
SEQ = 2048
DEPTH = 4

MIX_WIDTH = D_MODEL
A_HEADS = 4
A_DV = MIX_WIDTH // 2 // A_HEADS
A_DK = A_DV // 2
B_HEADS = 4
B_DV = MIX_WIDTH // 2 // B_HEADS
B_DK = B_DV // 2
A_QK = A_HEADS * A_DK
A_V = A_HEADS * A_DV
B_QK = B_HEADS * B_DK
B_V = B_HEADS * B_DV
GLA_RANK = 16
GLA_GATE_TEMP = 16.0
CONV_W = 3
D_FF = 2816
CHUNK = 64
EPS = 1e-6
IN_WIDTHS = (2 * A_QK, A_V, A_V, 4 * A_HEADS, B_QK, B_QK, B_V, B_V, 2 * GLA_RANK)
IN_COLS = 2 * A_QK + A_V + A_V + 4 * A_HEADS + B_QK + B_QK + B_V + B_V + 2 * GLA_RANK

kernel_name = "hybrid_mlstm_gla_convffn_encoder"


def rms_norm(x, g):
    xf = x.astype(jnp.float32)
    y = xf * lax.rsqrt(jnp.mean(xf * xf, axis=-1, keepdims=True) + EPS)
    return (y * g.astype(jnp.float32)).astype(x.dtype)


def head_rms_norm(y, g):
    y = y * lax.rsqrt(jnp.mean(y * y, axis=-1, keepdims=True) + EPS)
    return y * g.astype(jnp.float32)


def dwconv3(x, w, b):
    xp = jnp.pad(x, ((0, 0), (1, 1), (0, 0)))
    return xp[:, :-2] * w[0] + xp[:, 1:-1] * w[1] + xp[:, 2:] * w[2] + b


def flip(t):
    return t[:, ::-1]


def to_chunks(t):
    b, s, h = t.shape[:3]
    t = t.reshape((b, s // CHUNK, CHUNK, h) + t.shape[3:])
    return jnp.moveaxis(t, (1, 3), (0, 2))


def from_chunks(y):
    y = jnp.moveaxis(y, (0, 2), (1, 3))
    b, nc, l, h, d = y.shape
    return y.reshape(b, nc * l, h, d)


def mlstm_chunkwise(q, k, v, i_pre, log_f):
    bsz, _, h, dk = q.shape
    dv = v.shape[-1]
    causal = jnp.tril(jnp.ones((CHUNK, CHUNK), dtype=bool))

    def step(carry, inp):
        c_st, n_st, m_st = carry
        q_, k_, v_, i_, f_ = inp
        b = jnp.cumsum(f_, axis=-1)
        d_intra = jnp.where(causal, b[..., :, None] - b[..., None, :] + i_[..., None, :], -jnp.inf)
        m_inter = b + m_st[..., None]
        m_t = jnp.maximum(m_inter, jnp.max(d_intra, axis=-1))
        s = jnp.einsum('bhtd,bhsd->bhts', q_, k_) * jnp.exp(d_intra - m_t[..., None])
        inter = jnp.exp(m_inter - m_t)
        num = jnp.einsum('bhts,bhse->bhte', s, v_) + inter[..., None] * jnp.einsum('bhtd,bhde->bhte', q_, c_st)
        den = jnp.sum(s, axis=-1) + inter * jnp.einsum('bhtd,bhd->bht', q_, n_st)
        h_out = num / jnp.maximum(jnp.abs(den), jnp.exp(-m_t))[..., None]
        b_last = b[..., -1]
        g = b_last[..., None] - b + i_
        m_new = jnp.maximum(b_last + m_st, jnp.max(g, axis=-1))
        decay = jnp.exp(b_last + m_st - m_new)
        wk = jnp.exp(g - m_new[..., None])[..., None] * k_
        c_new = decay[..., None, None] * c_st + jnp.einsum('bhsd,bhse->bhde', wk, v_)
        n_new = decay[..., None] * n_st + jnp.sum(wk, axis=-2)
        return (c_new, n_new, m_new), h_out

    init = (jnp.zeros((bsz, h, dk, dv), jnp.float32),
            jnp.zeros((bsz, h, dk), jnp.float32),
            jnp.zeros((bsz, h), jnp.float32))
    _, hs = lax.scan(step, init, (to_chunks(q), to_chunks(k), to_chunks(v),
                                  to_chunks(i_pre), to_chunks(log_f)))
    return from_chunks(hs)


def gla_chunkwise(q, k, v, log_a):
    bsz, _, h, dk = q.shape
    dv = v.shape[-1]
    causal = jnp.tril(jnp.ones((CHUNK, CHUNK), dtype=bool))[..., None]

    def step(s_st, inp):
        q_, k_, v_, a_ = inp
        b = jnp.cumsum(a_, axis=-2)
        diff = b[..., :, None, :] - b[..., None, :, :]
        decay = jnp.exp(jnp.where(causal, diff, -jnp.inf))
        s = jnp.einsum('bhtd,bhsd,bhtsd->bhts', q_, k_, decay)
        o = jnp.einsum('bhts,bhse->bhte', s, v_) + jnp.einsum('bhtd,bhde->bhte', q_ * jnp.exp(b), s_st)
        b_last = b[..., -1:, :]
        k_dec = k_ * jnp.exp(b_last - b)
        s_new = jnp.exp(b_last[..., 0, :])[..., None] * s_st + jnp.einsum('bhsd,bhse->bhde', k_dec, v_)
        return s_new, o

    init = jnp.zeros((bsz, h, dk, dv), jnp.float32)
    _, os_ = lax.scan(step, init, (to_chunks(q), to_chunks(k), to_chunks(v), to_chunks(log_a)))
    return from_chunks(os_)


def hybrid_mixer(hn, w_in, mlstm_gate_b, mlstm_conv_w, mlstm_conv_b, mlstm_norm,
                 gla_w2, gla_b, gla_norm, w_out):
    bsz, s, _ = hn.shape
    proj = jnp.matmul(hn, w_in).astype(jnp.float32)
    idx = np.cumsum(np.array(IN_WIDTHS))[:-1].tolist()
    a_qk, a_v, a_o, a_gates, b_q, b_k, b_v, b_r, b_lr = jnp.split(proj, idx, axis=-1)

    qk = jax.nn.silu(dwconv3(a_qk, mlstm_conv_w.astype(jnp.float32), mlstm_conv_b.astype(jnp.float32)))
    q_a = qk[..., :A_QK].reshape(bsz, s, A_HEADS, A_DK)
    k_a = qk[..., A_QK:].reshape(bsz, s, A_HEADS, A_DK) * (A_DK ** -0.5)
    v_a = a_v.reshape(bsz, s, A_HEADS, A_DV)
    gates = a_gates + mlstm_gate_b.astype(jnp.float32)
    i_fw, f_fw, i_bw, f_bw = jnp.split(gates, 4, axis=-1)
    h_fw = mlstm_chunkwise(q_a, k_a, v_a, i_fw, jax.nn.log_sigmoid(f_fw))
    h_bw = flip(mlstm_chunkwise(flip(q_a), flip(k_a), flip(v_a), flip(i_bw), flip(jax.nn.log_sigmoid(f_bw))))
    y_a = jax.nn.sigmoid(a_o) * head_rms_norm(h_fw + h_bw, mlstm_norm).reshape(bsz, s, A_V)

    q_b = b_q.reshape(bsz, s, B_HEADS, B_DK) * (B_DK ** -0.5)
    k_b = b_k.reshape(bsz, s, B_HEADS, B_DK)
    v_b = b_v.reshape(bsz, s, B_HEADS, B_DV)
    lr_fw, lr_bw = jnp.split(b_lr, 2, axis=-1)
    w2 = gla_w2.astype(jnp.float32)
    gb = gla_b.astype(jnp.float32)
    la_fw = (jax.nn.log_sigmoid(lr_fw @ w2[0] + gb[0]) / GLA_GATE_TEMP).reshape(bsz, s, B_HEADS, B_DK)
    la_bw = (jax.nn.log_sigmoid(lr_bw @ w2[1] + gb[1]) / GLA_GATE_TEMP).reshape(bsz, s, B_HEADS, B_DK)
    o_fw = gla_chunkwise(q_b, k_b, v_b, la_fw)
    o_bw = flip(gla_chunkwise(flip(q_b), flip(k_b), flip(v_b), flip(la_bw)))
    y_b = jax.nn.silu(b_r) * head_rms_norm(o_fw + o_bw, gla_norm).reshape(bsz, s, B_V)

    y = jnp.concatenate([y_a, y_b], axis=-1).astype(hn.dtype)
    return jnp.matmul(y, w_out)


def conv_ffn(hn, w_gate, w_up, conv_w, conv_b, w_down):
    g = dwconv3(jnp.matmul(hn, w_gate), conv_w, conv_b)
    u = jnp.matmul(hn, w_up)
    return jnp.matmul(jax.nn.gelu(g, approximate=True) * u, w_down)


def setup_inputs(seed: int = 0) -> dict:
    key = jax.random.key(seed)
    ks = jax.random.split(key, 20)
    f32 = jnp.float32

    def nrm(k, shape, scale):
        return jax.random.normal(k, shape, f32) * scale

    def gain(k, shape):
        return 1.0 + 0.05 * jax.random.normal(k, shape, f32)

    f_base = jnp.linspace(3.0, 6.0, A_HEADS, dtype=f32)
    i_base = jnp.zeros((A_HEADS,), f32)
    gate_base = jnp.concatenate([i_base, f_base, i_base, f_base])
    return {
        "x": nrm(ks[0], (BATCH, SEQ, D_MODEL), 1.0),
        "norm_mix_pre": gain(ks[1], (DEPTH, D_MODEL)),
        "norm_mix_post": gain(ks[2], (DEPTH, D_MODEL)),
        "norm_ffn_pre": gain(ks[3], (DEPTH, D_MODEL)),
        "norm_ffn_post": gain(ks[4], (DEPTH, D_MODEL)),
        "w_in": nrm(ks[5], (DEPTH, D_MODEL, IN_COLS), D_MODEL ** -0.5),
        "mlstm_gate_b": gate_base + nrm(ks[6], (DEPTH, 4 * A_HEADS), 0.1),
        "mlstm_conv_w": nrm(ks[7], (DEPTH, CONV_W, 2 * A_QK), CONV_W ** -0.5),
        "mlstm_conv_b": nrm(ks[8], (DEPTH, 2 * A_QK), 0.02),
        "mlstm_norm": gain(ks[9], (DEPTH, A_HEADS, A_DV)),
        "gla_w2": nrm(ks[10], (DEPTH, 2, GLA_RANK, B_QK), GLA_RANK ** -0.5),
        "gla_b": nrm(ks[11], (DEPTH, 2, B_QK), 0.02),
        "gla_norm": gain(ks[12], (DEPTH, B_HEADS, B_DV)),
        "w_out": nrm(ks[13], (DEPTH, MIX_WIDTH, D_MODEL), MIX_WIDTH ** -0.5),
        "ffn_w_gate": nrm(ks[14], (DEPTH, D_MODEL, D_FF), D_MODEL ** -0.5),
        "ffn_w_up": nrm(ks[15], (DEPTH, D_MODEL, D_FF), D_MODEL ** -0.5),
        "ffn_conv_w": nrm(ks[16], (DEPTH, CONV_W, D_FF), CONV_W ** -0.5),
        "ffn_conv_b": nrm(ks[17], (DEPTH, D_FF), 0.02),
        "ffn_w_down": nrm(ks[18], (DEPTH, D_FF, D_MODEL), D_FF ** -0.5),
    }


def reference(x, norm_mix_pre, norm_mix_post, norm_ffn_pre, norm_ffn_post, w_in,
              mlstm_gate_b, mlstm_conv_w, mlstm_conv_b, mlstm_norm, gla_w2, gla_b, gla_norm,
              w_out, ffn_w_gate, ffn_w_up, ffn_conv_w, ffn_conv_b, ffn_w_down):
    for l in range(DEPTH):
        hn = rms_norm(x, norm_mix_pre[l])
        mix = hybrid_mixer(hn, w_in[l], mlstm_gate_b[l], mlstm_conv_w[l], mlstm_conv_b[l], mlstm_norm[l],
                           gla_w2[l], gla_b[l], gla_norm[l], w_out[l])
        x = x + rms_norm(mix, norm_mix_post[l])
        hn = rms_norm(x, norm_ffn_pre[l])
        ff = conv_ffn(hn, ffn_w_gate[l], ffn_w_up[l], ffn_conv_w[l], ffn_conv_b[l], ffn_w_down[l])
        x = x + rms_norm(ff, norm_ffn_post[l])
    return x
```

```python
import contextlib
import types
import numpy as np
import concourse.bass as bass
import concourse.mybir as mybir
from concourse.bass_utils import run_bass_kernel_spmd

F32 = mybir.dt.float32
BF16 = mybir.dt.bfloat16
ALU = mybir.AluOpType
AF = mybir.ActivationFunctionType

EPOCH = 30000
NCORES = 8
DEPTH = 4
D = 1024
S = 2048
NT = 16
NB = 4
KC = 8
FF = 2816
NF = 22
INC = 3120
EPS = 1e-6
PCOLS = 136
PBC = 1040


def _freeze(fn):
    if fn is None or fn.__closure__ is None:
        return fn
    cells = []
    for c in fn.__closure__:
        try:
            cells.append(types.CellType(c.cell_contents))
        except ValueError:
            cells.append(c)
    return types.FunctionType(fn.__code__, fn.__globals__, fn.__name__, fn.__defaults__, tuple(cells))


class _Op:
    __slots__ = ("eng", "fn", "R", "W", "dma", "deps", "marked", "tick", "dsem", "dtick", "dprev")

    def __init__(self, eng, fn, R, W, dma):
        self.eng = eng
        self.fn = fn
        self.R = tuple(R)
        self.W = tuple(W)
        self.dma = dma
        self.deps = ()
        self.marked = False
        self.tick = 0
        self.dsem = -1
        self.dtick = 0
        self.dprev = None


class Prog:
    ENGS = ("pe", "act", "dve", "pool", "sp")

    def __init__(self, n_dma_sems=16):
        self.ops = []
        self.nds = n_dma_sems

    def add(self, eng, fn, R=(), W=(), dma=False):
        self.ops.append(_Op(eng, _freeze(fn), R, W, dma))

    def pe(self, fn, R=(), W=()):
        self.add("pe", fn, R, W)

    def act(self, fn, R=(), W=()):
        self.add("act", fn, R, W)

    def dve(self, fn, R=(), W=()):
        self.add("dve", fn, R, W)

    def pool(self, fn, R=(), W=()):
        self.add("pool", fn, R, W)

    def dma(self, eng, fn, R=(), W=()):
        self.add(eng, fn, R, W, dma=True)

    def barrier(self):
        self.ops.append(_Op("__barrier__", None, (), (), False))

    def analyze(self):
        last_w = {}
        readers = {}
        last_on_eng = {e: None for e in self.ENGS}
        pend = {e: None for e in self.ENGS}
        out = []
        for op in self.ops:
            if op.eng == "__barrier__":
                snap = [v for v in last_on_eng.values() if v is not None]
                for e in self.ENGS:
                    pend[e] = snap
                continue
            g = len(out)
            deps = set()
            for k in op.R:
                if k in last_w:
                    deps.add(last_w[k])
            for k in op.W:
                if k in last_w:
                    deps.add(last_w[k])
                for r in readers.get(k, ()):
                    deps.add(r)
            if pend[op.eng] is not None:
                deps.update(pend[op.eng])
                pend[op.eng] = None
            for k in op.R:
                readers.setdefault(k, []).append(g)
            for k in op.W:
                last_w[k] = g
                readers[k] = []
            deps.discard(g)
            op.deps = tuple(sorted(deps))
            out.append(op)
            if op.fn is not None:
                last_on_eng[op.eng] = g
        self.lin = out
        ndma = 0
        nq = [0, 0]
        last_dma_on_sem = {}
        for g, op in enumerate(out):
            for d in op.deps:
                p = out[d]
                if p.dma:
                    continue
                if p.eng == "pe" and op.eng == "pe" and not op.dma:
                    continue
                p.marked = True
            if op.dma:
                half = self.nds // 2
                qi = 0 if op.eng == "sp" else 1
                s = qi * half + (nq[qi] % half)
                nq[qi] += 1
                ndma += 1
                op.dsem = s
                prev = last_dma_on_sem.get(s)
                op.dprev = prev
                op.dtick = (out[prev].dtick if prev is not None else 0) + 16
                last_dma_on_sem[s] = g
        cnt = {e: 0 for e in self.ENGS}
        for op in out:
            if op.marked and not op.dma:
                cnt[op.eng] += 1
                op.tick = cnt[op.eng]
        self.cnt = cnt
        self.ndma = ndma

    def emit(self, nc):
        self.analyze()
        out = self.lin
        with contextlib.ExitStack() as st:
            esems = {}
            for e in self.ENGS:
                nep = self.cnt[e] // EPOCH + 1
                esems[e] = [st.enter_context(nc.semaphore(f"s_{e}_{i}")) for i in range(nep)]
            dsems = [st.enter_context(nc.semaphore(f"s_dma_{i}")) for i in range(self.nds)]
            block = st.enter_context(nc.Block())
            by_eng = {e: [] for e in self.ENGS}
            for g, op in enumerate(out):
                by_eng[op.eng].append((g, op))

            def sem_of(p):
                if p.dma:
                    return ("d", p.dsem), dsems[p.dsem], p.dtick
                ep = (p.tick - 1) // EPOCH
                return (p.eng, ep), esems[p.eng][ep], p.tick - ep * EPOCH

            def run(eng_name, e):
                waited = {}
                for g, op in by_eng[eng_name]:
                    waits = {}
                    deps = list(op.deps)
                    if op.dma and op.dprev is not None:
                        deps.append(op.dprev)
                    for d in deps:
                        p = out[d]
                        if (not p.dma) and p.eng == "pe" and eng_name == "pe" and not op.dma:
                            continue
                        key, sem, val = sem_of(p)
                        if waited.get(key, 0) >= val:
                            continue
                        if key not in waits or waits[key][1] < val:
                            waits[key] = (sem, val)
                    for key, (sem, val) in waits.items():
                        e.wait_ge(sem, val)
                        waited[key] = val
                    if op.fn is None:
                        continue
                    ins = op.fn(e)
                    if op.dma:
                        ins.then_inc(dsems[op.dsem], 16)
                    elif op.marked:
                        ep = (op.tick - 1) // EPOCH
                        ins.then_inc(esems[eng_name][ep], 1)

            @block.tensor
            def _(e):
                run("pe", e)

            @block.scalar
            def _(e):
                run("act", e)

            @block.vector
            def _(e):
                run("dve", e)

            @block.gpsimd
            def _(e):
                run("pool", e)

            @block.sync
            def _(e):
                run("sp", e)


class _Stop(Exception):
    pass


def build_program(nl, dbg=None, stop_at=None):
    nc = bass.Bass("TRN2", target_bir_lowering=False)
    P = Prog()

    def din(name, shape):
        return nc.dram_tensor(name, shape, F32, kind="ExternalInput").ap()

    xT_d = din("xT", [D, S])
    w_in_d = din("w_in", [nl, D, INC])
    w_out_d = din("w_out", [nl, D, D])
    wg_d = din("wg", [nl, D, FF])
    wu_d = din("wu", [nl, D, FF])
    wd_d = din("wd", [nl, FF, D])
    pcol_d = din("pcol", [128, nl * PCOLS])
    pbc_d = din("pbc", [nl, PBC])
    w2_d = din("w2", [nl, 2, 16, 256])
    gb_d = din("gb", [nl, 512])
    outT_d = nc.dram_tensor("outT", [D, S], F32, kind="ExternalOutput").ap()
    dbg_d = {}
    if dbg:
        for k, shp in dbg.items():
            dbg_d[k] = nc.dram_tensor("dbg_" + k, shp, F32, kind="ExternalOutput").ap()

    st = contextlib.ExitStack()
    with st:
        def sb(name, shape, dt):
            return st.enter_context(nc.sbuf_tensor(name, shape, dt))

        def psum(name, shape, dt):
            return st.enter_context(nc.psum_tensor(name, shape, dt))

        xT = sb("xT_sb", [128, KC, S], F32)
        hnT = sb("hnT", [128, KC, S], BF16)
        pcol = sb("pcol_sb", [128, PCOLS], F32)
        pbc = sb("pbc_sb", [128, PBC], F32)
        w2bd = sb("w2bd", [128, 512], BF16)
        lrT = sb("lrT", [128, S], BF16)
        ones_bf = sb("ones_bf", [128, 128], BF16)
        negones_f = sb("negones_f", [128, 128], F32)
        ident = sb("ident", [128, 128], BF16)
        maskA = sb("maskA", [128, 2, 128], BF16)
        maskB = sb("maskB", [128, 2, 128], BF16)
        tri16 = sb("tri16", [128, 2, 128], BF16)
        trif = sb("trif", [128, 2, 128], F32)
        WB = 2048
        wbuf = [sb(f"wbuf{i}", [128, WB], BF16) for i in range(2)]
        A_YT = 0
        A_Q = A_YT + 6 * S
        A_K = A_Q + S
        A_2 = A_K + S
        A_3 = A_2 + 4096
        A_4 = A_3 + 4160
        A_5 = A_4 + 4096
        A_END = A_5 + 4100
        arena = sb("arena", [128, A_END], BF16)
        yT = arena[:, A_YT:A_Q].rearrange("p (c t) -> p c t", c=6)
        qT = arena[:, A_Q:A_K]
        kT = arena[:, A_K:A_2]
        kw = arena[:, A_2:A_3].rearrange("p (d i h e) -> p d i h e", d=2, i=NT, h=2)
        bv = arena[:, A_2:A_3].rearrange("p (i e) -> p i e", i=NT)
        vaug = arena[:, A_3:A_4].rearrange("p (i h e) -> p i h e", i=NT, h=2)
        sr = arena[:, A_3:A_3 + 4096].rearrange("p (i e) -> p i e", i=NT)
        sigo = arena[:, A_4:A_5].rearrange("p (i e) -> p i e", i=NT)
        la = arena[:, A_4:A_5].rearrange("p (i e) -> p i e", i=NT)
        hsum = arena[:, A_5:A_5 + 4096].rearrange("p (i e) -> p i e", i=NT)
        qkraw = arena[:, A_5:A_END].bitcast(F32)
        wo = arena[:, A_Q:A_Q + KC * D].rearrange("p (c n) -> p c n", c=KC)
        mixT = arena[:, A_Q + KC * D:A_Q + KC * D + 2 * KC * 512].bitcast(F32).rearrange("p (c t) -> p c t", c=KC)
        actT = arena[:, 0:NF * 1024].rearrange("p (f t) -> p f t", f=NF)
        ffT = arena[:, NF * 1024:NF * 1024 + 2 * KC * 512].bitcast(F32).rearrange("p (c t) -> p c t", c=KC)
        graw = arena[:, NF * 1024 + 2 * KC * 512:NF * 1024 + 2 * KC * 512 + 2 * 1026].bitcast(F32)
        assert NF * 1024 + 2 * KC * 512 + 2 * 1026 <= A_END

        gl = sb("gl", [128, NT, 16], F32)
        lf = sb("lf", [128, 2, NT * 4], F32)
        gtmp = sb("gtmp", [128, 2, NT * 4], F32)
        eb = sb("eb", [128, 2, NT, 4], F32)
        enb = sb("enb", [128, 2, NT, 4], F32)
        wsc = sb("wsc", [128, 2, NT, 4], F32)
        dec = sb("dec", [128, 2, NT, 4], F32)
        wdec = sb("wdec", [128, 2, NT, 4], F32)
        decsel = sb("decsel", [128, 2, NT], F32)
        Sm = sb("Sm", [128, 2, 2, 2, 128], BF16)
        C32 = sb("C32", [128, 2, 132], F32)
        Cbf = sb("Cbf", [128, 2, 2, 132], BF16)
        eBt = sb("eBt", [128, 2, 2, 128], F32)
        eNBt = sb("eNBt", [128, 2, 128], F32)
        qs = sb("qs", [128, 2, 2, 128], BF16)
        ks = sb("ks", [128, 2, 2, 128], BF16)
        ktok = sb("ktok", [128, 2, 2, 128], BF16)
        tmpS = sb("tmpS", [128, 2, 128], F32)
        osb = sb("osb", [128, 2, 260], F32)
        sml = sb("sml", [128, 3, 16], F32)
        sqb = sb("sqb", [128, 2, 512], BF16)
        rstd = sb("rstd", [128, 512], F32)
        acc = sb("acc", [128, 2, 512], F32)
        bsb = acc[:, 1, 0:256]
        gel = Sm[:].rearrange("p a b c t -> p (a b c t)").rearrange("p (b t) -> p b t", b=2)
        tot = acc[:, 0, 0:256]
        junk = sqb[:, 0, 0:256]
        ytile = sqb[:, 1, :].rearrange("p (b t) -> p b t", b=2)

        pb = [psum(f"pb{i}", [128, 512], F32) for i in range(6)]
        ptb = [psum(f"ptb{i}", [128, 1024], BF16) for i in range(2)]
        rot = {"n": 0, "t": 0}

        def bank():
            i = rot["n"] % 6
            rot["n"] += 1
            return pb[i], ("ps", i)

        def tbank():
            i = rot["t"] % 2
            rot["t"] += 1
            return ptb[i], ("pt", i)

        wrot = {"n": 0}

        def load_w(segs):
            i = wrot["n"] % 2
            wrot["n"] += 1
            views = []
            off = 0
            for src in segs:
                k, n = src.shape[1], src.shape[2]
                v = wbuf[i][:, off:off + k * n].rearrange("p (k n) -> p k n", k=k)
                P.dma("pool", lambda e, v=v, src=src: e.dma_start(out=v, in_=src), W=[("wbuf", i)])
                views.append(v)
                off += k * n
            assert off <= WB
            return views, ("wbuf", i)

        def wcols(wd3, l, a, n, kc=KC):
            return wd3[l, :, a:a + n].rearrange("(c p) n -> p c n", p=128)

        P.pool(lambda e: e.memset(ones_bf[:], 1.0), W=["ones_bf"])
        P.pool(lambda e: e.memset(negones_f[:], -1.0), W=["negones_f"])
        P.pool(lambda e: e.memset(ident[:], 0.0), W=["ident"])
        P.pool(lambda e: e.affine_select(out=ident[:], in_=ident[:], pattern=[[-1, 128]], compare_op=ALU.not_equal,
                                         fill=1.0, base=0, channel_multiplier=1), R=["ident"], W=["ident"])
        for t_, val in ((maskA, 0.125), (maskB, 1.0), (tri16, -1.0 / 16.0), (trif, -1.0)):
            P.pool(lambda e, t_=t_, val=val: e.memset(t_[:], val), W=[("const", id(t_))])
            P.pool(lambda e, t_=t_: e.affine_select(out=t_[:, 0, :], in_=t_[:, 0, :], pattern=[[1, 128]], compare_op=ALU.is_ge,
                                                    fill=0.0, base=0, channel_multiplier=-1), R=[("const", id(t_))], W=[("const", id(t_))])
            P.pool(lambda e, t_=t_: e.affine_select(out=t_[:, 1, :], in_=t_[:, 1, :], pattern=[[-1, 128]], compare_op=ALU.is_ge,
                                                    fill=0.0, base=0, channel_multiplier=1), R=[("const", id(t_))], W=[("const", id(t_))])
        CONSTS = ["ones_bf", "negones_f", "ident"] + [("const", id(t_)) for t_ in (maskA, maskB, tri16, trif)]
        P.pool(lambda e: e.memset(lrT[32:33, :], 1.0), W=["lrT1"])
        for c in range(KC):
            P.dma("sp", lambda e, c=c: e.dma_start(out=xT[:, c, :], in_=xT_d[c * 128:(c + 1) * 128, :]),
                  W=[("xT", c, j) for j in range(NB)])

        def blk(j):
            return slice(j * 512, (j + 1) * 512)

        def ykey(ch, i):
            return ("yT", ch, i) if ch < 6 else ("hnT", ch - 6, i // 4)

        def yview(ch):
            return yT[:, ch, :] if ch < 6 else hnT[:, ch - 6, :]

        def til(i):
            return slice(i * 128, (i + 1) * 128)

        def ss_and_rstd(srcs, src_keys, nparity):
            ps_, pk = bank()
            for c in range(KC):
                sq = sqb[:, c % 2, :]
                P.act(lambda e, sq=sq, s_=srcs[c]: e.activation(out=sq, in_=s_, func=AF.Square),
                      R=[src_keys[c]], W=[("sqb", c % 2)])
                P.pe(lambda e, sq=sq, c=c, ps_=ps_: e.matmul(ps_[:, :], lhsT=ones_bf[:], rhs=sq, start=(c == 0), stop=(c == KC - 1)),
                     R=[("sqb", c % 2), "ones_bf"], W=[pk])
            r_ = rstd[:, :]
            P.act(lambda e, ps_=ps_, r_=r_: e.activation(out=r_, in_=ps_[:, :], func=AF.Ln, scale=1.0 / D, bias=EPS),
                  R=[pk], W=["rstd"])
            P.act(lambda e, r_=r_: e.activation(out=r_, in_=r_, func=AF.Exp, scale=-0.5),
                  R=["rstd"], W=["rstd"])
            return r_, "rstd"

        def pre_norm(l, gofs):
            for j in range(NB):
                srcs = [xT[:, c, blk(j)] for c in range(KC)]
                keys = [("xT", c, j) for c in range(KC)]
                r_, rk = ss_and_rstd(srcs, keys, j % 2)
                for c in range(KC):
                    col = gofs + c
                    P.dve(lambda e, c=c, j=j, col=col, r_=r_: e.scalar_tensor_tensor(
                        out=hnT[:, c, blk(j)], in0=xT[:, c, blk(j)], scalar=pcol[:, col:col + 1], in1=r_,
                        op0=ALU.mult, op1=ALU.mult), R=[("xT", c, j), "pcol", rk], W=[("hnT", c, j)])

        def post_norm_residual(l, gofs, srcT, src_keyf, tok0, j_x):
            srcs = [srcT[:, c, :] for c in range(KC)]
            keys = [src_keyf(c) for c in range(KC)]
            r_, rk = ss_and_rstd(srcs, keys, j_x % 2)
            for c in range(KC):
                col = gofs + c
                P.dve(lambda e, c=c, col=col, r_=r_: e.scalar_tensor_tensor(
                    out=srcT[:, c, :], in0=srcT[:, c, :], scalar=pcol[:, col:col + 1], in1=r_,
                    op0=ALU.mult, op1=ALU.mult), R=[keys[c], "pcol", rk], W=[keys[c]])
                P.dve(lambda e, c=c: e.tensor_tensor(out=xT[:, c, tok0:tok0 + 512], in0=xT[:, c, tok0:tok0 + 512],
                                                     in1=srcT[:, c, :], op=ALU.add),
                      R=[keys[c], ("xT", c, j_x)], W=[("xT", c, j_x)])

        def proj_fm(wv, wkey, ncol_lo, m, evac):
            for j in range(NB):
                ps_, pk = bank()
                for c in range(KC):
                    P.pe(lambda e, c=c, j=j, ps_=ps_: e.matmul(ps_[0:m, :], lhsT=wv[:, c, ncol_lo:ncol_lo + m], rhs=hnT[:, c, blk(j)],
                                                               start=(c == 0), stop=(c == KC - 1)),
                         R=[wkey, ("hnT", c, j)], W=[pk])
                evac(j, ps_, pk)

        def proj_tm(wv, wkey, ncol_lo, n, per_bank, evac):
            for i0 in range(0, NT, per_bank):
                ps_, pk = bank()
                for ii in range(per_bank):
                    i = i0 + ii
                    for c in range(KC):
                        P.pe(lambda e, c=c, i=i, ii=ii, ps_=ps_: e.matmul(
                            ps_[:, ii * n:(ii + 1) * n], lhsT=hnT[:, c, til(i)], rhs=wv[:, c, ncol_lo:ncol_lo + n],
                            start=(c == 0), stop=(c == KC - 1)),
                            R=[wkey, ("hnT", c, i // 4)], W=[pk])
                evac(i0, ps_, pk)

        def head_out(i, p, grp, gate_t, gate_key):
            tt = tot[:, :]
            for hh in range(2):
                P.act(lambda e, hh=hh: e.activation(out=junk[:, hh * 128:(hh + 1) * 128], in_=tt[:, hh * 128:(hh + 1) * 128],
                                                    func=AF.Square, accum_out=sml[:, 0, hh:hh + 1]),
                      R=[("acc", 0)], W=[("sqb", 0), ("sml0", hh)])
            P.act(lambda e: e.activation(out=sml[:, 0, 2:4], in_=sml[:, 0, 0:2], func=AF.Ln, scale=1.0 / 128, bias=EPS),
                  R=[("sml0", 0), ("sml0", 1)], W=["sml0b"])
            P.act(lambda e: e.activation(out=sml[:, 0, 4:6], in_=sml[:, 0, 2:4], func=AF.Exp, scale=-0.5),
                  R=["sml0b"], W=["sml0c"])
            yt = ytile[:, i % 2, :]
            for hh in range(2):
                P.dve(lambda e, hh=hh, yt=yt: e.scalar_tensor_tensor(
                    out=yt[:, hh * 128:(hh + 1) * 128], in0=tt[:, hh * 128:(hh + 1) * 128], scalar=sml[:, 0, 4 + hh:5 + hh],
                    in1=gate_t[:, i, hh * 128:(hh + 1) * 128], op0=ALU.mult, op1=ALU.mult),
                    R=[("acc", 0), "sml0c", gate_key], W=[("sqb", 1)])
            pt_, ptk = tbank()
            for hh in range(2):
                P.pe(lambda e, hh=hh, yt=yt, pt_=pt_: e.transpose(pt_[:, hh * 128:(hh + 1) * 128], yt[:, hh * 128:(hh + 1) * 128], ident[:]),
                     R=[("sqb", 1), "ident"], W=[ptk])
            ch = grp * 4 + 2 * p
            for hh in range(2):
                dstv = yview(ch + hh)[:, til(i)]
                P.act(lambda e, pt_=pt_, dstv=dstv, hh=hh: e.copy(out=dstv, in_=pt_[:, hh * 128:(hh + 1) * 128]),
                      R=[ptk], W=[ykey(ch + hh, i)])

        def stage(name):
            if stop_at == name:
                raise _Stop()

        def layer(l):
            P.dma("sp", lambda e, l=l: e.dma_start(out=pcol[:], in_=pcol_d[:, l * PCOLS:(l + 1) * PCOLS]), W=["pcol"])
            P.dma("sp", lambda e, l=l: e.dma_start(out=pbc[:], in_=pbc_d[l:l + 1, :].partition_broadcast(128)), W=["pbc"])
            P.pool(lambda e: e.memset(w2bd[:], 0.0), W=["w2bd"])
            P.dma("pool", lambda e, l=l: e.dma_start(out=w2bd[0:16, 0:256], in_=w2_d[l, 0]), W=["w2bd"])
            P.dma("pool", lambda e, l=l: e.dma_start(out=w2bd[16:32, 256:512], in_=w2_d[l, 1]), W=["w2bd"])
            P.dma("pool", lambda e, l=l: e.dma_start(out=w2bd[32:33, :], in_=gb_d[l:l + 1, :]), W=["w2bd"])

            stage("params")
            pre_norm(l, 0)
            stage("prenorm")

            for p in range(2):
                h0 = 2 * p
                if p == 0:
                    (wv,), wk = load_w([wcols(w_in_d, l, 1424, 128)])
                    psg, pgk = bank()
                    for i in range(NT):
                        for c in range(KC):
                            P.pe(lambda e, c=c, i=i: e.matmul(psg[:, i * 16:(i + 1) * 16], lhsT=hnT[:, c, til(i)], rhs=wv[:, c, 112:128],
                                                              start=(c == 0), stop=(c == KC - 1)),
                                 R=[wk, ("hnT", c, i // 4)], W=[pgk])
                    P.dve(lambda e: e.tensor_tensor(out=gl[:], in0=psg[:, 0:256].rearrange("p (i g) -> p i g", i=NT),
                                                    in1=pbc[:, 0:16].unsqueeze(1).to_broadcast([128, NT, 16]), op=ALU.add),
                          R=[pgk, "pbc"], W=["gl"])
                    stage("g1")
                    for d in range(2):
                        fsl = gl[:, :, 4 + 8 * d:8 + 8 * d]
                        lfd = lf[:, d, :].rearrange("p (i h) -> p i h", i=NT)
                        P.act(lambda e, fsl=fsl, lfd=lfd: e.activation(out=lfd, in_=fsl, func=AF.Exp, scale=-1.0), R=["gl"], W=[("lf", d)])
                        P.act(lambda e, lfd=lfd: e.activation(out=lfd, in_=lfd, func=AF.Ln, bias=1.0), R=[("lf", d)], W=[("lf", d)])
                    stage("g2")
                    psb, pbk = bank()
                    for d in range(2):
                        P.pe(lambda e, d=d: e.matmul(psb[:, d * 64:(d + 1) * 64], lhsT=trif[:, d, :], rhs=lf[:, d, :], start=True, stop=True),
                             R=[("lf", d)] + CONSTS, W=[pbk])
                        P.pe(lambda e, d=d: e.matmul(psb[:, 128 + d * 64:128 + (d + 1) * 64], lhsT=negones_f[:], rhs=lf[:, d, :], start=True, stop=True),
                             R=[("lf", d)] + CONSTS, W=[pbk])
                    stage("g3")
                    P.act(lambda e: e.copy(out=bsb[:, :], in_=psb[:, 0:256]), R=[pbk], W=[("acc", 1)])
                    flat = lambda t_: t_[:].rearrange("p d i h -> p (d i h)")
                    P.act(lambda e: e.activation(out=flat(eb), in_=bsb[:, 0:128], func=AF.Exp), R=[("acc", 1)], W=["eb"])
                    P.act(lambda e: e.activation(out=flat(enb), in_=bsb[:, 0:128], func=AF.Exp, scale=-1.0), R=[("acc", 1)], W=["enb"])
                    P.act(lambda e: e.activation(out=flat(dec), in_=bsb[:, 128:256], func=AF.Exp), R=[("acc", 1)], W=["dec"])
                    for d in range(2):
                        isl = gl[:, :, 8 * d:8 * d + 4]
                        gt = gtmp[:, d, :].rearrange("p (i h) -> p i h", i=NT)
                        P.dve(lambda e, d=d, isl=isl, gt=gt: e.tensor_tensor(out=gt, in0=isl, in1=bsb[:, d * 64:(d + 1) * 64].rearrange("p (i h) -> p i h", i=NT),
                                                                             op=ALU.subtract), R=["gl", ("acc", 1)], W=[("gtmp", d)])
                        P.act(lambda e, d=d, gt=gt: e.activation(out=wsc[:, d].rearrange("p i h -> p (i h)"), in_=gtmp[:, d, :], func=AF.Exp), R=[("gtmp", d)], W=[("wsc", d)])
                        P.dve(lambda e, d=d: e.tensor_tensor(out=wdec[:, d], in0=wsc[:, d], in1=dec[:, d], op=ALU.mult),
                              R=[("wsc", d), "dec"], W=[("wdec", d)])
                stage("gates")
                for d in range(2):
                    for hh in range(2):
                        P.pool(lambda e, d=d, hh=hh: e.tensor_copy(out=decsel[hh * 64:(hh + 1) * 64, d, :], in_=dec[hh * 64:(hh + 1) * 64, d, :, h0 + hh]),
                               R=["dec"], W=[("decsel", hh)])
                (wq, wkk), wk = load_w([wcols(w_in_d, l, p * 128, 128), wcols(w_in_d, l, 256 + p * 128, 128)])
                for which, wv, dst in ((0, wq, qT), (1, wkk, kT)):
                    cj = which * 2 + p
                    cb = 32 + cj * 4
                    def qk_keys(j):
                        return [("qkraw", j)] + [("s5", ii) for ii in range(4 * j, min(NT, 4 * j + 5))]
                    P.pool(lambda e: e.memset(qkraw[:, 0:1], 0.0), W=qk_keys(0))
                    P.pool(lambda e: e.memset(qkraw[:, 2049:2050], 0.0), W=qk_keys(3))

                    def ev(j, ps_, pk):
                        P.act(lambda e, j=j, ps_=ps_: e.copy(out=qkraw[:, 1 + j * 512:1 + (j + 1) * 512], in_=ps_[:, :]),
                              R=[pk], W=qk_keys(j))
                    proj_fm(wv, wk, 0, 128, ev)
                    for j in range(NB):
                        a_ = acc[:, j % 2, :]
                        rk = []
                        for jj in range(max(0, j - 1), min(NB, j + 2)):
                            rk += qk_keys(jj)
                        P.dve(lambda e, j=j, a_=a_, cb=cb: e.tensor_scalar(out=a_, in0=qkraw[:, 1 + j * 512:1 + (j + 1) * 512],
                                                                           scalar1=pcol[:, cb + 1:cb + 2], scalar2=pcol[:, cb + 3:cb + 4],
                                                                           op0=ALU.mult, op1=ALU.add),
                              R=rk + ["pcol"], W=[("acc", j % 2)])
                        P.dve(lambda e, j=j, a_=a_, cb=cb: e.scalar_tensor_tensor(out=a_, in0=qkraw[:, j * 512:(j + 1) * 512],
                                                                                  scalar=pcol[:, cb:cb + 1], in1=a_, op0=ALU.mult, op1=ALU.add),
                              R=rk + ["pcol", ("acc", j % 2)], W=[("acc", j % 2)])
                        P.dve(lambda e, j=j, a_=a_, cb=cb: e.scalar_tensor_tensor(out=a_, in0=qkraw[:, 2 + j * 512:2 + (j + 1) * 512],
                                                                                  scalar=pcol[:, cb + 2:cb + 3], in1=a_, op0=ALU.mult, op1=ALU.add),
                              R=rk + ["pcol", ("acc", j % 2)], W=[("acc", j % 2)])
                        P.act(lambda e, j=j, a_=a_, dst=dst: e.activation(out=dst[:, blk(j)], in_=a_, func=AF.Silu),
                              R=[("acc", j % 2)], W=[("q" if which == 0 else "k", j)])
                stage("Aqk")
                for i in range(NT):
                    pt_, ptk = tbank()
                    P.pe(lambda e, i=i, pt_=pt_: e.transpose(pt_[:, 0:128], kT[:, til(i)], ident[:]), R=[("k", i // 4), "ident"], W=[ptk])
                    for d in range(2):
                        for hh in range(2):
                            P.dve(lambda e, i=i, d=d, hh=hh, pt_=pt_: e.tensor_scalar(
                                out=kw[:, d, i, hh, :], in0=pt_[:, hh * 64:(hh + 1) * 64], scalar1=wdec[:, d, i, h0 + hh:h0 + hh + 1], scalar2=0.125,
                                op0=ALU.mult, op1=ALU.mult), R=[ptk, ("wdec", d)], W=[("s2", i)])
                stage("Akw")
                for hh in range(2):
                    P.pool(lambda e, hh=hh: e.memset(vaug[:, :, hh, 128:130], 1.0), W=[("s3", i) for i in range(NT)])
                (wv,), wk = load_w([wcols(w_in_d, l, 512 + p * 256, 256)])

                def evv(i0, ps_, pk):
                    for ii in range(2):
                        for hh in range(2):
                            P.act(lambda e, i0=i0, ii=ii, hh=hh, ps_=ps_: e.copy(out=vaug[:, i0 + ii, hh, 0:128],
                                                                               in_=ps_[:, ii * 256 + hh * 128:ii * 256 + (hh + 1) * 128]),
                                  R=[pk], W=[("s3", i0 + ii)])
                proj_tm(wv, wk, 0, 256, 2, evv)
                (wv,), wk = load_w([wcols(w_in_d, l, 1024 + p * 256, 256)])

                def evo(i0, ps_, pk):
                    P.act(lambda e, i0=i0, ps_=ps_: e.activation(out=sigo[:, i0:i0 + 2, :].rearrange("p i e -> p (i e)"), in_=ps_[:, :],
                                                                func=AF.Sigmoid), R=[pk], W=[("s4", i0), ("s4", i0 + 1)])
                    P.pool(lambda e, i0=i0: e.tensor_tensor(out=sigo[:, i0:i0 + 2, :], in0=sigo[:, i0:i0 + 2, :],
                                                           in1=pbc[:, 16 + h0 * 128:16 + h0 * 128 + 256].unsqueeze(1).to_broadcast([128, 2, 256]),
                                                           op=ALU.mult), R=[("s4", i0), ("s4", i0 + 1), "pbc"], W=[("s4", i0), ("s4", i0 + 1)])
                proj_tm(wv, wk, 0, 256, 2, evo)

                stage("Aproj")
                orders = [list(range(NT)), list(range(NT - 1, -1, -1))]
                for d in range(2):
                    P.pool(lambda e, d=d: e.memset(C32[:, d, :], 0.0), W=[("C32", d)])
                    P.pool(lambda e, d=d: e.memset(Cbf[:, d, 0, :], 0.0), W=[("Cbf", d, 0)])

                def a_early(d, n):
                    i = orders[d][n]
                    par = n % 2
                    psSs = [bank(), bank()]
                    for hh in range(2):
                        rs = slice(hh * 64, (hh + 1) * 64)
                        psS, psk = psSs[hh]
                        P.pe(lambda e: e.matmul(psS[:, 0:128], lhsT=kT[rs, til(i)], rhs=qT[rs, til(i)], start=True, stop=True),
                             R=[("k", i // 4), ("q", i // 4)], W=[psk])
                    for hh in range(2):
                        psS, psk = psSs[hh]
                        P.dve(lambda e: e.scalar_tensor_tensor(
                            out=Sm[:, d, par, hh, :], in0=psS[:, 0:128], scalar=wsc[:, d, i, h0 + hh:h0 + hh + 1],
                            in1=maskA[:, d, :], op0=ALU.mult, op1=ALU.mult),
                            R=[psk, ("wsc", d)] + CONSTS, W=[("Sm", d, par, hh)])

                def a_main(d, n):
                    i = orders[d][n]
                    par = n % 2
                    psU, puk = bank()
                    P.pe(lambda e: e.matmul(psU[0:64, 0:130], lhsT=kw[:, d, i, 0, :], rhs=vaug[:, i, 0, 0:130], start=True, stop=True),
                         R=[("s2", i), ("s3", i)], W=[puk])
                    P.pe(lambda e: e.matmul(psU[64:128, 0:130], lhsT=kw[:, d, i, 1, :], rhs=vaug[:, i, 1, 0:130], start=True, stop=True,
                                            tile_position=(0, 64)),
                         R=[("s2", i), ("s3", i)], W=[puk])
                    psO, pok = bank()
                    for hh in range(2):
                        rs = slice(hh * 64, (hh + 1) * 64)
                        P.pe(lambda e: e.matmul(psO[:, hh * 130:hh * 130 + 130], lhsT=Sm[:, d, par, hh, :],
                                                rhs=vaug[:, i, hh, 0:130], start=True, stop=False),
                             R=[("Sm", d, par, hh), ("s3", i)], W=[pok])
                        P.pe(lambda e: e.matmul(psO[:, hh * 130:hh * 130 + 130], lhsT=qT[rs, til(i)],
                                                rhs=Cbf[rs, d, par, 0:130], start=False, stop=True),
                             R=[("q", i // 4), ("Cbf", d, par)], W=[pok])
                    P.dve(lambda e: e.scalar_tensor_tensor(
                        out=C32[:, d, 0:130], in0=C32[:, d, 0:130], scalar=decsel[:, d, i:i + 1], in1=psU[:, 0:130], op0=ALU.mult, op1=ALU.add),
                        R=[("C32", d), ("decsel", 0), ("decsel", 1), puk], W=[("C32", d)])
                    P.pool(lambda e: e.tensor_copy(out=Cbf[:, d, 1 - par, 0:130], in_=C32[:, d, 0:130]),
                           R=[("C32", d)], W=[("Cbf", d, 1 - par)])
                    ob = osb[:, d, :]
                    P.act(lambda e: e.copy(out=ob[:, 0:260], in_=psO[:, 0:260]), R=[pok], W=[("osb", d)])
                    sm = sml[:, 1 + d, :]
                    for hh in range(2):
                        P.dve(lambda e: e.scalar_tensor_tensor(out=sm[:, hh:hh + 1], in0=ob[:, hh * 130 + 128:hh * 130 + 129], scalar=-1.0,
                                                               in1=ob[:, hh * 130 + 128:hh * 130 + 129], op0=ALU.mult, op1=ALU.max),
                              R=[("osb", d)], W=[("sml1a", d)])
                    P.dve(lambda e: e.tensor_tensor(out=sm[:, 2:4], in0=sm[:, 0:2], in1=enb[:, d, i, h0:h0 + 2], op=ALU.max),
                          R=[("sml1a", d), "enb"], W=[("sml1b", d)])
                    P.dve(lambda e: e.reciprocal(out=sm[:, 4:6], in_=sm[:, 2:4]), R=[("sml1b", d)], W=[("sml1c", d)])
                    for hh in range(2):
                        if n < NT // 2:
                            P.dve(lambda e: e.tensor_scalar(
                                out=hsum[:, i, hh * 128:(hh + 1) * 128], in0=ob[:, hh * 130:hh * 130 + 128], scalar1=sm[:, 4 + hh:5 + hh], scalar2=None, op0=ALU.mult),
                                R=[("osb", d), ("sml1c", d)], W=[("s5", i)])
                        else:
                            P.dve(lambda e: e.scalar_tensor_tensor(
                                out=tot[:, hh * 128:(hh + 1) * 128], in0=ob[:, hh * 130:hh * 130 + 128], scalar=sm[:, 4 + hh:5 + hh],
                                in1=hsum[:, i, hh * 128:(hh + 1) * 128], op0=ALU.mult, op1=ALU.add),
                                R=[("osb", d), ("sml1c", d), ("s5", i)], W=[("acc", 0)])
                    if n >= NT // 2:
                        head_out(i, p, 0, sigo, ("s4", i))

                for d in range(2):
                    a_early(d, 0)
                for n in range(NT):
                    for d in range(2):
                        if n + 1 < NT:
                            a_early(d, n + 1)
                        a_main(d, n)

            stage("A")
            for p in range(2):
                h0 = 2 * p
                if p == 0:
                    (wv,), wk = load_w([wcols(w_in_d, l, 2992, 128)])

                    def evl(j, ps_, pk):
                        P.act(lambda e, j=j, ps_=ps_: e.copy(out=lrT[0:32, blk(j)], in_=ps_[0:32, :]), R=[pk], W=[("lrT", j)])
                    proj_fm(wv, wk, 96, 32, evl)
                (wq, wkk), wk = load_w([wcols(w_in_d, l, 1552 + p * 128, 128), wcols(w_in_d, l, 1808 + p * 128, 128)])
                for which, wv, dst in ((0, wq, qT), (1, wkk, kT)):
                    def ev(j, ps_, pk, dst=dst, which=which):
                        P.act(lambda e, j=j, ps_=ps_: e.copy(out=dst[:, blk(j)], in_=ps_[:, :]), R=[pk], W=[("q" if which == 0 else "k", j)])
                    proj_fm(wv, wk, 0, 128, ev)
                (wv,), wk = load_w([wcols(w_in_d, l, 2064 + p * 256, 256)])

                def evv(i0, ps_, pk):
                    P.act(lambda e, i0=i0, ps_=ps_: e.copy(out=bv[:, i0:i0 + 2, :].rearrange("p i e -> p (i e)"), in_=ps_[:, :]),
                          R=[pk], W=[("s2", i0), ("s2", i0 + 1)])
                proj_tm(wv, wk, 0, 256, 2, evv)
                (wv,), wk = load_w([wcols(w_in_d, l, 2576 + p * 256, 256)])

                def evr(i0, ps_, pk):
                    P.act(lambda e, i0=i0, ps_=ps_: e.activation(out=sr[:, i0:i0 + 2, :].rearrange("p i e -> p (i e)"), in_=ps_[:, :],
                                                                func=AF.Silu), R=[pk], W=[("s3", i0), ("s3", i0 + 1)])
                    P.pool(lambda e, i0=i0: e.tensor_tensor(out=sr[:, i0:i0 + 2, :], in0=sr[:, i0:i0 + 2, :],
                                                           in1=pbc[:, 528 + h0 * 128:528 + h0 * 128 + 256].unsqueeze(1).to_broadcast([128, 2, 256]),
                                                           op=ALU.mult), R=[("s3", i0), ("s3", i0 + 1), "pbc"], W=[("s3", i0), ("s3", i0 + 1)])
                proj_tm(wv, wk, 0, 256, 2, evr)
                for i0 in range(0, NT, 2):
                    ps_, pk = bank()
                    for ii in range(2):
                        i = i0 + ii
                        for d in range(2):
                            P.pe(lambda e, i=i, ii=ii, d=d, ps_=ps_: e.matmul(ps_[:, ii * 256 + d * 128:ii * 256 + (d + 1) * 128], lhsT=lrT[0:33, til(i)],
                                                                              rhs=w2bd[0:33, d * 256 + p * 128:d * 256 + (p + 1) * 128], start=True, stop=True),
                                 R=[("lrT", i // 4), "lrT1", "w2bd"], W=[pk])
                    g_ = acc[:, (i0 // 2) % 2, :]
                    P.act(lambda e, ps_=ps_, g_=g_: e.activation(out=g_, in_=ps_[:, :], func=AF.Exp, scale=-1.0), R=[pk], W=[("acc", (i0 // 2) % 2)])
                    P.act(lambda e, i0=i0, g_=g_: e.activation(out=la[:, i0:i0 + 2, :].rearrange("p i e -> p (i e)"), in_=g_, func=AF.Ln, bias=1.0),
                          R=[("acc", (i0 // 2) % 2)], W=[("s4", i0), ("s4", i0 + 1)])
                stage("Bproj")
                orders = [list(range(NT)), list(range(NT - 1, -1, -1))]
                for d in range(2):
                    P.pool(lambda e, d=d: e.memset(C32[:, d, :], 0.0), W=[("C32", d)])
                    P.pool(lambda e, d=d: e.memset(Cbf[:, d, 0, :], 0.0), W=[("Cbf", d, 0)])

                def b_early(d, n):
                    i = orders[d][n]
                    par = n % 2
                    psB, pbk2 = bank()
                    P.pe(lambda e: e.matmul(psB[:, 0:128], lhsT=la[:, i, d * 128:(d + 1) * 128], rhs=tri16[:, d, :], start=True, stop=True),
                         R=[("s4", i)] + CONSTS, W=[pbk2])
                    P.act(lambda e: e.activation(out=eBt[:, d, par, :], in_=psB[:, 0:128], func=AF.Exp), R=[pbk2], W=[("eBt", d, par)])
                    P.act(lambda e: e.activation(out=eNBt[:, d, :], in_=psB[:, 0:128], func=AF.Exp, scale=-1.0), R=[pbk2], W=[("eNBt", d)])
                    P.dve(lambda e: e.scalar_tensor_tensor(out=qs[:, d, par, :], in0=qT[:, til(i)], scalar=0.125, in1=eBt[:, d, par, :],
                                                           op0=ALU.mult, op1=ALU.mult), R=[("q", i // 4), ("eBt", d, par)], W=[("qs", d, par)])
                    P.dve(lambda e: e.tensor_tensor(out=ks[:, d, par, :], in0=kT[:, til(i)], in1=eNBt[:, d, :], op=ALU.mult),
                          R=[("k", i // 4), ("eNBt", d)], W=[("ks", d, par)])
                    psSs = [bank(), bank()]
                    for hh in range(2):
                        rs = slice(hh * 64, (hh + 1) * 64)
                        psS, psk = psSs[hh]
                        P.pe(lambda e: e.matmul(psS[:, 0:128], lhsT=ks[rs, d, par, :], rhs=qs[rs, d, par, :], start=True, stop=True),
                             R=[("ks", d, par), ("qs", d, par)], W=[psk])
                    pt_, ptk = tbank()
                    P.pe(lambda e: e.transpose(pt_[:, 0:128], ks[:, d, par, :], ident[:]), R=[("ks", d, par), "ident"], W=[ptk])
                    for hh in range(2):
                        psS, psk = psSs[hh]
                        P.dve(lambda e: e.tensor_tensor(out=Sm[:, d, par, hh, :], in0=psS[:, 0:128], in1=maskB[:, d, :], op=ALU.mult),
                              R=[psk] + CONSTS, W=[("Sm", d, par, hh)])
                    P.act(lambda e: e.copy(out=ktok[:, d, par, :], in_=pt_[:, 0:128]), R=[ptk], W=[("ktok", d, par)])

                def b_main(d, n):
                    i = orders[d][n]
                    par = n % 2
                    lastcol = 127 if d == 0 else 0
                    psU, puk = bank()
                    P.pe(lambda e: e.matmul(psU[0:64, 0:128], lhsT=ktok[:, d, par, 0:64], rhs=bv[:, i, 0:128], start=True, stop=True),
                         R=[("ktok", d, par), ("s2", i)], W=[puk])
                    P.pe(lambda e: e.matmul(psU[64:128, 0:128], lhsT=ktok[:, d, par, 64:128], rhs=bv[:, i, 128:256], start=True, stop=True,
                                            tile_position=(0, 64)), R=[("ktok", d, par), ("s2", i)], W=[puk])
                    psO, pok = bank()
                    for hh in range(2):
                        rs = slice(hh * 64, (hh + 1) * 64)
                        P.pe(lambda e: e.matmul(psO[:, hh * 128:(hh + 1) * 128], lhsT=Sm[:, d, par, hh, :],
                                                rhs=bv[:, i, hh * 128:(hh + 1) * 128], start=True, stop=False),
                             R=[("Sm", d, par, hh), ("s2", i)], W=[pok])
                        P.pe(lambda e: e.matmul(psO[:, hh * 128:(hh + 1) * 128], lhsT=qs[rs, d, par, :],
                                                rhs=Cbf[rs, d, par, 0:128], start=False, stop=True),
                             R=[("qs", d, par), ("Cbf", d, par)], W=[pok])
                    P.dve(lambda e: e.tensor_tensor(out=tmpS[:, d, :], in0=psU[:, 0:128], in1=C32[:, d, 0:128], op=ALU.add),
                          R=[puk, ("C32", d)], W=[("tmpS", d)])
                    P.dve(lambda e: e.tensor_scalar(out=C32[:, d, 0:128], in0=tmpS[:, d, :], scalar1=eBt[:, d, par, lastcol:lastcol + 1], scalar2=None,
                                                    op0=ALU.mult), R=[("tmpS", d), ("eBt", d, par)], W=[("C32", d)])
                    P.pool(lambda e: e.tensor_copy(out=Cbf[:, d, 1 - par, 0:128], in_=C32[:, d, 0:128]),
                           R=[("C32", d)], W=[("Cbf", d, 1 - par)])
                    if n < NT // 2:
                        P.act(lambda e: e.copy(out=hsum[:, i, :], in_=psO[:, 0:256]), R=[pok], W=[("s5", i)])
                    else:
                        P.dve(lambda e: e.tensor_tensor(out=tot[:, :], in0=psO[:, 0:256], in1=hsum[:, i, :], op=ALU.add),
                              R=[pok, ("s5", i)], W=[("acc", 0)])
                        head_out(i, p, 1, sr, ("s3", i))

                for d in range(2):
                    b_early(d, 0)
                for n in range(NT):
                    for d in range(2):
                        if n + 1 < NT:
                            b_early(d, n + 1)
                        b_main(d, n)

            stage("B")
            if dbg and "yT" in dbg and l == 0:
                P.barrier()
                for c in range(KC):
                    P.act(lambda e, c=c: e.copy(out=xT[:, c, :], in_=yT[:, c, :]), R=[], W=[("xT", c, j) for j in range(NB)])
                P.barrier()

            P.barrier()
            for q4 in range(4):
                P.dma("pool", lambda e, q4=q4, l=l: e.dma_start(out=wo[:, :, q4 * 256:(q4 + 1) * 256],
                                                                in_=w_out_d[l, :, q4 * 256:(q4 + 1) * 256].rearrange("(c p) n -> p c n", p=128)),
                      W=["wo"])
            for j in range(NB):
                for co in range(KC):
                    ps_, pk = bank()
                    for ch in range(KC):
                        P.pe(lambda e, co=co, ch=ch, j=j, ps_=ps_: e.matmul(ps_[:, :], lhsT=wo[:, ch, co * 128:(co + 1) * 128], rhs=yview(ch)[:, blk(j)],
                                                                            start=(ch == 0), stop=(ch == KC - 1)),
                             R=["wo"] + [ykey(ch, i) for i in range(4 * j, 4 * j + 4)], W=[pk])
                    P.act(lambda e, co=co, ps_=ps_: e.copy(out=mixT[:, co, :], in_=ps_[:, :]), R=[pk], W=[("mixT", co)])
                post_norm_residual(l, 8, mixT, lambda c: ("mixT", c), j * 512, j)

            stage("wout")
            pre_norm(l, 16)
            P.barrier()
            for H in range(2):
                t0 = H * 1024
                for f in range(NF):
                    (wgv, wuv), wk = load_w([wcols(wg_d, l, f * 128, 128), wcols(wu_d, l, f * 128, 128)])
                    cb = 48 + f * 4
                    if H == 0:
                        P.pool(lambda e: e.memset(graw[:, 0:1], 0.0), W=[("graw", 0)])
                        halo_tok, halo_col = 1024, 1025
                    else:
                        P.pool(lambda e: e.memset(graw[:, 1025:1026], 0.0), W=[("graw", 1)])
                        halo_tok, halo_col = 1023, 0
                    psh, phk = bank()
                    for c in range(KC):
                        P.pe(lambda e, c=c, psh=psh: e.matmul(psh[:, 0:1], lhsT=wgv[:, c, :], rhs=hnT[:, c, halo_tok:halo_tok + 1],
                                                              start=(c == 0), stop=(c == KC - 1)), R=[wk, ("hnT", c, halo_tok // 512)], W=[phk])
                    P.act(lambda e, psh=psh, halo_col=halo_col: e.copy(out=graw[:, halo_col:halo_col + 1], in_=psh[:, 0:1]),
                          R=[phk], W=[("graw", 0 if halo_col == 0 else 1)])
                    for b in range(2):
                        jj = 2 * H + b
                        ps_, pk = bank()
                        for c in range(KC):
                            P.pe(lambda e, c=c, jj=jj, ps_=ps_: e.matmul(ps_[:, :], lhsT=wgv[:, c, :], rhs=hnT[:, c, blk(jj)],
                                                                         start=(c == 0), stop=(c == KC - 1)), R=[wk, ("hnT", c, jj)], W=[pk])
                        P.act(lambda e, b=b, ps_=ps_: e.copy(out=graw[:, 1 + b * 512:1 + (b + 1) * 512], in_=ps_[:, :]), R=[pk], W=[("grawb", b)])
                    for b in range(2):
                        jj = 2 * H + b
                        a_ = acc[:, b, :]
                        rk = [("graw", 0), ("graw", 1), ("grawb", 0), ("grawb", 1)]
                        P.dve(lambda e, b=b, a_=a_, cb=cb: e.tensor_scalar(out=a_, in0=graw[:, 1 + b * 512:1 + (b + 1) * 512],
                                                                           scalar1=pcol[:, cb + 1:cb + 2], scalar2=pcol[:, cb + 3:cb + 4],
                                                                           op0=ALU.mult, op1=ALU.add), R=rk + ["pcol"], W=[("acc", b)])
                        P.dve(lambda e, b=b, a_=a_, cb=cb: e.scalar_tensor_tensor(out=a_, in0=graw[:, b * 512:(b + 1) * 512],
                                                                                  scalar=pcol[:, cb:cb + 1], in1=a_, op0=ALU.mult, op1=ALU.add),
                              R=rk + ["pcol", ("acc", b)], W=[("acc", b)])
                        P.dve(lambda e, b=b, a_=a_, cb=cb: e.scalar_tensor_tensor(out=a_, in0=graw[:, 2 + b * 512:2 + (b + 1) * 512],
                                                                                  scalar=pcol[:, cb + 2:cb + 3], in1=a_, op0=ALU.mult, op1=ALU.add),
                              R=rk + ["pcol", ("acc", b)], W=[("acc", b)])
                        P.act(lambda e, b=b, a_=a_: e.activation(out=gel[:, b, :], in_=a_, func=AF.Gelu_apprx_tanh), R=[("acc", b)], W=[("gel", b)])
                        ps_, pk = bank()
                        for c in range(KC):
                            P.pe(lambda e, c=c, jj=jj, ps_=ps_: e.matmul(ps_[:, :], lhsT=wuv[:, c, :], rhs=hnT[:, c, blk(jj)],
                                                                         start=(c == 0), stop=(c == KC - 1)), R=[wk, ("hnT", c, jj)], W=[pk])
                        P.dve(lambda e, b=b, f=f, ps_=ps_: e.tensor_tensor(out=actT[:, f, b * 512:(b + 1) * 512], in0=gel[:, b, :], in1=ps_[:, :], op=ALU.mult),
                              R=[("gel", b), pk], W=[("actT", f, b)])
                for b in range(2):
                    for co in range(KC):
                        (wda,), wka = load_w([wd_d[l, 0:11 * 128, co * 128:(co + 1) * 128].rearrange("(f p) n -> p f n", p=128)])
                        (wdb,), wkb = load_w([wd_d[l, 11 * 128:22 * 128, co * 128:(co + 1) * 128].rearrange("(f p) n -> p f n", p=128)])
                        ps_, pk = bank()
                        for f in range(NF):
                            wdv, wk = (wda, wka) if f < 11 else (wdb, wkb)
                            P.pe(lambda e, f=f, b=b, ps_=ps_, wdv=wdv, wk=wk: e.matmul(ps_[:, :], lhsT=wdv[:, f % 11, :], rhs=actT[:, f, b * 512:(b + 1) * 512],
                                                                                start=(f == 0), stop=(f == NF - 1)), R=[wk, ("actT", f, b)], W=[pk])
                        P.act(lambda e, co=co, ps_=ps_: e.copy(out=ffT[:, co, :], in_=ps_[:, :]), R=[pk], W=[("ffT", co)])
                    post_norm_residual(l, 24, ffT, lambda c: ("ffT", c), t0 + b * 512, 2 * H + b)
            P.barrier()

        try:
            stage("load")
            for l in range(nl):
                layer(l)
        except _Stop:
            pass
        for c in range(KC):
            P.dma("sp", lambda e, c=c: e.dma_start(out=outT_d[c * 128:(c + 1) * 128, :], in_=xT[:, c, :]),
                  R=[("xT", c, j) for j in range(NB)], W=[("out", c)])
        P.add("sp", None, R=[("out", c) for c in range(KC)])
        P.emit(nc)
    return nc


def _pack_params(ls, norm_mix_pre, norm_mix_post, norm_ffn_pre, norm_ffn_post, mlstm_conv_w, mlstm_conv_b,
                 ffn_conv_w, ffn_conv_b, mlstm_gate_b, mlstm_norm, gla_norm):
    nl = len(ls)
    pcol = np.zeros((128, nl * PCOLS), np.float32)
    pbc = np.zeros((nl, PBC), np.float32)
    for li, l in enumerate(ls):
        o = li * PCOLS
        for gi, g in enumerate((norm_mix_pre, norm_mix_post, norm_ffn_pre, norm_ffn_post)):
            pcol[:, o + gi * 8:o + gi * 8 + 8] = g[l].reshape(8, 128).T
        for cj in range(4):
            pcol[:, o + 32 + cj * 4:o + 32 + cj * 4 + 3] = mlstm_conv_w[l][:, cj * 128:(cj + 1) * 128].T
            pcol[:, o + 32 + cj * 4 + 3] = mlstm_conv_b[l][cj * 128:(cj + 1) * 128]
        for f in range(NF):
            pcol[:, o + 48 + f * 4:o + 48 + f * 4 + 3] = ffn_conv_w[l][:, f * 128:(f + 1) * 128].T
            pcol[:, o + 48 + f * 4 + 3] = ffn_conv_b[l][f * 128:(f + 1) * 128]
        pbc[li, 0:16] = mlstm_gate_b[l]
        pbc[li, 16:528] = mlstm_norm[l].reshape(-1)
        pbc[li, 528:1040] = gla_norm[l].reshape(-1)
    return pcol, pbc


_CACHE = {}


def _get_prog(nl):
    if nl not in _CACHE:
        _CACHE[nl] = build_program(nl)
    return _CACHE[nl]


FUSED = True


def kernel(x, norm_mix_pre, norm_mix_post, norm_ffn_pre, norm_ffn_post, w_in, mlstm_gate_b, mlstm_conv_w,
           mlstm_conv_b, mlstm_norm, gla_w2, gla_b, gla_norm, w_out, ffn_w_gate, ffn_w_up, ffn_conv_w,
           ffn_conv_b, ffn_w_down):
    f = lambda a: np.ascontiguousarray(np.asarray(a), dtype=np.float32)
    x = f(x)
    args = [f(a) for a in (norm_mix_pre, norm_mix_post, norm_ffn_pre, norm_ffn_post, mlstm_conv_w, mlstm_conv_b,
                           ffn_conv_w, ffn_conv_b, mlstm_gate_b, mlstm_norm, gla_norm)]
    w_in, w_out, wg, wu, wd = f(w_in), f(w_out), f(ffn_w_gate), f(ffn_w_up), f(ffn_w_down)
    w2, gb = f(gla_w2), f(gla_b)
    xTs = [np.ascontiguousarray(x[b].T) for b in range(NCORES)]
    groups = [list(range(DEPTH))] if FUSED else [[l] for l in range(DEPTH)]
    for ls in groups:
        nl = len(ls)
        nc = _get_prog(nl)
        pcol, pbc = _pack_params(ls, *args)
        sl = slice(ls[0], ls[-1] + 1)
        shared = {"w_in": w_in[sl], "w_out": w_out[sl], "wg": wg[sl], "wu": wu[sl], "wd": wd[sl], "pcol": pcol, "pbc": pbc,
                  "w2": w2[sl], "gb": np.ascontiguousarray(gb[sl].reshape(nl, 512))}
        in_maps = [dict(shared, xT=xTs[b]) for b in range(NCORES)]
        res = run_bass_kernel_spmd(nc, in_maps, core_ids=list(range(NCORES)))
        xTs = [np.asarray(r["outT"]) for r in res.results]
    return np.stack([np.ascontiguousarray(t.T) for t in xTs], axis=0).astype(np.float32)
```

```python
import contextlib
import types
import numpy as np
import concourse.bass as bass
import concourse.mybir as mybir
from concourse.bass_utils import run_bass_kernel_spmd

F32 = mybir.dt.float32
BF16 = mybir.dt.bfloat16
ALU = mybir.AluOpType
AF = mybir.ActivationFunctionType

EPOCH = 30000
NCORES = 8
DEPTH = 4
D = 1024
S = 2048
NT = 16
NB = 4
KC = 8
FF = 2816
NF = 22
INC = 3120
EPS = 1e-6
PCOLS = 136
PBC = 1040


def _freeze(fn):
    if fn is None or fn.__closure__ is None:
        return fn
    cells = []
    for c in fn.__closure__:
        try:
            cells.append(types.CellType(c.cell_contents))
        except ValueError:
            cells.append(c)
    return types.FunctionType(fn.__code__, fn.__globals__, fn.__name__, fn.__defaults__, tuple(cells))


class _Op:
    __slots__ = ("eng", "fn", "R", "W", "dma", "deps", "marked", "tick", "dsem", "dtick", "dprev")

    def __init__(self, eng, fn, R, W, dma):
        self.eng = eng
        self.fn = fn
        self.R = tuple(R)
        self.W = tuple(W)
        self.dma = dma
        self.deps = ()
        self.marked = False
        self.tick = 0
        self.dsem = -1
        self.dtick = 0
        self.dprev = None


class Prog:
    ENGS = ("pe", "act", "dve", "pool", "sp")

    def __init__(self, n_dma_sems=16):
        self.ops = []
        self.nds = n_dma_sems

    def add(self, eng, fn, R=(), W=(), dma=False):
        self.ops.append(_Op(eng, _freeze(fn), R, W, dma))

    def pe(self, fn, R=(), W=()):
        self.add("pe", fn, R, W)

    def act(self, fn, R=(), W=()):
        self.add("act", fn, R, W)

    def dve(self, fn, R=(), W=()):
        self.add("dve", fn, R, W)

    def pool(self, fn, R=(), W=()):
        self.add("pool", fn, R, W)

    def dma(self, eng, fn, R=(), W=()):
        self.add(eng, fn, R, W, dma=True)

    def barrier(self):
        self.ops.append(_Op("__barrier__", None, (), (), False))

    def analyze(self):
        last_w = {}
        readers = {}
        last_on_eng = {e: None for e in self.ENGS}
        pend = {e: None for e in self.ENGS}
        out = []
        for op in self.ops:
            if op.eng == "__barrier__":
                snap = [v for v in last_on_eng.values() if v is not None]
                for e in self.ENGS:
                    pend[e] = snap
                continue
            g = len(out)
            deps = set()
            for k in op.R:
                if k in last_w:
                    deps.add(last_w[k])
            for k in op.W:
                if k in last_w:
                    deps.add(last_w[k])
                for r in readers.get(k, ()):
                    deps.add(r)
            if pend[op.eng] is not None:
                deps.update(pend[op.eng])
                pend[op.eng] = None
            for k in op.R:
                readers.setdefault(k, []).append(g)
            for k in op.W:
                last_w[k] = g
                readers[k] = []
            deps.discard(g)
            op.deps = tuple(sorted(deps))
            out.append(op)
            if op.fn is not None:
                last_on_eng[op.eng] = g
        self.lin = out
        ndma = 0
        nq = [0, 0]
        last_dma_on_sem = {}
        for g, op in enumerate(out):
            for d in op.deps:
                p = out[d]
                if p.dma:
                    continue
                if p.eng == "pe" and op.eng == "pe" and not op.dma:
                    continue
                p.marked = True
            if op.dma:
                half = self.nds // 2
                qi = 0 if op.eng == "sp" else 1
                s = qi * half + (nq[qi] % half)
                nq[qi] += 1
                ndma += 1
                op.dsem = s
                prev = last_dma_on_sem.get(s)
                op.dprev = prev
                op.dtick = (out[prev].dtick if prev is not None else 0) + 16
                last_dma_on_sem[s] = g
        cnt = {e: 0 for e in self.ENGS}
        for op in out:
            if op.marked and not op.dma:
                cnt[op.eng] += 1
                op.tick = cnt[op.eng]
        self.cnt = cnt
        self.ndma = ndma

    def emit(self, nc):
        self.analyze()
        out = self.lin
        with contextlib.ExitStack() as st:
            esems = {}
            for e in self.ENGS:
                nep = self.cnt[e] // EPOCH + 1
                esems[e] = [st.enter_context(nc.semaphore(f"s_{e}_{i}")) for i in range(nep)]
            dsems = [st.enter_context(nc.semaphore(f"s_dma_{i}")) for i in range(self.nds)]
            block = st.enter_context(nc.Block())
            by_eng = {e: [] for e in self.ENGS}
            for g, op in enumerate(out):
                by_eng[op.eng].append((g, op))

            def sem_of(p):
                if p.dma:
                    return ("d", p.dsem), dsems[p.dsem], p.dtick
                ep = (p.tick - 1) // EPOCH
                return (p.eng, ep), esems[p.eng][ep], p.tick - ep * EPOCH

            def run(eng_name, e):
                waited = {}
                for g, op in by_eng[eng_name]:
                    waits = {}
                    deps = list(op.deps)
                    if op.dma and op.dprev is not None:
                        deps.append(op.dprev)
                    for d in deps:
                        p = out[d]
                        if (not p.dma) and p.eng == "pe" and eng_name == "pe" and not op.dma:
                            continue
                        key, sem, val = sem_of(p)
                        if waited.get(key, 0) >= val:
                            continue
                        if key not in waits or waits[key][1] < val:
                            waits[key] = (sem, val)
                    for key, (sem, val) in waits.items():
                        e.wait_ge(sem, val)
                        waited[key] = val
                    if op.fn is None:
                        continue
                    ins = op.fn(e)
                    if op.dma:
                        ins.then_inc(dsems[op.dsem], 16)
                    elif op.marked:
                        ep = (op.tick - 1) // EPOCH
                        ins.then_inc(esems[eng_name][ep], 1)

            @block.tensor
            def _(e):
                run("pe", e)

            @block.scalar
            def _(e):
                run("act", e)

            @block.vector
            def _(e):
                run("dve", e)

            @block.gpsimd
            def _(e):
                run("pool", e)

            @block.sync
            def _(e):
                run("sp", e)


class _Stop(Exception):
    pass


def build_program(nl, dbg=None, stop_at=None):
    nc = bass.Bass("TRN2", target_bir_lowering=False)
    P = Prog()

    def din(name, shape):
        return nc.dram_tensor(name, shape, F32, kind="ExternalInput").ap()

    xT_d = din("xT", [D, S])
    w_in_d = din("w_in", [nl, D, INC])
    w_out_d = din("w_out", [nl, D, D])
    wg_d = din("wg", [nl, D, FF])
    wu_d = din("wu", [nl, D, FF])
    wd_d = din("wd", [nl, FF, D])
    pcol_d = din("pcol", [128, nl * PCOLS])
    pbc_d = din("pbc", [nl, PBC])
    w2_d = din("w2", [nl, 2, 16, 256])
    gb_d = din("gb", [nl, 512])
    outT_d = nc.dram_tensor("outT", [D, S], F32, kind="ExternalOutput").ap()
    dbg_d = {}
    if dbg:
        for k, shp in dbg.items():
            dbg_d[k] = nc.dram_tensor("dbg_" + k, shp, F32, kind="ExternalOutput").ap()

    st = contextlib.ExitStack()
    with st:
        def sb(name, shape, dt):
            return st.enter_context(nc.sbuf_tensor(name, shape, dt))

        def psum(name, shape, dt):
            return st.enter_context(nc.psum_tensor(name, shape, dt))

        xT = sb("xT_sb", [128, KC, S], F32)
        hnT = sb("hnT", [128, KC, S], BF16)
        pcol = sb("pcol_sb", [128, PCOLS], F32)
        pbc = sb("pbc_sb", [128, PBC], F32)
        w2bd = sb("w2bd", [128, 512], BF16)
        lrT = sb("lrT", [128, S], BF16)
        ones_bf = sb("ones_bf", [128, 128], BF16)
        negones_f = sb("negones_f", [128, 128], F32)
        ident = sb("ident", [128, 128], BF16)
        maskA = sb("maskA", [128, 2, 128], BF16)
        maskB = sb("maskB", [128, 2, 128], BF16)
        tri16 = sb("tri16", [128, 2, 128], BF16)
        trif = sb("trif", [128, 2, 128], F32)
        WB = 2048
        wbuf = [sb(f"wbuf{i}", [128, WB], BF16) for i in range(2)]
        A_YT = 0
        A_Q = A_YT + 6 * S
        A_K = A_Q + S
        A_2 = A_K + S
        A_3 = A_2 + 4096
        A_4 = A_3 + 4160
        A_5 = A_4 + 4096
        A_END = A_5 + 4100
        arena = sb("arena", [128, A_END], BF16)
        yT = arena[:, A_YT:A_Q].rearrange("p (c t) -> p c t", c=6)
        qT = arena[:, A_Q:A_K]
        kT = arena[:, A_K:A_2]
        kw = arena[:, A_2:A_3].rearrange("p (d i h e) -> p d i h e", d=2, i=NT, h=2)
        bv = arena[:, A_2:A_3].rearrange("p (i e) -> p i e", i=NT)
        vaug = arena[:, A_3:A_4].rearrange("p (i h e) -> p i h e", i=NT, h=2)
        sr = arena[:, A_3:A_3 + 4096].rearrange("p (i e) -> p i e", i=NT)
        sigo = arena[:, A_4:A_5].rearrange("p (i e) -> p i e", i=NT)
        la = arena[:, A_4:A_5].rearrange("p (i e) -> p i e", i=NT)
        hsum = arena[:, A_5:A_5 + 4096].rearrange("p (i e) -> p i e", i=NT)
        qkraw = arena[:, A_5:A_END].bitcast(F32)
        wo = arena[:, A_Q:A_Q + KC * D].rearrange("p (c n) -> p c n", c=KC)
        mixT = arena[:, A_Q + KC * D:A_Q + KC * D + 2 * KC * 512].bitcast(F32).rearrange("p (c t) -> p c t", c=KC)
        actT = arena[:, 0:NF * 1024].rearrange("p (f t) -> p f t", f=NF)
        ffT = arena[:, NF * 1024:NF * 1024 + 2 * KC * 512].bitcast(F32).rearrange("p (c t) -> p c t", c=KC)
        graw = arena[:, NF * 1024 + 2 * KC * 512:NF * 1024 + 2 * KC * 512 + 2 * 1026].bitcast(F32)
        assert NF * 1024 + 2 * KC * 512 + 2 * 1026 <= A_END

        gl = sb("gl", [128, NT, 16], F32)
        lf = sb("lf", [128, 2, NT * 4], F32)
        gtmp = sb("gtmp", [128, 2, NT * 4], F32)
        eb = sb("eb", [128, 2, NT, 4], F32)
        enb = sb("enb", [128, 2, NT, 4], F32)
        wsc = sb("wsc", [128, 2, NT, 4], F32)
        dec = sb("dec", [128, 2, NT, 4], F32)
        wdec = sb("wdec", [128, 2, NT, 4], F32)
        decsel = sb("decsel", [128, 2, NT], F32)
        Sm = sb("Sm", [128, 2, 2, 2, 128], BF16)
        C32 = sb("C32", [128, 2, 132], F32)
        Cbf = sb("Cbf", [128, 2, 2, 132], BF16)
        eBt = sb("eBt", [128, 2, 2, 128], F32)
        eNBt = sb("eNBt", [128, 2, 128], F32)
        qs = sb("qs", [128, 2, 2, 128], BF16)
        ks = sb("ks", [128, 2, 2, 128], BF16)
        ktok = sb("ktok", [128, 2, 2, 128], BF16)
        tmpS = sb("tmpS", [128, 2, 128], F32)
        osb = sb("osb", [128, 2, 260], F32)
        sml = sb("sml", [128, 3, 16], F32)
        sqb = sb("sqb", [128, 2, 512], BF16)
        hhalo = sb("hhalo", [128, KC, 2], BF16)
        rstd = sb("rstd", [128, 512], F32)
        acc = sb("acc", [128, 2, 512], F32)
        bsb = acc[:, 1, 0:256]
        gel = Sm[:].rearrange("p a b c t -> p (a b c t)").rearrange("p (b t) -> p b t", b=2)
        tot = acc[:, 0, 0:256]
        junk = sqb[:, 0, 0:256]
        ytile = sqb[:, 1, :].rearrange("p (b t) -> p b t", b=2)

        pb = [psum(f"pb{i}", [128, 512], F32) for i in range(6)]
        ptb = [psum(f"ptb{i}", [128, 1024], BF16) for i in range(2)]
        rot = {"n": 0, "t": 0}

        def bank():
            i = rot["n"] % 6
            rot["n"] += 1
            return pb[i], ("ps", i)

        def tbank():
            i = rot["t"] % 2
            rot["t"] += 1
            return ptb[i], ("pt", i)

        wrot = {"n": 0}

        def load_into(bufspec, segs):
            ap2d, keys = bufspec
            views = []
            off = 0
            for src in segs:
                k, n = src.shape[1], src.shape[2]
                v = ap2d[:, off:off + k * n].rearrange("p (k n) -> p k n", k=k)
                P.dma("pool", lambda e: e.dma_start(out=v, in_=src), W=keys)
                views.append(v)
                off += k * n
            assert off <= ap2d.shape[1]
            return views, list(keys)

        def load_w(segs):
            i = wrot["n"] % 2
            wrot["n"] += 1
            views, _ = load_into((wbuf[i][:, :], [("wbuf", i)]), segs)
            return views, ("wbuf", i)

        def wcols(wd3, l, a, n, kc=KC):
            return wd3[l, :, a:a + n].rearrange("(c p) n -> p c n", p=128)

        P.pool(lambda e: e.memset(ones_bf[:], 1.0), W=["ones_bf"])
        P.pool(lambda e: e.memset(negones_f[:], -1.0), W=["negones_f"])
        P.pool(lambda e: e.memset(ident[:], 0.0), W=["ident"])
        P.pool(lambda e: e.affine_select(out=ident[:], in_=ident[:], pattern=[[-1, 128]], compare_op=ALU.not_equal,
                                         fill=1.0, base=0, channel_multiplier=1), R=["ident"], W=["ident"])
        for t_, val in ((maskA, 0.125), (maskB, 0.125), (tri16, -1.0 / 16.0), (trif, -1.0)):
            P.pool(lambda e, t_=t_, val=val: e.memset(t_[:], val), W=[("const", id(t_))])
            P.pool(lambda e, t_=t_: e.affine_select(out=t_[:, 0, :], in_=t_[:, 0, :], pattern=[[1, 128]], compare_op=ALU.is_ge,
                                                    fill=0.0, base=0, channel_multiplier=-1), R=[("const", id(t_))], W=[("const", id(t_))])
            P.pool(lambda e, t_=t_: e.affine_select(out=t_[:, 1, :], in_=t_[:, 1, :], pattern=[[-1, 128]], compare_op=ALU.is_ge,
                                                    fill=0.0, base=0, channel_multiplier=1), R=[("const", id(t_))], W=[("const", id(t_))])
        CONSTS = ["ones_bf", "negones_f", "ident"] + [("const", id(t_)) for t_ in (maskA, maskB, tri16, trif)]
        P.pool(lambda e: e.memset(lrT[32:33, :], 1.0), W=["lrT1"])
        for c in range(KC):
            P.dma("sp", lambda e, c=c: e.dma_start(out=xT[:, c, :], in_=xT_d[c * 128:(c + 1) * 128, :]),
                  W=[("xT", c, j) for j in range(NB)])

        def blk(j):
            return slice(j * 512, (j + 1) * 512)

        def ykey(ch, i):
            return ("yT", ch, i) if ch < 6 else ("hnT", ch - 6, i // 4)

        def yview(ch):
            return yT[:, ch, :] if ch < 6 else hnT[:, ch - 6, :]

        def til(i):
            return slice(i * 128, (i + 1) * 128)

        def ss_and_rstd(srcs, src_keys, nparity):
            ps_, pk = bank()
            for c in range(KC):
                sq = sqb[:, c % 2, :]
                P.act(lambda e, sq=sq, s_=srcs[c]: e.activation(out=sq, in_=s_, func=AF.Square),
                      R=[src_keys[c]], W=[("sqb", c % 2)])
                P.pe(lambda e, sq=sq, c=c, ps_=ps_: e.matmul(ps_[:, :], lhsT=ones_bf[:], rhs=sq, start=(c == 0), stop=(c == KC - 1)),
                     R=[("sqb", c % 2), "ones_bf"], W=[pk])
            r_ = rstd[:, :]
            P.act(lambda e, ps_=ps_, r_=r_: e.activation(out=r_, in_=ps_[:, :], func=AF.Ln, scale=1.0 / D, bias=EPS),
                  R=[pk], W=["rstd"])
            P.act(lambda e, r_=r_: e.activation(out=r_, in_=r_, func=AF.Exp, scale=-0.5),
                  R=["rstd"], W=["rstd"])
            return r_, "rstd"

        def pre_norm(l, gofs):
            for j in range(NB):
                srcs = [xT[:, c, blk(j)] for c in range(KC)]
                keys = [("xT", c, j) for c in range(KC)]
                r_, rk = ss_and_rstd(srcs, keys, j % 2)
                for c in range(KC):
                    col = gofs + c
                    P.dve(lambda e, c=c, j=j, col=col, r_=r_: e.scalar_tensor_tensor(
                        out=hnT[:, c, blk(j)], in0=xT[:, c, blk(j)], scalar=pcol[:, col:col + 1], in1=r_,
                        op0=ALU.mult, op1=ALU.mult), R=[("xT", c, j), "pcol", rk], W=[("hnT", c, j)])

        def post_norm_residual(l, gofs, srcT, src_keyf, tok0, j_x):
            srcs = [srcT[:, c, :] for c in range(KC)]
            keys = [src_keyf(c) for c in range(KC)]
            r_, rk = ss_and_rstd(srcs, keys, j_x % 2)
            for c in range(KC):
                col = gofs + c
                P.dve(lambda e, c=c, col=col, r_=r_: e.scalar_tensor_tensor(
                    out=srcT[:, c, :], in0=srcT[:, c, :], scalar=pcol[:, col:col + 1], in1=r_,
                    op0=ALU.mult, op1=ALU.mult), R=[keys[c], "pcol", rk], W=[keys[c]])
                P.dve(lambda e, c=c: e.tensor_tensor(out=xT[:, c, tok0:tok0 + 512], in0=xT[:, c, tok0:tok0 + 512],
                                                     in1=srcT[:, c, :], op=ALU.add),
                      R=[keys[c], ("xT", c, j_x)], W=[("xT", c, j_x)])

        def proj_fm(wv, wkey, ncol_lo, m, evac):
            for j in range(NB):
                ps_, pk = bank()
                for c in range(KC):
                    P.pe(lambda e, c=c, j=j, ps_=ps_: e.matmul(ps_[0:m, :], lhsT=wv[:, c, ncol_lo:ncol_lo + m], rhs=hnT[:, c, blk(j)],
                                                               start=(c == 0), stop=(c == KC - 1)),
                         R=[wkey, ("hnT", c, j)], W=[pk])
                evac(j, ps_, pk)

        def proj_tm(wv, wkey, ncol_lo, n, per_bank, evac):
            for i0 in range(0, NT, per_bank):
                ps_, pk = bank()
                for ii in range(per_bank):
                    i = i0 + ii
                    for c in range(KC):
                        P.pe(lambda e, c=c, i=i, ii=ii, ps_=ps_: e.matmul(
                            ps_[:, ii * n:(ii + 1) * n], lhsT=hnT[:, c, til(i)], rhs=wv[:, c, ncol_lo:ncol_lo + n],
                            start=(c == 0), stop=(c == KC - 1)),
                            R=[wkey, ("hnT", c, i // 4)], W=[pk])
                evac(i0, ps_, pk)

        def head_out(i, p, grp, gate_t, gate_key):
            tt = tot[:, :]
            for hh in range(2):
                P.act(lambda e, hh=hh: e.activation(out=junk[:, hh * 128:(hh + 1) * 128], in_=tt[:, hh * 128:(hh + 1) * 128],
                                                    func=AF.Square, accum_out=sml[:, 0, hh:hh + 1]),
                      R=[("acc", 0)], W=[("sqb", 0), ("sml0", hh)])
            P.act(lambda e: e.activation(out=sml[:, 0, 2:4], in_=sml[:, 0, 0:2], func=AF.Ln, scale=1.0 / 128, bias=EPS),
                  R=[("sml0", 0), ("sml0", 1)], W=["sml0b"])
            P.act(lambda e: e.activation(out=sml[:, 0, 4:6], in_=sml[:, 0, 2:4], func=AF.Exp, scale=-0.5),
                  R=["sml0b"], W=["sml0c"])
            yt = ytile[:, i % 2, :]
            for hh in range(2):
                P.dve(lambda e, hh=hh, yt=yt: e.scalar_tensor_tensor(
                    out=yt[:, hh * 128:(hh + 1) * 128], in0=tt[:, hh * 128:(hh + 1) * 128], scalar=sml[:, 0, 4 + hh:5 + hh],
                    in1=gate_t[:, i, hh * 128:(hh + 1) * 128], op0=ALU.mult, op1=ALU.mult),
                    R=[("acc", 0), "sml0c", gate_key], W=[("sqb", 1)])
            pt_, ptk = tbank()
            for hh in range(2):
                P.pe(lambda e, hh=hh, yt=yt, pt_=pt_: e.transpose(pt_[:, hh * 128:(hh + 1) * 128], yt[:, hh * 128:(hh + 1) * 128], ident[:]),
                     R=[("sqb", 1), "ident"], W=[ptk])
            ch = grp * 4 + 2 * p
            for hh in range(2):
                dstv = yview(ch + hh)[:, til(i)]
                P.act(lambda e, pt_=pt_, dstv=dstv, hh=hh: e.copy(out=dstv, in_=pt_[:, hh * 128:(hh + 1) * 128]),
                      R=[ptk], W=[ykey(ch + hh, i)])

        def stage(name):
            if stop_at == name:
                raise _Stop()

        def layer(l):
            P.dma("sp", lambda e, l=l: e.dma_start(out=pcol[:], in_=pcol_d[:, l * PCOLS:(l + 1) * PCOLS]), W=["pcol"])
            P.dma("sp", lambda e, l=l: e.dma_start(out=pbc[:], in_=pbc_d[l:l + 1, :].partition_broadcast(128)), W=["pbc"])
            P.pool(lambda e: e.memset(w2bd[:], 0.0), W=["w2bd"])
            P.dma("pool", lambda e, l=l: e.dma_start(out=w2bd[0:16, 0:256], in_=w2_d[l, 0]), W=["w2bd"])
            P.dma("pool", lambda e, l=l: e.dma_start(out=w2bd[16:32, 256:512], in_=w2_d[l, 1]), W=["w2bd"])
            P.dma("pool", lambda e, l=l: e.dma_start(out=w2bd[32:33, :], in_=gb_d[l:l + 1, :]), W=["w2bd"])

            stage("params")
            pre_norm(l, 0)
            stage("prenorm")

            for p in range(2):
                h0 = 2 * p
                if p == 0:
                    (wv,), wk = load_w([wcols(w_in_d, l, 1424, 128)])
                    psg, pgk = bank()
                    for i in range(NT):
                        for c in range(KC):
                            P.pe(lambda e, c=c, i=i: e.matmul(psg[:, i * 16:(i + 1) * 16], lhsT=hnT[:, c, til(i)], rhs=wv[:, c, 112:128],
                                                              start=(c == 0), stop=(c == KC - 1)),
                                 R=[wk, ("hnT", c, i // 4)], W=[pgk])
                    P.dve(lambda e: e.tensor_tensor(out=gl[:], in0=psg[:, 0:256].rearrange("p (i g) -> p i g", i=NT),
                                                    in1=pbc[:, 0:16].unsqueeze(1).to_broadcast([128, NT, 16]), op=ALU.add),
                          R=[pgk, "pbc"], W=["gl"])
                    stage("g1")
                    for d in range(2):
                        fsl = gl[:, :, 4 + 8 * d:8 + 8 * d]
                        lfd = lf[:, d, :].rearrange("p (i h) -> p i h", i=NT)
                        P.act(lambda e, fsl=fsl, lfd=lfd: e.activation(out=lfd, in_=fsl, func=AF.Exp, scale=-1.0), R=["gl"], W=[("lf", d)])
                        P.act(lambda e, lfd=lfd: e.activation(out=lfd, in_=lfd, func=AF.Ln, bias=1.0), R=[("lf", d)], W=[("lf", d)])
                    stage("g2")
                    psb, pbk = bank()
                    for d in range(2):
                        P.pe(lambda e, d=d: e.matmul(psb[:, d * 64:(d + 1) * 64], lhsT=trif[:, d, :], rhs=lf[:, d, :], start=True, stop=True),
                             R=[("lf", d)] + CONSTS, W=[pbk])
                        P.pe(lambda e, d=d: e.matmul(psb[:, 128 + d * 64:128 + (d + 1) * 64], lhsT=negones_f[:], rhs=lf[:, d, :], start=True, stop=True),
                             R=[("lf", d)] + CONSTS, W=[pbk])
                    stage("g3")
                    P.act(lambda e: e.copy(out=bsb[:, :], in_=psb[:, 0:256]), R=[pbk], W=[("acc", 1)])
                    flat = lambda t_: t_[:].rearrange("p d i h -> p (d i h)")
                    P.act(lambda e: e.activation(out=flat(eb), in_=bsb[:, 0:128], func=AF.Exp), R=[("acc", 1)], W=["eb"])
                    P.act(lambda e: e.activation(out=flat(enb), in_=bsb[:, 0:128], func=AF.Exp, scale=-1.0), R=[("acc", 1)], W=["enb"])
                    P.act(lambda e: e.activation(out=flat(dec), in_=bsb[:, 128:256], func=AF.Exp), R=[("acc", 1)], W=["dec"])
                    for d in range(2):
                        isl = gl[:, :, 8 * d:8 * d + 4]
                        gt = gtmp[:, d, :].rearrange("p (i h) -> p i h", i=NT)
                        P.dve(lambda e, d=d, isl=isl, gt=gt: e.tensor_tensor(out=gt, in0=isl, in1=bsb[:, d * 64:(d + 1) * 64].rearrange("p (i h) -> p i h", i=NT),
                                                                             op=ALU.subtract), R=["gl", ("acc", 1)], W=[("gtmp", d)])
                        P.act(lambda e, d=d, gt=gt: e.activation(out=wsc[:, d].rearrange("p i h -> p (i h)"), in_=gtmp[:, d, :], func=AF.Exp), R=[("gtmp", d)], W=[("wsc", d)])
                        P.dve(lambda e, d=d: e.tensor_tensor(out=wdec[:, d], in0=wsc[:, d], in1=dec[:, d], op=ALU.mult),
                              R=[("wsc", d), "dec"], W=[("wdec", d)])
                stage("gates")
                for d in range(2):
                    for hh in range(2):
                        P.pool(lambda e, d=d, hh=hh: e.tensor_copy(out=decsel[hh * 64:(hh + 1) * 64, d, :], in_=dec[hh * 64:(hh + 1) * 64, d, :, h0 + hh]),
                               R=["dec"], W=[("decsel", hh)])
                (wq, wkk), wk = load_w([wcols(w_in_d, l, p * 128, 128), wcols(w_in_d, l, 256 + p * 128, 128)])
                for which, wv, dst in ((0, wq, qT), (1, wkk, kT)):
                    cj = which * 2 + p
                    cb = 32 + cj * 4
                    def qk_keys(j):
                        return [("qkraw", j)] + [("s5", ii) for ii in range(4 * j, min(NT, 4 * j + 5))]
                    P.pool(lambda e: e.memset(qkraw[:, 0:1], 0.0), W=qk_keys(0))
                    P.pool(lambda e: e.memset(qkraw[:, 2049:2050], 0.0), W=qk_keys(3))

                    def ev(j, ps_, pk):
                        P.act(lambda e, j=j, ps_=ps_: e.copy(out=qkraw[:, 1 + j * 512:1 + (j + 1) * 512], in_=ps_[:, :]),
                              R=[pk], W=qk_keys(j))
                    proj_fm(wv, wk, 0, 128, ev)
                    for j in range(NB):
                        a_ = acc[:, j % 2, :]
                        rk = []
                        for jj in range(max(0, j - 1), min(NB, j + 2)):
                            rk += qk_keys(jj)
                        P.dve(lambda e, j=j, a_=a_, cb=cb: e.tensor_scalar(out=a_, in0=qkraw[:, 1 + j * 512:1 + (j + 1) * 512],
                                                                           scalar1=pcol[:, cb + 1:cb + 2], scalar2=pcol[:, cb + 3:cb + 4],
                                                                           op0=ALU.mult, op1=ALU.add),
                              R=rk + ["pcol"], W=[("acc", j % 2)])
                        P.dve(lambda e, j=j, a_=a_, cb=cb: e.scalar_tensor_tensor(out=a_, in0=qkraw[:, j * 512:(j + 1) * 512],
                                                                                  scalar=pcol[:, cb:cb + 1], in1=a_, op0=ALU.mult, op1=ALU.add),
                              R=rk + ["pcol", ("acc", j % 2)], W=[("acc", j % 2)])
                        P.dve(lambda e, j=j, a_=a_, cb=cb: e.scalar_tensor_tensor(out=a_, in0=qkraw[:, 2 + j * 512:2 + (j + 1) * 512],
                                                                                  scalar=pcol[:, cb + 2:cb + 3], in1=a_, op0=ALU.mult, op1=ALU.add),
                              R=rk + ["pcol", ("acc", j % 2)], W=[("acc", j % 2)])
                        P.act(lambda e, j=j, a_=a_, dst=dst: e.activation(out=dst[:, blk(j)], in_=a_, func=AF.Silu),
                              R=[("acc", j % 2)], W=[("q" if which == 0 else "k", j)])
                stage("Aqk")
                for i in range(NT):
                    pt_, ptk = tbank()
                    P.pe(lambda e, i=i, pt_=pt_: e.transpose(pt_[:, 0:128], kT[:, til(i)], ident[:]), R=[("k", i // 4), "ident"], W=[ptk])
                    for d in range(2):
                        for hh in range(2):
                            P.dve(lambda e, i=i, d=d, hh=hh, pt_=pt_: e.tensor_scalar(
                                out=kw[:, d, i, hh, :], in0=pt_[:, hh * 64:(hh + 1) * 64], scalar1=wdec[:, d, i, h0 + hh:h0 + hh + 1], scalar2=0.125,
                                op0=ALU.mult, op1=ALU.mult), R=[ptk, ("wdec", d)], W=[("s2", i)])
                stage("Akw")
                for hh in range(2):
                    P.pool(lambda e, hh=hh: e.memset(vaug[:, :, hh, 128:130], 1.0), W=[("s3", i) for i in range(NT)])
                (wv,), wk = load_w([wcols(w_in_d, l, 512 + p * 256, 256)])

                def evv(i0, ps_, pk):
                    for ii in range(2):
                        for hh in range(2):
                            P.act(lambda e, i0=i0, ii=ii, hh=hh, ps_=ps_: e.copy(out=vaug[:, i0 + ii, hh, 0:128],
                                                                               in_=ps_[:, ii * 256 + hh * 128:ii * 256 + (hh + 1) * 128]),
                                  R=[pk], W=[("s3", i0 + ii)])
                proj_tm(wv, wk, 0, 256, 2, evv)
                (wv,), wk = load_w([wcols(w_in_d, l, 1024 + p * 256, 256)])

                def evo(i0, ps_, pk):
                    P.act(lambda e, i0=i0, ps_=ps_: e.activation(out=sigo[:, i0:i0 + 2, :].rearrange("p i e -> p (i e)"), in_=ps_[:, :],
                                                                func=AF.Sigmoid), R=[pk], W=[("s4", i0), ("s4", i0 + 1)])
                    P.pool(lambda e, i0=i0: e.tensor_tensor(out=sigo[:, i0:i0 + 2, :], in0=sigo[:, i0:i0 + 2, :],
                                                           in1=pbc[:, 16 + h0 * 128:16 + h0 * 128 + 256].unsqueeze(1).to_broadcast([128, 2, 256]),
                                                           op=ALU.mult), R=[("s4", i0), ("s4", i0 + 1), "pbc"], W=[("s4", i0), ("s4", i0 + 1)])
                proj_tm(wv, wk, 0, 256, 2, evo)

                stage("Aproj")
                orders = [list(range(NT)), list(range(NT - 1, -1, -1))]
                for d in range(2):
                    P.pool(lambda e, d=d: e.memset(C32[:, d, :], 0.0), W=[("C32", d)])
                    P.pool(lambda e, d=d: e.memset(Cbf[:, d, 0, :], 0.0), W=[("Cbf", d, 0)])

                def a_early(d, n):
                    i = orders[d][n]
                    par = n % 2
                    psSs = [bank(), bank()]
                    for hh in range(2):
                        rs = slice(hh * 64, (hh + 1) * 64)
                        psS, psk = psSs[hh]
                        P.pe(lambda e: e.matmul(psS[:, 0:128], lhsT=kT[rs, til(i)], rhs=qT[rs, til(i)], start=True, stop=True),
                             R=[("k", i // 4), ("q", i // 4)], W=[psk])
                    for hh in range(2):
                        psS, psk = psSs[hh]
                        P.dve(lambda e: e.scalar_tensor_tensor(
                            out=Sm[:, d, par, hh, :], in0=psS[:, 0:128], scalar=wsc[:, d, i, h0 + hh:h0 + hh + 1],
                            in1=maskA[:, d, :], op0=ALU.mult, op1=ALU.mult),
                            R=[psk, ("wsc", d)] + CONSTS, W=[("Sm", d, par, hh)])

                def a_main(d, n):
                    i = orders[d][n]
                    par = n % 2
                    psU, puk = bank()
                    P.pe(lambda e: e.matmul(psU[0:64, 0:130], lhsT=kw[:, d, i, 0, :], rhs=vaug[:, i, 0, 0:130], start=True, stop=True),
                         R=[("s2", i), ("s3", i)], W=[puk])
                    P.pe(lambda e: e.matmul(psU[64:128, 0:130], lhsT=kw[:, d, i, 1, :], rhs=vaug[:, i, 1, 0:130], start=True, stop=True,
                                            tile_position=(0, 64)),
                         R=[("s2", i), ("s3", i)], W=[puk])
                    psO, pok = bank()
                    for hh in range(2):
                        rs = slice(hh * 64, (hh + 1) * 64)
                        P.pe(lambda e: e.matmul(psO[:, hh * 130:hh * 130 + 130], lhsT=Sm[:, d, par, hh, :],
                                                rhs=vaug[:, i, hh, 0:130], start=True, stop=False),
                             R=[("Sm", d, par, hh), ("s3", i)], W=[pok])
                        P.pe(lambda e: e.matmul(psO[:, hh * 130:hh * 130 + 130], lhsT=qT[rs, til(i)],
                                                rhs=Cbf[rs, d, par, 0:130], start=False, stop=True),
                             R=[("q", i // 4), ("Cbf", d, par)], W=[pok])
                    P.dve(lambda e: e.scalar_tensor_tensor(
                        out=C32[:, d, 0:130], in0=C32[:, d, 0:130], scalar=decsel[:, d, i:i + 1], in1=psU[:, 0:130], op0=ALU.mult, op1=ALU.add),
                        R=[("C32", d), ("decsel", 0), ("decsel", 1), puk], W=[("C32", d)])
                    P.pool(lambda e: e.tensor_copy(out=Cbf[:, d, 1 - par, 0:130], in_=C32[:, d, 0:130]),
                           R=[("C32", d)], W=[("Cbf", d, 1 - par)])
                    ob = osb[:, d, :]
                    P.act(lambda e: e.copy(out=ob[:, 0:260], in_=psO[:, 0:260]), R=[pok], W=[("osb", d)])
                    sm = sml[:, 1 + d, :]
                    P.act(lambda e: e.activation(out=sm[:, 0:2], in_=ob[:, 0:260].rearrange("p (h e) -> p h e", h=2)[:, :, 128], func=AF.Abs),
                          R=[("osb", d)], W=[("sml1a", d)])
                    P.dve(lambda e: e.tensor_tensor(out=sm[:, 2:4], in0=sm[:, 0:2], in1=enb[:, d, i, h0:h0 + 2], op=ALU.max),
                          R=[("sml1a", d), "enb"], W=[("sml1b", d)])
                    P.dve(lambda e: e.reciprocal(out=sm[:, 4:6], in_=sm[:, 2:4]), R=[("sml1b", d)], W=[("sml1c", d)])
                    for hh in range(2):
                        if n < NT // 2:
                            P.act(lambda e: e.activation(
                                out=hsum[:, i, hh * 128:(hh + 1) * 128], in_=ob[:, hh * 130:hh * 130 + 128], func=AF.Copy, scale=sm[:, 4 + hh:5 + hh]),
                                R=[("osb", d), ("sml1c", d)], W=[("s5", i)])
                        else:
                            P.dve(lambda e: e.scalar_tensor_tensor(
                                out=tot[:, hh * 128:(hh + 1) * 128], in0=ob[:, hh * 130:hh * 130 + 128], scalar=sm[:, 4 + hh:5 + hh],
                                in1=hsum[:, i, hh * 128:(hh + 1) * 128], op0=ALU.mult, op1=ALU.add),
                                R=[("osb", d), ("sml1c", d), ("s5", i)], W=[("acc", 0)])
                    if n >= NT // 2:
                        head_out(i, p, 0, sigo, ("s4", i))

                for d in range(2):
                    a_early(d, 0)
                for n in range(NT):
                    for d in range(2):
                        if n + 1 < NT:
                            a_early(d, n + 1)
                        a_main(d, n)

            stage("A")
            for p in range(2):
                h0 = 2 * p
                if p == 0:
                    (wv,), wk = load_w([wcols(w_in_d, l, 2992, 128)])

                    def evl(j, ps_, pk):
                        P.act(lambda e, j=j, ps_=ps_: e.copy(out=lrT[0:32, blk(j)], in_=ps_[0:32, :]), R=[pk], W=[("lrT", j)])
                    proj_fm(wv, wk, 96, 32, evl)
                (wq, wkk), wk = load_w([wcols(w_in_d, l, 1552 + p * 128, 128), wcols(w_in_d, l, 1808 + p * 128, 128)])
                for which, wv, dst in ((0, wq, qT), (1, wkk, kT)):
                    def ev(j, ps_, pk, dst=dst, which=which):
                        P.act(lambda e, j=j, ps_=ps_: e.copy(out=dst[:, blk(j)], in_=ps_[:, :]), R=[pk], W=[("q" if which == 0 else "k", j)])
                    proj_fm(wv, wk, 0, 128, ev)
                (wv,), wk = load_w([wcols(w_in_d, l, 2064 + p * 256, 256)])

                def evv(i0, ps_, pk):
                    P.act(lambda e, i0=i0, ps_=ps_: e.copy(out=bv[:, i0:i0 + 2, :].rearrange("p i e -> p (i e)"), in_=ps_[:, :]),
                          R=[pk], W=[("s2", i0), ("s2", i0 + 1)])
                proj_tm(wv, wk, 0, 256, 2, evv)
                (wv,), wk = load_w([wcols(w_in_d, l, 2576 + p * 256, 256)])

                def evr(i0, ps_, pk):
                    P.act(lambda e, i0=i0, ps_=ps_: e.activation(out=sr[:, i0:i0 + 2, :].rearrange("p i e -> p (i e)"), in_=ps_[:, :],
                                                                func=AF.Silu), R=[pk], W=[("s3", i0), ("s3", i0 + 1)])
                    P.pool(lambda e, i0=i0: e.tensor_tensor(out=sr[:, i0:i0 + 2, :], in0=sr[:, i0:i0 + 2, :],
                                                           in1=pbc[:, 528 + h0 * 128:528 + h0 * 128 + 256].unsqueeze(1).to_broadcast([128, 2, 256]),
                                                           op=ALU.mult), R=[("s3", i0), ("s3", i0 + 1), "pbc"], W=[("s3", i0), ("s3", i0 + 1)])
                proj_tm(wv, wk, 0, 256, 2, evr)
                for i0 in range(0, NT, 2):
                    ps_, pk = bank()
                    for ii in range(2):
                        i = i0 + ii
                        for d in range(2):
                            P.pe(lambda e, i=i, ii=ii, d=d, ps_=ps_: e.matmul(ps_[:, ii * 256 + d * 128:ii * 256 + (d + 1) * 128], lhsT=lrT[0:33, til(i)],
                                                                              rhs=w2bd[0:33, d * 256 + p * 128:d * 256 + (p + 1) * 128], start=True, stop=True),
                                 R=[("lrT", i // 4), "lrT1", "w2bd"], W=[pk])
                    g_ = acc[:, (i0 // 2) % 2, :]
                    P.act(lambda e, ps_=ps_, g_=g_: e.activation(out=g_, in_=ps_[:, :], func=AF.Exp, scale=-1.0), R=[pk], W=[("acc", (i0 // 2) % 2)])
                    P.act(lambda e, i0=i0, g_=g_: e.activation(out=la[:, i0:i0 + 2, :].rearrange("p i e -> p (i e)"), in_=g_, func=AF.Ln, bias=1.0),
                          R=[("acc", (i0 // 2) % 2)], W=[("s4", i0), ("s4", i0 + 1)])
                stage("Bproj")
                orders = [list(range(NT)), list(range(NT - 1, -1, -1))]
                for d in range(2):
                    P.pool(lambda e, d=d: e.memset(C32[:, d, :], 0.0), W=[("C32", d)])
                    P.pool(lambda e, d=d: e.memset(Cbf[:, d, 0, :], 0.0), W=[("Cbf", d, 0)])

                def b_early(d, n):
                    i = orders[d][n]
                    par = n % 2
                    psB, pbk2 = bank()
                    P.pe(lambda e: e.matmul(psB[:, 0:128], lhsT=la[:, i, d * 128:(d + 1) * 128], rhs=tri16[:, d, :], start=True, stop=True),
                         R=[("s4", i)] + CONSTS, W=[pbk2])
                    P.act(lambda e: e.activation(out=eBt[:, d, par, :], in_=psB[:, 0:128], func=AF.Exp), R=[pbk2], W=[("eBt", d, par)])
                    P.act(lambda e: e.activation(out=eNBt[:, d, :], in_=psB[:, 0:128], func=AF.Exp, scale=-1.0), R=[pbk2], W=[("eNBt", d)])
                    P.pool(lambda e: e.tensor_tensor(out=qs[:, d, par, :], in0=qT[:, til(i)], in1=eBt[:, d, par, :], op=ALU.mult),
                           R=[("q", i // 4), ("eBt", d, par)], W=[("qs", d, par)])
                    P.dve(lambda e: e.tensor_tensor(out=ks[:, d, par, :], in0=kT[:, til(i)], in1=eNBt[:, d, :], op=ALU.mult),
                          R=[("k", i // 4), ("eNBt", d)], W=[("ks", d, par)])
                    psSs = [bank(), bank()]
                    for hh in range(2):
                        rs = slice(hh * 64, (hh + 1) * 64)
                        psS, psk = psSs[hh]
                        P.pe(lambda e: e.matmul(psS[:, 0:128], lhsT=ks[rs, d, par, :], rhs=qs[rs, d, par, :], start=True, stop=True),
                             R=[("ks", d, par), ("qs", d, par)], W=[psk])
                    pt_, ptk = tbank()
                    P.pe(lambda e: e.transpose(pt_[:, 0:128], ks[:, d, par, :], ident[:]), R=[("ks", d, par), "ident"], W=[ptk])
                    for hh in range(2):
                        psS, psk = psSs[hh]
                        P.dve(lambda e: e.tensor_tensor(out=Sm[:, d, par, hh, :], in0=psS[:, 0:128], in1=maskB[:, d, :], op=ALU.mult),
                              R=[psk] + CONSTS, W=[("Sm", d, par, hh)])
                    P.act(lambda e: e.activation(out=ktok[:, d, par, :], in_=pt_[:, 0:128], func=AF.Copy, scale=0.125), R=[ptk], W=[("ktok", d, par)])

                def b_main(d, n):
                    i = orders[d][n]
                    par = n % 2
                    lastcol = 127 if d == 0 else 0
                    psU, puk = bank()
                    P.pe(lambda e: e.matmul(psU[0:64, 0:128], lhsT=ktok[:, d, par, 0:64], rhs=bv[:, i, 0:128], start=True, stop=True),
                         R=[("ktok", d, par), ("s2", i)], W=[puk])
                    P.pe(lambda e: e.matmul(psU[64:128, 0:128], lhsT=ktok[:, d, par, 64:128], rhs=bv[:, i, 128:256], start=True, stop=True,
                                            tile_position=(0, 64)), R=[("ktok", d, par), ("s2", i)], W=[puk])
                    psO, pok = bank()
                    for hh in range(2):
                        rs = slice(hh * 64, (hh + 1) * 64)
                        P.pe(lambda e: e.matmul(psO[:, hh * 128:(hh + 1) * 128], lhsT=Sm[:, d, par, hh, :],
                                                rhs=bv[:, i, hh * 128:(hh + 1) * 128], start=True, stop=False),
                             R=[("Sm", d, par, hh), ("s2", i)], W=[pok])
                        P.pe(lambda e: e.matmul(psO[:, hh * 128:(hh + 1) * 128], lhsT=qs[rs, d, par, :],
                                                rhs=Cbf[rs, d, par, 0:128], start=False, stop=True),
                             R=[("qs", d, par), ("Cbf", d, par)], W=[pok])
                    P.act(lambda e: e.activation(out=tmpS[:, d, :], in_=psU[:, 0:128], func=AF.Copy, scale=eBt[:, d, par, lastcol:lastcol + 1]),
                          R=[puk, ("eBt", d, par)], W=[("tmpS", d)])
                    P.dve(lambda e: e.scalar_tensor_tensor(out=C32[:, d, 0:128], in0=C32[:, d, 0:128], scalar=eBt[:, d, par, lastcol:lastcol + 1],
                                                           in1=tmpS[:, d, :], op0=ALU.mult, op1=ALU.add),
                          R=[("tmpS", d), ("eBt", d, par), ("C32", d)], W=[("C32", d)])
                    P.pool(lambda e: e.tensor_copy(out=Cbf[:, d, 1 - par, 0:128], in_=C32[:, d, 0:128]),
                           R=[("C32", d)], W=[("Cbf", d, 1 - par)])
                    if n < NT // 2:
                        P.act(lambda e: e.copy(out=hsum[:, i, :], in_=psO[:, 0:256]), R=[pok], W=[("s5", i)])
                    else:
                        P.dve(lambda e: e.tensor_tensor(out=tot[:, :], in0=psO[:, 0:256], in1=hsum[:, i, :], op=ALU.add),
                              R=[pok, ("s5", i)], W=[("acc", 0)])
                        head_out(i, p, 1, sr, ("s3", i))

                for d in range(2):
                    b_early(d, 0)
                for n in range(NT):
                    for d in range(2):
                        if n + 1 < NT:
                            b_early(d, n + 1)
                        b_main(d, n)

            stage("B")
            if dbg and "yT" in dbg and l == 0:
                P.barrier()
                for c in range(KC):
                    P.act(lambda e, c=c: e.copy(out=xT[:, c, :], in_=yT[:, c, :]), R=[], W=[("xT", c, j) for j in range(NB)])
                P.barrier()

            P.barrier()
            for q4 in range(4):
                P.dma("pool", lambda e, q4=q4, l=l: e.dma_start(out=wo[:, :, q4 * 256:(q4 + 1) * 256],
                                                                in_=w_out_d[l, :, q4 * 256:(q4 + 1) * 256].rearrange("(c p) n -> p c n", p=128)),
                      W=["wo"])
            for j in range(NB):
                for co in range(KC):
                    ps_, pk = bank()
                    for ch in range(KC):
                        P.pe(lambda e, co=co, ch=ch, j=j, ps_=ps_: e.matmul(ps_[:, :], lhsT=wo[:, ch, co * 128:(co + 1) * 128], rhs=yview(ch)[:, blk(j)],
                                                                            start=(ch == 0), stop=(ch == KC - 1)),
                             R=["wo"] + [ykey(ch, i) for i in range(4 * j, 4 * j + 4)], W=[pk])
                    P.act(lambda e, co=co, ps_=ps_: e.copy(out=mixT[:, co, :], in_=ps_[:, :]), R=[pk], W=[("mixT", co)])
                post_norm_residual(l, 8, mixT, lambda c: ("mixT", c), j * 512, j)

            stage("wout")
            pre_norm(l, 16)
            P.pool(lambda e: e.tensor_copy(out=hhalo[:, :, :], in_=hnT[:, :, 1023:1025]),
                   R=[("hnT", c, j) for c in range(KC) for j in (1, 2)], W=["hhalo"])
            P.barrier()
            ffT_bf = arena[:, NF * 1024:NF * 1024 + 2 * KC * 512]
            gu_pool = [(wbuf[0][:, :], [("wbuf", 0)]), (wbuf[1][:, :], [("wbuf", 1)])] + \
                      [(ffT_bf[:, j * 2048:(j + 1) * 2048], [("ffT", 2 * j), ("ffT", 2 * j + 1)]) for j in range(4)]
            gurot = 0
            for H in range(2):
                t0 = H * 1024
                for f in range(NF):
                    (wgv, wuv), wks = load_into(gu_pool[gurot % len(gu_pool)], [wcols(wg_d, l, f * 128, 128), wcols(wu_d, l, f * 128, 128)])
                    gurot += 1
                    cb = 48 + f * 4
                    if H == 0:
                        P.pool(lambda e: e.memset(graw[:, 0:1], 0.0), W=[("graw", 0)])
                        halo_tok, halo_col, hsel = 1024, 1025, 1
                    else:
                        P.pool(lambda e: e.memset(graw[:, 1025:1026], 0.0), W=[("graw", 1)])
                        halo_tok, halo_col, hsel = 1023, 0, 0
                    psh, phk = bank()
                    for c in range(KC):
                        P.pe(lambda e, c=c, psh=psh: e.matmul(psh[:, 0:1], lhsT=wgv[:, c, :], rhs=hhalo[:, c, hsel:hsel + 1],
                                                              start=(c == 0), stop=(c == KC - 1)), R=wks + ["hhalo"], W=[phk])
                    P.act(lambda e, psh=psh, halo_col=halo_col: e.copy(out=graw[:, halo_col:halo_col + 1], in_=psh[:, 0:1]),
                          R=[phk], W=[("graw", 0 if halo_col == 0 else 1)])
                    for b in range(2):
                        jj = 2 * H + b
                        ps_, pk = bank()
                        for c in range(KC):
                            P.pe(lambda e, c=c, jj=jj, ps_=ps_: e.matmul(ps_[:, :], lhsT=wgv[:, c, :], rhs=hnT[:, c, blk(jj)],
                                                                         start=(c == 0), stop=(c == KC - 1)), R=wks + [("hnT", c, jj)], W=[pk])
                        P.act(lambda e, b=b, ps_=ps_: e.copy(out=graw[:, 1 + b * 512:1 + (b + 1) * 512], in_=ps_[:, :]), R=[pk], W=[("grawb", b)])
                    for b in range(2):
                        jj = 2 * H + b
                        a_ = acc[:, b, :]
                        rk = [("graw", 0), ("graw", 1), ("grawb", 0), ("grawb", 1)]
                        P.dve(lambda e, b=b, a_=a_, cb=cb: e.tensor_scalar(out=a_, in0=graw[:, 1 + b * 512:1 + (b + 1) * 512],
                                                                           scalar1=pcol[:, cb + 1:cb + 2], scalar2=pcol[:, cb + 3:cb + 4],
                                                                           op0=ALU.mult, op1=ALU.add), R=rk + ["pcol"], W=[("acc", b)])
                        P.dve(lambda e, b=b, a_=a_, cb=cb: e.scalar_tensor_tensor(out=a_, in0=graw[:, b * 512:(b + 1) * 512],
                                                                                  scalar=pcol[:, cb:cb + 1], in1=a_, op0=ALU.mult, op1=ALU.add),
                              R=rk + ["pcol", ("acc", b)], W=[("acc", b)])
                        P.dve(lambda e, b=b, a_=a_, cb=cb: e.scalar_tensor_tensor(out=a_, in0=graw[:, 2 + b * 512:2 + (b + 1) * 512],
                                                                                  scalar=pcol[:, cb + 2:cb + 3], in1=a_, op0=ALU.mult, op1=ALU.add),
                              R=rk + ["pcol", ("acc", b)], W=[("acc", b)])
                        P.act(lambda e, b=b, a_=a_: e.activation(out=gel[:, b, :], in_=a_, func=AF.Gelu_apprx_tanh), R=[("acc", b)], W=[("gel", b)])
                        ps_, pk = bank()
                        for c in range(KC):
                            P.pe(lambda e, c=c, jj=jj, ps_=ps_: e.matmul(ps_[:, :], lhsT=wuv[:, c, :], rhs=hnT[:, c, blk(jj)],
                                                                         start=(c == 0), stop=(c == KC - 1)), R=wks + [("hnT", c, jj)], W=[pk])
                        P.dve(lambda e, b=b, f=f, ps_=ps_: e.tensor_tensor(out=actT[:, f, b * 512:(b + 1) * 512], in0=gel[:, b, :], in1=ps_[:, :], op=ALU.mult),
                              R=[("gel", b), pk], W=[("actT", f, b)])
                d_pool = [(hnT[:, c, t0:t0 + 1024], [("hnT", c, 2 * H), ("hnT", c, 2 * H + 1)]) for c in range(KC)]
                drot = 0
                for b in range(2):
                    for co in range(KC):
                        parts = []
                        for (fa, fb) in ((0, 8), (8, 16), (16, 22)):
                            (wv_,), wk_ = load_into(d_pool[drot % KC], [wd_d[l, fa * 128:fb * 128, co * 128:(co + 1) * 128].rearrange("(f p) n -> p f n", p=128)])
                            drot += 1
                            parts.append((fa, fb, wv_, wk_))
                        ps_, pk = bank()
                        for f in range(NF):
                            fa, fb, wdv, wk_ = parts[f // 8]
                            P.pe(lambda e: e.matmul(ps_[:, :], lhsT=wdv[:, f - fa, :], rhs=actT[:, f, b * 512:(b + 1) * 512],
                                                    start=(f == 0), stop=(f == NF - 1)), R=wk_ + [("actT", f, b)], W=[pk])
                        P.act(lambda e, co=co, ps_=ps_: e.copy(out=ffT[:, co, :], in_=ps_[:, :]), R=[pk], W=[("ffT", co)])
                    post_norm_residual(l, 24, ffT, lambda c: ("ffT", c), t0 + b * 512, 2 * H + b)
            P.barrier()

        try:
            stage("load")
            for l in range(nl):
                layer(l)
        except _Stop:
            pass
        for c in range(KC):
            P.dma("sp", lambda e, c=c: e.dma_start(out=outT_d[c * 128:(c + 1) * 128, :], in_=xT[:, c, :]),
                  R=[("xT", c, j) for j in range(NB)], W=[("out", c)])
        P.add("sp", None, R=[("out", c) for c in range(KC)])
        P.emit(nc)
    return nc


def _pack_params(ls, norm_mix_pre, norm_mix_post, norm_ffn_pre, norm_ffn_post, mlstm_conv_w, mlstm_conv_b,
                 ffn_conv_w, ffn_conv_b, mlstm_gate_b, mlstm_norm, gla_norm):
    nl = len(ls)
    pcol = np.zeros((128, nl * PCOLS), np.float32)
    pbc = np.zeros((nl, PBC), np.float32)
    for li, l in enumerate(ls):
        o = li * PCOLS
        for gi, g in enumerate((norm_mix_pre, norm_mix_post, norm_ffn_pre, norm_ffn_post)):
            pcol[:, o + gi * 8:o + gi * 8 + 8] = g[l].reshape(8, 128).T
        for cj in range(4):
            pcol[:, o + 32 + cj * 4:o + 32 + cj * 4 + 3] = mlstm_conv_w[l][:, cj * 128:(cj + 1) * 128].T
            pcol[:, o + 32 + cj * 4 + 3] = mlstm_conv_b[l][cj * 128:(cj + 1) * 128]
        for f in range(NF):
            pcol[:, o + 48 + f * 4:o + 48 + f * 4 + 3] = ffn_conv_w[l][:, f * 128:(f + 1) * 128].T
            pcol[:, o + 48 + f * 4 + 3] = ffn_conv_b[l][f * 128:(f + 1) * 128]
        pbc[li, 0:16] = mlstm_gate_b[l]
        pbc[li, 16:528] = mlstm_norm[l].reshape(-1)
        pbc[li, 528:1040] = gla_norm[l].reshape(-1)
    return pcol, pbc


_CACHE = {}


def _get_prog(nl):
    if nl not in _CACHE:
        _CACHE[nl] = build_program(nl)
    return _CACHE[nl]


FUSED = True


def kernel(x, norm_mix_pre, norm_mix_post, norm_ffn_pre, norm_ffn_post, w_in, mlstm_gate_b, mlstm_conv_w,
           mlstm_conv_b, mlstm_norm, gla_w2, gla_b, gla_norm, w_out, ffn_w_gate, ffn_w_up, ffn_conv_w,
           ffn_conv_b, ffn_w_down):
    f = lambda a: np.ascontiguousarray(np.asarray(a), dtype=np.float32)
    x = f(x)
    args = [f(a) for a in (norm_mix_pre, norm_mix_post, norm_ffn_pre, norm_ffn_post, mlstm_conv_w, mlstm_conv_b,
                           ffn_conv_w, ffn_conv_b, mlstm_gate_b, mlstm_norm, gla_norm)]
    w_in, w_out, wg, wu, wd = f(w_in), f(w_out), f(ffn_w_gate), f(ffn_w_up), f(ffn_w_down)
    w2, gb = f(gla_w2), f(gla_b)
    xTs = [np.ascontiguousarray(x[b].T) for b in range(NCORES)]
    groups = [list(range(DEPTH))] if FUSED else [[l] for l in range(DEPTH)]
    for ls in groups:
        nl = len(ls)
        nc = _get_prog(nl)
        pcol, pbc = _pack_params(ls, *args)
        sl = slice(ls[0], ls[-1] + 1)
        shared = {"w_in": w_in[sl], "w_out": w_out[sl], "wg": wg[sl], "wu": wu[sl], "wd": wd[sl], "pcol": pcol, "pbc": pbc,
                  "w2": w2[sl], "gb": np.ascontiguousarray(gb[sl].reshape(nl, 512))}
        in_maps = [dict(shared, xT=xTs[b]) for b in range(NCORES)]
        res = run_bass_kernel_spmd(nc, in_maps, core_ids=list(range(NCORES)))
        xTs = [np.asarray(r["outT"]) for r in res.results]
    return np.stack([np.ascontiguousarray(t.T) for t in xTs], axis=0).astype(np.float32)
```

```python
import contextlib
import types
import numpy as np
import concourse.bass as bass
import concourse.mybir as mybir
from concourse.bass_utils import run_bass_kernel_spmd

F32 = mybir.dt.float32
BF16 = mybir.dt.bfloat16
ALU = mybir.AluOpType
AF = mybir.ActivationFunctionType

EPOCH = 30000
HD1, HD2 = 1, 2
NCORES = 8
DEPTH = 4
D = 1024
S = 2048
NT = 16
NB = 4
KC = 8
FF = 2816
NF = 22
INC = 3120
EPS = 1e-6
PCOLS = 136
PBC = 1040


def _freeze(fn):
    if fn is None or fn.__closure__ is None:
        return fn
    cells = []
    for c in fn.__closure__:
        try:
            cells.append(types.CellType(c.cell_contents))
        except ValueError:
            cells.append(c)
    return types.FunctionType(fn.__code__, fn.__globals__, fn.__name__, fn.__defaults__, tuple(cells))


class _Op:
    __slots__ = ("eng", "fn", "R", "W", "dma", "deps", "marked", "tick", "dsem", "dtick", "dprev")

    def __init__(self, eng, fn, R, W, dma):
        self.eng = eng
        self.fn = fn
        self.R = tuple(R)
        self.W = tuple(W)
        self.dma = dma
        self.deps = ()
        self.marked = False
        self.tick = 0
        self.dsem = -1
        self.dtick = 0
        self.dprev = None


class Prog:
    ENGS = ("pe", "act", "dve", "pool", "sp")

    def __init__(self, n_dma_sems=16):
        self.ops = []
        self.nds = n_dma_sems

    def add(self, eng, fn, R=(), W=(), dma=False):
        self.ops.append(_Op(eng, _freeze(fn), R, W, dma))

    def pe(self, fn, R=(), W=()):
        self.add("pe", fn, R, W)

    def act(self, fn, R=(), W=()):
        self.add("act", fn, R, W)

    def dve(self, fn, R=(), W=()):
        self.add("dve", fn, R, W)

    def pool(self, fn, R=(), W=()):
        self.add("pool", fn, R, W)

    def dma(self, eng, fn, R=(), W=()):
        self.add(eng, fn, R, W, dma=True)

    def barrier(self):
        self.ops.append(_Op("__barrier__", None, (), (), False))

    def analyze(self):
        last_w = {}
        readers = {}
        last_on_eng = {e: None for e in self.ENGS}
        pend = {e: None for e in self.ENGS}
        out = []
        for op in self.ops:
            if op.eng == "__barrier__":
                snap = [v for v in last_on_eng.values() if v is not None]
                for e in self.ENGS:
                    pend[e] = snap
                continue
            g = len(out)
            deps = set()
            for k in op.R:
                if k in last_w:
                    deps.add(last_w[k])
            for k in op.W:
                if k in last_w:
                    deps.add(last_w[k])
                for r in readers.get(k, ()):
                    deps.add(r)
            if pend[op.eng] is not None:
                deps.update(pend[op.eng])
                pend[op.eng] = None
            for k in op.R:
                readers.setdefault(k, []).append(g)
            for k in op.W:
                last_w[k] = g
                readers[k] = []
            deps.discard(g)
            op.deps = tuple(sorted(deps))
            out.append(op)
            if op.fn is not None:
                last_on_eng[op.eng] = g
        self.lin = out
        ndma = 0
        nq = [0, 0]
        last_dma_on_sem = {}
        for g, op in enumerate(out):
            for d in op.deps:
                p = out[d]
                if p.dma:
                    continue
                if p.eng == "pe" and op.eng == "pe" and not op.dma:
                    continue
                p.marked = True
            if op.dma:
                half = self.nds // 2
                qi = 0 if op.eng == "sp" else 1
                s = qi * half + (nq[qi] % half)
                nq[qi] += 1
                ndma += 1
                op.dsem = s
                prev = last_dma_on_sem.get(s)
                op.dprev = prev
                op.dtick = (out[prev].dtick if prev is not None else 0) + 16
                last_dma_on_sem[s] = g
        cnt = {e: 0 for e in self.ENGS}
        for op in out:
            if op.marked and not op.dma:
                cnt[op.eng] += 1
                op.tick = cnt[op.eng]
        self.cnt = cnt
        self.ndma = ndma

    def emit(self, nc):
        self.analyze()
        out = self.lin
        with contextlib.ExitStack() as st:
            esems = {}
            for e in self.ENGS:
                nep = self.cnt[e] // EPOCH + 1
                esems[e] = [st.enter_context(nc.semaphore(f"s_{e}_{i}")) for i in range(nep)]
            dsems = [st.enter_context(nc.semaphore(f"s_dma_{i}")) for i in range(self.nds)]
            block = st.enter_context(nc.Block())
            by_eng = {e: [] for e in self.ENGS}
            for g, op in enumerate(out):
                by_eng[op.eng].append((g, op))

            def sem_of(p):
                if p.dma:
                    return ("d", p.dsem), dsems[p.dsem], p.dtick
                ep = (p.tick - 1) // EPOCH
                return (p.eng, ep), esems[p.eng][ep], p.tick - ep * EPOCH

            def run(eng_name, e):
                waited = {}
                for g, op in by_eng[eng_name]:
                    waits = {}
                    deps = list(op.deps)
                    if op.dma and op.dprev is not None:
                        deps.append(op.dprev)
                    for d in deps:
                        p = out[d]
                        if (not p.dma) and p.eng == "pe" and eng_name == "pe" and not op.dma:
                            continue
                        key, sem, val = sem_of(p)
                        if waited.get(key, 0) >= val:
                            continue
                        if key not in waits or waits[key][1] < val:
                            waits[key] = (sem, val)
                    for key, (sem, val) in waits.items():
                        e.wait_ge(sem, val)
                        waited[key] = val
                    if op.fn is None:
                        continue
                    ins = op.fn(e)
                    if op.dma:
                        ins.then_inc(dsems[op.dsem], 16)
                    elif op.marked:
                        ep = (op.tick - 1) // EPOCH
                        ins.then_inc(esems[eng_name][ep], 1)

            @block.tensor
            def _(e):
                run("pe", e)

            @block.scalar
            def _(e):
                run("act", e)

            @block.vector
            def _(e):
                run("dve", e)

            @block.gpsimd
            def _(e):
                run("pool", e)

            @block.sync
            def _(e):
                run("sp", e)


class _Stop(Exception):
    pass


def build_program(nl, dbg=None, stop_at=None):
    nc = bass.Bass("TRN2", target_bir_lowering=False)
    P = Prog()

    def din(name, shape):
        return nc.dram_tensor(name, shape, F32, kind="ExternalInput").ap()

    xT_d = din("xT", [D, S])
    w_in_d = din("w_in", [nl, D, INC])
    w_out_d = din("w_out", [nl, D, D])
    wg_d = din("wg", [nl, D, FF])
    wu_d = din("wu", [nl, D, FF])
    wd_d = din("wd", [nl, FF, D])
    pcol_d = din("pcol", [128, nl * PCOLS])
    pbc_d = din("pbc", [nl, PBC])
    w2_d = din("w2", [nl, 2, 16, 256])
    gb_d = din("gb", [nl, 512])
    outT_d = nc.dram_tensor("outT", [D, S], F32, kind="ExternalOutput").ap()
    dbg_d = {}
    if dbg:
        for k, shp in dbg.items():
            dbg_d[k] = nc.dram_tensor("dbg_" + k, shp, F32, kind="ExternalOutput").ap()

    st = contextlib.ExitStack()
    with st:
        def sb(name, shape, dt):
            return st.enter_context(nc.sbuf_tensor(name, shape, dt))

        def psum(name, shape, dt):
            return st.enter_context(nc.psum_tensor(name, shape, dt))

        xT = sb("xT_sb", [128, KC, S], F32)
        hnT = sb("hnT", [128, KC, S], BF16)
        pcol = sb("pcol_sb", [128, PCOLS], F32)
        pbc = sb("pbc_sb", [128, PBC], F32)
        w2bd = sb("w2bd", [128, 512], BF16)
        lrT = sb("lrT", [128, S], BF16)
        ones_bf = sb("ones_bf", [128, 128], BF16)
        negones_f = sb("negones_f", [128, 128], F32)
        ident = sb("ident", [128, 128], BF16)
        maskA = sb("maskA", [128, 2, 128], BF16)
        maskB = sb("maskB", [128, 2, 128], BF16)
        tri16 = sb("tri16", [128, 2, 128], BF16)
        trif = sb("trif", [128, 2, 128], F32)
        WB = 2048
        wbuf = [sb(f"wbuf{i}", [128, WB], BF16) for i in range(2)]
        A_YT = 0
        A_Q = A_YT + 6 * S
        A_K = A_Q + S
        A_2 = A_K + S
        A_3 = A_2 + 4096
        A_4 = A_3 + 4160
        A_5 = A_4 + 4096
        A_END = A_5 + 4100
        arena = sb("arena", [128, A_END], BF16)
        yT = arena[:, A_YT:A_Q].rearrange("p (c t) -> p c t", c=6)
        qT = arena[:, A_Q:A_K]
        kT = arena[:, A_K:A_2]
        kw = arena[:, A_2:A_3].rearrange("p (d i h e) -> p d i h e", d=2, i=NT, h=2)
        bv = arena[:, A_2:A_3].rearrange("p (i e) -> p i e", i=NT)
        vaug = arena[:, A_3:A_4].rearrange("p (i h e) -> p i h e", i=NT, h=2)
        sr = arena[:, A_3:A_3 + 4096].rearrange("p (i e) -> p i e", i=NT)
        sigo = arena[:, A_4:A_5].rearrange("p (i e) -> p i e", i=NT)
        la = arena[:, A_4:A_5].rearrange("p (i e) -> p i e", i=NT)
        hsum = arena[:, A_5:A_5 + 4096].rearrange("p (i e) -> p i e", i=NT)
        qkraw = arena[:, A_5:A_END].bitcast(F32)
        wo = arena[:, A_Q:A_Q + KC * D].rearrange("p (c n) -> p c n", c=KC)
        mixT = arena[:, A_Q + KC * D:A_Q + KC * D + 2 * KC * 512].bitcast(F32).rearrange("p (c t) -> p c t", c=KC)
        actT = arena[:, 0:NF * 1024].rearrange("p (f t) -> p f t", f=NF)
        ffT = arena[:, NF * 1024:NF * 1024 + 2 * KC * 512].bitcast(F32).rearrange("p (c t) -> p c t", c=KC)
        graw = arena[:, NF * 1024 + 2 * KC * 512:NF * 1024 + 2 * KC * 512 + 2 * 1026].bitcast(F32)
        assert NF * 1024 + 2 * KC * 512 + 2 * 1026 <= A_END

        gl = sb("gl", [128, NT, 16], F32)
        lf = sb("lf", [128, 2, NT * 4], F32)
        gtmp = sb("gtmp", [128, 2, NT * 4], F32)
        eb = sb("eb", [128, 2, NT, 4], F32)
        enb = sb("enb", [128, 2, NT, 4], F32)
        wsc = sb("wsc", [128, 2, NT, 4], F32)
        dec = sb("dec", [128, 2, NT, 4], F32)
        wdec = sb("wdec", [128, 2, NT, 4], F32)
        decsel = sb("decsel", [128, 2, NT], F32)
        Sm = sb("Sm", [128, 2, 2, 2, 128], BF16)
        C32 = sb("C32", [128, 2, 132], F32)
        Cbf = sb("Cbf", [128, 2, 2, 132], BF16)
        eBt = sb("eBt", [128, 2, 2, 128], F32)
        eNBt = sb("eNBt", [128, 2, 128], F32)
        qs = sb("qs", [128, 2, 2, 128], BF16)
        ks = sb("ks", [128, 2, 2, 128], BF16)
        ktok = sb("ktok", [128, 2, 2, 128], BF16)
        tmpS = sb("tmpS", [128, 2, 128], F32)
        osb = sb("osb", [128, 2, 260], F32)
        sml = sb("sml", [128, 5, 16], F32)
        sqb = sb("sqb", [128, 2, 512], BF16)
        hhalo = sb("hhalo", [128, KC, 2], BF16)
        rstd = sb("rstd", [128, 512], F32)
        acc = sb("acc", [128, 2, 512], F32)
        bsb = acc[:, 1, 0:256]
        gel = Sm[:].rearrange("p a b c t -> p (a b c t)").rearrange("p (b t) -> p b t", b=2)
        tot2 = acc[:, :, 0:256]
        junk2 = sqb[:, 0, :].rearrange("p (q h t) -> p q h t", q=2, h=2)
        ytile = sqb[:, 1, :].rearrange("p (b t) -> p b t", b=2)
        orders_g = [list(range(NT)), list(range(NT - 1, -1, -1))]

        pb = [psum(f"pb{i}", [128, 512], F32) for i in range(6)]
        ptb = [psum(f"ptb{i}", [128, 1024], BF16) for i in range(2)]
        rot = {"n": 0, "t": 0}

        def bank():
            i = rot["n"] % 6
            rot["n"] += 1
            return pb[i], ("ps", i)

        def tbank():
            i = rot["t"] % 2
            rot["t"] += 1
            return ptb[i], ("pt", i)

        wrot = {"n": 0}

        def load_into(bufspec, segs):
            ap2d, keys = bufspec
            views = []
            off = 0
            for src in segs:
                k, n = src.shape[1], src.shape[2]
                v = ap2d[:, off:off + k * n].rearrange("p (k n) -> p k n", k=k)
                P.dma("pool", lambda e: e.dma_start(out=v, in_=src), W=keys)
                views.append(v)
                off += k * n
            assert off <= ap2d.shape[1]
            return views, list(keys)

        def load_w(segs):
            i = wrot["n"] % 2
            wrot["n"] += 1
            views, _ = load_into((wbuf[i][:, :], [("wbuf", i)]), segs)
            return views, ("wbuf", i)

        def wcols(wd3, l, a, n, kc=KC):
            return wd3[l, :, a:a + n].rearrange("(c p) n -> p c n", p=128)

        P.pool(lambda e: e.memset(ones_bf[:], 1.0), W=["ones_bf"])
        P.pool(lambda e: e.memset(negones_f[:], -1.0), W=["negones_f"])
        P.pool(lambda e: e.memset(ident[:], 0.0), W=["ident"])
        P.pool(lambda e: e.affine_select(out=ident[:], in_=ident[:], pattern=[[-1, 128]], compare_op=ALU.not_equal,
                                         fill=1.0, base=0, channel_multiplier=1), R=["ident"], W=["ident"])
        for t_, val in ((maskA, 0.125), (maskB, 0.125), (tri16, -1.0 / 16.0), (trif, -1.0)):
            P.pool(lambda e, t_=t_, val=val: e.memset(t_[:], val), W=[("const", id(t_))])
            P.pool(lambda e, t_=t_: e.affine_select(out=t_[:, 0, :], in_=t_[:, 0, :], pattern=[[1, 128]], compare_op=ALU.is_ge,
                                                    fill=0.0, base=0, channel_multiplier=-1), R=[("const", id(t_))], W=[("const", id(t_))])
            P.pool(lambda e, t_=t_: e.affine_select(out=t_[:, 1, :], in_=t_[:, 1, :], pattern=[[-1, 128]], compare_op=ALU.is_ge,
                                                    fill=0.0, base=0, channel_multiplier=1), R=[("const", id(t_))], W=[("const", id(t_))])
        CONSTS = ["ones_bf", "negones_f", "ident"] + [("const", id(t_)) for t_ in (maskA, maskB, tri16, trif)]
        P.pool(lambda e: e.memset(lrT[32:33, :], 1.0), W=["lrT1"])
        for c in range(KC):
            P.dma("sp", lambda e, c=c: e.dma_start(out=xT[:, c, :], in_=xT_d[c * 128:(c + 1) * 128, :]),
                  W=[("xT", c, j) for j in range(NB)])

        def blk(j):
            return slice(j * 512, (j + 1) * 512)

        def ykey(ch, i):
            return ("yT", ch, i) if ch < 6 else ("hnT", ch - 6, i // 4)

        def yview(ch):
            return yT[:, ch, :] if ch < 6 else hnT[:, ch - 6, :]

        def til(i):
            return slice(i * 128, (i + 1) * 128)

        def ss_and_rstd(srcs, src_keys, nparity):
            ps_, pk = bank()
            for c in range(KC):
                sq = sqb[:, c % 2, :]
                P.act(lambda e, sq=sq, s_=srcs[c]: e.activation(out=sq, in_=s_, func=AF.Square),
                      R=[src_keys[c]], W=[("sqb", c % 2)])
                P.pe(lambda e, sq=sq, c=c, ps_=ps_: e.matmul(ps_[:, :], lhsT=ones_bf[:], rhs=sq, start=(c == 0), stop=(c == KC - 1)),
                     R=[("sqb", c % 2), "ones_bf"], W=[pk])
            r_ = rstd[:, :]
            P.act(lambda e, ps_=ps_, r_=r_: e.activation(out=r_, in_=ps_[:, :], func=AF.Ln, scale=1.0 / D, bias=EPS),
                  R=[pk], W=["rstd"])
            P.act(lambda e, r_=r_: e.activation(out=r_, in_=r_, func=AF.Exp, scale=-0.5),
                  R=["rstd"], W=["rstd"])
            return r_, "rstd"

        def pre_norm(l, gofs):
            for j in range(NB):
                srcs = [xT[:, c, blk(j)] for c in range(KC)]
                keys = [("xT", c, j) for c in range(KC)]
                r_, rk = ss_and_rstd(srcs, keys, j % 2)
                for c in range(KC):
                    col = gofs + c
                    P.dve(lambda e, c=c, j=j, col=col, r_=r_: e.scalar_tensor_tensor(
                        out=hnT[:, c, blk(j)], in0=xT[:, c, blk(j)], scalar=pcol[:, col:col + 1], in1=r_,
                        op0=ALU.mult, op1=ALU.mult), R=[("xT", c, j), "pcol", rk], W=[("hnT", c, j)])

        def post_norm_residual(l, gofs, srcT, src_keyf, tok0, j_x):
            srcs = [srcT[:, c, :] for c in range(KC)]
            keys = [src_keyf(c) for c in range(KC)]
            r_, rk = ss_and_rstd(srcs, keys, j_x % 2)
            for c in range(KC):
                col = gofs + c
                P.dve(lambda e, c=c, col=col, r_=r_: e.scalar_tensor_tensor(
                    out=srcT[:, c, :], in0=srcT[:, c, :], scalar=pcol[:, col:col + 1], in1=r_,
                    op0=ALU.mult, op1=ALU.mult), R=[keys[c], "pcol", rk], W=[keys[c]])
                P.dve(lambda e, c=c: e.tensor_tensor(out=xT[:, c, tok0:tok0 + 512], in0=xT[:, c, tok0:tok0 + 512],
                                                     in1=srcT[:, c, :], op=ALU.add),
                      R=[keys[c], ("xT", c, j_x)], W=[("xT", c, j_x)])

        def proj_fm(wv, wkey, ncol_lo, m, evac):
            for j in range(NB):
                ps_, pk = bank()
                for c in range(KC):
                    P.pe(lambda e, c=c, j=j, ps_=ps_: e.matmul(ps_[0:m, :], lhsT=wv[:, c, ncol_lo:ncol_lo + m], rhs=hnT[:, c, blk(j)],
                                                               start=(c == 0), stop=(c == KC - 1)),
                         R=[wkey, ("hnT", c, j)], W=[pk])
                evac(j, ps_, pk)

        def proj_tm(wv, wkey, ncol_lo, n, per_bank, evac):
            for i0 in range(0, NT, per_bank):
                ps_, pk = bank()
                for ii in range(per_bank):
                    i = i0 + ii
                    for c in range(KC):
                        P.pe(lambda e, c=c, i=i, ii=ii, ps_=ps_: e.matmul(
                            ps_[:, ii * n:(ii + 1) * n], lhsT=hnT[:, c, til(i)], rhs=wv[:, c, ncol_lo:ncol_lo + n],
                            start=(c == 0), stop=(c == KC - 1)),
                            R=[wkey, ("hnT", c, i // 4)], W=[pk])
                evac(i0, ps_, pk)

        def head1(i, q, gate_t, gate_key):
            tt = tot2[:, q, :]
            sq_ = sml[:, 3 + q, :]
            for hh in range(2):
                P.act(lambda e: e.activation(out=junk2[:, q, hh, :],
                                             in_=tt[:, hh * 128:(hh + 1) * 128], func=AF.Square, accum_out=sq_[:, hh:hh + 1]),
                      R=[("acc", q)], W=[("junk", q, hh), ("sml0", q, hh)])
            P.act(lambda e: e.activation(out=sq_[:, 2:4], in_=sq_[:, 0:2], func=AF.Ln, scale=1.0 / 128, bias=EPS),
                  R=[("sml0", q, 0), ("sml0", q, 1)], W=[("sml0b", q)])
            P.act(lambda e: e.activation(out=sq_[:, 4:6], in_=sq_[:, 2:4], func=AF.Exp, scale=-0.5),
                  R=[("sml0b", q)], W=[("sml0c", q)])
            yt = ytile[:, q, :]
            for hh in range(2):
                P.dve(lambda e: e.scalar_tensor_tensor(
                    out=yt[:, hh * 128:(hh + 1) * 128], in0=tt[:, hh * 128:(hh + 1) * 128], scalar=sq_[:, 4 + hh:5 + hh],
                    in1=gate_t[:, i, hh * 128:(hh + 1) * 128], op0=ALU.mult, op1=ALU.mult),
                    R=[("acc", q), ("sml0c", q), gate_key], W=[("ytile", q)])

        def head2(i, q, p, grp):
            yt = ytile[:, q, :]
            pt_, ptk = tbank()
            for hh in range(2):
                P.pe(lambda e: e.transpose(pt_[:, hh * 128:(hh + 1) * 128], yt[:, hh * 128:(hh + 1) * 128], ident[:]),
                     R=[("ytile", q), "ident"], W=[ptk])
            ch = grp * 4 + 2 * p
            for hh in range(2):
                dstv = yview(ch + hh)[:, til(i)]
                P.act(lambda e: e.copy(out=dstv, in_=pt_[:, hh * 128:(hh + 1) * 128]), R=[ptk], W=[ykey(ch + hh, i)])

        def run_pipeline(core, tail, p, grp, gate_t, gate_key_fn, hd1, hd2, td=1):
            its = [(n, d) for n in range(NT) for d in range(2)]
            nit = len(its)
            seconds = []
            for it in range(nit + 2 + hd2):
                if it < nit:
                    core(*its[it])
                if 0 <= it - td < nit:
                    n, d = its[it - td]
                    q = tail(n, d)
                    if q is not None:
                        seconds.append((it - 1, orders_g[d][n], q))
                for (jt, ti, q) in seconds:
                    if jt == it - 1 - hd1:
                        head1(ti, q, gate_t, gate_key_fn(ti))
                for (jt, ti, q) in seconds:
                    if jt == it - 1 - hd2:
                        head2(ti, q, p, grp)

        def stage(name):
            if stop_at == name:
                raise _Stop()

        def layer(l):
            P.dma("sp", lambda e, l=l: e.dma_start(out=pcol[:], in_=pcol_d[:, l * PCOLS:(l + 1) * PCOLS]), W=["pcol"])
            P.dma("sp", lambda e, l=l: e.dma_start(out=pbc[:], in_=pbc_d[l:l + 1, :].partition_broadcast(128)), W=["pbc"])
            P.pool(lambda e: e.memset(w2bd[:], 0.0), W=["w2bd"])
            P.dma("pool", lambda e, l=l: e.dma_start(out=w2bd[0:16, 0:256], in_=w2_d[l, 0]), W=["w2bd"])
            P.dma("pool", lambda e, l=l: e.dma_start(out=w2bd[16:32, 256:512], in_=w2_d[l, 1]), W=["w2bd"])
            P.dma("pool", lambda e, l=l: e.dma_start(out=w2bd[32:33, :], in_=gb_d[l:l + 1, :]), W=["w2bd"])

            stage("params")
            pre_norm(l, 0)
            stage("prenorm")

            for p in range(2):
                h0 = 2 * p
                if p == 0:
                    (wv,), wk = load_w([wcols(w_in_d, l, 1424, 128)])
                    psg, pgk = bank()
                    for i in range(NT):
                        for c in range(KC):
                            P.pe(lambda e, c=c, i=i: e.matmul(psg[:, i * 16:(i + 1) * 16], lhsT=hnT[:, c, til(i)], rhs=wv[:, c, 112:128],
                                                              start=(c == 0), stop=(c == KC - 1)),
                                 R=[wk, ("hnT", c, i // 4)], W=[pgk])
                    P.dve(lambda e: e.tensor_tensor(out=gl[:], in0=psg[:, 0:256].rearrange("p (i g) -> p i g", i=NT),
                                                    in1=pbc[:, 0:16].unsqueeze(1).to_broadcast([128, NT, 16]), op=ALU.add),
                          R=[pgk, "pbc"], W=["gl"])
                    stage("g1")
                    for d in range(2):
                        fsl = gl[:, :, 4 + 8 * d:8 + 8 * d]
                        lfd = lf[:, d, :].rearrange("p (i h) -> p i h", i=NT)
                        P.act(lambda e, fsl=fsl, lfd=lfd: e.activation(out=lfd, in_=fsl, func=AF.Exp, scale=-1.0), R=["gl"], W=[("lf", d)])
                        P.act(lambda e, lfd=lfd: e.activation(out=lfd, in_=lfd, func=AF.Ln, bias=1.0), R=[("lf", d)], W=[("lf", d)])
                    stage("g2")
                    psb, pbk = bank()
                    for d in range(2):
                        P.pe(lambda e, d=d: e.matmul(psb[:, d * 64:(d + 1) * 64], lhsT=trif[:, d, :], rhs=lf[:, d, :], start=True, stop=True),
                             R=[("lf", d)] + CONSTS, W=[pbk])
                        P.pe(lambda e, d=d: e.matmul(psb[:, 128 + d * 64:128 + (d + 1) * 64], lhsT=negones_f[:], rhs=lf[:, d, :], start=True, stop=True),
                             R=[("lf", d)] + CONSTS, W=[pbk])
                    stage("g3")
                    P.act(lambda e: e.copy(out=bsb[:, :], in_=psb[:, 0:256]), R=[pbk], W=[("acc", 1)])
                    flat = lambda t_: t_[:].rearrange("p d i h -> p (d i h)")
                    P.act(lambda e: e.activation(out=flat(eb), in_=bsb[:, 0:128], func=AF.Exp), R=[("acc", 1)], W=["eb"])
                    P.act(lambda e: e.activation(out=flat(enb), in_=bsb[:, 0:128], func=AF.Exp, scale=-1.0), R=[("acc", 1)], W=["enb"])
                    P.act(lambda e: e.activation(out=flat(dec), in_=bsb[:, 128:256], func=AF.Exp), R=[("acc", 1)], W=["dec"])
                    for d in range(2):
                        isl = gl[:, :, 8 * d:8 * d + 4]
                        gt = gtmp[:, d, :].rearrange("p (i h) -> p i h", i=NT)
                        P.dve(lambda e, d=d, isl=isl, gt=gt: e.tensor_tensor(out=gt, in0=isl, in1=bsb[:, d * 64:(d + 1) * 64].rearrange("p (i h) -> p i h", i=NT),
                                                                             op=ALU.subtract), R=["gl", ("acc", 1)], W=[("gtmp", d)])
                        P.act(lambda e, d=d, gt=gt: e.activation(out=wsc[:, d].rearrange("p i h -> p (i h)"), in_=gtmp[:, d, :], func=AF.Exp), R=[("gtmp", d)], W=[("wsc", d)])
                        P.dve(lambda e, d=d: e.tensor_tensor(out=wdec[:, d], in0=wsc[:, d], in1=dec[:, d], op=ALU.mult),
                              R=[("wsc", d), "dec"], W=[("wdec", d)])
                stage("gates")
                for d in range(2):
                    for hh in range(2):
                        P.pool(lambda e, d=d, hh=hh: e.tensor_copy(out=decsel[hh * 64:(hh + 1) * 64, d, :], in_=dec[hh * 64:(hh + 1) * 64, d, :, h0 + hh]),
                               R=["dec"], W=[("decsel", hh)])
                (wq, wkk), wk = load_w([wcols(w_in_d, l, p * 128, 128), wcols(w_in_d, l, 256 + p * 128, 128)])
                for which, wv, dst in ((0, wq, qT), (1, wkk, kT)):
                    cj = which * 2 + p
                    cb = 32 + cj * 4
                    def qk_keys(j):
                        return [("qkraw", j)] + [("s5", ii) for ii in range(4 * j, min(NT, 4 * j + 5))]
                    P.pool(lambda e: e.memset(qkraw[:, 0:1], 0.0), W=qk_keys(0))
                    P.pool(lambda e: e.memset(qkraw[:, 2049:2050], 0.0), W=qk_keys(3))

                    def ev(j, ps_, pk):
                        P.act(lambda e, j=j, ps_=ps_: e.copy(out=qkraw[:, 1 + j * 512:1 + (j + 1) * 512], in_=ps_[:, :]),
                              R=[pk], W=qk_keys(j))
                    proj_fm(wv, wk, 0, 128, ev)
                    for j in range(NB):
                        a_ = acc[:, j % 2, :]
                        rk = []
                        for jj in range(max(0, j - 1), min(NB, j + 2)):
                            rk += qk_keys(jj)
                        P.dve(lambda e, j=j, a_=a_, cb=cb: e.tensor_scalar(out=a_, in0=qkraw[:, 1 + j * 512:1 + (j + 1) * 512],
                                                                           scalar1=pcol[:, cb + 1:cb + 2], scalar2=pcol[:, cb + 3:cb + 4],
                                                                           op0=ALU.mult, op1=ALU.add),
                              R=rk + ["pcol"], W=[("acc", j % 2)])
                        P.dve(lambda e, j=j, a_=a_, cb=cb: e.scalar_tensor_tensor(out=a_, in0=qkraw[:, j * 512:(j + 1) * 512],
                                                                                  scalar=pcol[:, cb:cb + 1], in1=a_, op0=ALU.mult, op1=ALU.add),
                              R=rk + ["pcol", ("acc", j % 2)], W=[("acc", j % 2)])
                        P.dve(lambda e, j=j, a_=a_, cb=cb: e.scalar_tensor_tensor(out=a_, in0=qkraw[:, 2 + j * 512:2 + (j + 1) * 512],
                                                                                  scalar=pcol[:, cb + 2:cb + 3], in1=a_, op0=ALU.mult, op1=ALU.add),
                              R=rk + ["pcol", ("acc", j % 2)], W=[("acc", j % 2)])
                        P.act(lambda e, j=j, a_=a_, dst=dst: e.activation(out=dst[:, blk(j)], in_=a_, func=AF.Silu),
                              R=[("acc", j % 2)], W=[("q" if which == 0 else "k", j)])
                stage("Aqk")
                for i in range(NT):
                    pt_, ptk = tbank()
                    P.pe(lambda e, i=i, pt_=pt_: e.transpose(pt_[:, 0:128], kT[:, til(i)], ident[:]), R=[("k", i // 4), "ident"], W=[ptk])
                    for d in range(2):
                        for hh in range(2):
                            P.dve(lambda e, i=i, d=d, hh=hh, pt_=pt_: e.tensor_scalar(
                                out=kw[:, d, i, hh, :], in0=pt_[:, hh * 64:(hh + 1) * 64], scalar1=wdec[:, d, i, h0 + hh:h0 + hh + 1], scalar2=0.125,
                                op0=ALU.mult, op1=ALU.mult), R=[ptk, ("wdec", d)], W=[("s2", i)])
                stage("Akw")
                for hh in range(2):
                    P.pool(lambda e, hh=hh: e.memset(vaug[:, :, hh, 128:130], 1.0), W=[("s3", i) for i in range(NT)])
                (wv,), wk = load_w([wcols(w_in_d, l, 512 + p * 256, 256)])

                def evv(i0, ps_, pk):
                    for ii in range(2):
                        for hh in range(2):
                            P.act(lambda e, i0=i0, ii=ii, hh=hh, ps_=ps_: e.copy(out=vaug[:, i0 + ii, hh, 0:128],
                                                                               in_=ps_[:, ii * 256 + hh * 128:ii * 256 + (hh + 1) * 128]),
                                  R=[pk], W=[("s3", i0 + ii)])
                proj_tm(wv, wk, 0, 256, 2, evv)
                (wv,), wk = load_w([wcols(w_in_d, l, 1024 + p * 256, 256)])

                def evo(i0, ps_, pk):
                    P.act(lambda e, i0=i0, ps_=ps_: e.activation(out=sigo[:, i0:i0 + 2, :].rearrange("p i e -> p (i e)"), in_=ps_[:, :],
                                                                func=AF.Sigmoid), R=[pk], W=[("s4", i0), ("s4", i0 + 1)])
                    P.pool(lambda e, i0=i0: e.tensor_tensor(out=sigo[:, i0:i0 + 2, :], in0=sigo[:, i0:i0 + 2, :],
                                                           in1=pbc[:, 16 + h0 * 128:16 + h0 * 128 + 256].unsqueeze(1).to_broadcast([128, 2, 256]),
                                                           op=ALU.mult), R=[("s4", i0), ("s4", i0 + 1), "pbc"], W=[("s4", i0), ("s4", i0 + 1)])
                proj_tm(wv, wk, 0, 256, 2, evo)

                stage("Aproj")
                orders = [list(range(NT)), list(range(NT - 1, -1, -1))]
                for d in range(2):
                    P.pool(lambda e, d=d: e.memset(C32[:, d, :], 0.0), W=[("C32", d)])
                    P.pool(lambda e, d=d: e.memset(Cbf[:, d, 0, :], 0.0), W=[("Cbf", d, 0)])

                def a_early(d, n):
                    i = orders[d][n]
                    par = n % 2
                    psSs = [bank(), bank()]
                    for hh in range(2):
                        rs = slice(hh * 64, (hh + 1) * 64)
                        psS, psk = psSs[hh]
                        P.pe(lambda e: e.matmul(psS[:, 0:128], lhsT=kT[rs, til(i)], rhs=qT[rs, til(i)], start=True, stop=True),
                             R=[("k", i // 4), ("q", i // 4)], W=[psk])
                    for hh in range(2):
                        psS, psk = psSs[hh]
                        P.dve(lambda e: e.scalar_tensor_tensor(
                            out=Sm[:, d, par, hh, :], in0=psS[:, 0:128], scalar=wsc[:, d, i, h0 + hh:h0 + hh + 1],
                            in1=maskA[:, d, :], op0=ALU.mult, op1=ALU.mult),
                            R=[psk, ("wsc", d)] + CONSTS, W=[("Sm", d, par, hh)])

                def a_main(d, n):
                    i = orders[d][n]
                    par = n % 2
                    psU, puk = bank()
                    P.pe(lambda e: e.matmul(psU[0:64, 0:130], lhsT=kw[:, d, i, 0, :], rhs=vaug[:, i, 0, 0:130], start=True, stop=True),
                         R=[("s2", i), ("s3", i)], W=[puk])
                    P.pe(lambda e: e.matmul(psU[64:128, 0:130], lhsT=kw[:, d, i, 1, :], rhs=vaug[:, i, 1, 0:130], start=True, stop=True,
                                            tile_position=(0, 64)),
                         R=[("s2", i), ("s3", i)], W=[puk])
                    psO, pok = bank()
                    for hh in range(2):
                        rs = slice(hh * 64, (hh + 1) * 64)
                        P.pe(lambda e: e.matmul(psO[:, hh * 130:hh * 130 + 130], lhsT=Sm[:, d, par, hh, :],
                                                rhs=vaug[:, i, hh, 0:130], start=True, stop=False),
                             R=[("Sm", d, par, hh), ("s3", i)], W=[pok])
                        P.pe(lambda e: e.matmul(psO[:, hh * 130:hh * 130 + 130], lhsT=qT[rs, til(i)],
                                                rhs=Cbf[rs, d, par, 0:130], start=False, stop=True),
                             R=[("q", i // 4), ("Cbf", d, par)], W=[pok])
                    P.dve(lambda e: e.scalar_tensor_tensor(
                        out=C32[:, d, 0:130], in0=C32[:, d, 0:130], scalar=decsel[:, d, i:i + 1], in1=psU[:, 0:130], op0=ALU.mult, op1=ALU.add),
                        R=[("C32", d), ("decsel", 0), ("decsel", 1), puk], W=[("C32", d)])
                    P.pool(lambda e: e.tensor_copy(out=Cbf[:, d, 1 - par, 0:130], in_=C32[:, d, 0:130]),
                           R=[("C32", d)], W=[("Cbf", d, 1 - par)])
                    ob = osb[:, d, :]
                    P.act(lambda e: e.copy(out=ob[:, 0:260], in_=psO[:, 0:260]), R=[pok], W=[("osb", d)])
                    sm = sml[:, 1 + d, :]
                    P.act(lambda e: e.activation(out=sm[:, 0:2], in_=ob[:, 0:260].rearrange("p (h e) -> p h e", h=2)[:, :, 128], func=AF.Abs),
                          R=[("osb", d)], W=[("sml1a", d)])

                def a_tail(n, d):
                    i = orders[d][n]
                    ob = osb[:, d, :]
                    sm = sml[:, 1 + d, :]
                    q = None
                    if n >= NT // 2:
                        q = acnt[0] % 2
                        acnt[0] += 1
                    P.dve(lambda e: e.tensor_tensor(out=sm[:, 2:4], in0=sm[:, 0:2], in1=enb[:, d, i, h0:h0 + 2], op=ALU.max),
                          R=[("sml1a", d), "enb"], W=[("sml1b", d)])
                    P.dve(lambda e: e.reciprocal(out=sm[:, 4:6], in_=sm[:, 2:4]), R=[("sml1b", d)], W=[("sml1c", d)])
                    for hh in range(2):
                        if n < NT // 2:
                            P.act(lambda e: e.activation(
                                out=hsum[:, i, hh * 128:(hh + 1) * 128], in_=ob[:, hh * 130:hh * 130 + 128], func=AF.Copy, scale=sm[:, 4 + hh:5 + hh]),
                                R=[("osb", d), ("sml1c", d)], W=[("s5", i)])
                        else:
                            P.dve(lambda e: e.scalar_tensor_tensor(
                                out=tot2[:, q, hh * 128:(hh + 1) * 128], in0=ob[:, hh * 130:hh * 130 + 128], scalar=sm[:, 4 + hh:5 + hh],
                                in1=hsum[:, i, hh * 128:(hh + 1) * 128], op0=ALU.mult, op1=ALU.add),
                                R=[("osb", d), ("sml1c", d), ("s5", i)], W=[("acc", q)])
                    return q

                acnt = [0]

                def a_core(n, d):
                    if n == 0:
                        a_early(d, 0)
                    if n + 1 < NT:
                        a_early(d, n + 1)
                    a_main(d, n)

                run_pipeline(a_core, a_tail, p, 0, sigo, lambda ti: ("s4", ti), hd1=0, hd2=1, td=0)

            stage("A")
            for p in range(2):
                h0 = 2 * p
                if p == 0:
                    (wv,), wk = load_w([wcols(w_in_d, l, 2992, 128)])

                    def evl(j, ps_, pk):
                        P.act(lambda e, j=j, ps_=ps_: e.copy(out=lrT[0:32, blk(j)], in_=ps_[0:32, :]), R=[pk], W=[("lrT", j)])
                    proj_fm(wv, wk, 96, 32, evl)
                (wq, wkk), wk = load_w([wcols(w_in_d, l, 1552 + p * 128, 128), wcols(w_in_d, l, 1808 + p * 128, 128)])
                for which, wv, dst in ((0, wq, qT), (1, wkk, kT)):
                    def ev(j, ps_, pk, dst=dst, which=which):
                        P.act(lambda e, j=j, ps_=ps_: e.copy(out=dst[:, blk(j)], in_=ps_[:, :]), R=[pk], W=[("q" if which == 0 else "k", j)])
                    proj_fm(wv, wk, 0, 128, ev)
                (wv,), wk = load_w([wcols(w_in_d, l, 2064 + p * 256, 256)])

                def evv(i0, ps_, pk):
                    P.act(lambda e, i0=i0, ps_=ps_: e.copy(out=bv[:, i0:i0 + 2, :].rearrange("p i e -> p (i e)"), in_=ps_[:, :]),
                          R=[pk], W=[("s2", i0), ("s2", i0 + 1)])
                proj_tm(wv, wk, 0, 256, 2, evv)
                (wv,), wk = load_w([wcols(w_in_d, l, 2576 + p * 256, 256)])

                def evr(i0, ps_, pk):
                    P.act(lambda e, i0=i0, ps_=ps_: e.activation(out=sr[:, i0:i0 + 2, :].rearrange("p i e -> p (i e)"), in_=ps_[:, :],
                                                                func=AF.Silu), R=[pk], W=[("s3", i0), ("s3", i0 + 1)])
                    P.pool(lambda e, i0=i0: e.tensor_tensor(out=sr[:, i0:i0 + 2, :], in0=sr[:, i0:i0 + 2, :],
                                                           in1=pbc[:, 528 + h0 * 128:528 + h0 * 128 + 256].unsqueeze(1).to_broadcast([128, 2, 256]),
                                                           op=ALU.mult), R=[("s3", i0), ("s3", i0 + 1), "pbc"], W=[("s3", i0), ("s3", i0 + 1)])
                proj_tm(wv, wk, 0, 256, 2, evr)
                for i0 in range(0, NT, 2):
                    ps_, pk = bank()
                    for ii in range(2):
                        i = i0 + ii
                        for d in range(2):
                            P.pe(lambda e, i=i, ii=ii, d=d, ps_=ps_: e.matmul(ps_[:, ii * 256 + d * 128:ii * 256 + (d + 1) * 128], lhsT=lrT[0:33, til(i)],
                                                                              rhs=w2bd[0:33, d * 256 + p * 128:d * 256 + (p + 1) * 128], start=True, stop=True),
                                 R=[("lrT", i // 4), "lrT1", "w2bd"], W=[pk])
                    g_ = acc[:, (i0 // 2) % 2, :]
                    P.act(lambda e, ps_=ps_, g_=g_: e.activation(out=g_, in_=ps_[:, :], func=AF.Exp, scale=-1.0), R=[pk], W=[("acc", (i0 // 2) % 2)])
                    P.act(lambda e, i0=i0, g_=g_: e.activation(out=la[:, i0:i0 + 2, :].rearrange("p i e -> p (i e)"), in_=g_, func=AF.Ln, bias=1.0),
                          R=[("acc", (i0 // 2) % 2)], W=[("s4", i0), ("s4", i0 + 1)])
                stage("Bproj")
                orders = [list(range(NT)), list(range(NT - 1, -1, -1))]
                for d in range(2):
                    P.pool(lambda e, d=d: e.memset(C32[:, d, :], 0.0), W=[("C32", d)])
                    P.pool(lambda e, d=d: e.memset(Cbf[:, d, 0, :], 0.0), W=[("Cbf", d, 0)])

                def b_early(d, n):
                    i = orders[d][n]
                    par = n % 2
                    psB, pbk2 = bank()
                    P.pe(lambda e: e.matmul(psB[:, 0:128], lhsT=la[:, i, d * 128:(d + 1) * 128], rhs=tri16[:, d, :], start=True, stop=True),
                         R=[("s4", i)] + CONSTS, W=[pbk2])
                    P.act(lambda e: e.activation(out=eBt[:, d, par, :], in_=psB[:, 0:128], func=AF.Exp), R=[pbk2], W=[("eBt", d, par)])
                    P.act(lambda e: e.activation(out=eNBt[:, d, :], in_=psB[:, 0:128], func=AF.Exp, scale=-1.0), R=[pbk2], W=[("eNBt", d)])
                    P.pool(lambda e: e.tensor_tensor(out=qs[:, d, par, :], in0=qT[:, til(i)], in1=eBt[:, d, par, :], op=ALU.mult),
                           R=[("q", i // 4), ("eBt", d, par)], W=[("qs", d, par)])
                    P.dve(lambda e: e.tensor_tensor(out=ks[:, d, par, :], in0=kT[:, til(i)], in1=eNBt[:, d, :], op=ALU.mult),
                          R=[("k", i // 4), ("eNBt", d)], W=[("ks", d, par)])
                    psSs = [bank(), bank()]
                    for hh in range(2):
                        rs = slice(hh * 64, (hh + 1) * 64)
                        psS, psk = psSs[hh]
                        P.pe(lambda e: e.matmul(psS[:, 0:128], lhsT=ks[rs, d, par, :], rhs=qs[rs, d, par, :], start=True, stop=True),
                             R=[("ks", d, par), ("qs", d, par)], W=[psk])
                    pt_, ptk = tbank()
                    P.pe(lambda e: e.transpose(pt_[:, 0:128], ks[:, d, par, :], ident[:]), R=[("ks", d, par), "ident"], W=[ptk])
                    for hh in range(2):
                        psS, psk = psSs[hh]
                        P.dve(lambda e: e.tensor_tensor(out=Sm[:, d, par, hh, :], in0=psS[:, 0:128], in1=maskB[:, d, :], op=ALU.mult),
                              R=[psk] + CONSTS, W=[("Sm", d, par, hh)])
                    P.act(lambda e: e.activation(out=ktok[:, d, par, :], in_=pt_[:, 0:128], func=AF.Copy, scale=0.125), R=[ptk], W=[("ktok", d, par)])

                def b_main(d, n):
                    i = orders[d][n]
                    par = n % 2
                    lastcol = 127 if d == 0 else 0
                    psU, puk = bank()
                    P.pe(lambda e: e.matmul(psU[0:64, 0:128], lhsT=ktok[:, d, par, 0:64], rhs=bv[:, i, 0:128], start=True, stop=True),
                         R=[("ktok", d, par), ("s2", i)], W=[puk])
                    P.pe(lambda e: e.matmul(psU[64:128, 0:128], lhsT=ktok[:, d, par, 64:128], rhs=bv[:, i, 128:256], start=True, stop=True,
                                            tile_position=(0, 64)), R=[("ktok", d, par), ("s2", i)], W=[puk])
                    psO, pok = bank()
                    for hh in range(2):
                        rs = slice(hh * 64, (hh + 1) * 64)
                        P.pe(lambda e: e.matmul(psO[:, hh * 128:(hh + 1) * 128], lhsT=Sm[:, d, par, hh, :],
                                                rhs=bv[:, i, hh * 128:(hh + 1) * 128], start=True, stop=False),
                             R=[("Sm", d, par, hh), ("s2", i)], W=[pok])
                        P.pe(lambda e: e.matmul(psO[:, hh * 128:(hh + 1) * 128], lhsT=qs[rs, d, par, :],
                                                rhs=Cbf[rs, d, par, 0:128], start=False, stop=True),
                             R=[("qs", d, par), ("Cbf", d, par)], W=[pok])
                    P.act(lambda e: e.activation(out=tmpS[:, d, :], in_=psU[:, 0:128], func=AF.Copy, scale=eBt[:, d, par, lastcol:lastcol + 1]),
                          R=[puk, ("eBt", d, par)], W=[("tmpS", d)])
                    P.dve(lambda e: e.scalar_tensor_tensor(out=C32[:, d, 0:128], in0=C32[:, d, 0:128], scalar=eBt[:, d, par, lastcol:lastcol + 1],
                                                           in1=tmpS[:, d, :], op0=ALU.mult, op1=ALU.add),
                          R=[("tmpS", d), ("eBt", d, par), ("C32", d)], W=[("C32", d)])
                    P.pool(lambda e: e.tensor_copy(out=Cbf[:, d, 1 - par, 0:128], in_=C32[:, d, 0:128]),
                           R=[("C32", d)], W=[("Cbf", d, 1 - par)])
                    if n < NT // 2:
                        P.act(lambda e: e.copy(out=hsum[:, i, :], in_=psO[:, 0:256]), R=[pok], W=[("s5", i)])
                    else:
                        q = bcnt[0] % 2
                        bcnt[0] += 1
                        bq[(n, d)] = q
                        P.dve(lambda e: e.tensor_tensor(out=tot2[:, q, :], in0=psO[:, 0:256], in1=hsum[:, i, :], op=ALU.add),
                              R=[pok, ("s5", i)], W=[("acc", q)])

                bcnt = [0]
                bq = {}

                def b_core(n, d):
                    if n == 0:
                        b_early(d, 0)
                    if n + 1 < NT:
                        b_early(d, n + 1)
                    b_main(d, n)

                run_pipeline(b_core, lambda n, d: bq.get((n, d)), p, 1, sr, lambda ti: ("s3", ti), hd1=0, hd2=1, td=0)

            stage("B")
            if dbg and "yT" in dbg and l == 0:
                P.barrier()
                for c in range(KC):
                    P.act(lambda e, c=c: e.copy(out=xT[:, c, :], in_=yT[:, c, :]), R=[], W=[("xT", c, j) for j in range(NB)])
                P.barrier()

            P.barrier()
            for q4 in range(4):
                P.dma("pool", lambda e, q4=q4, l=l: e.dma_start(out=wo[:, :, q4 * 256:(q4 + 1) * 256],
                                                                in_=w_out_d[l, :, q4 * 256:(q4 + 1) * 256].rearrange("(c p) n -> p c n", p=128)),
                      W=["wo"])
            for j in range(NB):
                for co in range(KC):
                    ps_, pk = bank()
                    for ch in range(KC):
                        P.pe(lambda e, co=co, ch=ch, j=j, ps_=ps_: e.matmul(ps_[:, :], lhsT=wo[:, ch, co * 128:(co + 1) * 128], rhs=yview(ch)[:, blk(j)],
                                                                            start=(ch == 0), stop=(ch == KC - 1)),
                             R=["wo"] + [ykey(ch, i) for i in range(4 * j, 4 * j + 4)], W=[pk])
                    P.act(lambda e, co=co, ps_=ps_: e.copy(out=mixT[:, co, :], in_=ps_[:, :]), R=[pk], W=[("mixT", co)])
                post_norm_residual(l, 8, mixT, lambda c: ("mixT", c), j * 512, j)

            stage("wout")
            pre_norm(l, 16)
            P.pool(lambda e: e.tensor_copy(out=hhalo[:, :, :], in_=hnT[:, :, 1023:1025]),
                   R=[("hnT", c, j) for c in range(KC) for j in (1, 2)], W=["hhalo"])
            P.barrier()
            ffT_bf = arena[:, NF * 1024:NF * 1024 + 2 * KC * 512]
            gu_pool = [(wbuf[0][:, :], [("wbuf", 0)]), (wbuf[1][:, :], [("wbuf", 1)])] + \
                      [(ffT_bf[:, j * 2048:(j + 1) * 2048], [("ffT", 2 * j), ("ffT", 2 * j + 1)]) for j in range(4)]
            gurot = 0
            for H in range(2):
                t0 = H * 1024
                for f in range(NF):
                    (wgv, wuv), wks = load_into(gu_pool[gurot % len(gu_pool)], [wcols(wg_d, l, f * 128, 128), wcols(wu_d, l, f * 128, 128)])
                    gurot += 1
                    cb = 48 + f * 4
                    if H == 0:
                        P.pool(lambda e: e.memset(graw[:, 0:1], 0.0), W=[("graw", 0)])
                        halo_tok, halo_col, hsel = 1024, 1025, 1
                    else:
                        P.pool(lambda e: e.memset(graw[:, 1025:1026], 0.0), W=[("graw", 1)])
                        halo_tok, halo_col, hsel = 1023, 0, 0
                    psh, phk = bank()
                    for c in range(KC):
                        P.pe(lambda e, c=c, psh=psh: e.matmul(psh[:, 0:1], lhsT=wgv[:, c, :], rhs=hhalo[:, c, hsel:hsel + 1],
                                                              start=(c == 0), stop=(c == KC - 1)), R=wks + ["hhalo"], W=[phk])
                    P.act(lambda e, psh=psh, halo_col=halo_col: e.copy(out=graw[:, halo_col:halo_col + 1], in_=psh[:, 0:1]),
                          R=[phk], W=[("graw", 0 if halo_col == 0 else 1)])
                    for b in range(2):
                        jj = 2 * H + b
                        ps_, pk = bank()
                        for c in range(KC):
                            P.pe(lambda e, c=c, jj=jj, ps_=ps_: e.matmul(ps_[:, :], lhsT=wgv[:, c, :], rhs=hnT[:, c, blk(jj)],
                                                                         start=(c == 0), stop=(c == KC - 1)), R=wks + [("hnT", c, jj)], W=[pk])
                        P.act(lambda e, b=b, ps_=ps_: e.copy(out=graw[:, 1 + b * 512:1 + (b + 1) * 512], in_=ps_[:, :]), R=[pk], W=[("grawb", b)])
                    for b in range(2):
                        jj = 2 * H + b
                        a_ = acc[:, b, :]
                        rk = [("graw", 0), ("graw", 1), ("grawb", 0), ("grawb", 1)]
                        P.dve(lambda e, b=b, a_=a_, cb=cb: e.tensor_scalar(out=a_, in0=graw[:, 1 + b * 512:1 + (b + 1) * 512],
                                                                           scalar1=pcol[:, cb + 1:cb + 2], scalar2=pcol[:, cb + 3:cb + 4],
                                                                           op0=ALU.mult, op1=ALU.add), R=rk + ["pcol"], W=[("acc", b)])
                        P.dve(lambda e, b=b, a_=a_, cb=cb: e.scalar_tensor_tensor(out=a_, in0=graw[:, b * 512:(b + 1) * 512],
                                                                                  scalar=pcol[:, cb:cb + 1], in1=a_, op0=ALU.mult, op1=ALU.add),
                              R=rk + ["pcol", ("acc", b)], W=[("acc", b)])
                        P.dve(lambda e, b=b, a_=a_, cb=cb: e.scalar_tensor_tensor(out=a_, in0=graw[:, 2 + b * 512:2 + (b + 1) * 512],
                                                                                  scalar=pcol[:, cb + 2:cb + 3], in1=a_, op0=ALU.mult, op1=ALU.add),
                              R=rk + ["pcol", ("acc", b)], W=[("acc", b)])
                        P.act(lambda e, b=b, a_=a_: e.activation(out=gel[:, b, :], in_=a_, func=AF.Gelu_apprx_tanh), R=[("acc", b)], W=[("gel", b)])
                        ps_, pk = bank()
                        for c in range(KC):
                            P.pe(lambda e, c=c, jj=jj, ps_=ps_: e.matmul(ps_[:, :], lhsT=wuv[:, c, :], rhs=hnT[:, c, blk(jj)],
                                                                         start=(c == 0), stop=(c == KC - 1)), R=wks + [("hnT", c, jj)], W=[pk])
                        P.dve(lambda e, b=b, f=f, ps_=ps_: e.tensor_tensor(out=actT[:, f, b * 512:(b + 1) * 512], in0=gel[:, b, :], in1=ps_[:, :], op=ALU.mult),
                              R=[("gel", b), pk], W=[("actT", f, b)])
                d_pool = [(hnT[:, c, t0:t0 + 1024], [("hnT", c, 2 * H), ("hnT", c, 2 * H + 1)]) for c in range(KC)]
                drot = 0
                for b in range(2):
                    for co in range(KC):
                        parts = []
                        for (fa, fb) in ((0, 8), (8, 16), (16, 22)):
                            (wv_,), wk_ = load_into(d_pool[drot % KC], [wd_d[l, fa * 128:fb * 128, co * 128:(co + 1) * 128].rearrange("(f p) n -> p f n", p=128)])
                            drot += 1
                            parts.append((fa, fb, wv_, wk_))
                        ps_, pk = bank()
                        for f in range(NF):
                            fa, fb, wdv, wk_ = parts[f // 8]
                            P.pe(lambda e: e.matmul(ps_[:, :], lhsT=wdv[:, f - fa, :], rhs=actT[:, f, b * 512:(b + 1) * 512],
                                                    start=(f == 0), stop=(f == NF - 1)), R=wk_ + [("actT", f, b)], W=[pk])
                        P.act(lambda e, co=co, ps_=ps_: e.copy(out=ffT[:, co, :], in_=ps_[:, :]), R=[pk], W=[("ffT", co)])
                    post_norm_residual(l, 24, ffT, lambda c: ("ffT", c), t0 + b * 512, 2 * H + b)
            P.barrier()

        try:
            stage("load")
            for l in range(nl):
                layer(l)
        except _Stop:
            pass
        for c in range(KC):
            P.dma("sp", lambda e, c=c: e.dma_start(out=outT_d[c * 128:(c + 1) * 128, :], in_=xT[:, c, :]),
                  R=[("xT", c, j) for j in range(NB)], W=[("out", c)])
        P.add("sp", None, R=[("out", c) for c in range(KC)])
        P.emit(nc)
    return nc


def _pack_params(ls, norm_mix_pre, norm_mix_post, norm_ffn_pre, norm_ffn_post, mlstm_conv_w, mlstm_conv_b,
                 ffn_conv_w, ffn_conv_b, mlstm_gate_b, mlstm_norm, gla_norm):
    nl = len(ls)
    pcol = np.zeros((128, nl * PCOLS), np.float32)
    pbc = np.zeros((nl, PBC), np.float32)
    for li, l in enumerate(ls):
        o = li * PCOLS
        for gi, g in enumerate((norm_mix_pre, norm_mix_post, norm_ffn_pre, norm_ffn_post)):
            pcol[:, o + gi * 8:o + gi * 8 + 8] = g[l].reshape(8, 128).T
        for cj in range(4):
            pcol[:, o + 32 + cj * 4:o + 32 + cj * 4 + 3] = mlstm_conv_w[l][:, cj * 128:(cj + 1) * 128].T
            pcol[:, o + 32 + cj * 4 + 3] = mlstm_conv_b[l][cj * 128:(cj + 1) * 128]
        for f in range(NF):
            pcol[:, o + 48 + f * 4:o + 48 + f * 4 + 3] = ffn_conv_w[l][:, f * 128:(f + 1) * 128].T
            pcol[:, o + 48 + f * 4 + 3] = ffn_conv_b[l][f * 128:(f + 1) * 128]
        pbc[li, 0:16] = mlstm_gate_b[l]
        pbc[li, 16:528] = mlstm_norm[l].reshape(-1)
        pbc[li, 528:1040] = gla_norm[l].reshape(-1)
    return pcol, pbc


_CACHE = {}


def _get_prog(nl):
    if nl not in _CACHE:
        _CACHE[nl] = build_program(nl)
    return _CACHE[nl]


FUSED = True


def kernel(x, norm_mix_pre, norm_mix_post, norm_ffn_pre, norm_ffn_post, w_in, mlstm_gate_b, mlstm_conv_w,
           mlstm_conv_b, mlstm_norm, gla_w2, gla_b, gla_norm, w_out, ffn_w_gate, ffn_w_up, ffn_conv_w,
           ffn_conv_b, ffn_w_down):
    f = lambda a: np.ascontiguousarray(np.asarray(a), dtype=np.float32)
    x = f(x)
    args = [f(a) for a in (norm_mix_pre, norm_mix_post, norm_ffn_pre, norm_ffn_post, mlstm_conv_w, mlstm_conv_b,
                           ffn_conv_w, ffn_conv_b, mlstm_gate_b, mlstm_norm, gla_norm)]
    w_in, w_out, wg, wu, wd = f(w_in), f(w_out), f(ffn_w_gate), f(ffn_w_up), f(ffn_w_down)
    w2, gb = f(gla_w2), f(gla_b)
    xTs = [np.ascontiguousarray(x[b].T) for b in range(NCORES)]
    groups = [list(range(DEPTH))] if FUSED else [[l] for l in range(DEPTH)]
    for ls in groups:
        nl = len(ls)
        nc = _get_prog(nl)
        pcol, pbc = _pack_params(ls, *args)
        sl = slice(ls[0], ls[-1] + 1)
        shared = {"w_in": w_in[sl], "w_out": w_out[sl], "wg": wg[sl], "wu": wu[sl], "wd": wd[sl], "pcol": pcol, "pbc": pbc,
                  "w2": w2[sl], "gb": np.ascontiguousarray(gb[sl].reshape(nl, 512))}
        in_maps = [dict(shared, xT=xTs[b]) for b in range(NCORES)]
        res = run_bass_kernel_spmd(nc, in_maps, core_ids=list(range(NCORES)))
        xTs = [np.asarray(r["outT"]) for r in res.results]
    return np.stack([np.ascontiguousarray(t.T) for t in xTs], axis=0).astype(np.float32)
```

```python
import contextlib
import types
import numpy as np
import concourse.bass as bass
import concourse.mybir as mybir
from concourse.bass_utils import run_bass_kernel_spmd

F32 = mybir.dt.float32
BF16 = mybir.dt.bfloat16
ALU = mybir.AluOpType
AF = mybir.ActivationFunctionType

EPOCH = 30000
HD1, HD2 = 1, 2
NCORES = 8
DEPTH = 4
D = 1024
S = 2048
NT = 16
NB = 4
KC = 8
FF = 2816
NF = 22
INC = 3120
EPS = 1e-6
PCOLS = 136
PBC = 1040


def _freeze(fn):
    if fn is None or fn.__closure__ is None:
        return fn
    cells = []
    for c in fn.__closure__:
        try:
            cells.append(types.CellType(c.cell_contents))
        except ValueError:
            cells.append(c)
    return types.FunctionType(fn.__code__, fn.__globals__, fn.__name__, fn.__defaults__, tuple(cells))


class _Op:
    __slots__ = ("eng", "fn", "R", "W", "dma", "deps", "marked", "tick", "dsem", "dtick", "dprev")

    def __init__(self, eng, fn, R, W, dma):
        self.eng = eng
        self.fn = fn
        self.R = tuple(R)
        self.W = tuple(W)
        self.dma = dma
        self.deps = ()
        self.marked = False
        self.tick = 0
        self.dsem = -1
        self.dtick = 0
        self.dprev = None


class Prog:
    ENGS = ("pe", "act", "dve", "pool", "sp")

    def __init__(self, n_dma_sems=16):
        self.ops = []
        self.nds = n_dma_sems

    def add(self, eng, fn, R=(), W=(), dma=False):
        self.ops.append(_Op(eng, _freeze(fn), R, W, dma))

    def pe(self, fn, R=(), W=()):
        self.add("pe", fn, R, W)

    def act(self, fn, R=(), W=()):
        self.add("act", fn, R, W)

    def dve(self, fn, R=(), W=()):
        self.add("dve", fn, R, W)

    def pool(self, fn, R=(), W=()):
        self.add("pool", fn, R, W)

    def dma(self, eng, fn, R=(), W=()):
        self.add(eng, fn, R, W, dma=True)

    def barrier(self):
        self.ops.append(_Op("__barrier__", None, (), (), False))

    def analyze(self):
        last_w = {}
        readers = {}
        last_on_eng = {e: None for e in self.ENGS}
        pend = {e: None for e in self.ENGS}
        out = []
        for op in self.ops:
            if op.eng == "__barrier__":
                snap = [v for v in last_on_eng.values() if v is not None]
                for e in self.ENGS:
                    pend[e] = snap
                continue
            g = len(out)
            deps = set()
            for k in op.R:
                if k in last_w:
                    deps.add(last_w[k])
            for k in op.W:
                if k in last_w:
                    deps.add(last_w[k])
                for r in readers.get(k, ()):
                    deps.add(r)
            if pend[op.eng] is not None:
                deps.update(pend[op.eng])
                pend[op.eng] = None
            for k in op.R:
                readers.setdefault(k, []).append(g)
            for k in op.W:
                last_w[k] = g
                readers[k] = []
            deps.discard(g)
            op.deps = tuple(sorted(deps))
            out.append(op)
            if op.fn is not None:
                last_on_eng[op.eng] = g
        self.lin = out
        ndma = 0
        nq = [0, 0]
        last_dma_on_sem = {}
        for g, op in enumerate(out):
            for d in op.deps:
                p = out[d]
                if p.dma:
                    continue
                if p.eng == "pe" and op.eng == "pe" and not op.dma:
                    continue
                p.marked = True
            if op.dma:
                half = self.nds // 2
                qi = 0 if op.eng == "sp" else 1
                s = qi * half + (nq[qi] % half)
                nq[qi] += 1
                ndma += 1
                op.dsem = s
                prev = last_dma_on_sem.get(s)
                op.dprev = prev
                op.dtick = (out[prev].dtick if prev is not None else 0) + 16
                last_dma_on_sem[s] = g
        cnt = {e: 0 for e in self.ENGS}
        for op in out:
            if op.marked and not op.dma:
                cnt[op.eng] += 1
                op.tick = cnt[op.eng]
        self.cnt = cnt
        self.ndma = ndma

    def emit(self, nc):
        self.analyze()
        out = self.lin
        with contextlib.ExitStack() as st:
            esems = {}
            for e in self.ENGS:
                nep = self.cnt[e] // EPOCH + 1
                esems[e] = [st.enter_context(nc.semaphore(f"s_{e}_{i}")) for i in range(nep)]
            dsems = [st.enter_context(nc.semaphore(f"s_dma_{i}")) for i in range(self.nds)]
            block = st.enter_context(nc.Block())
            by_eng = {e: [] for e in self.ENGS}
            for g, op in enumerate(out):
                by_eng[op.eng].append((g, op))

            def sem_of(p):
                if p.dma:
                    return ("d", p.dsem), dsems[p.dsem], p.dtick
                ep = (p.tick - 1) // EPOCH
                return (p.eng, ep), esems[p.eng][ep], p.tick - ep * EPOCH

            def run(eng_name, e):
                waited = {}
                for g, op in by_eng[eng_name]:
                    waits = {}
                    deps = list(op.deps)
                    if op.dma and op.dprev is not None:
                        deps.append(op.dprev)
                    for d in deps:
                        p = out[d]
                        if (not p.dma) and p.eng == "pe" and eng_name == "pe" and not op.dma:
                            continue
                        key, sem, val = sem_of(p)
                        if waited.get(key, 0) >= val:
                            continue
                        if key not in waits or waits[key][1] < val:
                            waits[key] = (sem, val)
                    for key, (sem, val) in waits.items():
                        e.wait_ge(sem, val)
                        waited[key] = val
                    if op.fn is None:
                        continue
                    ins = op.fn(e)
                    if op.dma:
                        ins.then_inc(dsems[op.dsem], 16)
                    elif op.marked:
                        ep = (op.tick - 1) // EPOCH
                        ins.then_inc(esems[eng_name][ep], 1)

            @block.tensor
            def _(e):
                run("pe", e)

            @block.scalar
            def _(e):
                run("act", e)

            @block.vector
            def _(e):
                run("dve", e)

            @block.gpsimd
            def _(e):
                run("pool", e)

            @block.sync
            def _(e):
                run("sp", e)


class _Stop(Exception):
    pass


def build_program(nl, dbg=None, stop_at=None):
    nc = bass.Bass("TRN2", target_bir_lowering=False)
    P = Prog()

    def din(name, shape):
        return nc.dram_tensor(name, shape, F32, kind="ExternalInput").ap()

    xT_d = din("xT", [D, S])
    w_in_d = din("w_in", [nl, D, INC])
    w_out_d = din("w_out", [nl, D, D])
    wg_d = din("wg", [nl, D, FF])
    wu_d = din("wu", [nl, D, FF])
    wd_d = din("wd", [nl, FF, D])
    pcol_d = din("pcol", [128, nl * PCOLS])
    pbc_d = din("pbc", [nl, PBC])
    w2_d = din("w2", [nl, 2, 16, 256])
    gb_d = din("gb", [nl, 512])
    outT_d = nc.dram_tensor("outT", [D, S], F32, kind="ExternalOutput").ap()
    dbg_d = {}
    if dbg:
        for k, shp in dbg.items():
            dbg_d[k] = nc.dram_tensor("dbg_" + k, shp, F32, kind="ExternalOutput").ap()

    st = contextlib.ExitStack()
    with st:
        def sb(name, shape, dt):
            return st.enter_context(nc.sbuf_tensor(name, shape, dt))

        def psum(name, shape, dt):
            return st.enter_context(nc.psum_tensor(name, shape, dt))

        xT = sb("xT_sb", [128, KC, S], F32)
        hnT = sb("hnT", [128, KC, S], BF16)
        pcol = sb("pcol_sb", [128, PCOLS], F32)
        pbc = sb("pbc_sb", [128, PBC], F32)
        w2bd = sb("w2bd", [128, 512], BF16)
        lrT = sb("lrT", [128, S], BF16)
        ones_bf = sb("ones_bf", [128, 128], BF16)
        negones_f = sb("negones_f", [128, 128], F32)
        ident = sb("ident", [128, 128], BF16)
        maskA = sb("maskA", [128, 2, 128], BF16)
        maskB = sb("maskB", [128, 2, 128], BF16)
        tri16 = sb("tri16", [128, 2, 128], BF16)
        trif = sb("trif", [128, 2, 128], F32)
        WB = 2048
        wbuf = [sb(f"wbuf{i}", [128, WB], BF16) for i in range(2)]
        A_YT = 0
        A_Q = A_YT + 6 * S
        A_K = A_Q + S
        A_2 = A_K + S
        A_3 = A_2 + 4096
        A_4 = A_3 + 4160
        A_5 = A_4 + 4096
        A_END = A_5 + 4100
        arena = sb("arena", [128, A_END], BF16)
        yT = arena[:, A_YT:A_Q].rearrange("p (c t) -> p c t", c=6)
        qT = arena[:, A_Q:A_K]
        kT = arena[:, A_K:A_2]
        kw = arena[:, A_2:A_3].rearrange("p (d i h e) -> p d i h e", d=2, i=NT, h=2)
        bv = arena[:, A_2:A_3].rearrange("p (i e) -> p i e", i=NT)
        vaug = arena[:, A_3:A_4].rearrange("p (i h e) -> p i h e", i=NT, h=2)
        sr = arena[:, A_3:A_3 + 4096].rearrange("p (i e) -> p i e", i=NT)
        sigo = arena[:, A_4:A_5].rearrange("p (i e) -> p i e", i=NT)
        la = arena[:, A_4:A_5].rearrange("p (i e) -> p i e", i=NT)
        hsum = arena[:, A_5:A_5 + 4096].rearrange("p (i e) -> p i e", i=NT)
        qkraw = arena[:, A_5:A_END].bitcast(F32)
        wo = arena[:, A_Q:A_Q + KC * D].rearrange("p (c n) -> p c n", c=KC)
        mixT = arena[:, A_Q + KC * D:A_Q + KC * D + 2 * KC * 512].bitcast(F32).rearrange("p (c t) -> p c t", c=KC)
        actT = arena[:, 0:NF * 1024].rearrange("p (f t) -> p f t", f=NF)
        ffT = arena[:, NF * 1024:NF * 1024 + 2 * KC * 512].bitcast(F32).rearrange("p (c t) -> p c t", c=KC)
        graw = arena[:, NF * 1024 + 2 * KC * 512:NF * 1024 + 2 * KC * 512 + 2 * 1026].bitcast(F32)
        assert NF * 1024 + 2 * KC * 512 + 2 * 1026 <= A_END

        gl = sb("gl", [128, NT, 16], F32)
        lf = sb("lf", [128, 2, NT * 4], F32)
        gtmp = sb("gtmp", [128, 2, NT * 4], F32)
        eb = sb("eb", [128, 2, NT, 4], F32)
        enb = sb("enb", [128, 2, NT, 4], F32)
        wsc = sb("wsc", [128, 2, NT, 4], F32)
        dec = sb("dec", [128, 2, NT, 4], F32)
        wdec = sb("wdec", [128, 2, NT, 4], F32)
        decsel = sb("decsel", [128, 2, NT], F32)
        Sm = sb("Sm", [128, 2, 2, 2, 128], BF16)
        C32 = sb("C32", [128, 2, 132], F32)
        Cbf = sb("Cbf", [128, 2, 2, 132], BF16)
        eBt = sb("eBt", [128, 2, 2, 128], F32)
        eNBt = sb("eNBt", [128, 2, 128], F32)
        qs = sb("qs", [128, 2, 2, 128], BF16)
        ks = sb("ks", [128, 2, 2, 128], BF16)
        ktok = sb("ktok", [128, 2, 2, 128], BF16)
        tmpS = sb("tmpS", [128, 2, 128], F32)
        osb = sb("osb", [128, 2, 260], F32)
        sml = sb("sml", [128, 5, 16], F32)
        sqb = sb("sqb", [128, 2, 512], BF16)
        hhalo = sb("hhalo", [128, KC, 2], BF16)
        rstd = sb("rstd", [128, 512], F32)
        acc = sb("acc", [128, 2, 512], F32)
        bsb = acc[:, 1, 0:256]
        gel = Sm[:].rearrange("p a b c t -> p (a b c t)").rearrange("p (b t) -> p b t", b=2)
        tot2 = acc[:, :, 0:256]
        junk2 = sqb[:, 0, :].rearrange("p (q h t) -> p q h t", q=2, h=2)
        ytile = sqb[:, 1, :].rearrange("p (b t) -> p b t", b=2)
        orders_g = [list(range(NT)), list(range(NT - 1, -1, -1))]

        pb = [psum(f"pb{i}", [128, 512], F32) for i in range(6)]
        ptb = [psum(f"ptb{i}", [128, 1024], BF16) for i in range(2)]
        rot = {"n": 0, "t": 0}

        def bank():
            i = rot["n"] % 6
            rot["n"] += 1
            return pb[i], ("ps", i)

        def tbank():
            i = rot["t"] % 2
            rot["t"] += 1
            return ptb[i], ("pt", i)

        wrot = {"n": 0}

        def load_into(bufspec, segs):
            ap2d, keys = bufspec
            views = []
            off = 0
            for src in segs:
                k, n = src.shape[1], src.shape[2]
                v = ap2d[:, off:off + k * n].rearrange("p (k n) -> p k n", k=k)
                P.dma("pool", lambda e: e.dma_start(out=v, in_=src), W=keys)
                views.append(v)
                off += k * n
            assert off <= ap2d.shape[1]
            return views, list(keys)

        def load_w(segs):
            i = wrot["n"] % 2
            wrot["n"] += 1
            views, _ = load_into((wbuf[i][:, :], [("wbuf", i)]), segs)
            return views, ("wbuf", i)

        def wcols(wd3, l, a, n, kc=KC):
            return wd3[l, :, a:a + n].rearrange("(c p) n -> p c n", p=128)

        P.pool(lambda e: e.memset(ones_bf[:], 1.0), W=["ones_bf"])
        P.pool(lambda e: e.memset(negones_f[:], -1.0), W=["negones_f"])
        P.pool(lambda e: e.memset(ident[:], 0.0), W=["ident"])
        P.pool(lambda e: e.affine_select(out=ident[:], in_=ident[:], pattern=[[-1, 128]], compare_op=ALU.not_equal,
                                         fill=1.0, base=0, channel_multiplier=1), R=["ident"], W=["ident"])
        for t_, val in ((maskA, 0.125), (maskB, 0.125), (tri16, -1.0 / 16.0), (trif, -1.0)):
            P.pool(lambda e, t_=t_, val=val: e.memset(t_[:], val), W=[("const", id(t_))])
            P.pool(lambda e, t_=t_: e.affine_select(out=t_[:, 0, :], in_=t_[:, 0, :], pattern=[[1, 128]], compare_op=ALU.is_ge,
                                                    fill=0.0, base=0, channel_multiplier=-1), R=[("const", id(t_))], W=[("const", id(t_))])
            P.pool(lambda e, t_=t_: e.affine_select(out=t_[:, 1, :], in_=t_[:, 1, :], pattern=[[-1, 128]], compare_op=ALU.is_ge,
                                                    fill=0.0, base=0, channel_multiplier=1), R=[("const", id(t_))], W=[("const", id(t_))])
        CONSTS = ["ones_bf", "negones_f", "ident"] + [("const", id(t_)) for t_ in (maskA, maskB, tri16, trif)]
        P.pool(lambda e: e.memset(lrT[32:33, :], 1.0), W=["lrT1"])
        for c in range(KC):
            P.dma("sp", lambda e, c=c: e.dma_start(out=xT[:, c, :], in_=xT_d[c * 128:(c + 1) * 128, :]),
                  W=[("xT", c, j) for j in range(NB)])

        def blk(j):
            return slice(j * 512, (j + 1) * 512)

        def ykey(ch, i):
            return ("yT", ch, i) if ch < 6 else ("hnT", ch - 6, i // 4)

        def yview(ch):
            return yT[:, ch, :] if ch < 6 else hnT[:, ch - 6, :]

        def til(i):
            return slice(i * 128, (i + 1) * 128)

        def ss_and_rstd(srcs, src_keys, nparity):
            ps_, pk = bank()
            for c in range(KC):
                sq = sqb[:, c % 2, :]
                P.act(lambda e, sq=sq, s_=srcs[c]: e.activation(out=sq, in_=s_, func=AF.Square),
                      R=[src_keys[c]], W=[("sqb", c % 2)])
                P.pe(lambda e, sq=sq, c=c, ps_=ps_: e.matmul(ps_[:, :], lhsT=ones_bf[:], rhs=sq, start=(c == 0), stop=(c == KC - 1)),
                     R=[("sqb", c % 2), "ones_bf"], W=[pk])
            r_ = rstd[:, :]
            P.act(lambda e, ps_=ps_, r_=r_: e.activation(out=r_, in_=ps_[:, :], func=AF.Ln, scale=1.0 / D, bias=EPS),
                  R=[pk], W=["rstd"])
            P.act(lambda e, r_=r_: e.activation(out=r_, in_=r_, func=AF.Exp, scale=-0.5),
                  R=["rstd"], W=["rstd"])
            return r_, "rstd"

        def pre_norm(l, gofs):
            for j in range(NB):
                srcs = [xT[:, c, blk(j)] for c in range(KC)]
                keys = [("xT", c, j) for c in range(KC)]
                r_, rk = ss_and_rstd(srcs, keys, j % 2)
                for c in range(KC):
                    col = gofs + c
                    P.dve(lambda e, c=c, j=j, col=col, r_=r_: e.scalar_tensor_tensor(
                        out=hnT[:, c, blk(j)], in0=xT[:, c, blk(j)], scalar=pcol[:, col:col + 1], in1=r_,
                        op0=ALU.mult, op1=ALU.mult), R=[("xT", c, j), "pcol", rk], W=[("hnT", c, j)])

        def post_norm_residual(l, gofs, srcT, src_keyf, tok0, j_x):
            srcs = [srcT[:, c, :] for c in range(KC)]
            keys = [src_keyf(c) for c in range(KC)]
            r_, rk = ss_and_rstd(srcs, keys, j_x % 2)
            for c in range(KC):
                col = gofs + c
                P.dve(lambda e, c=c, col=col, r_=r_: e.scalar_tensor_tensor(
                    out=srcT[:, c, :], in0=srcT[:, c, :], scalar=pcol[:, col:col + 1], in1=r_,
                    op0=ALU.mult, op1=ALU.mult), R=[keys[c], "pcol", rk], W=[keys[c]])
                P.dve(lambda e, c=c: e.tensor_tensor(out=xT[:, c, tok0:tok0 + 512], in0=xT[:, c, tok0:tok0 + 512],
                                                     in1=srcT[:, c, :], op=ALU.add),
                      R=[keys[c], ("xT", c, j_x)], W=[("xT", c, j_x)])

        def proj_fm(wv, wkey, ncol_lo, m, evac):
            for j in range(NB):
                ps_, pk = bank()
                for c in range(KC):
                    P.pe(lambda e, c=c, j=j, ps_=ps_: e.matmul(ps_[0:m, :], lhsT=wv[:, c, ncol_lo:ncol_lo + m], rhs=hnT[:, c, blk(j)],
                                                               start=(c == 0), stop=(c == KC - 1)),
                         R=[wkey, ("hnT", c, j)], W=[pk])
                evac(j, ps_, pk)

        def proj_tm(wv, wkey, ncol_lo, n, per_bank, evac):
            for i0 in range(0, NT, per_bank):
                ps_, pk = bank()
                for ii in range(per_bank):
                    i = i0 + ii
                    for c in range(KC):
                        P.pe(lambda e, c=c, i=i, ii=ii, ps_=ps_: e.matmul(
                            ps_[:, ii * n:(ii + 1) * n], lhsT=hnT[:, c, til(i)], rhs=wv[:, c, ncol_lo:ncol_lo + n],
                            start=(c == 0), stop=(c == KC - 1)),
                            R=[wkey, ("hnT", c, i // 4)], W=[pk])
                evac(i0, ps_, pk)

        def head1(i, q, gate_t, gate_key):
            tt = tot2[:, q, :]
            sq_ = sml[:, 3 + q, :]
            for hh in range(2):
                P.act(lambda e: e.activation(out=junk2[:, q, hh, :],
                                             in_=tt[:, hh * 128:(hh + 1) * 128], func=AF.Square, accum_out=sq_[:, hh:hh + 1]),
                      R=[("acc", q)], W=[("junk", q, hh), ("sml0", q, hh)])
            P.act(lambda e: e.activation(out=sq_[:, 2:4], in_=sq_[:, 0:2], func=AF.Ln, scale=1.0 / 128, bias=EPS),
                  R=[("sml0", q, 0), ("sml0", q, 1)], W=[("sml0b", q)])
            P.act(lambda e: e.activation(out=sq_[:, 4:6], in_=sq_[:, 2:4], func=AF.Exp, scale=-0.5),
                  R=[("sml0b", q)], W=[("sml0c", q)])
            yt = ytile[:, q, :]
            for hh in range(2):
                P.dve(lambda e: e.scalar_tensor_tensor(
                    out=yt[:, hh * 128:(hh + 1) * 128], in0=tt[:, hh * 128:(hh + 1) * 128], scalar=sq_[:, 4 + hh:5 + hh],
                    in1=gate_t[:, i, hh * 128:(hh + 1) * 128], op0=ALU.mult, op1=ALU.mult),
                    R=[("acc", q), ("sml0c", q), gate_key], W=[("ytile", q)])

        def head2(i, q, p, grp):
            yt = ytile[:, q, :]
            pt_, ptk = tbank()
            for hh in range(2):
                P.pe(lambda e: e.transpose(pt_[:, hh * 128:(hh + 1) * 128], yt[:, hh * 128:(hh + 1) * 128], ident[:]),
                     R=[("ytile", q), "ident"], W=[ptk])
            ch = grp * 4 + 2 * p
            for hh in range(2):
                dstv = yview(ch + hh)[:, til(i)]
                P.act(lambda e: e.copy(out=dstv, in_=pt_[:, hh * 128:(hh + 1) * 128]), R=[ptk], W=[ykey(ch + hh, i)])

        def run_pipeline(core, tail, p, grp, gate_t, gate_key_fn, hd1, hd2, td=1):
            its = [(n, d) for n in range(NT) for d in range(2)]
            nit = len(its)
            seconds = []
            for it in range(nit + 2 + hd2):
                if it < nit:
                    core(*its[it])
                if 0 <= it - td < nit:
                    n, d = its[it - td]
                    q = tail(n, d)
                    if q is not None:
                        seconds.append((it - 1, orders_g[d][n], q))
                for (jt, ti, q) in seconds:
                    if jt == it - 1 - hd1:
                        head1(ti, q, gate_t, gate_key_fn(ti))
                for (jt, ti, q) in seconds:
                    if jt == it - 1 - hd2:
                        head2(ti, q, p, grp)

        def stage(name):
            if stop_at == name:
                raise _Stop()

        def layer(l):
            P.dma("sp", lambda e, l=l: e.dma_start(out=pcol[:], in_=pcol_d[:, l * PCOLS:(l + 1) * PCOLS]), W=["pcol"])
            P.dma("sp", lambda e, l=l: e.dma_start(out=pbc[:], in_=pbc_d[l:l + 1, :].partition_broadcast(128)), W=["pbc"])
            P.pool(lambda e: e.memset(w2bd[:], 0.0), W=["w2bd"])
            P.dma("pool", lambda e, l=l: e.dma_start(out=w2bd[0:16, 0:256], in_=w2_d[l, 0]), W=["w2bd"])
            P.dma("pool", lambda e, l=l: e.dma_start(out=w2bd[16:32, 256:512], in_=w2_d[l, 1]), W=["w2bd"])
            P.dma("pool", lambda e, l=l: e.dma_start(out=w2bd[32:33, :], in_=gb_d[l:l + 1, :]), W=["w2bd"])

            stage("params")
            pre_norm(l, 0)
            stage("prenorm")

            for p in range(2):
                h0 = 2 * p
                if p == 0:
                    (wv,), wk = load_w([wcols(w_in_d, l, 1424, 128)])
                    psg, pgk = bank()
                    for i in range(NT):
                        for c in range(KC):
                            P.pe(lambda e, c=c, i=i: e.matmul(psg[:, i * 16:(i + 1) * 16], lhsT=hnT[:, c, til(i)], rhs=wv[:, c, 112:128],
                                                              start=(c == 0), stop=(c == KC - 1)),
                                 R=[wk, ("hnT", c, i // 4)], W=[pgk])
                    P.dve(lambda e: e.tensor_tensor(out=gl[:], in0=psg[:, 0:256].rearrange("p (i g) -> p i g", i=NT),
                                                    in1=pbc[:, 0:16].unsqueeze(1).to_broadcast([128, NT, 16]), op=ALU.add),
                          R=[pgk, "pbc"], W=["gl"])
                    stage("g1")
                    for d in range(2):
                        fsl = gl[:, :, 4 + 8 * d:8 + 8 * d]
                        lfd = lf[:, d, :].rearrange("p (i h) -> p i h", i=NT)
                        P.act(lambda e, fsl=fsl, lfd=lfd: e.activation(out=lfd, in_=fsl, func=AF.Exp, scale=-1.0), R=["gl"], W=[("lf", d)])
                        P.act(lambda e, lfd=lfd: e.activation(out=lfd, in_=lfd, func=AF.Ln, bias=1.0), R=[("lf", d)], W=[("lf", d)])
                    stage("g2")
                    psb, pbk = bank()
                    for d in range(2):
                        P.pe(lambda e, d=d: e.matmul(psb[:, d * 64:(d + 1) * 64], lhsT=trif[:, d, :], rhs=lf[:, d, :], start=True, stop=True),
                             R=[("lf", d)] + CONSTS, W=[pbk])
                        P.pe(lambda e, d=d: e.matmul(psb[:, 128 + d * 64:128 + (d + 1) * 64], lhsT=negones_f[:], rhs=lf[:, d, :], start=True, stop=True),
                             R=[("lf", d)] + CONSTS, W=[pbk])
                    stage("g3")
                    P.act(lambda e: e.copy(out=bsb[:, :], in_=psb[:, 0:256]), R=[pbk], W=[("acc", 1)])
                    flat = lambda t_: t_[:].rearrange("p d i h -> p (d i h)")
                    P.act(lambda e: e.activation(out=flat(eb), in_=bsb[:, 0:128], func=AF.Exp), R=[("acc", 1)], W=["eb"])
                    P.act(lambda e: e.activation(out=flat(enb), in_=bsb[:, 0:128], func=AF.Exp, scale=-1.0), R=[("acc", 1)], W=["enb"])
                    P.act(lambda e: e.activation(out=flat(dec), in_=bsb[:, 128:256], func=AF.Exp), R=[("acc", 1)], W=["dec"])
                    for d in range(2):
                        isl = gl[:, :, 8 * d:8 * d + 4]
                        gt = gtmp[:, d, :].rearrange("p (i h) -> p i h", i=NT)
                        P.dve(lambda e, d=d, isl=isl, gt=gt: e.tensor_tensor(out=gt, in0=isl, in1=bsb[:, d * 64:(d + 1) * 64].rearrange("p (i h) -> p i h", i=NT),
                                                                             op=ALU.subtract), R=["gl", ("acc", 1)], W=[("gtmp", d)])
                        P.act(lambda e, d=d, gt=gt: e.activation(out=wsc[:, d].rearrange("p i h -> p (i h)"), in_=gtmp[:, d, :], func=AF.Exp), R=[("gtmp", d)], W=[("wsc", d)])
                        P.dve(lambda e, d=d: e.tensor_tensor(out=wdec[:, d], in0=wsc[:, d], in1=dec[:, d], op=ALU.mult),
                              R=[("wsc", d), "dec"], W=[("wdec", d)])
                stage("gates")
                for d in range(2):
                    for hh in range(2):
                        P.pool(lambda e, d=d, hh=hh: e.tensor_copy(out=decsel[hh * 64:(hh + 1) * 64, d, :], in_=dec[hh * 64:(hh + 1) * 64, d, :, h0 + hh]),
                               R=["dec"], W=[("decsel", hh)])
                (wq, wkk), wk = load_w([wcols(w_in_d, l, p * 128, 128), wcols(w_in_d, l, 256 + p * 128, 128)])
                for which, wv, dst in ((0, wq, qT), (1, wkk, kT)):
                    cj = which * 2 + p
                    cb = 32 + cj * 4
                    def qk_keys(j):
                        return [("qkraw", j)] + [("s5", ii) for ii in range(4 * j, min(NT, 4 * j + 5))]
                    P.pool(lambda e: e.memset(qkraw[:, 0:1], 0.0), W=qk_keys(0))
                    P.pool(lambda e: e.memset(qkraw[:, 2049:2050], 0.0), W=qk_keys(3))

                    def ev(j, ps_, pk):
                        P.act(lambda e, j=j, ps_=ps_: e.copy(out=qkraw[:, 1 + j * 512:1 + (j + 1) * 512], in_=ps_[:, :]),
                              R=[pk], W=qk_keys(j))
                    proj_fm(wv, wk, 0, 128, ev)
                    for j in range(NB):
                        a_ = acc[:, j % 2, :]
                        rk = []
                        for jj in range(max(0, j - 1), min(NB, j + 2)):
                            rk += qk_keys(jj)
                        P.dve(lambda e, j=j, a_=a_, cb=cb: e.tensor_scalar(out=a_, in0=qkraw[:, 1 + j * 512:1 + (j + 1) * 512],
                                                                           scalar1=pcol[:, cb + 1:cb + 2], scalar2=pcol[:, cb + 3:cb + 4],
                                                                           op0=ALU.mult, op1=ALU.add),
                              R=rk + ["pcol"], W=[("acc", j % 2)])
                        P.dve(lambda e, j=j, a_=a_, cb=cb: e.scalar_tensor_tensor(out=a_, in0=qkraw[:, j * 512:(j + 1) * 512],
                                                                                  scalar=pcol[:, cb:cb + 1], in1=a_, op0=ALU.mult, op1=ALU.add),
                              R=rk + ["pcol", ("acc", j % 2)], W=[("acc", j % 2)])
                        P.dve(lambda e, j=j, a_=a_, cb=cb: e.scalar_tensor_tensor(out=a_, in0=qkraw[:, 2 + j * 512:2 + (j + 1) * 512],
                                                                                  scalar=pcol[:, cb + 2:cb + 3], in1=a_, op0=ALU.mult, op1=ALU.add),
                              R=rk + ["pcol", ("acc", j % 2)], W=[("acc", j % 2)])
                        P.act(lambda e, j=j, a_=a_, dst=dst: e.activation(out=dst[:, blk(j)], in_=a_, func=AF.Silu),
                              R=[("acc", j % 2)], W=[("q" if which == 0 else "k", j)])
                stage("Aqk")
                for hh in range(2):
                    P.pool(lambda e, hh=hh: e.memset(vaug[:, :, hh, 128:130], 1.0), W=[("s3", i) for i in range(NT)])
                (wv,), wk = load_w([wcols(w_in_d, l, 512 + p * 256, 256)])

                def evv(i0, ps_, pk):
                    for ii in range(2):
                        for hh in range(2):
                            P.act(lambda e, i0=i0, ii=ii, hh=hh, ps_=ps_: e.copy(out=vaug[:, i0 + ii, hh, 0:128],
                                                                               in_=ps_[:, ii * 256 + hh * 128:ii * 256 + (hh + 1) * 128]),
                                  R=[pk], W=[("s3", i0 + ii)])
                proj_tm(wv, wk, 0, 256, 2, evv)
                (wv,), wk = load_w([wcols(w_in_d, l, 1024 + p * 256, 256)])

                def evo(i0, ps_, pk):
                    P.act(lambda e, i0=i0, ps_=ps_: e.activation(out=sigo[:, i0:i0 + 2, :].rearrange("p i e -> p (i e)"), in_=ps_[:, :],
                                                                func=AF.Sigmoid), R=[pk], W=[("s4", i0), ("s4", i0 + 1)])
                    P.pool(lambda e, i0=i0: e.tensor_tensor(out=sigo[:, i0:i0 + 2, :], in0=sigo[:, i0:i0 + 2, :],
                                                           in1=pbc[:, 16 + h0 * 128:16 + h0 * 128 + 256].unsqueeze(1).to_broadcast([128, 2, 256]),
                                                           op=ALU.mult), R=[("s4", i0), ("s4", i0 + 1), "pbc"], W=[("s4", i0), ("s4", i0 + 1)])
                proj_tm(wv, wk, 0, 256, 2, evo)

                for i in range(NT):
                    pt_, ptk = tbank()
                    P.pe(lambda e, i=i, pt_=pt_: e.transpose(pt_[:, 0:128], kT[:, til(i)], ident[:]), R=[("k", i // 4), "ident"], W=[ptk])
                    for d in range(2):
                        for hh in range(2):
                            P.dve(lambda e, i=i, d=d, hh=hh, pt_=pt_: e.tensor_scalar(
                                out=kw[:, d, i, hh, :], in0=pt_[:, hh * 64:(hh + 1) * 64], scalar1=wdec[:, d, i, h0 + hh:h0 + hh + 1], scalar2=0.125,
                                op0=ALU.mult, op1=ALU.mult), R=[ptk, ("wdec", d)], W=[("s2", i)])
                stage("Aproj")
                orders = [list(range(NT)), list(range(NT - 1, -1, -1))]
                for d in range(2):
                    P.pool(lambda e, d=d: e.memset(C32[:, d, :], 0.0), W=[("C32", d)])
                    P.pool(lambda e, d=d: e.memset(Cbf[:, d, 0, :], 0.0), W=[("Cbf", d, 0)])

                def a_early(d, n):
                    i = orders[d][n]
                    par = n % 2
                    psSs = [bank(), bank()]
                    for hh in range(2):
                        rs = slice(hh * 64, (hh + 1) * 64)
                        psS, psk = psSs[hh]
                        P.pe(lambda e: e.matmul(psS[:, 0:128], lhsT=kT[rs, til(i)], rhs=qT[rs, til(i)], start=True, stop=True),
                             R=[("k", i // 4), ("q", i // 4)], W=[psk])
                    for hh in range(2):
                        psS, psk = psSs[hh]
                        P.dve(lambda e: e.scalar_tensor_tensor(
                            out=Sm[:, d, par, hh, :], in0=psS[:, 0:128], scalar=wsc[:, d, i, h0 + hh:h0 + hh + 1],
                            in1=maskA[:, d, :], op0=ALU.mult, op1=ALU.mult),
                            R=[psk, ("wsc", d)] + CONSTS, W=[("Sm", d, par, hh)])

                def a_main(d, n):
                    i = orders[d][n]
                    par = n % 2
                    psU, puk = bank()
                    P.pe(lambda e: e.matmul(psU[0:64, 0:130], lhsT=kw[:, d, i, 0, :], rhs=vaug[:, i, 0, 0:130], start=True, stop=True),
                         R=[("s2", i), ("s3", i)], W=[puk])
                    P.pe(lambda e: e.matmul(psU[64:128, 0:130], lhsT=kw[:, d, i, 1, :], rhs=vaug[:, i, 1, 0:130], start=True, stop=True,
                                            tile_position=(0, 64)),
                         R=[("s2", i), ("s3", i)], W=[puk])
                    psO, pok = bank()
                    for hh in range(2):
                        rs = slice(hh * 64, (hh + 1) * 64)
                        P.pe(lambda e: e.matmul(psO[:, hh * 130:hh * 130 + 130], lhsT=Sm[:, d, par, hh, :],
                                                rhs=vaug[:, i, hh, 0:130], start=True, stop=False),
                             R=[("Sm", d, par, hh), ("s3", i)], W=[pok])
                        P.pe(lambda e: e.matmul(psO[:, hh * 130:hh * 130 + 130], lhsT=qT[rs, til(i)],
                                                rhs=Cbf[rs, d, par, 0:130], start=False, stop=True),
                             R=[("q", i // 4), ("Cbf", d, par)], W=[pok])
                    P.dve(lambda e: e.scalar_tensor_tensor(
                        out=C32[:, d, 0:130], in0=C32[:, d, 0:130], scalar=decsel[:, d, i:i + 1], in1=psU[:, 0:130], op0=ALU.mult, op1=ALU.add),
                        R=[("C32", d), ("decsel", 0), ("decsel", 1), puk], W=[("C32", d)])
                    P.pool(lambda e: e.tensor_copy(out=Cbf[:, d, 1 - par, 0:130], in_=C32[:, d, 0:130]),
                           R=[("C32", d)], W=[("Cbf", d, 1 - par)])
                    ob = osb[:, d, :]
                    P.act(lambda e: e.copy(out=ob[:, 0:260], in_=psO[:, 0:260]), R=[pok], W=[("osb", d)])
                    sm = sml[:, 1 + d, :]
                    P.act(lambda e: e.activation(out=sm[:, 0:2], in_=ob[:, 0:260].rearrange("p (h e) -> p h e", h=2)[:, :, 128], func=AF.Abs),
                          R=[("osb", d)], W=[("sml1a", d)])

                def a_tail(n, d):
                    i = orders[d][n]
                    ob = osb[:, d, :]
                    sm = sml[:, 1 + d, :]
                    q = None
                    if n >= NT // 2:
                        q = acnt[0] % 2
                        acnt[0] += 1
                    P.dve(lambda e: e.tensor_tensor(out=sm[:, 2:4], in0=sm[:, 0:2], in1=enb[:, d, i, h0:h0 + 2], op=ALU.max),
                          R=[("sml1a", d), "enb"], W=[("sml1b", d)])
                    P.dve(lambda e: e.reciprocal(out=sm[:, 4:6], in_=sm[:, 2:4]), R=[("sml1b", d)], W=[("sml1c", d)])
                    for hh in range(2):
                        if n < NT // 2:
                            P.act(lambda e: e.activation(
                                out=hsum[:, i, hh * 128:(hh + 1) * 128], in_=ob[:, hh * 130:hh * 130 + 128], func=AF.Copy, scale=sm[:, 4 + hh:5 + hh]),
                                R=[("osb", d), ("sml1c", d)], W=[("s5", i)])
                        else:
                            P.dve(lambda e: e.scalar_tensor_tensor(
                                out=tot2[:, q, hh * 128:(hh + 1) * 128], in0=ob[:, hh * 130:hh * 130 + 128], scalar=sm[:, 4 + hh:5 + hh],
                                in1=hsum[:, i, hh * 128:(hh + 1) * 128], op0=ALU.mult, op1=ALU.add),
                                R=[("osb", d), ("sml1c", d), ("s5", i)], W=[("acc", q)])
                    return q

                acnt = [0]

                def a_core(n, d):
                    if n == 0:
                        a_early(d, 0)
                    if n + 1 < NT:
                        a_early(d, n + 1)
                    a_main(d, n)

                run_pipeline(a_core, a_tail, p, 0, sigo, lambda ti: ("s4", ti), hd1=0, hd2=1, td=0)

            stage("A")
            for p in range(2):
                h0 = 2 * p
                if p == 0:
                    (wv,), wk = load_w([wcols(w_in_d, l, 2992, 128)])

                    def evl(j, ps_, pk):
                        P.act(lambda e, j=j, ps_=ps_: e.copy(out=lrT[0:32, blk(j)], in_=ps_[0:32, :]), R=[pk], W=[("lrT", j)])
                    proj_fm(wv, wk, 96, 32, evl)
                (wq, wkk), wk = load_w([wcols(w_in_d, l, 1552 + p * 128, 128), wcols(w_in_d, l, 1808 + p * 128, 128)])
                for which, wv, dst in ((0, wq, qT), (1, wkk, kT)):
                    def ev(j, ps_, pk, dst=dst, which=which):
                        P.act(lambda e, j=j, ps_=ps_: e.copy(out=dst[:, blk(j)], in_=ps_[:, :]), R=[pk], W=[("q" if which == 0 else "k", j)])
                    proj_fm(wv, wk, 0, 128, ev)
                (wv,), wk = load_w([wcols(w_in_d, l, 2064 + p * 256, 256)])

                def evv(i0, ps_, pk):
                    P.act(lambda e, i0=i0, ps_=ps_: e.copy(out=bv[:, i0:i0 + 2, :].rearrange("p i e -> p (i e)"), in_=ps_[:, :]),
                          R=[pk], W=[("s2", i0), ("s2", i0 + 1)])
                proj_tm(wv, wk, 0, 256, 2, evv)
                (wv,), wk = load_w([wcols(w_in_d, l, 2576 + p * 256, 256)])

                def evr(i0, ps_, pk):
                    P.act(lambda e, i0=i0, ps_=ps_: e.activation(out=sr[:, i0:i0 + 2, :].rearrange("p i e -> p (i e)"), in_=ps_[:, :],
                                                                func=AF.Silu), R=[pk], W=[("s3", i0), ("s3", i0 + 1)])
                    P.pool(lambda e, i0=i0: e.tensor_tensor(out=sr[:, i0:i0 + 2, :], in0=sr[:, i0:i0 + 2, :],
                                                           in1=pbc[:, 528 + h0 * 128:528 + h0 * 128 + 256].unsqueeze(1).to_broadcast([128, 2, 256]),
                                                           op=ALU.mult), R=[("s3", i0), ("s3", i0 + 1), "pbc"], W=[("s3", i0), ("s3", i0 + 1)])
                proj_tm(wv, wk, 0, 256, 2, evr)
                for i0 in range(0, NT, 2):
                    ps_, pk = bank()
                    for ii in range(2):
                        i = i0 + ii
                        for d in range(2):
                            P.pe(lambda e, i=i, ii=ii, d=d, ps_=ps_: e.matmul(ps_[:, ii * 256 + d * 128:ii * 256 + (d + 1) * 128], lhsT=lrT[0:33, til(i)],
                                                                              rhs=w2bd[0:33, d * 256 + p * 128:d * 256 + (p + 1) * 128], start=True, stop=True),
                                 R=[("lrT", i // 4), "lrT1", "w2bd"], W=[pk])
                    g_ = acc[:, (i0 // 2) % 2, :]
                    P.act(lambda e, ps_=ps_, g_=g_: e.activation(out=g_, in_=ps_[:, :], func=AF.Exp, scale=-1.0), R=[pk], W=[("acc", (i0 // 2) % 2)])
                    P.act(lambda e, i0=i0, g_=g_: e.activation(out=la[:, i0:i0 + 2, :].rearrange("p i e -> p (i e)"), in_=g_, func=AF.Ln, bias=1.0),
                          R=[("acc", (i0 // 2) % 2)], W=[("s4", i0), ("s4", i0 + 1)])
                stage("Bproj")
                orders = [list(range(NT)), list(range(NT - 1, -1, -1))]
                for d in range(2):
                    P.pool(lambda e, d=d: e.memset(C32[:, d, :], 0.0), W=[("C32", d)])
                    P.pool(lambda e, d=d: e.memset(Cbf[:, d, 0, :], 0.0), W=[("Cbf", d, 0)])

                def b_early(d, n):
                    i = orders[d][n]
                    par = n % 2
                    psB, pbk2 = bank()
                    P.pe(lambda e: e.matmul(psB[:, 0:128], lhsT=la[:, i, d * 128:(d + 1) * 128], rhs=tri16[:, d, :], start=True, stop=True),
                         R=[("s4", i)] + CONSTS, W=[pbk2])
                    P.act(lambda e: e.activation(out=eBt[:, d, par, :], in_=psB[:, 0:128], func=AF.Exp), R=[pbk2], W=[("eBt", d, par)])
                    P.act(lambda e: e.activation(out=eNBt[:, d, :], in_=psB[:, 0:128], func=AF.Exp, scale=-1.0), R=[pbk2], W=[("eNBt", d)])
                    P.pool(lambda e: e.tensor_tensor(out=qs[:, d, par, :], in0=qT[:, til(i)], in1=eBt[:, d, par, :], op=ALU.mult),
                           R=[("q", i // 4), ("eBt", d, par)], W=[("qs", d, par)])
                    P.dve(lambda e: e.tensor_tensor(out=ks[:, d, par, :], in0=kT[:, til(i)], in1=eNBt[:, d, :], op=ALU.mult),
                          R=[("k", i // 4), ("eNBt", d)], W=[("ks", d, par)])
                    psSs = [bank(), bank()]
                    for hh in range(2):
                        rs = slice(hh * 64, (hh + 1) * 64)
                        psS, psk = psSs[hh]
                        P.pe(lambda e: e.matmul(psS[:, 0:128], lhsT=ks[rs, d, par, :], rhs=qs[rs, d, par, :], start=True, stop=True),
                             R=[("ks", d, par), ("qs", d, par)], W=[psk])
                    pt_, ptk = tbank()
                    P.pe(lambda e: e.transpose(pt_[:, 0:128], ks[:, d, par, :], ident[:]), R=[("ks", d, par), "ident"], W=[ptk])
                    for hh in range(2):
                        psS, psk = psSs[hh]
                        P.dve(lambda e: e.tensor_tensor(out=Sm[:, d, par, hh, :], in0=psS[:, 0:128], in1=maskB[:, d, :], op=ALU.mult),
                              R=[psk] + CONSTS, W=[("Sm", d, par, hh)])
                    P.act(lambda e: e.activation(out=ktok[:, d, par, :], in_=pt_[:, 0:128], func=AF.Copy, scale=0.125), R=[ptk], W=[("ktok", d, par)])

                def b_main(d, n):
                    i = orders[d][n]
                    par = n % 2
                    lastcol = 127 if d == 0 else 0
                    psU, puk = bank()
                    P.pe(lambda e: e.matmul(psU[0:64, 0:128], lhsT=ktok[:, d, par, 0:64], rhs=bv[:, i, 0:128], start=True, stop=True),
                         R=[("ktok", d, par), ("s2", i)], W=[puk])
                    P.pe(lambda e: e.matmul(psU[64:128, 0:128], lhsT=ktok[:, d, par, 64:128], rhs=bv[:, i, 128:256], start=True, stop=True,
                                            tile_position=(0, 64)), R=[("ktok", d, par), ("s2", i)], W=[puk])
                    psO, pok = bank()
                    for hh in range(2):
                        rs = slice(hh * 64, (hh + 1) * 64)
                        P.pe(lambda e: e.matmul(psO[:, hh * 128:(hh + 1) * 128], lhsT=Sm[:, d, par, hh, :],
                                                rhs=bv[:, i, hh * 128:(hh + 1) * 128], start=True, stop=False),
                             R=[("Sm", d, par, hh), ("s2", i)], W=[pok])
                        P.pe(lambda e: e.matmul(psO[:, hh * 128:(hh + 1) * 128], lhsT=qs[rs, d, par, :],
                                                rhs=Cbf[rs, d, par, 0:128], start=False, stop=True),
                             R=[("qs", d, par), ("Cbf", d, par)], W=[pok])
                    P.act(lambda e: e.activation(out=tmpS[:, d, :], in_=psU[:, 0:128], func=AF.Copy, scale=eBt[:, d, par, lastcol:lastcol + 1]),
                          R=[puk, ("eBt", d, par)], W=[("tmpS", d)])
                    P.dve(lambda e: e.scalar_tensor_tensor(out=C32[:, d, 0:128], in0=C32[:, d, 0:128], scalar=eBt[:, d, par, lastcol:lastcol + 1],
                                                           in1=tmpS[:, d, :], op0=ALU.mult, op1=ALU.add),
                          R=[("tmpS", d), ("eBt", d, par), ("C32", d)], W=[("C32", d)])
                    P.pool(lambda e: e.tensor_copy(out=Cbf[:, d, 1 - par, 0:128], in_=C32[:, d, 0:128]),
                           R=[("C32", d)], W=[("Cbf", d, 1 - par)])
                    if n < NT // 2:
                        P.act(lambda e: e.copy(out=hsum[:, i, :], in_=psO[:, 0:256]), R=[pok], W=[("s5", i)])
                    else:
                        q = bcnt[0] % 2
                        bcnt[0] += 1
                        bq[(n, d)] = q
                        P.dve(lambda e: e.tensor_tensor(out=tot2[:, q, :], in0=psO[:, 0:256], in1=hsum[:, i, :], op=ALU.add),
                              R=[pok, ("s5", i)], W=[("acc", q)])

                bcnt = [0]
                bq = {}

                def b_core(n, d):
                    if n == 0:
                        b_early(d, 0)
                    if n + 1 < NT:
                        b_early(d, n + 1)
                    b_main(d, n)

                run_pipeline(b_core, lambda n, d: bq.get((n, d)), p, 1, sr, lambda ti: ("s3", ti), hd1=0, hd2=1, td=0)

            stage("B")
            if dbg and "yT" in dbg and l == 0:
                P.barrier()
                for c in range(KC):
                    P.act(lambda e, c=c: e.copy(out=xT[:, c, :], in_=yT[:, c, :]), R=[], W=[("xT", c, j) for j in range(NB)])
                P.barrier()

            P.barrier()
            for q4 in range(4):
                P.dma("pool", lambda e, q4=q4, l=l: e.dma_start(out=wo[:, :, q4 * 256:(q4 + 1) * 256],
                                                                in_=w_out_d[l, :, q4 * 256:(q4 + 1) * 256].rearrange("(c p) n -> p c n", p=128)),
                      W=["wo"])
            for j in range(NB):
                for co in range(KC):
                    ps_, pk = bank()
                    for ch in range(KC):
                        P.pe(lambda e, co=co, ch=ch, j=j, ps_=ps_: e.matmul(ps_[:, :], lhsT=wo[:, ch, co * 128:(co + 1) * 128], rhs=yview(ch)[:, blk(j)],
                                                                            start=(ch == 0), stop=(ch == KC - 1)),
                             R=["wo"] + [ykey(ch, i) for i in range(4 * j, 4 * j + 4)], W=[pk])
                    P.act(lambda e, co=co, ps_=ps_: e.copy(out=mixT[:, co, :], in_=ps_[:, :]), R=[pk], W=[("mixT", co)])
                post_norm_residual(l, 8, mixT, lambda c: ("mixT", c), j * 512, j)

            stage("wout")
            pre_norm(l, 16)
            P.pool(lambda e: e.tensor_copy(out=hhalo[:, :, :], in_=hnT[:, :, 1023:1025]),
                   R=[("hnT", c, j) for c in range(KC) for j in (1, 2)], W=["hhalo"])
            P.barrier()
            ffT_bf = arena[:, NF * 1024:NF * 1024 + 2 * KC * 512]
            gu_pool = [(wbuf[0][:, :], [("wbuf", 0)]), (wbuf[1][:, :], [("wbuf", 1)])] + \
                      [(ffT_bf[:, j * 2048:(j + 1) * 2048], [("ffT", 2 * j), ("ffT", 2 * j + 1)]) for j in range(4)]
            gurot = [0]
            for H in range(2):
                t0 = H * 1024
                graws = [graw, pbc[:, 0:1026]]
                wst = {}

                def G(f):
                    gr = graws[f % 2]
                    (wgv, wuv), wks = load_into(gu_pool[gurot[0] % len(gu_pool)], [wcols(wg_d, l, f * 128, 128), wcols(wu_d, l, f * 128, 128)])
                    gurot[0] += 1
                    wst[f] = (wuv, wks)
                    if H == 0:
                        P.pool(lambda e: e.memset(gr[:, 0:1], 0.0), W=[("graw", f % 2, 0)])
                        halo_col, hsel = 1025, 1
                    else:
                        P.pool(lambda e: e.memset(gr[:, 1025:1026], 0.0), W=[("graw", f % 2, 1)])
                        halo_col, hsel = 0, 0
                    psh, phk = bank()
                    for c in range(KC):
                        P.pe(lambda e: e.matmul(psh[:, 0:1], lhsT=wgv[:, c, :], rhs=hhalo[:, c, hsel:hsel + 1],
                                                start=(c == 0), stop=(c == KC - 1)), R=wks + ["hhalo"], W=[phk])
                    P.act(lambda e: e.copy(out=gr[:, halo_col:halo_col + 1], in_=psh[:, 0:1]),
                          R=[phk], W=[("graw", f % 2, 0 if halo_col == 0 else 1)])
                    for b in range(2):
                        jj = 2 * H + b
                        ps_, pk = bank()
                        for c in range(KC):
                            P.pe(lambda e: e.matmul(ps_[:, :], lhsT=wgv[:, c, :], rhs=hnT[:, c, blk(jj)],
                                                    start=(c == 0), stop=(c == KC - 1)), R=wks + [("hnT", c, jj)], W=[pk])
                        P.act(lambda e: e.copy(out=gr[:, 1 + b * 512:1 + (b + 1) * 512], in_=ps_[:, :]), R=[pk], W=[("grawb", f % 2, b)])

                def C(f):
                    gr = graws[f % 2]
                    cb = 48 + f * 4
                    rk = [("graw", f % 2, 0), ("graw", f % 2, 1), ("grawb", f % 2, 0), ("grawb", f % 2, 1)]
                    for b in range(2):
                        a_ = acc[:, b, :]
                        P.dve(lambda e: e.tensor_scalar(out=a_, in0=gr[:, 1 + b * 512:1 + (b + 1) * 512],
                                                        scalar1=pcol[:, cb + 1:cb + 2], scalar2=pcol[:, cb + 3:cb + 4],
                                                        op0=ALU.mult, op1=ALU.add), R=rk + ["pcol"], W=[("acc", b)])
                        P.dve(lambda e: e.scalar_tensor_tensor(out=a_, in0=gr[:, b * 512:(b + 1) * 512],
                                                               scalar=pcol[:, cb:cb + 1], in1=a_, op0=ALU.mult, op1=ALU.add),
                              R=rk + ["pcol", ("acc", b)], W=[("acc", b)])
                        P.dve(lambda e: e.scalar_tensor_tensor(out=a_, in0=gr[:, 2 + b * 512:2 + (b + 1) * 512],
                                                               scalar=pcol[:, cb + 2:cb + 3], in1=a_, op0=ALU.mult, op1=ALU.add),
                              R=rk + ["pcol", ("acc", b)], W=[("acc", b)])
                        P.act(lambda e: e.activation(out=gel[:, b, :], in_=a_, func=AF.Gelu_apprx_tanh), R=[("acc", b)], W=[("gel", b)])

                def U(f):
                    wuv, wks = wst.pop(f)
                    for b in range(2):
                        jj = 2 * H + b
                        ps_, pk = bank()
                        for c in range(KC):
                            P.pe(lambda e: e.matmul(ps_[:, :], lhsT=wuv[:, c, :], rhs=hnT[:, c, blk(jj)],
                                                    start=(c == 0), stop=(c == KC - 1)), R=wks + [("hnT", c, jj)], W=[pk])
                        P.dve(lambda e: e.tensor_tensor(out=actT[:, f, b * 512:(b + 1) * 512], in0=gel[:, b, :], in1=ps_[:, :], op=ALU.mult),
                              R=[("gel", b), pk], W=[("actT", f, b)])

                G(0)
                for f in range(NF):
                    if f + 1 < NF:
                        G(f + 1)
                    C(f)
                    U(f)

                d_pool = [(hnT[:, c, t0:t0 + 1024], [("hnT", c, 2 * H), ("hnT", c, 2 * H + 1)]) for c in range(KC)]
                drot = 0
                for b in range(2):
                    for co in range(KC):
                        parts = []
                        for (fa, fb) in ((0, 8), (8, 16), (16, 22)):
                            (wv_,), wk_ = load_into(d_pool[drot % KC], [wd_d[l, fa * 128:fb * 128, co * 128:(co + 1) * 128].rearrange("(f p) n -> p f n", p=128)])
                            drot += 1
                            parts.append((fa, fb, wv_, wk_))
                        ps_, pk = bank()
                        for f in range(NF):
                            fa, fb, wdv, wk_ = parts[f // 8]
                            P.pe(lambda e: e.matmul(ps_[:, :], lhsT=wdv[:, f - fa, :], rhs=actT[:, f, b * 512:(b + 1) * 512],
                                                    start=(f == 0), stop=(f == NF - 1)), R=wk_ + [("actT", f, b)], W=[pk])
                        P.act(lambda e, co=co, ps_=ps_: e.copy(out=ffT[:, co, :], in_=ps_[:, :]), R=[pk], W=[("ffT", co)])
                    post_norm_residual(l, 24, ffT, lambda c: ("ffT", c), t0 + b * 512, 2 * H + b)
            P.barrier()

        try:
            stage("load")
            for l in range(nl):
                layer(l)
        except _Stop:
            pass
        for c in range(KC):
            P.dma("sp", lambda e, c=c: e.dma_start(out=outT_d[c * 128:(c + 1) * 128, :], in_=xT[:, c, :]),
                  R=[("xT", c, j) for j in range(NB)], W=[("out", c)])
        P.add("sp", None, R=[("out", c) for c in range(KC)])
        P.emit(nc)
    return nc


def _pack_params(ls, norm_mix_pre, norm_mix_post, norm_ffn_pre, norm_ffn_post, mlstm_conv_w, mlstm_conv_b,
                 ffn_conv_w, ffn_conv_b, mlstm_gate_b, mlstm_norm, gla_norm):
    nl = len(ls)
    pcol = np.zeros((128, nl * PCOLS), np.float32)
    pbc = np.zeros((nl, PBC), np.float32)
    for li, l in enumerate(ls):
        o = li * PCOLS
        for gi, g in enumerate((norm_mix_pre, norm_mix_post, norm_ffn_pre, norm_ffn_post)):
            pcol[:, o + gi * 8:o + gi * 8 + 8] = g[l].reshape(8, 128).T
        for cj in range(4):
            pcol[:, o + 32 + cj * 4:o + 32 + cj * 4 + 3] = mlstm_conv_w[l][:, cj * 128:(cj + 1) * 128].T
            pcol[:, o + 32 + cj * 4 + 3] = mlstm_conv_b[l][cj * 128:(cj + 1) * 128]
        for f in range(NF):
            pcol[:, o + 48 + f * 4:o + 48 + f * 4 + 3] = ffn_conv_w[l][:, f * 128:(f + 1) * 128].T
            pcol[:, o + 48 + f * 4 + 3] = ffn_conv_b[l][f * 128:(f + 1) * 128]
        pbc[li, 0:16] = mlstm_gate_b[l]
        pbc[li, 16:528] = mlstm_norm[l].reshape(-1)
        pbc[li, 528:1040] = gla_norm[l].reshape(-1)
    return pcol, pbc


_CACHE = {}


def _get_prog(nl):
    if nl not in _CACHE:
        _CACHE[nl] = build_program(nl)
    return _CACHE[nl]


FUSED = True


def kernel(x, norm_mix_pre, norm_mix_post, norm_ffn_pre, norm_ffn_post, w_in, mlstm_gate_b, mlstm_conv_w,
           mlstm_conv_b, mlstm_norm, gla_w2, gla_b, gla_norm, w_out, ffn_w_gate, ffn_w_up, ffn_conv_w,
           ffn_conv_b, ffn_w_down):
    f = lambda a: np.ascontiguousarray(np.asarray(a), dtype=np.float32)
    x = f(x)
    args = [f(a) for a in (norm_mix_pre, norm_mix_post, norm_ffn_pre, norm_ffn_post, mlstm_conv_w, mlstm_conv_b,
                           ffn_conv_w, ffn_conv_b, mlstm_gate_b, mlstm_norm, gla_norm)]
    w_in, w_out, wg, wu, wd = f(w_in), f(w_out), f(ffn_w_gate), f(ffn_w_up), f(ffn_w_down)
    w2, gb = f(gla_w2), f(gla_b)
    xTs = [np.ascontiguousarray(x[b].T) for b in range(NCORES)]
    groups = [list(range(DEPTH))] if FUSED else [[l] for l in range(DEPTH)]
    for ls in groups:
        nl = len(ls)
        nc = _get_prog(nl)
        pcol, pbc = _pack_params(ls, *args)
        sl = slice(ls[0], ls[-1] + 1)
        shared = {"w_in": w_in[sl], "w_out": w_out[sl], "wg": wg[sl], "wu": wu[sl], "wd": wd[sl], "pcol": pcol, "pbc": pbc,
                  "w2": w2[sl], "gb": np.ascontiguousarray(gb[sl].reshape(nl, 512))}
        in_maps = [dict(shared, xT=xTs[b]) for b in range(NCORES)]
        res = run_bass_kernel_spmd(nc, in_maps, core_ids=list(range(NCORES)))
        xTs = [np.asarray(r["outT"]) for r in res.results]
    return np.stack([np.ascontiguousarray(t.T) for t in xTs], axis=0).astype(np.float32)
```

```python
import contextlib
import types
import numpy as np
import concourse.bass as bass
import concourse.mybir as mybir
from concourse.bass_utils import run_bass_kernel_spmd

F32 = mybir.dt.float32
BF16 = mybir.dt.bfloat16
ALU = mybir.AluOpType
AF = mybir.ActivationFunctionType

EPOCH = 30000
HD1, HD2 = 1, 2
NCORES = 8
DEPTH = 4
D = 1024
S = 2048
NT = 16
NB = 4
KC = 8
FF = 2816
NF = 22
INC = 3120
EPS = 1e-6
PCOLS = 136
PBC = 1040


def _freeze(fn):
    if fn is None or fn.__closure__ is None:
        return fn
    cells = []
    for c in fn.__closure__:
        try:
            cells.append(types.CellType(c.cell_contents))
        except ValueError:
            cells.append(c)
    return types.FunctionType(fn.__code__, fn.__globals__, fn.__name__, fn.__defaults__, tuple(cells))


class _Op:
    __slots__ = ("eng", "fn", "R", "W", "dma", "deps", "marked", "tick", "dsem", "dtick", "dprev")

    def __init__(self, eng, fn, R, W, dma):
        self.eng = eng
        self.fn = fn
        self.R = tuple(R)
        self.W = tuple(W)
        self.dma = dma
        self.deps = ()
        self.marked = False
        self.tick = 0
        self.dsem = -1
        self.dtick = 0
        self.dprev = None


class Prog:
    ENGS = ("pe", "act", "dve", "pool", "sp")

    def __init__(self, n_dma_sems=16):
        self.ops = []
        self.nds = n_dma_sems

    def add(self, eng, fn, R=(), W=(), dma=False):
        self.ops.append(_Op(eng, _freeze(fn), R, W, dma))

    def pe(self, fn, R=(), W=()):
        self.add("pe", fn, R, W)

    def act(self, fn, R=(), W=()):
        self.add("act", fn, R, W)

    def dve(self, fn, R=(), W=()):
        self.add("dve", fn, R, W)

    def pool(self, fn, R=(), W=()):
        self.add("pool", fn, R, W)

    def dma(self, eng, fn, R=(), W=()):
        self.add(eng, fn, R, W, dma=True)

    def barrier(self):
        self.ops.append(_Op("__barrier__", None, (), (), False))

    def analyze(self):
        last_w = {}
        readers = {}
        last_on_eng = {e: None for e in self.ENGS}
        pend = {e: None for e in self.ENGS}
        out = []
        for op in self.ops:
            if op.eng == "__barrier__":
                snap = [v for v in last_on_eng.values() if v is not None]
                for e in self.ENGS:
                    pend[e] = snap
                continue
            g = len(out)
            deps = set()
            for k in op.R:
                if k in last_w:
                    deps.add(last_w[k])
            for k in op.W:
                if k in last_w:
                    deps.add(last_w[k])
                for r in readers.get(k, ()):
                    deps.add(r)
            if pend[op.eng] is not None:
                deps.update(pend[op.eng])
                pend[op.eng] = None
            for k in op.R:
                readers.setdefault(k, []).append(g)
            for k in op.W:
                last_w[k] = g
                readers[k] = []
            deps.discard(g)
            op.deps = tuple(sorted(deps))
            out.append(op)
            if op.fn is not None:
                last_on_eng[op.eng] = g
        self.lin = out
        ndma = 0
        nq = [0, 0]
        last_dma_on_sem = {}
        for g, op in enumerate(out):
            for d in op.deps:
                p = out[d]
                if p.dma:
                    continue
                if p.eng == "pe" and op.eng == "pe" and not op.dma:
                    continue
                p.marked = True
            if op.dma:
                half = self.nds // 2
                qi = 0 if op.eng == "sp" else 1
                s = qi * half + (nq[qi] % half)
                nq[qi] += 1
                ndma += 1
                op.dsem = s
                prev = last_dma_on_sem.get(s)
                op.dprev = prev
                op.dtick = (out[prev].dtick if prev is not None else 0) + 16
                last_dma_on_sem[s] = g
        cnt = {e: 0 for e in self.ENGS}
        for op in out:
            if op.marked and not op.dma:
                cnt[op.eng] += 1
                op.tick = cnt[op.eng]
        self.cnt = cnt
        self.ndma = ndma

    def emit(self, nc):
        self.analyze()
        out = self.lin
        with contextlib.ExitStack() as st:
            esems = {}
            for e in self.ENGS:
                nep = self.cnt[e] // EPOCH + 1
                esems[e] = [st.enter_context(nc.semaphore(f"s_{e}_{i}")) for i in range(nep)]
            dsems = [st.enter_context(nc.semaphore(f"s_dma_{i}")) for i in range(self.nds)]
            block = st.enter_context(nc.Block())
            by_eng = {e: [] for e in self.ENGS}
            for g, op in enumerate(out):
                by_eng[op.eng].append((g, op))

            def sem_of(p):
                if p.dma:
                    return ("d", p.dsem), dsems[p.dsem], p.dtick
                ep = (p.tick - 1) // EPOCH
                return (p.eng, ep), esems[p.eng][ep], p.tick - ep * EPOCH

            def run(eng_name, e):
                waited = {}
                for g, op in by_eng[eng_name]:
                    waits = {}
                    deps = list(op.deps)
                    if op.dma and op.dprev is not None:
                        deps.append(op.dprev)
                    for d in deps:
                        p = out[d]
                        if (not p.dma) and p.eng == "pe" and eng_name == "pe" and not op.dma:
                            continue
                        key, sem, val = sem_of(p)
                        if waited.get(key, 0) >= val:
                            continue
                        if key not in waits or waits[key][1] < val:
                            waits[key] = (sem, val)
                    for key, (sem, val) in waits.items():
                        e.wait_ge(sem, val)
                        waited[key] = val
                    if op.fn is None:
                        continue
                    ins = op.fn(e)
                    if op.dma:
                        ins.then_inc(dsems[op.dsem], 16)
                    elif op.marked:
                        ep = (op.tick - 1) // EPOCH
                        ins.then_inc(esems[eng_name][ep], 1)

            @block.tensor
            def _(e):
                run("pe", e)

            @block.scalar
            def _(e):
                run("act", e)

            @block.vector
            def _(e):
                run("dve", e)

            @block.gpsimd
            def _(e):
                run("pool", e)

            @block.sync
            def _(e):
                run("sp", e)


class _Stop(Exception):
    pass


def build_program(nl, dbg=None, stop_at=None):
    nc = bass.Bass("TRN2", target_bir_lowering=False)
    P = Prog()

    def din(name, shape):
        return nc.dram_tensor(name, shape, F32, kind="ExternalInput").ap()

    xT_d = din("xT", [D, S])
    w_in_d = din("w_in", [nl, D, INC])
    w_out_d = din("w_out", [nl, D, D])
    wg_d = din("wg", [nl, D, FF])
    wu_d = din("wu", [nl, D, FF])
    wd_d = din("wd", [nl, FF, D])
    pcol_d = din("pcol", [128, nl * PCOLS])
    pbc_d = din("pbc", [nl, PBC])
    w2_d = din("w2", [nl, 2, 16, 256])
    gb_d = din("gb", [nl, 512])
    outT_d = nc.dram_tensor("outT", [D, S], F32, kind="ExternalOutput").ap()
    dbg_d = {}
    if dbg:
        for k, shp in dbg.items():
            dbg_d[k] = nc.dram_tensor("dbg_" + k, shp, F32, kind="ExternalOutput").ap()

    st = contextlib.ExitStack()
    with st:
        def sb(name, shape, dt):
            return st.enter_context(nc.sbuf_tensor(name, shape, dt))

        def psum(name, shape, dt):
            return st.enter_context(nc.psum_tensor(name, shape, dt))

        xT = sb("xT_sb", [128, KC, S], F32)
        hnT = sb("hnT", [128, KC, S], BF16)
        pcol = sb("pcol_sb", [128, PCOLS], F32)
        pbc = sb("pbc_sb", [128, PBC], F32)
        w2bd = sb("w2bd", [128, 512], BF16)
        lrT = sb("lrT", [128, S], BF16)
        ones_bf = sb("ones_bf", [128, 128], BF16)
        negones_f = sb("negones_f", [128, 128], F32)
        ident = sb("ident", [128, 128], BF16)
        maskA = sb("maskA", [128, 2, 128], BF16)
        maskB = sb("maskB", [128, 2, 128], BF16)
        tri16 = sb("tri16", [128, 2, 128], BF16)
        trif = sb("trif", [128, 2, 128], F32)
        WB = 2048
        wbuf = [sb(f"wbuf{i}", [128, WB], BF16) for i in range(2)]
        A_YT = 0
        A_Q = A_YT + 6 * S
        A_K = A_Q + S
        A_2 = A_K + S
        A_3 = A_2 + 4096
        A_4 = A_3 + 4160
        A_5 = A_4 + 4096
        A_END = A_5 + 4100
        arena = sb("arena", [128, A_END], BF16)
        yT = arena[:, A_YT:A_Q].rearrange("p (c t) -> p c t", c=6)
        qT = arena[:, A_Q:A_K]
        kT = arena[:, A_K:A_2]
        kw = arena[:, A_2:A_3].rearrange("p (d i h e) -> p d i h e", d=2, i=NT, h=2)
        bv = arena[:, A_2:A_3].rearrange("p (i e) -> p i e", i=NT)
        vaug = arena[:, A_3:A_4].rearrange("p (i h e) -> p i h e", i=NT, h=2)
        sr = arena[:, A_3:A_3 + 4096].rearrange("p (i e) -> p i e", i=NT)
        sigo = arena[:, A_4:A_5].rearrange("p (i e) -> p i e", i=NT)
        la = arena[:, A_4:A_5].rearrange("p (i e) -> p i e", i=NT)
        hsum = arena[:, A_5:A_5 + 4096].rearrange("p (i e) -> p i e", i=NT)
        qkraw = arena[:, A_5:A_END].bitcast(F32)
        wo = arena[:, A_Q:A_Q + KC * D].rearrange("p (c n) -> p c n", c=KC)
        mixT = arena[:, A_Q + KC * D:A_Q + KC * D + 2 * KC * 512].bitcast(F32).rearrange("p (c t) -> p c t", c=KC)
        actT = arena[:, 0:NF * 1024].rearrange("p (f t) -> p f t", f=NF)
        ffT = arena[:, NF * 1024:NF * 1024 + 2 * KC * 512].bitcast(F32).rearrange("p (c t) -> p c t", c=KC)
        graw = arena[:, NF * 1024 + 2 * KC * 512:NF * 1024 + 2 * KC * 512 + 2 * 1026].bitcast(F32)
        assert NF * 1024 + 2 * KC * 512 + 2 * 1026 <= A_END

        gl = sb("gl", [128, NT, 16], F32)
        lf = sb("lf", [128, 2, NT * 4], F32)
        gtmp = sb("gtmp", [128, 2, NT * 4], F32)
        eb = sb("eb", [128, 2, NT, 4], F32)
        enb = sb("enb", [128, 2, NT, 4], F32)
        wsc = sb("wsc", [128, 2, NT, 4], F32)
        dec = sb("dec", [128, 2, NT, 4], F32)
        wdec = sb("wdec", [128, 2, NT, 4], F32)
        decsel = sb("decsel", [128, 2, NT], F32)
        Sm = sb("Sm", [128, 2, 2, 2, 128], BF16)
        C32 = sb("C32", [128, 2, 132], F32)
        Cbf = sb("Cbf", [128, 2, 2, 132], BF16)
        eBt = sb("eBt", [128, 2, 2, 128], F32)
        eNBt = sb("eNBt", [128, 2, 128], F32)
        qs = sb("qs", [128, 2, 2, 128], BF16)
        ks = sb("ks", [128, 2, 2, 128], BF16)
        ktok = sb("ktok", [128, 2, 2, 128], BF16)
        tmpS = sb("tmpS", [128, 2, 128], F32)
        osb = sb("osb", [128, 2, 260], F32)
        sml = sb("sml", [128, 5, 16], F32)
        sqb = sb("sqb", [128, 2, 512], BF16)
        hhalo = sb("hhalo", [128, KC, 2], BF16)
        rstd = sb("rstd", [128, 512], F32)
        acc = sb("acc", [128, 2, 512], F32)
        bsb = acc[:, 1, 0:256]
        gel = Sm[:].rearrange("p a b c t -> p (a b c t)").rearrange("p (b t) -> p b t", b=2)
        tot2 = acc[:, :, 0:256]
        junk2 = sqb[:, 0, :].rearrange("p (q h t) -> p q h t", q=2, h=2)
        ytile = sqb[:, 1, :].rearrange("p (b t) -> p b t", b=2)
        orders_g = [list(range(NT)), list(range(NT - 1, -1, -1))]

        pb = [psum(f"pb{i}", [128, 512], F32) for i in range(6)]
        ptb = [psum(f"ptb{i}", [128, 1024], BF16) for i in range(2)]
        rot = {"n": 0, "t": 0}

        def bank():
            i = rot["n"] % 6
            rot["n"] += 1
            return pb[i], ("ps", i)

        def tbank():
            i = rot["t"] % 2
            rot["t"] += 1
            return ptb[i], ("pt", i)

        wrot = {"n": 0}

        def load_into(bufspec, segs):
            ap2d, keys = bufspec
            views = []
            off = 0
            for src in segs:
                k, n = src.shape[1], src.shape[2]
                v = ap2d[:, off:off + k * n].rearrange("p (k n) -> p k n", k=k)
                P.dma("pool", lambda e: e.dma_start(out=v, in_=src), W=keys)
                views.append(v)
                off += k * n
            assert off <= ap2d.shape[1]
            return views, list(keys)

        def load_w(segs):
            i = wrot["n"] % 2
            wrot["n"] += 1
            views, _ = load_into((wbuf[i][:, :], [("wbuf", i)]), segs)
            return views, ("wbuf", i)

        def wcols(wd3, l, a, n, kc=KC):
            return wd3[l, :, a:a + n].rearrange("(c p) n -> p c n", p=128)

        P.pool(lambda e: e.memset(ones_bf[:], 1.0), W=["ones_bf"])
        P.pool(lambda e: e.memset(negones_f[:], -1.0), W=["negones_f"])
        P.pool(lambda e: e.memset(ident[:], 0.0), W=["ident"])
        P.pool(lambda e: e.affine_select(out=ident[:], in_=ident[:], pattern=[[-1, 128]], compare_op=ALU.not_equal,
                                         fill=1.0, base=0, channel_multiplier=1), R=["ident"], W=["ident"])
        for t_, val in ((maskA, 0.125), (maskB, 0.125), (tri16, -1.0 / 16.0), (trif, -1.0)):
            P.pool(lambda e, t_=t_, val=val: e.memset(t_[:], val), W=[("const", id(t_))])
            P.pool(lambda e, t_=t_: e.affine_select(out=t_[:, 0, :], in_=t_[:, 0, :], pattern=[[1, 128]], compare_op=ALU.is_ge,
                                                    fill=0.0, base=0, channel_multiplier=-1), R=[("const", id(t_))], W=[("const", id(t_))])
            P.pool(lambda e, t_=t_: e.affine_select(out=t_[:, 1, :], in_=t_[:, 1, :], pattern=[[-1, 128]], compare_op=ALU.is_ge,
                                                    fill=0.0, base=0, channel_multiplier=1), R=[("const", id(t_))], W=[("const", id(t_))])
        CONSTS = ["ones_bf", "negones_f", "ident"] + [("const", id(t_)) for t_ in (maskA, maskB, tri16, trif)]
        P.pool(lambda e: e.memset(lrT[32:33, :], 1.0), W=["lrT1"])
        for c in range(KC):
            P.dma("sp", lambda e, c=c: e.dma_start(out=xT[:, c, :], in_=xT_d[c * 128:(c + 1) * 128, :]),
                  W=[("xT", c, j) for j in range(NB)])

        def blk(j):
            return slice(j * 512, (j + 1) * 512)

        def ykey(ch, i):
            return ("yT", ch, i) if ch < 6 else ("hnT", ch - 6, i // 4)

        def yview(ch):
            return yT[:, ch, :] if ch < 6 else hnT[:, ch - 6, :]

        def til(i):
            return slice(i * 128, (i + 1) * 128)

        def ss_and_rstd(srcs, src_keys, nparity):
            ps_, pk = bank()
            for c in range(KC):
                sq = sqb[:, c % 2, :]
                P.act(lambda e, sq=sq, s_=srcs[c]: e.activation(out=sq, in_=s_, func=AF.Square),
                      R=[src_keys[c]], W=[("sqb", c % 2)])
                P.pe(lambda e, sq=sq, c=c, ps_=ps_: e.matmul(ps_[:, :], lhsT=ones_bf[:], rhs=sq, start=(c == 0), stop=(c == KC - 1)),
                     R=[("sqb", c % 2), "ones_bf"], W=[pk])
            r_ = rstd[:, :]
            P.act(lambda e, ps_=ps_, r_=r_: e.activation(out=r_, in_=ps_[:, :], func=AF.Ln, scale=1.0 / D, bias=EPS),
                  R=[pk], W=["rstd"])
            P.act(lambda e, r_=r_: e.activation(out=r_, in_=r_, func=AF.Exp, scale=-0.5),
                  R=["rstd"], W=["rstd"])
            return r_, "rstd"

        def pre_norm_block(l, gofs, j):
            srcs = [xT[:, c, blk(j)] for c in range(KC)]
            keys = [("xT", c, j) for c in range(KC)]
            r_, rk = ss_and_rstd(srcs, keys, j % 2)
            for c in range(KC):
                col = gofs + c
                P.dve(lambda e, c=c, j=j, col=col, r_=r_: e.scalar_tensor_tensor(
                    out=hnT[:, c, blk(j)], in0=xT[:, c, blk(j)], scalar=pcol[:, col:col + 1], in1=r_,
                    op0=ALU.mult, op1=ALU.mult), R=[("xT", c, j), "pcol", rk], W=[("hnT", c, j)])

        def pre_norm(l, gofs):
            for j in range(NB):
                pre_norm_block(l, gofs, j)

        def post_norm_residual(l, gofs, srcT, src_keyf, tok0, j_x):
            srcs = [srcT[:, c, :] for c in range(KC)]
            keys = [src_keyf(c) for c in range(KC)]
            r_, rk = ss_and_rstd(srcs, keys, j_x % 2)
            for c in range(KC):
                col = gofs + c
                P.dve(lambda e, c=c, col=col, r_=r_: e.scalar_tensor_tensor(
                    out=srcT[:, c, :], in0=srcT[:, c, :], scalar=pcol[:, col:col + 1], in1=r_,
                    op0=ALU.mult, op1=ALU.mult), R=[keys[c], "pcol", rk], W=[keys[c]])
                P.dve(lambda e, c=c: e.tensor_tensor(out=xT[:, c, tok0:tok0 + 512], in0=xT[:, c, tok0:tok0 + 512],
                                                     in1=srcT[:, c, :], op=ALU.add),
                      R=[keys[c], ("xT", c, j_x)], W=[("xT", c, j_x)])

        def proj_fm(wv, wkey, ncol_lo, m, evac):
            for j in range(NB):
                ps_, pk = bank()
                for c in range(KC):
                    P.pe(lambda e, c=c, j=j, ps_=ps_: e.matmul(ps_[0:m, :], lhsT=wv[:, c, ncol_lo:ncol_lo + m], rhs=hnT[:, c, blk(j)],
                                                               start=(c == 0), stop=(c == KC - 1)),
                         R=[wkey, ("hnT", c, j)], W=[pk])
                evac(j, ps_, pk)

        def proj_tm(wv, wkey, ncol_lo, n, per_bank, evac):
            for i0 in range(0, NT, per_bank):
                ps_, pk = bank()
                for ii in range(per_bank):
                    i = i0 + ii
                    for c in range(KC):
                        P.pe(lambda e, c=c, i=i, ii=ii, ps_=ps_: e.matmul(
                            ps_[:, ii * n:(ii + 1) * n], lhsT=hnT[:, c, til(i)], rhs=wv[:, c, ncol_lo:ncol_lo + n],
                            start=(c == 0), stop=(c == KC - 1)),
                            R=[wkey, ("hnT", c, i // 4)], W=[pk])
                evac(i0, ps_, pk)

        def head1(i, q, gate_t, gate_key):
            tt = tot2[:, q, :]
            sq_ = sml[:, 3 + q, :]
            for hh in range(2):
                P.act(lambda e: e.activation(out=junk2[:, q, hh, :],
                                             in_=tt[:, hh * 128:(hh + 1) * 128], func=AF.Square, accum_out=sq_[:, hh:hh + 1]),
                      R=[("acc", q)], W=[("junk", q, hh), ("sml0", q, hh)])
            P.act(lambda e: e.activation(out=sq_[:, 2:4], in_=sq_[:, 0:2], func=AF.Ln, scale=1.0 / 128, bias=EPS),
                  R=[("sml0", q, 0), ("sml0", q, 1)], W=[("sml0b", q)])
            P.act(lambda e: e.activation(out=sq_[:, 4:6], in_=sq_[:, 2:4], func=AF.Exp, scale=-0.5),
                  R=[("sml0b", q)], W=[("sml0c", q)])
            yt = ytile[:, q, :]
            for hh in range(2):
                P.dve(lambda e: e.scalar_tensor_tensor(
                    out=yt[:, hh * 128:(hh + 1) * 128], in0=tt[:, hh * 128:(hh + 1) * 128], scalar=sq_[:, 4 + hh:5 + hh],
                    in1=gate_t[:, i, hh * 128:(hh + 1) * 128], op0=ALU.mult, op1=ALU.mult),
                    R=[("acc", q), ("sml0c", q), gate_key], W=[("ytile", q)])

        def head2(i, q, p, grp):
            yt = ytile[:, q, :]
            pt_, ptk = tbank()
            for hh in range(2):
                P.pe(lambda e: e.transpose(pt_[:, hh * 128:(hh + 1) * 128], yt[:, hh * 128:(hh + 1) * 128], ident[:]),
                     R=[("ytile", q), "ident"], W=[ptk])
            ch = grp * 4 + 2 * p
            for hh in range(2):
                dstv = yview(ch + hh)[:, til(i)]
                P.act(lambda e: e.copy(out=dstv, in_=pt_[:, hh * 128:(hh + 1) * 128]), R=[ptk], W=[ykey(ch + hh, i)])

        def run_pipeline(core, tail, p, grp, gate_t, gate_key_fn, hd1, hd2, td=1):
            its = [(n, d) for n in range(NT) for d in range(2)]
            nit = len(its)
            seconds = []
            for it in range(nit + 2 + hd2):
                if it < nit:
                    core(*its[it])
                if 0 <= it - td < nit:
                    n, d = its[it - td]
                    q = tail(n, d)
                    if q is not None:
                        seconds.append((it - 1, orders_g[d][n], q))
                for (jt, ti, q) in seconds:
                    if jt == it - 1 - hd1:
                        head1(ti, q, gate_t, gate_key_fn(ti))
                for (jt, ti, q) in seconds:
                    if jt == it - 1 - hd2:
                        head2(ti, q, p, grp)

        def stage(name):
            if stop_at == name:
                raise _Stop()

        def layer(l):
            P.dma("sp", lambda e, l=l: e.dma_start(out=pcol[:], in_=pcol_d[:, l * PCOLS:(l + 1) * PCOLS]), W=["pcol"])
            P.dma("sp", lambda e, l=l: e.dma_start(out=pbc[:], in_=pbc_d[l:l + 1, :].partition_broadcast(128)), W=["pbc"])
            P.pool(lambda e: e.memset(w2bd[:], 0.0), W=["w2bd"])
            P.dma("pool", lambda e, l=l: e.dma_start(out=w2bd[0:16, 0:256], in_=w2_d[l, 0]), W=["w2bd"])
            P.dma("pool", lambda e, l=l: e.dma_start(out=w2bd[16:32, 256:512], in_=w2_d[l, 1]), W=["w2bd"])
            P.dma("pool", lambda e, l=l: e.dma_start(out=w2bd[32:33, :], in_=gb_d[l:l + 1, :]), W=["w2bd"])

            stage("params")
            pre_norm(l, 0)
            stage("prenorm")

            for p in range(2):
                h0 = 2 * p
                if p == 0:
                    (wv,), wk = load_w([wcols(w_in_d, l, 1424, 128)])
                    psg, pgk = bank()
                    for i in range(NT):
                        for c in range(KC):
                            P.pe(lambda e, c=c, i=i: e.matmul(psg[:, i * 16:(i + 1) * 16], lhsT=hnT[:, c, til(i)], rhs=wv[:, c, 112:128],
                                                              start=(c == 0), stop=(c == KC - 1)),
                                 R=[wk, ("hnT", c, i // 4)], W=[pgk])
                    P.dve(lambda e: e.tensor_tensor(out=gl[:], in0=psg[:, 0:256].rearrange("p (i g) -> p i g", i=NT),
                                                    in1=pbc[:, 0:16].unsqueeze(1).to_broadcast([128, NT, 16]), op=ALU.add),
                          R=[pgk, "pbc"], W=["gl"])
                    stage("g1")
                    for d in range(2):
                        fsl = gl[:, :, 4 + 8 * d:8 + 8 * d]
                        lfd = lf[:, d, :].rearrange("p (i h) -> p i h", i=NT)
                        P.act(lambda e, fsl=fsl, lfd=lfd: e.activation(out=lfd, in_=fsl, func=AF.Exp, scale=-1.0), R=["gl"], W=[("lf", d)])
                        P.act(lambda e, lfd=lfd: e.activation(out=lfd, in_=lfd, func=AF.Ln, bias=1.0), R=[("lf", d)], W=[("lf", d)])
                    stage("g2")
                    psb, pbk = bank()
                    for d in range(2):
                        P.pe(lambda e, d=d: e.matmul(psb[:, d * 64:(d + 1) * 64], lhsT=trif[:, d, :], rhs=lf[:, d, :], start=True, stop=True),
                             R=[("lf", d)] + CONSTS, W=[pbk])
                        P.pe(lambda e, d=d: e.matmul(psb[:, 128 + d * 64:128 + (d + 1) * 64], lhsT=negones_f[:], rhs=lf[:, d, :], start=True, stop=True),
                             R=[("lf", d)] + CONSTS, W=[pbk])
                    stage("g3")
                    P.act(lambda e: e.copy(out=bsb[:, :], in_=psb[:, 0:256]), R=[pbk], W=[("acc", 1)])
                    flat = lambda t_: t_[:].rearrange("p d i h -> p (d i h)")
                    P.act(lambda e: e.activation(out=flat(eb), in_=bsb[:, 0:128], func=AF.Exp), R=[("acc", 1)], W=["eb"])
                    P.act(lambda e: e.activation(out=flat(enb), in_=bsb[:, 0:128], func=AF.Exp, scale=-1.0), R=[("acc", 1)], W=["enb"])
                    P.act(lambda e: e.activation(out=flat(dec), in_=bsb[:, 128:256], func=AF.Exp), R=[("acc", 1)], W=["dec"])
                    for d in range(2):
                        isl = gl[:, :, 8 * d:8 * d + 4]
                        gt = gtmp[:, d, :].rearrange("p (i h) -> p i h", i=NT)
                        P.dve(lambda e, d=d, isl=isl, gt=gt: e.tensor_tensor(out=gt, in0=isl, in1=bsb[:, d * 64:(d + 1) * 64].rearrange("p (i h) -> p i h", i=NT),
                                                                             op=ALU.subtract), R=["gl", ("acc", 1)], W=[("gtmp", d)])
                        P.act(lambda e, d=d, gt=gt: e.activation(out=wsc[:, d].rearrange("p i h -> p (i h)"), in_=gtmp[:, d, :], func=AF.Exp), R=[("gtmp", d)], W=[("wsc", d)])
                        P.dve(lambda e, d=d: e.tensor_tensor(out=wdec[:, d], in0=wsc[:, d], in1=dec[:, d], op=ALU.mult),
                              R=[("wsc", d), "dec"], W=[("wdec", d)])
                stage("gates")
                for d in range(2):
                    for hh in range(2):
                        P.pool(lambda e, d=d, hh=hh: e.tensor_copy(out=decsel[hh * 64:(hh + 1) * 64, d, :], in_=dec[hh * 64:(hh + 1) * 64, d, :, h0 + hh]),
                               R=["dec"], W=[("decsel", hh)])
                (wq, wkk), wk = load_w([wcols(w_in_d, l, p * 128, 128), wcols(w_in_d, l, 256 + p * 128, 128)])
                for which, wv, dst in ((0, wq, qT), (1, wkk, kT)):
                    cj = which * 2 + p
                    cb = 32 + cj * 4
                    def qk_keys(j):
                        return [("qkraw", j)] + [("s5", ii) for ii in range(4 * j, min(NT, 4 * j + 5))]
                    P.pool(lambda e: e.memset(qkraw[:, 0:1], 0.0), W=qk_keys(0))
                    P.pool(lambda e: e.memset(qkraw[:, 2049:2050], 0.0), W=qk_keys(3))

                    def ev(j, ps_, pk):
                        P.act(lambda e, j=j, ps_=ps_: e.copy(out=qkraw[:, 1 + j * 512:1 + (j + 1) * 512], in_=ps_[:, :]),
                              R=[pk], W=qk_keys(j))
                    proj_fm(wv, wk, 0, 128, ev)
                    for j in range(NB):
                        a_ = acc[:, j % 2, :]
                        rk = []
                        for jj in range(max(0, j - 1), min(NB, j + 2)):
                            rk += qk_keys(jj)
                        P.dve(lambda e, j=j, a_=a_, cb=cb: e.tensor_scalar(out=a_, in0=qkraw[:, 1 + j * 512:1 + (j + 1) * 512],
                                                                           scalar1=pcol[:, cb + 1:cb + 2], scalar2=pcol[:, cb + 3:cb + 4],
                                                                           op0=ALU.mult, op1=ALU.add),
                              R=rk + ["pcol"], W=[("acc", j % 2)])
                        P.dve(lambda e, j=j, a_=a_, cb=cb: e.scalar_tensor_tensor(out=a_, in0=qkraw[:, j * 512:(j + 1) * 512],
                                                                                  scalar=pcol[:, cb:cb + 1], in1=a_, op0=ALU.mult, op1=ALU.add),
                              R=rk + ["pcol", ("acc", j % 2)], W=[("acc", j % 2)])
                        P.dve(lambda e, j=j, a_=a_, cb=cb: e.scalar_tensor_tensor(out=a_, in0=qkraw[:, 2 + j * 512:2 + (j + 1) * 512],
                                                                                  scalar=pcol[:, cb + 2:cb + 3], in1=a_, op0=ALU.mult, op1=ALU.add),
                              R=rk + ["pcol", ("acc", j % 2)], W=[("acc", j % 2)])
                        P.act(lambda e, j=j, a_=a_, dst=dst: e.activation(out=dst[:, blk(j)], in_=a_, func=AF.Silu),
                              R=[("acc", j % 2)], W=[("q" if which == 0 else "k", j)])
                stage("Aqk")
                for hh in range(2):
                    P.pool(lambda e, hh=hh: e.memset(vaug[:, :, hh, 128:130], 1.0), W=[("s3", i) for i in range(NT)])
                (wv,), wk = load_w([wcols(w_in_d, l, 512 + p * 256, 256)])

                def evv(i0, ps_, pk):
                    for ii in range(2):
                        for hh in range(2):
                            P.act(lambda e, i0=i0, ii=ii, hh=hh, ps_=ps_: e.copy(out=vaug[:, i0 + ii, hh, 0:128],
                                                                               in_=ps_[:, ii * 256 + hh * 128:ii * 256 + (hh + 1) * 128]),
                                  R=[pk], W=[("s3", i0 + ii)])
                proj_tm(wv, wk, 0, 256, 2, evv)
                (wv,), wk = load_w([wcols(w_in_d, l, 1024 + p * 256, 256)])

                def evo(i0, ps_, pk):
                    P.act(lambda e, i0=i0, ps_=ps_: e.activation(out=sigo[:, i0:i0 + 2, :].rearrange("p i e -> p (i e)"), in_=ps_[:, :],
                                                                func=AF.Sigmoid), R=[pk], W=[("s4", i0), ("s4", i0 + 1)])
                    P.pool(lambda e, i0=i0: e.tensor_tensor(out=sigo[:, i0:i0 + 2, :], in0=sigo[:, i0:i0 + 2, :],
                                                           in1=pbc[:, 16 + h0 * 128:16 + h0 * 128 + 256].unsqueeze(1).to_broadcast([128, 2, 256]),
                                                           op=ALU.mult), R=[("s4", i0), ("s4", i0 + 1), "pbc"], W=[("s4", i0), ("s4", i0 + 1)])
                proj_tm(wv, wk, 0, 256, 2, evo)

                for i in range(NT):
                    pt_, ptk = tbank()
                    P.pe(lambda e, i=i, pt_=pt_: e.transpose(pt_[:, 0:128], kT[:, til(i)], ident[:]), R=[("k", i // 4), "ident"], W=[ptk])
                    for d in range(2):
                        for hh in range(2):
                            P.dve(lambda e, i=i, d=d, hh=hh, pt_=pt_: e.tensor_scalar(
                                out=kw[:, d, i, hh, :], in0=pt_[:, hh * 64:(hh + 1) * 64], scalar1=wdec[:, d, i, h0 + hh:h0 + hh + 1], scalar2=0.125,
                                op0=ALU.mult, op1=ALU.mult), R=[ptk, ("wdec", d)], W=[("s2", i)])
                stage("Aproj")
                orders = [list(range(NT)), list(range(NT - 1, -1, -1))]
                for d in range(2):
                    P.pool(lambda e, d=d: e.memset(C32[:, d, :], 0.0), W=[("C32", d)])
                    P.pool(lambda e, d=d: e.memset(Cbf[:, d, 0, :], 0.0), W=[("Cbf", d, 0)])

                def a_early(d, n):
                    i = orders[d][n]
                    par = n % 2
                    psSs = [bank(), bank()]
                    for hh in range(2):
                        rs = slice(hh * 64, (hh + 1) * 64)
                        psS, psk = psSs[hh]
                        P.pe(lambda e: e.matmul(psS[:, 0:128], lhsT=kT[rs, til(i)], rhs=qT[rs, til(i)], start=True, stop=True),
                             R=[("k", i // 4), ("q", i // 4)], W=[psk])
                    for hh in range(2):
                        psS, psk = psSs[hh]
                        P.dve(lambda e: e.scalar_tensor_tensor(
                            out=Sm[:, d, par, hh, :], in0=psS[:, 0:128], scalar=wsc[:, d, i, h0 + hh:h0 + hh + 1],
                            in1=maskA[:, d, :], op0=ALU.mult, op1=ALU.mult),
                            R=[psk, ("wsc", d)] + CONSTS, W=[("Sm", d, par, hh)])

                def a_main(d, n):
                    i = orders[d][n]
                    par = n % 2
                    psU, puk = bank()
                    P.pe(lambda e: e.matmul(psU[0:64, 0:130], lhsT=kw[:, d, i, 0, :], rhs=vaug[:, i, 0, 0:130], start=True, stop=True),
                         R=[("s2", i), ("s3", i)], W=[puk])
                    P.pe(lambda e: e.matmul(psU[64:128, 0:130], lhsT=kw[:, d, i, 1, :], rhs=vaug[:, i, 1, 0:130], start=True, stop=True,
                                            tile_position=(0, 64)),
                         R=[("s2", i), ("s3", i)], W=[puk])
                    psO, pok = bank()
                    for hh in range(2):
                        rs = slice(hh * 64, (hh + 1) * 64)
                        P.pe(lambda e: e.matmul(psO[:, hh * 130:hh * 130 + 130], lhsT=Sm[:, d, par, hh, :],
                                                rhs=vaug[:, i, hh, 0:130], start=True, stop=False),
                             R=[("Sm", d, par, hh), ("s3", i)], W=[pok])
                        P.pe(lambda e: e.matmul(psO[:, hh * 130:hh * 130 + 130], lhsT=qT[rs, til(i)],
                                                rhs=Cbf[rs, d, par, 0:130], start=False, stop=True),
                             R=[("q", i // 4), ("Cbf", d, par)], W=[pok])
                    P.dve(lambda e: e.scalar_tensor_tensor(
                        out=C32[:, d, 0:130], in0=C32[:, d, 0:130], scalar=decsel[:, d, i:i + 1], in1=psU[:, 0:130], op0=ALU.mult, op1=ALU.add),
                        R=[("C32", d), ("decsel", 0), ("decsel", 1), puk], W=[("C32", d)])
                    P.pool(lambda e: e.tensor_copy(out=Cbf[:, d, 1 - par, 0:130], in_=C32[:, d, 0:130]),
                           R=[("C32", d)], W=[("Cbf", d, 1 - par)])
                    sm = sml[:, 1 + d, :]
                    P.act(lambda e: e.activation(out=sm[:, 0:2], in_=psO[:, 0:260].rearrange("p (h e) -> p h e", h=2)[:, :, 128], func=AF.Abs),
                          R=[pok], W=[("sml1a", d)])
                    aps[(n, d)] = (psO, pok)

                aps = {}

                def a_tail(n, d):
                    i = orders[d][n]
                    psO, pok = aps.pop((n, d))
                    sm = sml[:, 1 + d, :]
                    q = None
                    if n >= NT // 2:
                        q = acnt[0] % 2
                        acnt[0] += 1
                    P.dve(lambda e: e.tensor_tensor(out=sm[:, 2:4], in0=sm[:, 0:2], in1=enb[:, d, i, h0:h0 + 2], op=ALU.max),
                          R=[("sml1a", d), "enb"], W=[("sml1b", d)])
                    P.dve(lambda e: e.reciprocal(out=sm[:, 4:6], in_=sm[:, 2:4]), R=[("sml1b", d)], W=[("sml1c", d)])
                    for hh in range(2):
                        if n < NT // 2:
                            P.act(lambda e: e.activation(
                                out=hsum[:, i, hh * 128:(hh + 1) * 128], in_=psO[:, hh * 130:hh * 130 + 128], func=AF.Copy, scale=sm[:, 4 + hh:5 + hh]),
                                R=[pok, ("sml1c", d)], W=[("s5", i)])
                        else:
                            P.dve(lambda e: e.scalar_tensor_tensor(
                                out=tot2[:, q, hh * 128:(hh + 1) * 128], in0=psO[:, hh * 130:hh * 130 + 128], scalar=sm[:, 4 + hh:5 + hh],
                                in1=hsum[:, i, hh * 128:(hh + 1) * 128], op0=ALU.mult, op1=ALU.add),
                                R=[pok, ("sml1c", d), ("s5", i)], W=[("acc", q)])
                    return q

                acnt = [0]

                def a_core(n, d):
                    if n == 0:
                        a_early(d, 0)
                    if n + 1 < NT:
                        a_early(d, n + 1)
                    a_main(d, n)

                run_pipeline(a_core, a_tail, p, 0, sigo, lambda ti: ("s4", ti), hd1=0, hd2=1, td=0)

            stage("A")
            for p in range(2):
                h0 = 2 * p
                if p == 0:
                    (wv,), wk = load_w([wcols(w_in_d, l, 2992, 128)])

                    def evl(j, ps_, pk):
                        P.act(lambda e, j=j, ps_=ps_: e.copy(out=lrT[0:32, blk(j)], in_=ps_[0:32, :]), R=[pk], W=[("lrT", j)])
                    proj_fm(wv, wk, 96, 32, evl)
                (wq, wkk), wk = load_w([wcols(w_in_d, l, 1552 + p * 128, 128), wcols(w_in_d, l, 1808 + p * 128, 128)])
                for which, wv, dst in ((0, wq, qT), (1, wkk, kT)):
                    def ev(j, ps_, pk, dst=dst, which=which):
                        P.act(lambda e, j=j, ps_=ps_: e.copy(out=dst[:, blk(j)], in_=ps_[:, :]), R=[pk], W=[("q" if which == 0 else "k", j)])
                    proj_fm(wv, wk, 0, 128, ev)
                (wv,), wk = load_w([wcols(w_in_d, l, 2064 + p * 256, 256)])

                def evv(i0, ps_, pk):
                    P.act(lambda e, i0=i0, ps_=ps_: e.copy(out=bv[:, i0:i0 + 2, :].rearrange("p i e -> p (i e)"), in_=ps_[:, :]),
                          R=[pk], W=[("s2", i0), ("s2", i0 + 1)])
                proj_tm(wv, wk, 0, 256, 2, evv)
                (wv,), wk = load_w([wcols(w_in_d, l, 2576 + p * 256, 256)])

                def evr(i0, ps_, pk):
                    P.act(lambda e, i0=i0, ps_=ps_: e.activation(out=sr[:, i0:i0 + 2, :].rearrange("p i e -> p (i e)"), in_=ps_[:, :],
                                                                func=AF.Silu), R=[pk], W=[("s3", i0), ("s3", i0 + 1)])
                    P.pool(lambda e, i0=i0: e.tensor_tensor(out=sr[:, i0:i0 + 2, :], in0=sr[:, i0:i0 + 2, :],
                                                           in1=pbc[:, 528 + h0 * 128:528 + h0 * 128 + 256].unsqueeze(1).to_broadcast([128, 2, 256]),
                                                           op=ALU.mult), R=[("s3", i0), ("s3", i0 + 1), "pbc"], W=[("s3", i0), ("s3", i0 + 1)])
                proj_tm(wv, wk, 0, 256, 2, evr)
                for i0 in range(0, NT, 2):
                    ps_, pk = bank()
                    for ii in range(2):
                        i = i0 + ii
                        for d in range(2):
                            P.pe(lambda e, i=i, ii=ii, d=d, ps_=ps_: e.matmul(ps_[:, ii * 256 + d * 128:ii * 256 + (d + 1) * 128], lhsT=lrT[0:33, til(i)],
                                                                              rhs=w2bd[0:33, d * 256 + p * 128:d * 256 + (p + 1) * 128], start=True, stop=True),
                                 R=[("lrT", i // 4), "lrT1", "w2bd"], W=[pk])
                    g_ = acc[:, (i0 // 2) % 2, :]
                    P.act(lambda e, ps_=ps_, g_=g_: e.activation(out=g_, in_=ps_[:, :], func=AF.Exp, scale=-1.0), R=[pk], W=[("acc", (i0 // 2) % 2)])
                    P.act(lambda e, i0=i0, g_=g_: e.activation(out=la[:, i0:i0 + 2, :].rearrange("p i e -> p (i e)"), in_=g_, func=AF.Ln, bias=1.0),
                          R=[("acc", (i0 // 2) % 2)], W=[("s4", i0), ("s4", i0 + 1)])
                stage("Bproj")
                orders = [list(range(NT)), list(range(NT - 1, -1, -1))]
                for d in range(2):
                    P.pool(lambda e, d=d: e.memset(C32[:, d, :], 0.0), W=[("C32", d)])
                    P.pool(lambda e, d=d: e.memset(Cbf[:, d, 0, :], 0.0), W=[("Cbf", d, 0)])

                def b_early(d, n):
                    i = orders[d][n]
                    par = n % 2
                    psB, pbk2 = bank()
                    P.pe(lambda e: e.matmul(psB[:, 0:128], lhsT=la[:, i, d * 128:(d + 1) * 128], rhs=tri16[:, d, :], start=True, stop=True),
                         R=[("s4", i)] + CONSTS, W=[pbk2])
                    P.act(lambda e: e.activation(out=eBt[:, d, par, :], in_=psB[:, 0:128], func=AF.Exp), R=[pbk2], W=[("eBt", d, par)])
                    P.act(lambda e: e.activation(out=eNBt[:, d, :], in_=psB[:, 0:128], func=AF.Exp, scale=-1.0), R=[pbk2], W=[("eNBt", d)])
                    P.pool(lambda e: e.tensor_tensor(out=qs[:, d, par, :], in0=qT[:, til(i)], in1=eBt[:, d, par, :], op=ALU.mult),
                           R=[("q", i // 4), ("eBt", d, par)], W=[("qs", d, par)])
                    P.dve(lambda e: e.tensor_tensor(out=ks[:, d, par, :], in0=kT[:, til(i)], in1=eNBt[:, d, :], op=ALU.mult),
                          R=[("k", i // 4), ("eNBt", d)], W=[("ks", d, par)])
                    psSs = [bank(), bank()]
                    for hh in range(2):
                        rs = slice(hh * 64, (hh + 1) * 64)
                        psS, psk = psSs[hh]
                        P.pe(lambda e: e.matmul(psS[:, 0:128], lhsT=ks[rs, d, par, :], rhs=qs[rs, d, par, :], start=True, stop=True),
                             R=[("ks", d, par), ("qs", d, par)], W=[psk])
                    pt_, ptk = tbank()
                    P.pe(lambda e: e.transpose(pt_[:, 0:128], ks[:, d, par, :], ident[:]), R=[("ks", d, par), "ident"], W=[ptk])
                    for hh in range(2):
                        psS, psk = psSs[hh]
                        P.dve(lambda e: e.tensor_tensor(out=Sm[:, d, par, hh, :], in0=psS[:, 0:128], in1=maskB[:, d, :], op=ALU.mult),
                              R=[psk] + CONSTS, W=[("Sm", d, par, hh)])
                    P.act(lambda e: e.activation(out=ktok[:, d, par, :], in_=pt_[:, 0:128], func=AF.Copy, scale=0.125), R=[ptk], W=[("ktok", d, par)])

                def b_main(d, n):
                    i = orders[d][n]
                    par = n % 2
                    lastcol = 127 if d == 0 else 0
                    psU, puk = bank()
                    P.pe(lambda e: e.matmul(psU[0:64, 0:128], lhsT=ktok[:, d, par, 0:64], rhs=bv[:, i, 0:128], start=True, stop=True),
                         R=[("ktok", d, par), ("s2", i)], W=[puk])
                    P.pe(lambda e: e.matmul(psU[64:128, 0:128], lhsT=ktok[:, d, par, 64:128], rhs=bv[:, i, 128:256], start=True, stop=True,
                                            tile_position=(0, 64)), R=[("ktok", d, par), ("s2", i)], W=[puk])
                    psO, pok = bank()
                    for hh in range(2):
                        rs = slice(hh * 64, (hh + 1) * 64)
                        P.pe(lambda e: e.matmul(psO[:, hh * 128:(hh + 1) * 128], lhsT=Sm[:, d, par, hh, :],
                                                rhs=bv[:, i, hh * 128:(hh + 1) * 128], start=True, stop=False),
                             R=[("Sm", d, par, hh), ("s2", i)], W=[pok])
                        P.pe(lambda e: e.matmul(psO[:, hh * 128:(hh + 1) * 128], lhsT=qs[rs, d, par, :],
                                                rhs=Cbf[rs, d, par, 0:128], start=False, stop=True),
                             R=[("qs", d, par), ("Cbf", d, par)], W=[pok])
                    P.act(lambda e: e.activation(out=tmpS[:, d, :], in_=psU[:, 0:128], func=AF.Copy, scale=eBt[:, d, par, lastcol:lastcol + 1]),
                          R=[puk, ("eBt", d, par)], W=[("tmpS", d)])
                    P.dve(lambda e: e.scalar_tensor_tensor(out=C32[:, d, 0:128], in0=C32[:, d, 0:128], scalar=eBt[:, d, par, lastcol:lastcol + 1],
                                                           in1=tmpS[:, d, :], op0=ALU.mult, op1=ALU.add),
                          R=[("tmpS", d), ("eBt", d, par), ("C32", d)], W=[("C32", d)])
                    P.pool(lambda e: e.tensor_copy(out=Cbf[:, d, 1 - par, 0:128], in_=C32[:, d, 0:128]),
                           R=[("C32", d)], W=[("Cbf", d, 1 - par)])
                    if n < NT // 2:
                        P.act(lambda e: e.copy(out=hsum[:, i, :], in_=psO[:, 0:256]), R=[pok], W=[("s5", i)])
                    else:
                        q = bcnt[0] % 2
                        bcnt[0] += 1
                        bq[(n, d)] = q
                        P.dve(lambda e: e.tensor_tensor(out=tot2[:, q, :], in0=psO[:, 0:256], in1=hsum[:, i, :], op=ALU.add),
                              R=[pok, ("s5", i)], W=[("acc", q)])

                bcnt = [0]
                bq = {}

                def b_core(n, d):
                    if n == 0:
                        b_early(d, 0)
                    if n + 1 < NT:
                        b_early(d, n + 1)
                    b_main(d, n)

                run_pipeline(b_core, lambda n, d: bq.get((n, d)), p, 1, sr, lambda ti: ("s3", ti), hd1=0, hd2=1, td=0)

            stage("B")
            if dbg and "yT" in dbg and l == 0:
                P.barrier()
                for c in range(KC):
                    P.act(lambda e, c=c: e.copy(out=xT[:, c, :], in_=yT[:, c, :]), R=[], W=[("xT", c, j) for j in range(NB)])
                P.barrier()

            P.barrier()
            for q4 in range(4):
                P.dma("pool", lambda e, q4=q4, l=l: e.dma_start(out=wo[:, :, q4 * 256:(q4 + 1) * 256],
                                                                in_=w_out_d[l, :, q4 * 256:(q4 + 1) * 256].rearrange("(c p) n -> p c n", p=128)),
                      W=["wo"])
            for j in range(NB):
                for co in range(KC):
                    ps_, pk = bank()
                    for ch in range(KC):
                        P.pe(lambda e, co=co, ch=ch, j=j, ps_=ps_: e.matmul(ps_[:, :], lhsT=wo[:, ch, co * 128:(co + 1) * 128], rhs=yview(ch)[:, blk(j)],
                                                                            start=(ch == 0), stop=(ch == KC - 1)),
                             R=["wo"] + [ykey(ch, i) for i in range(4 * j, 4 * j + 4)], W=[pk])
                    P.act(lambda e, co=co, ps_=ps_: e.copy(out=mixT[:, co, :], in_=ps_[:, :]), R=[pk], W=[("mixT", co)])
                post_norm_residual(l, 8, mixT, lambda c: ("mixT", c), j * 512, j)
                if j >= 1:
                    pre_norm_block(l, 16, j - 1)
            pre_norm_block(l, 16, NB - 1)

            stage("wout")
            P.pool(lambda e: e.tensor_copy(out=hhalo[:, :, :], in_=hnT[:, :, 1023:1025]),
                   R=[("hnT", c, j) for c in range(KC) for j in (1, 2)], W=["hhalo"])
            P.barrier()
            ffT_bf = arena[:, NF * 1024:NF * 1024 + 2 * KC * 512]
            gu_pool = [(wbuf[0][:, :], [("wbuf", 0)]), (wbuf[1][:, :], [("wbuf", 1)])] + \
                      [(ffT_bf[:, j * 2048:(j + 1) * 2048], [("ffT", 2 * j), ("ffT", 2 * j + 1)]) for j in range(4)]
            gurot = [0]
            for H in range(2):
                t0 = H * 1024
                graws = [graw, pbc[:, 0:1026]]
                wst = {}

                def G(f):
                    gr = graws[f % 2]
                    (wgv, wuv), wks = load_into(gu_pool[gurot[0] % len(gu_pool)], [wcols(wg_d, l, f * 128, 128), wcols(wu_d, l, f * 128, 128)])
                    gurot[0] += 1
                    wst[f] = (wuv, wks)
                    if H == 0:
                        P.pool(lambda e: e.memset(gr[:, 0:1], 0.0), W=[("graw", f % 2, 0)])
                        halo_col, hsel = 1025, 1
                    else:
                        P.pool(lambda e: e.memset(gr[:, 1025:1026], 0.0), W=[("graw", f % 2, 1)])
                        halo_col, hsel = 0, 0
                    psh, phk = bank()
                    for c in range(KC):
                        P.pe(lambda e: e.matmul(psh[:, 0:1], lhsT=wgv[:, c, :], rhs=hhalo[:, c, hsel:hsel + 1],
                                                start=(c == 0), stop=(c == KC - 1)), R=wks + ["hhalo"], W=[phk])
                    P.act(lambda e: e.copy(out=gr[:, halo_col:halo_col + 1], in_=psh[:, 0:1]),
                          R=[phk], W=[("graw", f % 2, 0 if halo_col == 0 else 1)])
                    for b in range(2):
                        jj = 2 * H + b
                        ps_, pk = bank()
                        for c in range(KC):
                            P.pe(lambda e: e.matmul(ps_[:, :], lhsT=wgv[:, c, :], rhs=hnT[:, c, blk(jj)],
                                                    start=(c == 0), stop=(c == KC - 1)), R=wks + [("hnT", c, jj)], W=[pk])
                        P.act(lambda e: e.copy(out=gr[:, 1 + b * 512:1 + (b + 1) * 512], in_=ps_[:, :]), R=[pk], W=[("grawb", f % 2, b)])

                def C(f):
                    gr = graws[f % 2]
                    cb = 48 + f * 4
                    rk = [("graw", f % 2, 0), ("graw", f % 2, 1), ("grawb", f % 2, 0), ("grawb", f % 2, 1)]
                    for b in range(2):
                        a_ = acc[:, b, :]
                        P.dve(lambda e: e.tensor_scalar(out=a_, in0=gr[:, 1 + b * 512:1 + (b + 1) * 512],
                                                        scalar1=pcol[:, cb + 1:cb + 2], scalar2=pcol[:, cb + 3:cb + 4],
                                                        op0=ALU.mult, op1=ALU.add), R=rk + ["pcol"], W=[("acc", b)])
                        P.dve(lambda e: e.scalar_tensor_tensor(out=a_, in0=gr[:, b * 512:(b + 1) * 512],
                                                               scalar=pcol[:, cb:cb + 1], in1=a_, op0=ALU.mult, op1=ALU.add),
                              R=rk + ["pcol", ("acc", b)], W=[("acc", b)])
                        P.dve(lambda e: e.scalar_tensor_tensor(out=a_, in0=gr[:, 2 + b * 512:2 + (b + 1) * 512],
                                                               scalar=pcol[:, cb + 2:cb + 3], in1=a_, op0=ALU.mult, op1=ALU.add),
                              R=rk + ["pcol", ("acc", b)], W=[("acc", b)])
                        P.act(lambda e: e.activation(out=gel[:, b, :], in_=a_, func=AF.Gelu_apprx_tanh), R=[("acc", b)], W=[("gel", b)])

                def U(f):
                    wuv, wks = wst.pop(f)
                    for b in range(2):
                        jj = 2 * H + b
                        ps_, pk = bank()
                        for c in range(KC):
                            P.pe(lambda e: e.matmul(ps_[:, :], lhsT=wuv[:, c, :], rhs=hnT[:, c, blk(jj)],
                                                    start=(c == 0), stop=(c == KC - 1)), R=wks + [("hnT", c, jj)], W=[pk])
                        P.dve(lambda e: e.tensor_tensor(out=actT[:, f, b * 512:(b + 1) * 512], in0=gel[:, b, :], in1=ps_[:, :], op=ALU.mult),
                              R=[("gel", b), pk], W=[("actT", f, b)])

                G(0)
                for f in range(NF):
                    if f + 1 < NF:
                        G(f + 1)
                    C(f)
                    U(f)

                d_pool = [(hnT[:, c, t0:t0 + 1024], [("hnT", c, 2 * H), ("hnT", c, 2 * H + 1)]) for c in range(KC)]
                drot = 0
                for b in range(2):
                    for co in range(KC):
                        parts = []
                        for (fa, fb) in ((0, 8), (8, 16), (16, 22)):
                            (wv_,), wk_ = load_into(d_pool[drot % KC], [wd_d[l, fa * 128:fb * 128, co * 128:(co + 1) * 128].rearrange("(f p) n -> p f n", p=128)])
                            drot += 1
                            parts.append((fa, fb, wv_, wk_))
                        ps_, pk = bank()
                        for f in range(NF):
                            fa, fb, wdv, wk_ = parts[f // 8]
                            P.pe(lambda e: e.matmul(ps_[:, :], lhsT=wdv[:, f - fa, :], rhs=actT[:, f, b * 512:(b + 1) * 512],
                                                    start=(f == 0), stop=(f == NF - 1)), R=wk_ + [("actT", f, b)], W=[pk])
                        P.act(lambda e, co=co, ps_=ps_: e.copy(out=ffT[:, co, :], in_=ps_[:, :]), R=[pk], W=[("ffT", co)])
                    post_norm_residual(l, 24, ffT, lambda c: ("ffT", c), t0 + b * 512, 2 * H + b)
            P.barrier()

        try:
            stage("load")
            for l in range(nl):
                layer(l)
        except _Stop:
            pass
        for c in range(KC):
            P.dma("sp", lambda e, c=c: e.dma_start(out=outT_d[c * 128:(c + 1) * 128, :], in_=xT[:, c, :]),
                  R=[("xT", c, j) for j in range(NB)], W=[("out", c)])
        P.add("sp", None, R=[("out", c) for c in range(KC)])
        P.emit(nc)
    return nc


def _pack_params(ls, norm_mix_pre, norm_mix_post, norm_ffn_pre, norm_ffn_post, mlstm_conv_w, mlstm_conv_b,
                 ffn_conv_w, ffn_conv_b, mlstm_gate_b, mlstm_norm, gla_norm):
    nl = len(ls)
    pcol = np.zeros((128, nl * PCOLS), np.float32)
    pbc = np.zeros((nl, PBC), np.float32)
    for li, l in enumerate(ls):
        o = li * PCOLS
        for gi, g in enumerate((norm_mix_pre, norm_mix_post, norm_ffn_pre, norm_ffn_post)):
            pcol[:, o + gi * 8:o + gi * 8 + 8] = g[l].reshape(8, 128).T
        for cj in range(4):
            pcol[:, o + 32 + cj * 4:o + 32 + cj * 4 + 3] = mlstm_conv_w[l][:, cj * 128:(cj + 1) * 128].T
            pcol[:, o + 32 + cj * 4 + 3] = mlstm_conv_b[l][cj * 128:(cj + 1) * 128]
        for f in range(NF):
            pcol[:, o + 48 + f * 4:o + 48 + f * 4 + 3] = ffn_conv_w[l][:, f * 128:(f + 1) * 128].T
            pcol[:, o + 48 + f * 4 + 3] = ffn_conv_b[l][f * 128:(f + 1) * 128]
        pbc[li, 0:16] = mlstm_gate_b[l]
        pbc[li, 16:528] = mlstm_norm[l].reshape(-1)
        pbc[li, 528:1040] = gla_norm[l].reshape(-1)
    return pcol, pbc


_CACHE = {}


def _get_prog(nl):
    if nl not in _CACHE:
        _CACHE[nl] = build_program(nl)
    return _CACHE[nl]


FUSED = True


def kernel(x, norm_mix_pre, norm_mix_post, norm_ffn_pre, norm_ffn_post, w_in, mlstm_gate_b, mlstm_conv_w,
           mlstm_conv_b, mlstm_norm, gla_w2, gla_b, gla_norm, w_out, ffn_w_gate, ffn_w_up, ffn_conv_w,
           ffn_conv_b, ffn_w_down):
    f = lambda a: np.ascontiguousarray(np.asarray(a), dtype=np.float32)
    x = f(x)
    args = [f(a) for a in (norm_mix_pre, norm_mix_post, norm_ffn_pre, norm_ffn_post, mlstm_conv_w, mlstm_conv_b,
                           ffn_conv_w, ffn_conv_b, mlstm_gate_b, mlstm_norm, gla_norm)]
    w_in, w_out, wg, wu, wd = f(w_in), f(w_out), f(ffn_w_gate), f(ffn_w_up), f(ffn_w_down)
    w2, gb = f(gla_w2), f(gla_b)
    xTs = [np.ascontiguousarray(x[b].T) for b in range(NCORES)]
    groups = [list(range(DEPTH))] if FUSED else [[l] for l in range(DEPTH)]
    for ls in groups:
        nl = len(ls)
        nc = _get_prog(nl)
        pcol, pbc = _pack_params(ls, *args)
        sl = slice(ls[0], ls[-1] + 1)
        shared = {"w_in": w_in[sl], "w_out": w_out[sl], "wg": wg[sl], "wu": wu[sl], "wd": wd[sl], "pcol": pcol, "pbc": pbc,
                  "w2": w2[sl], "gb": np.ascontiguousarray(gb[sl].reshape(nl, 512))}
        in_maps = [dict(shared, xT=xTs[b]) for b in range(NCORES)]
        res = run_bass_kernel_spmd(nc, in_maps, core_ids=list(range(NCORES)))
        xTs = [np.asarray(r["outT"]) for r in res.results]
    return np.stack([np.ascontiguousarray(t.T) for t in xTs], axis=0).astype(np.float32)
```

```python
import contextlib
import types
import numpy as np
import concourse.bass as bass
import concourse.mybir as mybir
from concourse.bass_utils import run_bass_kernel_spmd

F32 = mybir.dt.float32
BF16 = mybir.dt.bfloat16
ALU = mybir.AluOpType
AF = mybir.ActivationFunctionType

EPOCH = 30000
HD1, HD2 = 1, 2
NCORES = 8
DEPTH = 4
D = 1024
S = 2048
NT = 16
NB = 4
KC = 8
FF = 2816
NF = 22
INC = 3120
EPS = 1e-6
PCOLS = 136
PBC = 1040


def _freeze(fn):
    if fn is None or fn.__closure__ is None:
        return fn
    cells = []
    for c in fn.__closure__:
        try:
            cells.append(types.CellType(c.cell_contents))
        except ValueError:
            cells.append(c)
    return types.FunctionType(fn.__code__, fn.__globals__, fn.__name__, fn.__defaults__, tuple(cells))


class _Op:
    __slots__ = ("eng", "fn", "R", "W", "dma", "deps", "marked", "tick", "dsem", "dtick", "dprev")

    def __init__(self, eng, fn, R, W, dma):
        self.eng = eng
        self.fn = fn
        self.R = tuple(R)
        self.W = tuple(W)
        self.dma = dma
        self.deps = ()
        self.marked = False
        self.tick = 0
        self.dsem = -1
        self.dtick = 0
        self.dprev = None


class Prog:
    ENGS = ("pe", "act", "dve", "pool", "sp")

    def __init__(self, n_dma_sems=16):
        self.ops = []
        self.nds = n_dma_sems

    def add(self, eng, fn, R=(), W=(), dma=False):
        self.ops.append(_Op(eng, _freeze(fn), R, W, dma))

    def pe(self, fn, R=(), W=()):
        self.add("pe", fn, R, W)

    def act(self, fn, R=(), W=()):
        self.add("act", fn, R, W)

    def dve(self, fn, R=(), W=()):
        self.add("dve", fn, R, W)

    def pool(self, fn, R=(), W=()):
        self.add("pool", fn, R, W)

    def dma(self, eng, fn, R=(), W=()):
        self.add(eng, fn, R, W, dma=True)

    def barrier(self):
        self.ops.append(_Op("__barrier__", None, (), (), False))

    def analyze(self):
        last_w = {}
        readers = {}
        last_on_eng = {e: None for e in self.ENGS}
        pend = {e: None for e in self.ENGS}
        out = []
        for op in self.ops:
            if op.eng == "__barrier__":
                snap = [v for v in last_on_eng.values() if v is not None]
                for e in self.ENGS:
                    pend[e] = snap
                continue
            g = len(out)
            deps = set()
            for k in op.R:
                if k in last_w:
                    deps.add(last_w[k])
            for k in op.W:
                if k in last_w:
                    deps.add(last_w[k])
                for r in readers.get(k, ()):
                    deps.add(r)
            if pend[op.eng] is not None:
                deps.update(pend[op.eng])
                pend[op.eng] = None
            for k in op.R:
                readers.setdefault(k, []).append(g)
            for k in op.W:
                last_w[k] = g
                readers[k] = []
            deps.discard(g)
            op.deps = tuple(sorted(deps))
            out.append(op)
            if op.fn is not None:
                last_on_eng[op.eng] = g
        self.lin = out
        ndma = 0
        nq = [0, 0]
        last_dma_on_sem = {}
        for g, op in enumerate(out):
            for d in op.deps:
                p = out[d]
                if p.dma:
                    continue
                if p.eng == "pe" and op.eng == "pe" and not op.dma:
                    continue
                p.marked = True
            if op.dma:
                half = self.nds // 2
                qi = 0 if op.eng == "sp" else 1
                s = qi * half + (nq[qi] % half)
                nq[qi] += 1
                ndma += 1
                op.dsem = s
                prev = last_dma_on_sem.get(s)
                op.dprev = prev
                op.dtick = (out[prev].dtick if prev is not None else 0) + 16
                last_dma_on_sem[s] = g
        cnt = {e: 0 for e in self.ENGS}
        for op in out:
            if op.marked and not op.dma:
                cnt[op.eng] += 1
                op.tick = cnt[op.eng]
        self.cnt = cnt
        self.ndma = ndma

    def emit(self, nc):
        self.analyze()
        out = self.lin
        with contextlib.ExitStack() as st:
            esems = {}
            for e in self.ENGS:
                nep = self.cnt[e] // EPOCH + 1
                esems[e] = [st.enter_context(nc.semaphore(f"s_{e}_{i}")) for i in range(nep)]
            dsems = [st.enter_context(nc.semaphore(f"s_dma_{i}")) for i in range(self.nds)]
            block = st.enter_context(nc.Block())
            by_eng = {e: [] for e in self.ENGS}
            for g, op in enumerate(out):
                by_eng[op.eng].append((g, op))

            def sem_of(p):
                if p.dma:
                    return ("d", p.dsem), dsems[p.dsem], p.dtick
                ep = (p.tick - 1) // EPOCH
                return (p.eng, ep), esems[p.eng][ep], p.tick - ep * EPOCH

            def run(eng_name, e):
                waited = {}
                for g, op in by_eng[eng_name]:
                    waits = {}
                    deps = list(op.deps)
                    if op.dma and op.dprev is not None:
                        deps.append(op.dprev)
                    for d in deps:
                        p = out[d]
                        if (not p.dma) and p.eng == "pe" and eng_name == "pe" and not op.dma:
                            continue
                        key, sem, val = sem_of(p)
                        if waited.get(key, 0) >= val:
                            continue
                        if key not in waits or waits[key][1] < val:
                            waits[key] = (sem, val)
                    for key, (sem, val) in waits.items():
                        e.wait_ge(sem, val)
                        waited[key] = val
                    if op.fn is None:
                        continue
                    ins = op.fn(e)
                    if op.dma:
                        ins.then_inc(dsems[op.dsem], 16)
                    elif op.marked:
                        ep = (op.tick - 1) // EPOCH
                        ins.then_inc(esems[eng_name][ep], 1)

            @block.tensor
            def _(e):
                run("pe", e)

            @block.scalar
            def _(e):
                run("act", e)

            @block.vector
            def _(e):
                run("dve", e)

            @block.gpsimd
            def _(e):
                run("pool", e)

            @block.sync
            def _(e):
                run("sp", e)


class _Stop(Exception):
    pass


def build_program(nl, dbg=None, stop_at=None):
    nc = bass.Bass("TRN2", target_bir_lowering=False)
    P = Prog()

    def din(name, shape):
        return nc.dram_tensor(name, shape, F32, kind="ExternalInput").ap()

    xT_d = din("xT", [D, S])
    w_in_d = din("w_in", [nl, D, INC])
    w_out_d = din("w_out", [nl, D, D])
    wg_d = din("wg", [nl, D, FF])
    wu_d = din("wu", [nl, D, FF])
    wd_d = din("wd", [nl, FF, D])
    pcol_d = din("pcol", [128, nl * PCOLS])
    pbc_d = din("pbc", [nl, PBC])
    w2_d = din("w2", [nl, 2, 16, 256])
    gb_d = din("gb", [nl, 512])
    outT_d = nc.dram_tensor("outT", [D, S], F32, kind="ExternalOutput").ap()
    dbg_d = {}
    if dbg:
        for k, shp in dbg.items():
            dbg_d[k] = nc.dram_tensor("dbg_" + k, shp, F32, kind="ExternalOutput").ap()

    st = contextlib.ExitStack()
    with st:
        def sb(name, shape, dt):
            return st.enter_context(nc.sbuf_tensor(name, shape, dt))

        def psum(name, shape, dt):
            return st.enter_context(nc.psum_tensor(name, shape, dt))

        xT = sb("xT_sb", [128, KC, S], F32)
        hnT = sb("hnT", [128, KC, S], BF16)
        pcol = sb("pcol_sb", [128, PCOLS], F32)
        pbc = sb("pbc_sb", [128, PBC], F32)
        w2bd = sb("w2bd", [128, 512], BF16)
        lrT = sb("lrT", [128, S], BF16)
        ones_bf = sb("ones_bf", [128, 128], BF16)
        negones_f = sb("negones_f", [128, 128], F32)
        ident = sb("ident", [128, 128], BF16)
        maskA = sb("maskA", [128, 2, 128], BF16)
        maskB = sb("maskB", [128, 2, 128], BF16)
        tri16 = sb("tri16", [128, 2, 128], BF16)
        trif = sb("trif", [128, 2, 128], F32)
        WB = 2048
        wbuf = [sb(f"wbuf{i}", [128, WB], BF16) for i in range(2)]
        A_YT = 0
        A_Q = A_YT + 6 * S
        A_K = A_Q + S
        A_2 = A_K + S
        A_3 = A_2 + 4096
        A_4 = A_3 + 4160
        A_5 = A_4 + 4096
        A_END = A_5 + 4100
        arena = sb("arena", [128, A_END], BF16)
        yT = arena[:, A_YT:A_Q].rearrange("p (c t) -> p c t", c=6)
        qT = arena[:, A_Q:A_K]
        kT = arena[:, A_K:A_2]
        kw = arena[:, A_2:A_3].rearrange("p (d i h e) -> p d i h e", d=2, i=NT, h=2)
        bv = arena[:, A_2:A_3].rearrange("p (i e) -> p i e", i=NT)
        vaug = arena[:, A_3:A_4].rearrange("p (i h e) -> p i h e", i=NT, h=2)
        sr = arena[:, A_3:A_3 + 4096].rearrange("p (i e) -> p i e", i=NT)
        sigo = arena[:, A_4:A_5].rearrange("p (i e) -> p i e", i=NT)
        la = arena[:, A_4:A_5].rearrange("p (i e) -> p i e", i=NT)
        hsum = arena[:, A_5:A_5 + 4096].rearrange("p (i e) -> p i e", i=NT)
        qkraw = arena[:, A_5:A_END].bitcast(F32)
        wo = arena[:, A_Q:A_Q + KC * D].rearrange("p (c n) -> p c n", c=KC)
        mixT = arena[:, A_Q + KC * D:A_Q + KC * D + 2 * KC * 512].bitcast(F32).rearrange("p (c t) -> p c t", c=KC)
        actT = arena[:, 0:NF * 1024].rearrange("p (f t) -> p f t", f=NF)
        ffT = arena[:, NF * 1024:NF * 1024 + 2 * KC * 512].bitcast(F32).rearrange("p (c t) -> p c t", c=KC)
        graw = arena[:, NF * 1024 + 2 * KC * 512:NF * 1024 + 2 * KC * 512 + 2 * 1026].bitcast(F32)
        assert NF * 1024 + 2 * KC * 512 + 2 * 1026 <= A_END

        gl = sb("gl", [128, NT, 16], F32)
        lf = sb("lf", [128, 2, NT * 4], F32)
        gtmp = sb("gtmp", [128, 2, NT * 4], F32)
        eb = sb("eb", [128, 2, NT, 4], F32)
        enb = sb("enb", [128, 2, NT, 4], F32)
        wsc = sb("wsc", [128, 2, NT, 4], F32)
        dec = sb("dec", [128, 2, NT, 4], F32)
        wdec = sb("wdec", [128, 2, NT, 4], F32)
        decsel = sb("decsel", [128, 2, NT], F32)
        Sm = sb("Sm", [128, 2, 2, 2, 128], BF16)
        C32 = sb("C32", [128, 2, 132], F32)
        Cbf = sb("Cbf", [128, 2, 2, 132], BF16)
        eBt = sb("eBt", [128, 2, 2, 128], F32)
        eNBt = sb("eNBt", [128, 2, 128], F32)
        qs = sb("qs", [128, 2, 2, 128], BF16)
        ks = sb("ks", [128, 2, 2, 128], BF16)
        ktok = sb("ktok", [128, 2, 2, 128], BF16)
        tmpS = sb("tmpS", [128, 2, 128], F32)
        osb = sb("osb", [128, 2, 260], F32)
        sml = sb("sml", [128, 5, 16], F32)
        sqb = sb("sqb", [128, 2, 512], BF16)
        hhalo = sb("hhalo", [128, KC, 2], BF16)
        rstd = sb("rstd", [128, 512], F32)
        acc = sb("acc", [128, 2, 512], F32)
        bsb = acc[:, 1, 0:256]
        gel = Sm[:].rearrange("p a b c t -> p (a b c t)").rearrange("p (b t) -> p b t", b=2)
        tot2 = acc[:, :, 0:256]
        junk2 = sqb[:, 0, :].rearrange("p (q h t) -> p q h t", q=2, h=2)
        ytile = sqb[:, 1, :].rearrange("p (b t) -> p b t", b=2)
        orders_g = [list(range(NT)), list(range(NT - 1, -1, -1))]

        pb = [psum(f"pb{i}", [128, 512], F32) for i in range(6)]
        ptb = [psum(f"ptb{i}", [128, 1024], BF16) for i in range(2)]
        rot = {"n": 0, "t": 0}

        def bank():
            i = rot["n"] % 6
            rot["n"] += 1
            return pb[i], ("ps", i)

        def tbank():
            i = rot["t"] % 2
            rot["t"] += 1
            return ptb[i], ("pt", i)

        wrot = {"n": 0}

        def load_into(bufspec, segs):
            ap2d, keys = bufspec
            views = []
            off = 0
            for src in segs:
                k, n = src.shape[1], src.shape[2]
                v = ap2d[:, off:off + k * n].rearrange("p (k n) -> p k n", k=k)
                P.dma("pool", lambda e: e.dma_start(out=v, in_=src), W=keys)
                views.append(v)
                off += k * n
            assert off <= ap2d.shape[1]
            return views, list(keys)

        def load_w(segs):
            i = wrot["n"] % 2
            wrot["n"] += 1
            views, _ = load_into((wbuf[i][:, :], [("wbuf", i)]), segs)
            return views, ("wbuf", i)

        def wcols(wd3, l, a, n, kc=KC):
            return wd3[l, :, a:a + n].rearrange("(c p) n -> p c n", p=128)

        P.pool(lambda e: e.memset(ones_bf[:], 1.0), W=["ones_bf"])
        P.pool(lambda e: e.memset(negones_f[:], -1.0), W=["negones_f"])
        P.pool(lambda e: e.memset(ident[:], 0.0), W=["ident"])
        P.pool(lambda e: e.affine_select(out=ident[:], in_=ident[:], pattern=[[-1, 128]], compare_op=ALU.not_equal,
                                         fill=1.0, base=0, channel_multiplier=1), R=["ident"], W=["ident"])
        for t_, val in ((maskA, 0.125), (maskB, 0.125), (tri16, -1.0 / 16.0), (trif, -1.0)):
            P.pool(lambda e, t_=t_, val=val: e.memset(t_[:], val), W=[("const", id(t_))])
            P.pool(lambda e, t_=t_: e.affine_select(out=t_[:, 0, :], in_=t_[:, 0, :], pattern=[[1, 128]], compare_op=ALU.is_ge,
                                                    fill=0.0, base=0, channel_multiplier=-1), R=[("const", id(t_))], W=[("const", id(t_))])
            P.pool(lambda e, t_=t_: e.affine_select(out=t_[:, 1, :], in_=t_[:, 1, :], pattern=[[-1, 128]], compare_op=ALU.is_ge,
                                                    fill=0.0, base=0, channel_multiplier=1), R=[("const", id(t_))], W=[("const", id(t_))])
        CONSTS = ["ones_bf", "negones_f", "ident"] + [("const", id(t_)) for t_ in (maskA, maskB, tri16, trif)]
        P.pool(lambda e: e.memset(lrT[32:33, :], 1.0), W=["lrT1"])
        for c in range(KC):
            P.dma("sp", lambda e, c=c: e.dma_start(out=xT[:, c, :], in_=xT_d[c * 128:(c + 1) * 128, :]),
                  W=[("xT", c, j) for j in range(NB)])

        def blk(j):
            return slice(j * 512, (j + 1) * 512)

        def ykey(ch, i):
            return ("yT", ch, i) if ch < 6 else ("hnT", ch - 6, i // 4)

        def yview(ch):
            return yT[:, ch, :] if ch < 6 else hnT[:, ch - 6, :]

        def til(i):
            return slice(i * 128, (i + 1) * 128)

        def ss_and_rstd(srcs, src_keys, nparity):
            ps_, pk = bank()
            for c in range(KC):
                sq = sqb[:, c % 2, :]
                P.act(lambda e, sq=sq, s_=srcs[c]: e.activation(out=sq, in_=s_, func=AF.Square),
                      R=[src_keys[c]], W=[("sqb", c % 2)])
                P.pe(lambda e, sq=sq, c=c, ps_=ps_: e.matmul(ps_[:, :], lhsT=ones_bf[:], rhs=sq, start=(c == 0), stop=(c == KC - 1)),
                     R=[("sqb", c % 2), "ones_bf"], W=[pk])
            r_ = rstd[:, :]
            P.act(lambda e, ps_=ps_, r_=r_: e.activation(out=r_, in_=ps_[:, :], func=AF.Ln, scale=1.0 / D, bias=EPS),
                  R=[pk], W=["rstd"])
            P.act(lambda e, r_=r_: e.activation(out=r_, in_=r_, func=AF.Exp, scale=-0.5),
                  R=["rstd"], W=["rstd"])
            return r_, "rstd"

        def pre_norm_block(l, gofs, j):
            srcs = [xT[:, c, blk(j)] for c in range(KC)]
            keys = [("xT", c, j) for c in range(KC)]
            r_, rk = ss_and_rstd(srcs, keys, j % 2)
            for c in range(KC):
                col = gofs + c
                P.dve(lambda e, c=c, j=j, col=col, r_=r_: e.scalar_tensor_tensor(
                    out=hnT[:, c, blk(j)], in0=xT[:, c, blk(j)], scalar=pcol[:, col:col + 1], in1=r_,
                    op0=ALU.mult, op1=ALU.mult), R=[("xT", c, j), "pcol", rk], W=[("hnT", c, j)])

        def pre_norm(l, gofs):
            for j in range(NB):
                pre_norm_block(l, gofs, j)

        def post_norm_residual(l, gofs, srcT, src_keyf, tok0, j_x):
            srcs = [srcT[:, c, :] for c in range(KC)]
            keys = [src_keyf(c) for c in range(KC)]
            r_, rk = ss_and_rstd(srcs, keys, j_x % 2)
            for c in range(KC):
                col = gofs + c
                P.dve(lambda e, c=c, col=col, r_=r_: e.scalar_tensor_tensor(
                    out=srcT[:, c, :], in0=srcT[:, c, :], scalar=pcol[:, col:col + 1], in1=r_,
                    op0=ALU.mult, op1=ALU.mult), R=[keys[c], "pcol", rk], W=[keys[c]])
                P.dve(lambda e, c=c: e.tensor_tensor(out=xT[:, c, tok0:tok0 + 512], in0=xT[:, c, tok0:tok0 + 512],
                                                     in1=srcT[:, c, :], op=ALU.add),
                      R=[keys[c], ("xT", c, j_x)], W=[("xT", c, j_x)])

        def proj_fm(wv, wkey, ncol_lo, m, evac):
            for j in range(NB):
                ps_, pk = bank()
                for c in range(KC):
                    P.pe(lambda e, c=c, j=j, ps_=ps_: e.matmul(ps_[0:m, :], lhsT=wv[:, c, ncol_lo:ncol_lo + m], rhs=hnT[:, c, blk(j)],
                                                               start=(c == 0), stop=(c == KC - 1)),
                         R=[wkey, ("hnT", c, j)], W=[pk])
                evac(j, ps_, pk)

        def proj_tm(wv, wkey, ncol_lo, n, per_bank, evac):
            for i0 in range(0, NT, per_bank):
                ps_, pk = bank()
                for ii in range(per_bank):
                    i = i0 + ii
                    for c in range(KC):
                        P.pe(lambda e, c=c, i=i, ii=ii, ps_=ps_: e.matmul(
                            ps_[:, ii * n:(ii + 1) * n], lhsT=hnT[:, c, til(i)], rhs=wv[:, c, ncol_lo:ncol_lo + n],
                            start=(c == 0), stop=(c == KC - 1)),
                            R=[wkey, ("hnT", c, i // 4)], W=[pk])
                evac(i0, ps_, pk)

        def head1(i, q, gate_t, gate_key):
            tt = tot2[:, q, :]
            sq_ = sml[:, 3 + q, :]
            for hh in range(2):
                P.act(lambda e: e.activation(out=junk2[:, q, hh, :],
                                             in_=tt[:, hh * 128:(hh + 1) * 128], func=AF.Square, accum_out=sq_[:, hh:hh + 1]),
                      R=[("acc", q)], W=[("junk", q, hh), ("sml0", q, hh)])
            P.act(lambda e: e.activation(out=sq_[:, 2:4], in_=sq_[:, 0:2], func=AF.Ln, scale=1.0 / 128, bias=EPS),
                  R=[("sml0", q, 0), ("sml0", q, 1)], W=[("sml0b", q)])
            P.act(lambda e: e.activation(out=sq_[:, 4:6], in_=sq_[:, 2:4], func=AF.Exp, scale=-0.5),
                  R=[("sml0b", q)], W=[("sml0c", q)])
            yt = ytile[:, q, :]
            for hh in range(2):
                P.dve(lambda e: e.scalar_tensor_tensor(
                    out=yt[:, hh * 128:(hh + 1) * 128], in0=tt[:, hh * 128:(hh + 1) * 128], scalar=sq_[:, 4 + hh:5 + hh],
                    in1=gate_t[:, i, hh * 128:(hh + 1) * 128], op0=ALU.mult, op1=ALU.mult),
                    R=[("acc", q), ("sml0c", q), gate_key], W=[("ytile", q)])

        def head2(i, q, p, grp):
            yt = ytile[:, q, :]
            pt_, ptk = tbank()
            for hh in range(2):
                P.pe(lambda e: e.transpose(pt_[:, hh * 128:(hh + 1) * 128], yt[:, hh * 128:(hh + 1) * 128], ident[:]),
                     R=[("ytile", q), "ident"], W=[ptk])
            ch = grp * 4 + 2 * p
            for hh in range(2):
                dstv = yview(ch + hh)[:, til(i)]
                P.act(lambda e: e.copy(out=dstv, in_=pt_[:, hh * 128:(hh + 1) * 128]), R=[ptk], W=[ykey(ch + hh, i)])

        def run_pipeline(core, tail, p, grp, gate_t, gate_key_fn, hd1, hd2, td=1):
            its = [(n, d) for n in range(NT) for d in range(2)]
            nit = len(its)
            seconds = []
            for it in range(nit + 2 + hd2):
                if it < nit:
                    core(*its[it])
                if 0 <= it - td < nit:
                    n, d = its[it - td]
                    q = tail(n, d)
                    if q is not None:
                        seconds.append((it - 1, orders_g[d][n], q))
                for (jt, ti, q) in seconds:
                    if jt == it - 1 - hd1:
                        head1(ti, q, gate_t, gate_key_fn(ti))
                for (jt, ti, q) in seconds:
                    if jt == it - 1 - hd2:
                        head2(ti, q, p, grp)

        def stage(name):
            if stop_at == name:
                raise _Stop()

        def layer(l):
            P.dma("sp", lambda e, l=l: e.dma_start(out=pcol[:], in_=pcol_d[:, l * PCOLS:(l + 1) * PCOLS]), W=["pcol"])
            P.dma("sp", lambda e, l=l: e.dma_start(out=pbc[:], in_=pbc_d[l:l + 1, :].partition_broadcast(128)), W=["pbc"])
            P.pool(lambda e: e.memset(w2bd[:], 0.0), W=["w2bd"])
            P.dma("pool", lambda e, l=l: e.dma_start(out=w2bd[0:16, 0:256], in_=w2_d[l, 0]), W=["w2bd"])
            P.dma("pool", lambda e, l=l: e.dma_start(out=w2bd[16:32, 256:512], in_=w2_d[l, 1]), W=["w2bd"])
            P.dma("pool", lambda e, l=l: e.dma_start(out=w2bd[32:33, :], in_=gb_d[l:l + 1, :]), W=["w2bd"])

            stage("params")
            pre_norm(l, 0)
            stage("prenorm")

            for p in range(2):
                h0 = 2 * p
                if p == 0:
                    (wv,), wk = load_w([wcols(w_in_d, l, 1424, 128)])
                    psg, pgk = bank()
                    for i in range(NT):
                        for c in range(KC):
                            P.pe(lambda e, c=c, i=i: e.matmul(psg[:, i * 16:(i + 1) * 16], lhsT=hnT[:, c, til(i)], rhs=wv[:, c, 112:128],
                                                              start=(c == 0), stop=(c == KC - 1)),
                                 R=[wk, ("hnT", c, i // 4)], W=[pgk])
                    P.dve(lambda e: e.tensor_tensor(out=gl[:], in0=psg[:, 0:256].rearrange("p (i g) -> p i g", i=NT),
                                                    in1=pbc[:, 0:16].unsqueeze(1).to_broadcast([128, NT, 16]), op=ALU.add),
                          R=[pgk, "pbc"], W=["gl"])
                    stage("g1")
                    for d in range(2):
                        fsl = gl[:, :, 4 + 8 * d:8 + 8 * d]
                        lfd = lf[:, d, :].rearrange("p (i h) -> p i h", i=NT)
                        P.act(lambda e, fsl=fsl, lfd=lfd: e.activation(out=lfd, in_=fsl, func=AF.Exp, scale=-1.0), R=["gl"], W=[("lf", d)])
                        P.act(lambda e, lfd=lfd: e.activation(out=lfd, in_=lfd, func=AF.Ln, bias=1.0), R=[("lf", d)], W=[("lf", d)])
                    stage("g2")
                    psb, pbk = bank()
                    for d in range(2):
                        P.pe(lambda e, d=d: e.matmul(psb[:, d * 64:(d + 1) * 64], lhsT=trif[:, d, :], rhs=lf[:, d, :], start=True, stop=True),
                             R=[("lf", d)] + CONSTS, W=[pbk])
                        P.pe(lambda e, d=d: e.matmul(psb[:, 128 + d * 64:128 + (d + 1) * 64], lhsT=negones_f[:], rhs=lf[:, d, :], start=True, stop=True),
                             R=[("lf", d)] + CONSTS, W=[pbk])
                    stage("g3")
                    P.act(lambda e: e.copy(out=bsb[:, :], in_=psb[:, 0:256]), R=[pbk], W=[("acc", 1)])
                    flat = lambda t_: t_[:].rearrange("p d i h -> p (d i h)")
                    P.act(lambda e: e.activation(out=flat(eb), in_=bsb[:, 0:128], func=AF.Exp), R=[("acc", 1)], W=["eb"])
                    P.act(lambda e: e.activation(out=flat(enb), in_=bsb[:, 0:128], func=AF.Exp, scale=-1.0), R=[("acc", 1)], W=["enb"])
                    P.act(lambda e: e.activation(out=flat(dec), in_=bsb[:, 128:256], func=AF.Exp), R=[("acc", 1)], W=["dec"])
                    for d in range(2):
                        isl = gl[:, :, 8 * d:8 * d + 4]
                        gt = gtmp[:, d, :].rearrange("p (i h) -> p i h", i=NT)
                        P.dve(lambda e, d=d, isl=isl, gt=gt: e.tensor_tensor(out=gt, in0=isl, in1=bsb[:, d * 64:(d + 1) * 64].rearrange("p (i h) -> p i h", i=NT),
                                                                             op=ALU.subtract), R=["gl", ("acc", 1)], W=[("gtmp", d)])
                        P.act(lambda e, d=d, gt=gt: e.activation(out=wsc[:, d].rearrange("p i h -> p (i h)"), in_=gtmp[:, d, :], func=AF.Exp), R=[("gtmp", d)], W=[("wsc", d)])
                        P.dve(lambda e, d=d: e.tensor_tensor(out=wdec[:, d], in0=wsc[:, d], in1=dec[:, d], op=ALU.mult),
                              R=[("wsc", d), "dec"], W=[("wdec", d)])
                stage("gates")
                for d in range(2):
                    for hh in range(2):
                        P.pool(lambda e, d=d, hh=hh: e.tensor_copy(out=decsel[hh * 64:(hh + 1) * 64, d, :], in_=dec[hh * 64:(hh + 1) * 64, d, :, h0 + hh]),
                               R=["dec"], W=[("decsel", hh)])
                (wq, wkk), wk = load_w([wcols(w_in_d, l, p * 128, 128), wcols(w_in_d, l, 256 + p * 128, 128)])
                for which, wv, dst in ((0, wq, qT), (1, wkk, kT)):
                    cj = which * 2 + p
                    cb = 32 + cj * 4
                    def qk_keys(j):
                        return [("qkraw", j)] + [("s5", ii) for ii in range(4 * j, min(NT, 4 * j + 5))]
                    P.pool(lambda e: e.memset(qkraw[:, 0:1], 0.0), W=qk_keys(0))
                    P.pool(lambda e: e.memset(qkraw[:, 2049:2050], 0.0), W=qk_keys(3))

                    def ev(j, ps_, pk):
                        P.act(lambda e, j=j, ps_=ps_: e.copy(out=qkraw[:, 1 + j * 512:1 + (j + 1) * 512], in_=ps_[:, :]),
                              R=[pk], W=qk_keys(j))
                    proj_fm(wv, wk, 0, 128, ev)
                    for j in range(NB):
                        a_ = acc[:, j % 2, :]
                        rk = []
                        for jj in range(max(0, j - 1), min(NB, j + 2)):
                            rk += qk_keys(jj)
                        P.dve(lambda e, j=j, a_=a_, cb=cb: e.tensor_scalar(out=a_, in0=qkraw[:, 1 + j * 512:1 + (j + 1) * 512],
                                                                           scalar1=pcol[:, cb + 1:cb + 2], scalar2=pcol[:, cb + 3:cb + 4],
                                                                           op0=ALU.mult, op1=ALU.add),
                              R=rk + ["pcol"], W=[("acc", j % 2)])
                        P.dve(lambda e, j=j, a_=a_, cb=cb: e.scalar_tensor_tensor(out=a_, in0=qkraw[:, j * 512:(j + 1) * 512],
                                                                                  scalar=pcol[:, cb:cb + 1], in1=a_, op0=ALU.mult, op1=ALU.add),
                              R=rk + ["pcol", ("acc", j % 2)], W=[("acc", j % 2)])
                        P.dve(lambda e, j=j, a_=a_, cb=cb: e.scalar_tensor_tensor(out=a_, in0=qkraw[:, 2 + j * 512:2 + (j + 1) * 512],
                                                                                  scalar=pcol[:, cb + 2:cb + 3], in1=a_, op0=ALU.mult, op1=ALU.add),
                              R=rk + ["pcol", ("acc", j % 2)], W=[("acc", j % 2)])
                        P.act(lambda e, j=j, a_=a_, dst=dst: e.activation(out=dst[:, blk(j)], in_=a_, func=AF.Silu),
                              R=[("acc", j % 2)], W=[("q" if which == 0 else "k", j)])
                stage("Aqk")
                for hh in range(2):
                    P.pool(lambda e, hh=hh: e.memset(vaug[:, :, hh, 128:130], 1.0), W=[("s3", i) for i in range(NT)])
                (wv,), wk = load_w([wcols(w_in_d, l, 512 + p * 256, 256)])

                def evv(i0, ps_, pk):
                    for ii in range(2):
                        for hh in range(2):
                            P.act(lambda e, i0=i0, ii=ii, hh=hh, ps_=ps_: e.copy(out=vaug[:, i0 + ii, hh, 0:128],
                                                                               in_=ps_[:, ii * 256 + hh * 128:ii * 256 + (hh + 1) * 128]),
                                  R=[pk], W=[("s3", i0 + ii)])
                proj_tm(wv, wk, 0, 256, 2, evv)
                (wv,), wk = load_w([wcols(w_in_d, l, 1024 + p * 256, 256)])

                def evo(i0, ps_, pk):
                    P.act(lambda e, i0=i0, ps_=ps_: e.activation(out=sigo[:, i0:i0 + 2, :].rearrange("p i e -> p (i e)"), in_=ps_[:, :],
                                                                func=AF.Sigmoid), R=[pk], W=[("s4", i0), ("s4", i0 + 1)])
                    P.pool(lambda e, i0=i0: e.tensor_tensor(out=sigo[:, i0:i0 + 2, :], in0=sigo[:, i0:i0 + 2, :],
                                                           in1=pbc[:, 16 + h0 * 128:16 + h0 * 128 + 256].unsqueeze(1).to_broadcast([128, 2, 256]),
                                                           op=ALU.mult), R=[("s4", i0), ("s4", i0 + 1), "pbc"], W=[("s4", i0), ("s4", i0 + 1)])
                proj_tm(wv, wk, 0, 256, 2, evo)

                for i in range(NT):
                    pt_, ptk = tbank()
                    P.pe(lambda e, i=i, pt_=pt_: e.transpose(pt_[:, 0:128], kT[:, til(i)], ident[:]), R=[("k", i // 4), "ident"], W=[ptk])
                    for d in range(2):
                        for hh in range(2):
                            P.dve(lambda e, i=i, d=d, hh=hh, pt_=pt_: e.tensor_scalar(
                                out=kw[:, d, i, hh, :], in0=pt_[:, hh * 64:(hh + 1) * 64], scalar1=wdec[:, d, i, h0 + hh:h0 + hh + 1], scalar2=0.125,
                                op0=ALU.mult, op1=ALU.mult), R=[ptk, ("wdec", d)], W=[("s2", i)])
                stage("Aproj")
                orders = [list(range(NT)), list(range(NT - 1, -1, -1))]
                for d in range(2):
                    P.pool(lambda e, d=d: e.memset(C32[:, d, :], 0.0), W=[("C32", d)])
                    P.pool(lambda e, d=d: e.memset(Cbf[:, d, 0, :], 0.0), W=[("Cbf", d, 0)])

                def a_early(d, n):
                    i = orders[d][n]
                    par = n % 2
                    psSs = [bank(), bank()]
                    for hh in range(2):
                        rs = slice(hh * 64, (hh + 1) * 64)
                        psS, psk = psSs[hh]
                        P.pe(lambda e: e.matmul(psS[:, 0:128], lhsT=kT[rs, til(i)], rhs=qT[rs, til(i)], start=True, stop=True),
                             R=[("k", i // 4), ("q", i // 4)], W=[psk])
                    for hh in range(2):
                        psS, psk = psSs[hh]
                        P.dve(lambda e: e.scalar_tensor_tensor(
                            out=Sm[:, d, par, hh, :], in0=psS[:, 0:128], scalar=wsc[:, d, i, h0 + hh:h0 + hh + 1],
                            in1=maskA[:, d, :], op0=ALU.mult, op1=ALU.mult),
                            R=[psk, ("wsc", d)] + CONSTS, W=[("Sm", d, par, hh)])

                def a_main(d, n):
                    i = orders[d][n]
                    par = n % 2
                    psU, puk = bank()
                    P.pe(lambda e: e.matmul(psU[0:64, 0:130], lhsT=kw[:, d, i, 0, :], rhs=vaug[:, i, 0, 0:130], start=True, stop=True),
                         R=[("s2", i), ("s3", i)], W=[puk])
                    P.pe(lambda e: e.matmul(psU[64:128, 0:130], lhsT=kw[:, d, i, 1, :], rhs=vaug[:, i, 1, 0:130], start=True, stop=True,
                                            tile_position=(0, 64)),
                         R=[("s2", i), ("s3", i)], W=[puk])
                    psO, pok = bank()
                    for hh in range(2):
                        rs = slice(hh * 64, (hh + 1) * 64)
                        P.pe(lambda e: e.matmul(psO[:, hh * 130:hh * 130 + 130], lhsT=Sm[:, d, par, hh, :],
                                                rhs=vaug[:, i, hh, 0:130], start=True, stop=False),
                             R=[("Sm", d, par, hh), ("s3", i)], W=[pok])
                        P.pe(lambda e: e.matmul(psO[:, hh * 130:hh * 130 + 130], lhsT=qT[rs, til(i)],
                                                rhs=Cbf[rs, d, par, 0:130], start=False, stop=True),
                             R=[("q", i // 4), ("Cbf", d, par)], W=[pok])
                    P.dve(lambda e: e.scalar_tensor_tensor(
                        out=C32[:, d, 0:130], in0=C32[:, d, 0:130], scalar=decsel[:, d, i:i + 1], in1=psU[:, 0:130], op0=ALU.mult, op1=ALU.add),
                        R=[("C32", d), ("decsel", 0), ("decsel", 1), puk], W=[("C32", d)])
                    P.pool(lambda e: e.tensor_copy(out=Cbf[:, d, 1 - par, 0:130], in_=C32[:, d, 0:130]),
                           R=[("C32", d)], W=[("Cbf", d, 1 - par)])
                    sm = sml[:, 1 + d, :]
                    P.act(lambda e: e.activation(out=sm[:, 0:2], in_=psO[:, 0:260].rearrange("p (h e) -> p h e", h=2)[:, :, 128], func=AF.Abs),
                          R=[pok], W=[("sml1a", d)])
                    aps[(n, d)] = (psO, pok)

                aps = {}

                def a_tail(n, d):
                    i = orders[d][n]
                    psO, pok = aps.pop((n, d))
                    sm = sml[:, 1 + d, :]
                    q = None
                    if n >= NT // 2:
                        q = acnt[0] % 2
                        acnt[0] += 1
                    P.dve(lambda e: e.tensor_tensor(out=sm[:, 2:4], in0=sm[:, 0:2], in1=enb[:, d, i, h0:h0 + 2], op=ALU.max),
                          R=[("sml1a", d), "enb"], W=[("sml1b", d)])
                    P.dve(lambda e: e.reciprocal(out=sm[:, 4:6], in_=sm[:, 2:4]), R=[("sml1b", d)], W=[("sml1c", d)])
                    for hh in range(2):
                        if n < NT // 2:
                            P.act(lambda e: e.activation(
                                out=hsum[:, i, hh * 128:(hh + 1) * 128], in_=psO[:, hh * 130:hh * 130 + 128], func=AF.Copy, scale=sm[:, 4 + hh:5 + hh]),
                                R=[pok, ("sml1c", d)], W=[("s5", i)])
                        else:
                            P.dve(lambda e: e.scalar_tensor_tensor(
                                out=tot2[:, q, hh * 128:(hh + 1) * 128], in0=psO[:, hh * 130:hh * 130 + 128], scalar=sm[:, 4 + hh:5 + hh],
                                in1=hsum[:, i, hh * 128:(hh + 1) * 128], op0=ALU.mult, op1=ALU.add),
                                R=[pok, ("sml1c", d), ("s5", i)], W=[("acc", q)])
                    return q

                acnt = [0]

                def a_core(n, d):
                    if n == 0:
                        a_early(d, 0)
                    if n + 1 < NT:
                        a_early(d, n + 1)
                    a_main(d, n)

                run_pipeline(a_core, a_tail, p, 0, sigo, lambda ti: ("s4", ti), hd1=1, hd2=2, td=0)

            stage("A")
            for p in range(2):
                h0 = 2 * p
                if p == 0:
                    (wv,), wk = load_w([wcols(w_in_d, l, 2992, 128)])

                    def evl(j, ps_, pk):
                        P.act(lambda e, j=j, ps_=ps_: e.copy(out=lrT[0:32, blk(j)], in_=ps_[0:32, :]), R=[pk], W=[("lrT", j)])
                    proj_fm(wv, wk, 96, 32, evl)
                (wq, wkk), wk = load_w([wcols(w_in_d, l, 1552 + p * 128, 128), wcols(w_in_d, l, 1808 + p * 128, 128)])
                for which, wv, dst in ((0, wq, qT), (1, wkk, kT)):
                    def ev(j, ps_, pk, dst=dst, which=which):
                        P.act(lambda e, j=j, ps_=ps_: e.copy(out=dst[:, blk(j)], in_=ps_[:, :]), R=[pk], W=[("q" if which == 0 else "k", j)])
                    proj_fm(wv, wk, 0, 128, ev)
                (wv,), wk = load_w([wcols(w_in_d, l, 2064 + p * 256, 256)])

                def evv(i0, ps_, pk):
                    P.act(lambda e, i0=i0, ps_=ps_: e.copy(out=bv[:, i0:i0 + 2, :].rearrange("p i e -> p (i e)"), in_=ps_[:, :]),
                          R=[pk], W=[("s2", i0), ("s2", i0 + 1)])
                proj_tm(wv, wk, 0, 256, 2, evv)
                (wv,), wk = load_w([wcols(w_in_d, l, 2576 + p * 256, 256)])

                def evr(i0, ps_, pk):
                    P.act(lambda e, i0=i0, ps_=ps_: e.activation(out=sr[:, i0:i0 + 2, :].rearrange("p i e -> p (i e)"), in_=ps_[:, :],
                                                                func=AF.Silu), R=[pk], W=[("s3", i0), ("s3", i0 + 1)])
                    P.pool(lambda e, i0=i0: e.tensor_tensor(out=sr[:, i0:i0 + 2, :], in0=sr[:, i0:i0 + 2, :],
                                                           in1=pbc[:, 528 + h0 * 128:528 + h0 * 128 + 256].unsqueeze(1).to_broadcast([128, 2, 256]),
                                                           op=ALU.mult), R=[("s3", i0), ("s3", i0 + 1), "pbc"], W=[("s3", i0), ("s3", i0 + 1)])
                proj_tm(wv, wk, 0, 256, 2, evr)
                for i0 in range(0, NT, 2):
                    ps_, pk = bank()
                    for ii in range(2):
                        i = i0 + ii
                        for d in range(2):
                            P.pe(lambda e, i=i, ii=ii, d=d, ps_=ps_: e.matmul(ps_[:, ii * 256 + d * 128:ii * 256 + (d + 1) * 128], lhsT=lrT[0:33, til(i)],
                                                                              rhs=w2bd[0:33, d * 256 + p * 128:d * 256 + (p + 1) * 128], start=True, stop=True),
                                 R=[("lrT", i // 4), "lrT1", "w2bd"], W=[pk])
                    g_ = acc[:, (i0 // 2) % 2, :]
                    P.act(lambda e, ps_=ps_, g_=g_: e.activation(out=g_, in_=ps_[:, :], func=AF.Exp, scale=-1.0), R=[pk], W=[("acc", (i0 // 2) % 2)])
                    P.act(lambda e, i0=i0, g_=g_: e.activation(out=la[:, i0:i0 + 2, :].rearrange("p i e -> p (i e)"), in_=g_, func=AF.Ln, bias=1.0),
                          R=[("acc", (i0 // 2) % 2)], W=[("s4", i0), ("s4", i0 + 1)])
                stage("Bproj")
                orders = [list(range(NT)), list(range(NT - 1, -1, -1))]
                for d in range(2):
                    P.pool(lambda e, d=d: e.memset(C32[:, d, :], 0.0), W=[("C32", d)])
                    P.pool(lambda e, d=d: e.memset(Cbf[:, d, 0, :], 0.0), W=[("Cbf", d, 0)])

                def b_early(d, n):
                    i = orders[d][n]
                    par = n % 2
                    psB, pbk2 = bank()
                    P.pe(lambda e: e.matmul(psB[:, 0:128], lhsT=la[:, i, d * 128:(d + 1) * 128], rhs=tri16[:, d, :], start=True, stop=True),
                         R=[("s4", i)] + CONSTS, W=[pbk2])
                    P.act(lambda e: e.activation(out=eBt[:, d, par, :], in_=psB[:, 0:128], func=AF.Exp), R=[pbk2], W=[("eBt", d, par)])
                    P.act(lambda e: e.activation(out=eNBt[:, d, :], in_=psB[:, 0:128], func=AF.Exp, scale=-1.0), R=[pbk2], W=[("eNBt", d)])
                    P.pool(lambda e: e.tensor_tensor(out=qs[:, d, par, :], in0=qT[:, til(i)], in1=eBt[:, d, par, :], op=ALU.mult),
                           R=[("q", i // 4), ("eBt", d, par)], W=[("qs", d, par)])
                    P.dve(lambda e: e.tensor_tensor(out=ks[:, d, par, :], in0=kT[:, til(i)], in1=eNBt[:, d, :], op=ALU.mult),
                          R=[("k", i // 4), ("eNBt", d)], W=[("ks", d, par)])
                    psSs = [bank(), bank()]
                    for hh in range(2):
                        rs = slice(hh * 64, (hh + 1) * 64)
                        psS, psk = psSs[hh]
                        P.pe(lambda e: e.matmul(psS[:, 0:128], lhsT=ks[rs, d, par, :], rhs=qs[rs, d, par, :], start=True, stop=True),
                             R=[("ks", d, par), ("qs", d, par)], W=[psk])
                    pt_, ptk = tbank()
                    P.pe(lambda e: e.transpose(pt_[:, 0:128], ks[:, d, par, :], ident[:]), R=[("ks", d, par), "ident"], W=[ptk])
                    for hh in range(2):
                        psS, psk = psSs[hh]
                        P.dve(lambda e: e.tensor_tensor(out=Sm[:, d, par, hh, :], in0=psS[:, 0:128], in1=maskB[:, d, :], op=ALU.mult),
                              R=[psk] + CONSTS, W=[("Sm", d, par, hh)])
                    P.act(lambda e: e.activation(out=ktok[:, d, par, :], in_=pt_[:, 0:128], func=AF.Copy, scale=0.125), R=[ptk], W=[("ktok", d, par)])

                def b_main(d, n):
                    i = orders[d][n]
                    par = n % 2
                    lastcol = 127 if d == 0 else 0
                    psU, puk = bank()
                    P.pe(lambda e: e.matmul(psU[0:64, 0:128], lhsT=ktok[:, d, par, 0:64], rhs=bv[:, i, 0:128], start=True, stop=True),
                         R=[("ktok", d, par), ("s2", i)], W=[puk])
                    P.pe(lambda e: e.matmul(psU[64:128, 0:128], lhsT=ktok[:, d, par, 64:128], rhs=bv[:, i, 128:256], start=True, stop=True,
                                            tile_position=(0, 64)), R=[("ktok", d, par), ("s2", i)], W=[puk])
                    psO, pok = bank()
                    for hh in range(2):
                        rs = slice(hh * 64, (hh + 1) * 64)
                        P.pe(lambda e: e.matmul(psO[:, hh * 128:(hh + 1) * 128], lhsT=Sm[:, d, par, hh, :],
                                                rhs=bv[:, i, hh * 128:(hh + 1) * 128], start=True, stop=False),
                             R=[("Sm", d, par, hh), ("s2", i)], W=[pok])
                        P.pe(lambda e: e.matmul(psO[:, hh * 128:(hh + 1) * 128], lhsT=qs[rs, d, par, :],
                                                rhs=Cbf[rs, d, par, 0:128], start=False, stop=True),
                             R=[("qs", d, par), ("Cbf", d, par)], W=[pok])
                    P.act(lambda e: e.activation(out=tmpS[:, d, :], in_=psU[:, 0:128], func=AF.Copy, scale=eBt[:, d, par, lastcol:lastcol + 1]),
                          R=[puk, ("eBt", d, par)], W=[("tmpS", d)])
                    P.dve(lambda e: e.scalar_tensor_tensor(out=C32[:, d, 0:128], in0=C32[:, d, 0:128], scalar=eBt[:, d, par, lastcol:lastcol + 1],
                                                           in1=tmpS[:, d, :], op0=ALU.mult, op1=ALU.add),
                          R=[("tmpS", d), ("eBt", d, par), ("C32", d)], W=[("C32", d)])
                    P.pool(lambda e: e.tensor_copy(out=Cbf[:, d, 1 - par, 0:128], in_=C32[:, d, 0:128]),
                           R=[("C32", d)], W=[("Cbf", d, 1 - par)])
                    if n < NT // 2:
                        P.act(lambda e: e.copy(out=hsum[:, i, :], in_=psO[:, 0:256]), R=[pok], W=[("s5", i)])
                    else:
                        q = bcnt[0] % 2
                        bcnt[0] += 1
                        bq[(n, d)] = q
                        P.dve(lambda e: e.tensor_tensor(out=tot2[:, q, :], in0=psO[:, 0:256], in1=hsum[:, i, :], op=ALU.add),
                              R=[pok, ("s5", i)], W=[("acc", q)])

                bcnt = [0]
                bq = {}

                def b_core(n, d):
                    if n == 0:
                        b_early(d, 0)
                    if n + 1 < NT:
                        b_early(d, n + 1)
                    b_main(d, n)

                run_pipeline(b_core, lambda n, d: bq.get((n, d)), p, 1, sr, lambda ti: ("s3", ti), hd1=0, hd2=1, td=0)

            stage("B")
            if dbg and "yT" in dbg and l == 0:
                P.barrier()
                for c in range(KC):
                    P.act(lambda e, c=c: e.copy(out=xT[:, c, :], in_=yT[:, c, :]), R=[], W=[("xT", c, j) for j in range(NB)])
                P.barrier()

            P.barrier()
            for q4 in range(4):
                P.dma("pool", lambda e, q4=q4, l=l: e.dma_start(out=wo[:, :, q4 * 256:(q4 + 1) * 256],
                                                                in_=w_out_d[l, :, q4 * 256:(q4 + 1) * 256].rearrange("(c p) n -> p c n", p=128)),
                      W=["wo"])
            for j in range(NB):
                for co in range(KC):
                    ps_, pk = bank()
                    for ch in range(KC):
                        P.pe(lambda e, co=co, ch=ch, j=j, ps_=ps_: e.matmul(ps_[:, :], lhsT=wo[:, ch, co * 128:(co + 1) * 128], rhs=yview(ch)[:, blk(j)],
                                                                            start=(ch == 0), stop=(ch == KC - 1)),
                             R=["wo"] + [ykey(ch, i) for i in range(4 * j, 4 * j + 4)], W=[pk])
                    P.act(lambda e, co=co, ps_=ps_: e.copy(out=mixT[:, co, :], in_=ps_[:, :]), R=[pk], W=[("mixT", co)])
                post_norm_residual(l, 8, mixT, lambda c: ("mixT", c), j * 512, j)
                if j >= 1:
                    pre_norm_block(l, 16, j - 1)
            pre_norm_block(l, 16, NB - 1)

            stage("wout")
            P.pool(lambda e: e.tensor_copy(out=hhalo[:, :, :], in_=hnT[:, :, 1023:1025]),
                   R=[("hnT", c, j) for c in range(KC) for j in (1, 2)], W=["hhalo"])
            P.barrier()
            ffT_bf = arena[:, NF * 1024:NF * 1024 + 2 * KC * 512]
            gu_pool = [(wbuf[0][:, :], [("wbuf", 0)]), (wbuf[1][:, :], [("wbuf", 1)])] + \
                      [(ffT_bf[:, j * 2048:(j + 1) * 2048], [("ffT", 2 * j), ("ffT", 2 * j + 1)]) for j in range(4)]
            gurot = [0]
            for H in range(2):
                t0 = H * 1024
                graws = [graw, pbc[:, 0:1026]]
                wst = {}

                def G(f):
                    gr = graws[f % 2]
                    (wgv, wuv), wks = load_into(gu_pool[gurot[0] % len(gu_pool)], [wcols(wg_d, l, f * 128, 128), wcols(wu_d, l, f * 128, 128)])
                    gurot[0] += 1
                    wst[f] = (wuv, wks)
                    if H == 0:
                        P.pool(lambda e: e.memset(gr[:, 0:1], 0.0), W=[("graw", f % 2, 0)])
                        halo_col, hsel = 1025, 1
                    else:
                        P.pool(lambda e: e.memset(gr[:, 1025:1026], 0.0), W=[("graw", f % 2, 1)])
                        halo_col, hsel = 0, 0
                    psh, phk = bank()
                    for c in range(KC):
                        P.pe(lambda e: e.matmul(psh[:, 0:1], lhsT=wgv[:, c, :], rhs=hhalo[:, c, hsel:hsel + 1],
                                                start=(c == 0), stop=(c == KC - 1)), R=wks + ["hhalo"], W=[phk])
                    P.act(lambda e: e.copy(out=gr[:, halo_col:halo_col + 1], in_=psh[:, 0:1]),
                          R=[phk], W=[("graw", f % 2, 0 if halo_col == 0 else 1)])
                    for b in range(2):
                        jj = 2 * H + b
                        ps_, pk = bank()
                        for c in range(KC):
                            P.pe(lambda e: e.matmul(ps_[:, :], lhsT=wgv[:, c, :], rhs=hnT[:, c, blk(jj)],
                                                    start=(c == 0), stop=(c == KC - 1)), R=wks + [("hnT", c, jj)], W=[pk])
                        P.act(lambda e: e.copy(out=gr[:, 1 + b * 512:1 + (b + 1) * 512], in_=ps_[:, :]), R=[pk], W=[("grawb", f % 2, b)])

                def C(f):
                    gr = graws[f % 2]
                    cb = 48 + f * 4
                    rk = [("graw", f % 2, 0), ("graw", f % 2, 1), ("grawb", f % 2, 0), ("grawb", f % 2, 1)]
                    for b in range(2):
                        a_ = acc[:, b, :]
                        P.dve(lambda e: e.tensor_scalar(out=a_, in0=gr[:, 1 + b * 512:1 + (b + 1) * 512],
                                                        scalar1=pcol[:, cb + 1:cb + 2], scalar2=pcol[:, cb + 3:cb + 4],
                                                        op0=ALU.mult, op1=ALU.add), R=rk + ["pcol"], W=[("acc", b)])
                        P.dve(lambda e: e.scalar_tensor_tensor(out=a_, in0=gr[:, b * 512:(b + 1) * 512],
                                                               scalar=pcol[:, cb:cb + 1], in1=a_, op0=ALU.mult, op1=ALU.add),
                              R=rk + ["pcol", ("acc", b)], W=[("acc", b)])
                        P.dve(lambda e: e.scalar_tensor_tensor(out=a_, in0=gr[:, 2 + b * 512:2 + (b + 1) * 512],
                                                               scalar=pcol[:, cb + 2:cb + 3], in1=a_, op0=ALU.mult, op1=ALU.add),
                              R=rk + ["pcol", ("acc", b)], W=[("acc", b)])
                        P.act(lambda e: e.activation(out=gel[:, b, :], in_=a_, func=AF.Gelu_apprx_tanh), R=[("acc", b)], W=[("gel", b)])

                def U(f):
                    wuv, wks = wst.pop(f)
                    for b in range(2):
                        jj = 2 * H + b
                        ps_, pk = bank()
                        for c in range(KC):
                            P.pe(lambda e: e.matmul(ps_[:, :], lhsT=wuv[:, c, :], rhs=hnT[:, c, blk(jj)],
                                                    start=(c == 0), stop=(c == KC - 1)), R=wks + [("hnT", c, jj)], W=[pk])
                        P.dve(lambda e: e.tensor_tensor(out=actT[:, f, b * 512:(b + 1) * 512], in0=gel[:, b, :], in1=ps_[:, :], op=ALU.mult),
                              R=[("gel", b), pk], W=[("actT", f, b)])

                G(0)
                for f in range(NF):
                    if f + 1 < NF:
                        G(f + 1)
                    C(f)
                    U(f)

                d_pool = [(hnT[:, c, t0:t0 + 1024], [("hnT", c, 2 * H), ("hnT", c, 2 * H + 1)]) for c in range(KC)]
                drot = 0
                for b in range(2):
                    for co in range(KC):
                        parts = []
                        for (fa, fb) in ((0, 8), (8, 16), (16, 22)):
                            (wv_,), wk_ = load_into(d_pool[drot % KC], [wd_d[l, fa * 128:fb * 128, co * 128:(co + 1) * 128].rearrange("(f p) n -> p f n", p=128)])
                            drot += 1
                            parts.append((fa, fb, wv_, wk_))
                        ps_, pk = bank()
                        for f in range(NF):
                            fa, fb, wdv, wk_ = parts[f // 8]
                            P.pe(lambda e: e.matmul(ps_[:, :], lhsT=wdv[:, f - fa, :], rhs=actT[:, f, b * 512:(b + 1) * 512],
                                                    start=(f == 0), stop=(f == NF - 1)), R=wk_ + [("actT", f, b)], W=[pk])
                        P.act(lambda e, co=co, ps_=ps_: e.copy(out=ffT[:, co, :], in_=ps_[:, :]), R=[pk], W=[("ffT", co)])
                    post_norm_residual(l, 24, ffT, lambda c: ("ffT", c), t0 + b * 512, 2 * H + b)
            P.barrier()

        try:
            stage("load")
            for l in range(nl):
                layer(l)
        except _Stop:
            pass
        for c in range(KC):
            P.dma("sp", lambda e, c=c: e.dma_start(out=outT_d[c * 128:(c + 1) * 128, :], in_=xT[:, c, :]),
                  R=[("xT", c, j) for j in range(NB)], W=[("out", c)])
        P.add("sp", None, R=[("out", c) for c in range(KC)])
        P.emit(nc)
    return nc


def _pack_params(ls, norm_mix_pre, norm_mix_post, norm_ffn_pre, norm_ffn_post, mlstm_conv_w, mlstm_conv_b,
                 ffn_conv_w, ffn_conv_b, mlstm_gate_b, mlstm_norm, gla_norm):
    nl = len(ls)
    pcol = np.zeros((128, nl * PCOLS), np.float32)
    pbc = np.zeros((nl, PBC), np.float32)
    for li, l in enumerate(ls):
        o = li * PCOLS
        for gi, g in enumerate((norm_mix_pre, norm_mix_post, norm_ffn_pre, norm_ffn_post)):
            pcol[:, o + gi * 8:o + gi * 8 + 8] = g[l].reshape(8, 128).T
        for cj in range(4):
            pcol[:, o + 32 + cj * 4:o + 32 + cj * 4 + 3] = mlstm_conv_w[l][:, cj * 128:(cj + 1) * 128].T
            pcol[:, o + 32 + cj * 4 + 3] = mlstm_conv_b[l][cj * 128:(cj + 1) * 128]
        for f in range(NF):
            pcol[:, o + 48 + f * 4:o + 48 + f * 4 + 3] = ffn_conv_w[l][:, f * 128:(f + 1) * 128].T
            pcol[:, o + 48 + f * 4 + 3] = ffn_conv_b[l][f * 128:(f + 1) * 128]
        pbc[li, 0:16] = mlstm_gate_b[l]
        pbc[li, 16:528] = mlstm_norm[l].reshape(-1)
        pbc[li, 528:1040] = gla_norm[l].reshape(-1)
    return pcol, pbc


_CACHE = {}


def _get_prog(nl):
    if nl not in _CACHE:
        _CACHE[nl] = build_program(nl)
    return _CACHE[nl]


FUSED = True


def kernel(x, norm_mix_pre, norm_mix_post, norm_ffn_pre, norm_ffn_post, w_in, mlstm_gate_b, mlstm_conv_w,
           mlstm_conv_b, mlstm_norm, gla_w2, gla_b, gla_norm, w_out, ffn_w_gate, ffn_w_up, ffn_conv_w,
           ffn_conv_b, ffn_w_down):
    f = lambda a: np.ascontiguousarray(np.asarray(a), dtype=np.float32)
    x = f(x)
    args = [f(a) for a in (norm_mix_pre, norm_mix_post, norm_ffn_pre, norm_ffn_post, mlstm_conv_w, mlstm_conv_b,
                           ffn_conv_w, ffn_conv_b, mlstm_gate_b, mlstm_norm, gla_norm)]
    w_in, w_out, wg, wu, wd = f(w_in), f(w_out), f(ffn_w_gate), f(ffn_w_up), f(ffn_w_down)
    w2, gb = f(gla_w2), f(gla_b)
    xTs = [np.ascontiguousarray(x[b].T) for b in range(NCORES)]
    groups = [list(range(DEPTH))] if FUSED else [[l] for l in range(DEPTH)]
    for ls in groups:
        nl = len(ls)
        nc = _get_prog(nl)
        pcol, pbc = _pack_params(ls, *args)
        sl = slice(ls[0], ls[-1] + 1)
        shared = {"w_in": w_in[sl], "w_out": w_out[sl], "wg": wg[sl], "wu": wu[sl], "wd": wd[sl], "pcol": pcol, "pbc": pbc,
                  "w2": w2[sl], "gb": np.ascontiguousarray(gb[sl].reshape(nl, 512))}
        in_maps = [dict(shared, xT=xTs[b]) for b in range(NCORES)]
        res = run_bass_kernel_spmd(nc, in_maps, core_ids=list(range(NCORES)))
        xTs = [np.asarray(r["outT"]) for r in res.results]
    return np.stack([np.ascontiguousarray(t.T) for t in xTs], axis=0).astype(np.float32)
```

```python
import contextlib
import types
import numpy as np
import concourse.bass as bass
import concourse.mybir as mybir
from concourse.bass_utils import run_bass_kernel_spmd

F32 = mybir.dt.float32
BF16 = mybir.dt.bfloat16
ALU = mybir.AluOpType
AF = mybir.ActivationFunctionType

EPOCH = 30000
HD1, HD2 = 1, 2
NCORES = 8
DEPTH = 4
D = 1024
S = 2048
NT = 16
NB = 4
KC = 8
FF = 2816
NF = 22
INC = 3120
EPS = 1e-6
PCOLS = 136
PBC = 1040


def _freeze(fn):
    if fn is None or fn.__closure__ is None:
        return fn
    cells = []
    for c in fn.__closure__:
        try:
            cells.append(types.CellType(c.cell_contents))
        except ValueError:
            cells.append(c)
    return types.FunctionType(fn.__code__, fn.__globals__, fn.__name__, fn.__defaults__, tuple(cells))


class _Op:
    __slots__ = ("eng", "fn", "R", "W", "dma", "deps", "marked", "tick", "dsem", "dtick", "dprev")

    def __init__(self, eng, fn, R, W, dma):
        self.eng = eng
        self.fn = fn
        self.R = tuple(R)
        self.W = tuple(W)
        self.dma = dma
        self.deps = ()
        self.marked = False
        self.tick = 0
        self.dsem = -1
        self.dtick = 0
        self.dprev = None


class Prog:
    ENGS = ("pe", "act", "dve", "pool", "sp")

    def __init__(self, n_dma_sems=16):
        self.ops = []
        self.nds = n_dma_sems

    def add(self, eng, fn, R=(), W=(), dma=False):
        self.ops.append(_Op(eng, _freeze(fn), R, W, dma))

    def pe(self, fn, R=(), W=()):
        self.add("pe", fn, R, W)

    def act(self, fn, R=(), W=()):
        self.add("act", fn, R, W)

    def dve(self, fn, R=(), W=()):
        self.add("dve", fn, R, W)

    def pool(self, fn, R=(), W=()):
        self.add("pool", fn, R, W)

    def dma(self, eng, fn, R=(), W=()):
        self.add(eng, fn, R, W, dma=True)

    def barrier(self):
        self.ops.append(_Op("__barrier__", None, (), (), False))

    def analyze(self):
        last_w = {}
        readers = {}
        last_on_eng = {e: None for e in self.ENGS}
        pend = {e: None for e in self.ENGS}
        out = []
        for op in self.ops:
            if op.eng == "__barrier__":
                snap = [v for v in last_on_eng.values() if v is not None]
                for e in self.ENGS:
                    pend[e] = snap
                continue
            g = len(out)
            deps = set()
            for k in op.R:
                if k in last_w:
                    deps.add(last_w[k])
            for k in op.W:
                if k in last_w:
                    deps.add(last_w[k])
                for r in readers.get(k, ()):
                    deps.add(r)
            if pend[op.eng] is not None:
                deps.update(pend[op.eng])
                pend[op.eng] = None
            for k in op.R:
                readers.setdefault(k, []).append(g)
            for k in op.W:
                last_w[k] = g
                readers[k] = []
            deps.discard(g)
            op.deps = tuple(sorted(deps))
            out.append(op)
            if op.fn is not None:
                last_on_eng[op.eng] = g
        self.lin = out
        ndma = 0
        nq = [0, 0]
        last_dma_on_sem = {}
        for g, op in enumerate(out):
            for d in op.deps:
                p = out[d]
                if p.dma:
                    continue
                if p.eng == "pe" and op.eng == "pe" and not op.dma:
                    continue
                p.marked = True
            if op.dma:
                half = self.nds // 2
                qi = 0 if op.eng == "sp" else 1
                s = qi * half + (nq[qi] % half)
                nq[qi] += 1
                ndma += 1
                op.dsem = s
                prev = last_dma_on_sem.get(s)
                op.dprev = prev
                op.dtick = (out[prev].dtick if prev is not None else 0) + 16
                last_dma_on_sem[s] = g
        cnt = {e: 0 for e in self.ENGS}
        for op in out:
            if op.marked and not op.dma:
                cnt[op.eng] += 1
                op.tick = cnt[op.eng]
        self.cnt = cnt
        self.ndma = ndma

    def emit(self, nc):
        self.analyze()
        out = self.lin
        with contextlib.ExitStack() as st:
            esems = {}
            for e in self.ENGS:
                nep = self.cnt[e] // EPOCH + 1
                esems[e] = [st.enter_context(nc.semaphore(f"s_{e}_{i}")) for i in range(nep)]
            dsems = [st.enter_context(nc.semaphore(f"s_dma_{i}")) for i in range(self.nds)]
            block = st.enter_context(nc.Block())
            by_eng = {e: [] for e in self.ENGS}
            for g, op in enumerate(out):
                by_eng[op.eng].append((g, op))

            def sem_of(p):
                if p.dma:
                    return ("d", p.dsem), dsems[p.dsem], p.dtick
                ep = (p.tick - 1) // EPOCH
                return (p.eng, ep), esems[p.eng][ep], p.tick - ep * EPOCH

            def run(eng_name, e):
                waited = {}
                for g, op in by_eng[eng_name]:
                    waits = {}
                    deps = list(op.deps)
                    if op.dma and op.dprev is not None:
                        deps.append(op.dprev)
                    for d in deps:
                        p = out[d]
                        if (not p.dma) and p.eng == "pe" and eng_name == "pe" and not op.dma:
                            continue
                        key, sem, val = sem_of(p)
                        if waited.get(key, 0) >= val:
                            continue
                        if key not in waits or waits[key][1] < val:
                            waits[key] = (sem, val)
                    for key, (sem, val) in waits.items():
                        e.wait_ge(sem, val)
                        waited[key] = val
                    if op.fn is None:
                        continue
                    ins = op.fn(e)
                    if op.dma:
                        ins.then_inc(dsems[op.dsem], 16)
                    elif op.marked:
                        ep = (op.tick - 1) // EPOCH
                        ins.then_inc(esems[eng_name][ep], 1)

            @block.tensor
            def _(e):
                run("pe", e)

            @block.scalar
            def _(e):
                run("act", e)

            @block.vector
            def _(e):
                run("dve", e)

            @block.gpsimd
            def _(e):
                run("pool", e)

            @block.sync
            def _(e):
                run("sp", e)


class _Stop(Exception):
    pass


def build_program(nl, dbg=None, stop_at=None):
    nc = bass.Bass("TRN2", target_bir_lowering=False)
    P = Prog()

    def din(name, shape):
        return nc.dram_tensor(name, shape, F32, kind="ExternalInput").ap()

    xT_d = din("xT", [D, S])
    w_in_d = din("w_in", [nl, D, INC])
    w_out_d = din("w_out", [nl, D, D])
    wg_d = din("wg", [nl, D, FF])
    wu_d = din("wu", [nl, D, FF])
    wd_d = din("wd", [nl, FF, D])
    pcol_d = din("pcol", [128, nl * PCOLS])
    pbc_d = din("pbc", [nl, PBC])
    w2_d = din("w2", [nl, 2, 16, 256])
    gb_d = din("gb", [nl, 512])
    outT_d = nc.dram_tensor("outT", [D, S], F32, kind="ExternalOutput").ap()
    dbg_d = {}
    if dbg:
        for k, shp in dbg.items():
            dbg_d[k] = nc.dram_tensor("dbg_" + k, shp, F32, kind="ExternalOutput").ap()

    st = contextlib.ExitStack()
    with st:
        def sb(name, shape, dt):
            return st.enter_context(nc.sbuf_tensor(name, shape, dt))

        def psum(name, shape, dt):
            return st.enter_context(nc.psum_tensor(name, shape, dt))

        xT = sb("xT_sb", [128, KC, S], F32)
        hnT = sb("hnT", [128, KC, S], BF16)
        pcol = sb("pcol_sb", [128, PCOLS], F32)
        pbc = sb("pbc_sb", [128, PBC], F32)
        w2bd = sb("w2bd", [128, 512], BF16)
        lrT = sb("lrT", [128, S], BF16)
        ones_bf = sb("ones_bf", [128, 128], BF16)
        negones_f = sb("negones_f", [128, 128], F32)
        ident = sb("ident", [128, 128], BF16)
        maskA = sb("maskA", [128, 2, 128], BF16)
        maskB = sb("maskB", [128, 2, 128], BF16)
        tri16 = sb("tri16", [128, 2, 128], BF16)
        trif = sb("trif", [128, 2, 128], F32)
        WB = 2048
        wbuf = [sb(f"wbuf{i}", [128, WB], BF16) for i in range(2)]
        A_YT = 0
        A_Q = A_YT + 6 * S
        A_K = A_Q + S
        A_2 = A_K + S
        A_3 = A_2 + 4096
        A_4 = A_3 + 4160
        A_5 = A_4 + 4096
        A_END = A_5 + 4100
        arena = sb("arena", [128, A_END], BF16)
        yT = arena[:, A_YT:A_Q].rearrange("p (c t) -> p c t", c=6)
        qT = arena[:, A_Q:A_K]
        kT = arena[:, A_K:A_2]
        kw = arena[:, A_2:A_3].rearrange("p (d i h e) -> p d i h e", d=2, i=NT, h=2)
        bv = arena[:, A_2:A_3].rearrange("p (i e) -> p i e", i=NT)
        vaug = arena[:, A_3:A_4].rearrange("p (i h e) -> p i h e", i=NT, h=2)
        sr = arena[:, A_3:A_3 + 4096].rearrange("p (i e) -> p i e", i=NT)
        sigo = arena[:, A_4:A_5].rearrange("p (i e) -> p i e", i=NT)
        la = arena[:, A_4:A_5].rearrange("p (i e) -> p i e", i=NT)
        hsum = arena[:, A_5:A_5 + 4096].rearrange("p (i e) -> p i e", i=NT)
        qkraw = arena[:, A_5:A_END].bitcast(F32)
        wo = arena[:, A_Q:A_Q + KC * D].rearrange("p (c n) -> p c n", c=KC)
        mixT = arena[:, A_Q + KC * D:A_Q + KC * D + 2 * KC * 512].bitcast(F32).rearrange("p (c t) -> p c t", c=KC)
        actT = arena[:, 0:NF * 1024].rearrange("p (f t) -> p f t", f=NF)
        ffT = arena[:, NF * 1024:NF * 1024 + 2 * KC * 512].bitcast(F32).rearrange("p (c t) -> p c t", c=KC)
        graw = arena[:, NF * 1024 + 2 * KC * 512:NF * 1024 + 2 * KC * 512 + 2 * 1026].bitcast(F32)
        assert NF * 1024 + 2 * KC * 512 + 2 * 1026 <= A_END

        gl = sb("gl", [128, NT, 16], F32)
        lf = sb("lf", [128, 2, NT * 4], F32)
        gtmp = sb("gtmp", [128, 2, NT * 4], F32)
        eb = sb("eb", [128, 2, NT, 4], F32)
        enb = sb("enb", [128, 2, NT, 4], F32)
        wsc = sb("wsc", [128, 2, NT, 4], F32)
        dec = sb("dec", [128, 2, NT, 4], F32)
        wdec = sb("wdec", [128, 2, NT, 4], F32)
        decsel = sb("decsel", [128, 2, NT], F32)
        Sm = sb("Sm", [128, 2, 2, 2, 128], BF16)
        C32 = sb("C32", [128, 2, 132], F32)
        Cbf = sb("Cbf", [128, 2, 2, 132], BF16)
        eBt = sb("eBt", [128, 2, 2, 128], F32)
        eNBt = sb("eNBt", [128, 2, 128], F32)
        qs = sb("qs", [128, 2, 2, 128], BF16)
        ks = sb("ks", [128, 2, 2, 128], BF16)
        ktok = sb("ktok", [128, 2, 2, 128], BF16)
        tmpS = sb("tmpS", [128, 2, 128], F32)
        osb = sb("osb", [128, 2, 260], F32)
        sml = sb("sml", [128, 5, 16], F32)
        sqb = sb("sqb", [128, 2, 512], BF16)
        hhalo = sb("hhalo", [128, KC, 2], BF16)
        rstd = sb("rstd", [128, 512], F32)
        acc = sb("acc", [128, 2, 512], F32)
        bsb = acc[:, 1, 0:256]
        gel = Sm[:].rearrange("p a b c t -> p (a b c t)").rearrange("p (b t) -> p b t", b=2)
        tot2 = acc[:, :, 0:256]
        junk2 = sqb[:, 0, :].rearrange("p (q h t) -> p q h t", q=2, h=2)
        ytile = sqb[:, 1, :].rearrange("p (b t) -> p b t", b=2)
        orders_g = [list(range(NT)), list(range(NT - 1, -1, -1))]

        pb = [psum(f"pb{i}", [128, 512], F32) for i in range(6)]
        ptb = [psum(f"ptb{i}", [128, 1024], BF16) for i in range(2)]
        rot = {"n": 0, "t": 0}

        def bank():
            i = rot["n"] % 6
            rot["n"] += 1
            return pb[i], ("ps", i)

        def tbank():
            i = rot["t"] % 2
            rot["t"] += 1
            return ptb[i], ("pt", i)

        wrot = {"n": 0}

        def load_into(bufspec, segs):
            ap2d, keys = bufspec
            views = []
            off = 0
            for src in segs:
                k, n = src.shape[1], src.shape[2]
                v = ap2d[:, off:off + k * n].rearrange("p (k n) -> p k n", k=k)
                P.dma("pool", lambda e: e.dma_start(out=v, in_=src), W=keys)
                views.append(v)
                off += k * n
            assert off <= ap2d.shape[1]
            return views, list(keys)

        def load_w(segs):
            i = wrot["n"] % 2
            wrot["n"] += 1
            views, _ = load_into((wbuf[i][:, :], [("wbuf", i)]), segs)
            return views, ("wbuf", i)

        def wcols(wd3, l, a, n, kc=KC):
            return wd3[l, :, a:a + n].rearrange("(c p) n -> p c n", p=128)

        P.pool(lambda e: e.memset(ones_bf[:], 1.0), W=["ones_bf"])
        P.pool(lambda e: e.memset(negones_f[:], -1.0), W=["negones_f"])
        P.pool(lambda e: e.memset(ident[:], 0.0), W=["ident"])
        P.pool(lambda e: e.affine_select(out=ident[:], in_=ident[:], pattern=[[-1, 128]], compare_op=ALU.not_equal,
                                         fill=1.0, base=0, channel_multiplier=1), R=["ident"], W=["ident"])
        for t_, val in ((maskA, 0.125), (maskB, 0.125), (tri16, -1.0 / 16.0), (trif, -1.0)):
            P.pool(lambda e, t_=t_, val=val: e.memset(t_[:], val), W=[("const", id(t_))])
            P.pool(lambda e, t_=t_: e.affine_select(out=t_[:, 0, :], in_=t_[:, 0, :], pattern=[[1, 128]], compare_op=ALU.is_ge,
                                                    fill=0.0, base=0, channel_multiplier=-1), R=[("const", id(t_))], W=[("const", id(t_))])
            P.pool(lambda e, t_=t_: e.affine_select(out=t_[:, 1, :], in_=t_[:, 1, :], pattern=[[-1, 128]], compare_op=ALU.is_ge,
                                                    fill=0.0, base=0, channel_multiplier=1), R=[("const", id(t_))], W=[("const", id(t_))])
        CONSTS = ["ones_bf", "negones_f", "ident"] + [("const", id(t_)) for t_ in (maskA, maskB, tri16, trif)]
        P.pool(lambda e: e.memset(lrT[32:33, :], 1.0), W=["lrT1"])
        for c in range(KC):
            P.dma("sp", lambda e, c=c: e.dma_start(out=xT[:, c, :], in_=xT_d[c * 128:(c + 1) * 128, :]),
                  W=[("xT", c, j) for j in range(NB)])

        def blk(j):
            return slice(j * 512, (j + 1) * 512)

        def ykey(ch, i):
            return ("yT", ch, i) if ch < 6 else ("hnT", ch - 6, i // 4)

        def yview(ch):
            return yT[:, ch, :] if ch < 6 else hnT[:, ch - 6, :]

        def til(i):
            return slice(i * 128, (i + 1) * 128)

        def ss_and_rstd(srcs, src_keys, nparity):
            ps_, pk = bank()
            for c in range(KC):
                sq = sqb[:, c % 2, :]
                P.act(lambda e, sq=sq, s_=srcs[c]: e.activation(out=sq, in_=s_, func=AF.Square),
                      R=[src_keys[c]], W=[("sqb", c % 2)])
                P.pe(lambda e, sq=sq, c=c, ps_=ps_: e.matmul(ps_[:, :], lhsT=ones_bf[:], rhs=sq, start=(c == 0), stop=(c == KC - 1)),
                     R=[("sqb", c % 2), "ones_bf"], W=[pk])
            r_ = rstd[:, :]
            P.act(lambda e, ps_=ps_, r_=r_: e.activation(out=r_, in_=ps_[:, :], func=AF.Ln, scale=1.0 / D, bias=EPS),
                  R=[pk], W=["rstd"])
            P.act(lambda e, r_=r_: e.activation(out=r_, in_=r_, func=AF.Exp, scale=-0.5),
                  R=["rstd"], W=["rstd"])
            return r_, "rstd"

        def pre_norm_block(l, gofs, j):
            srcs = [xT[:, c, blk(j)] for c in range(KC)]
            keys = [("xT", c, j) for c in range(KC)]
            r_, rk = ss_and_rstd(srcs, keys, j % 2)
            for c in range(KC):
                col = gofs + c
                P.dve(lambda e, c=c, j=j, col=col, r_=r_: e.scalar_tensor_tensor(
                    out=hnT[:, c, blk(j)], in0=xT[:, c, blk(j)], scalar=pcol[:, col:col + 1], in1=r_,
                    op0=ALU.mult, op1=ALU.mult), R=[("xT", c, j), "pcol", rk], W=[("hnT", c, j)])

        def pre_norm(l, gofs):
            for j in range(NB):
                pre_norm_block(l, gofs, j)

        def post_norm_residual(l, gofs, srcT, src_keyf, tok0, j_x):
            srcs = [srcT[:, c, :] for c in range(KC)]
            keys = [src_keyf(c) for c in range(KC)]
            r_, rk = ss_and_rstd(srcs, keys, j_x % 2)
            for c in range(KC):
                col = gofs + c
                P.dve(lambda e, c=c, col=col, r_=r_: e.scalar_tensor_tensor(
                    out=srcT[:, c, :], in0=srcT[:, c, :], scalar=pcol[:, col:col + 1], in1=r_,
                    op0=ALU.mult, op1=ALU.mult), R=[keys[c], "pcol", rk], W=[keys[c]])
                P.dve(lambda e, c=c: e.tensor_tensor(out=xT[:, c, tok0:tok0 + 512], in0=xT[:, c, tok0:tok0 + 512],
                                                     in1=srcT[:, c, :], op=ALU.add),
                      R=[keys[c], ("xT", c, j_x)], W=[("xT", c, j_x)])

        def proj_fm(wv, wkey, ncol_lo, m, evac):
            for j in range(NB):
                ps_, pk = bank()
                for c in range(KC):
                    P.pe(lambda e, c=c, j=j, ps_=ps_: e.matmul(ps_[0:m, :], lhsT=wv[:, c, ncol_lo:ncol_lo + m], rhs=hnT[:, c, blk(j)],
                                                               start=(c == 0), stop=(c == KC - 1)),
                         R=[wkey, ("hnT", c, j)], W=[pk])
                evac(j, ps_, pk)

        def proj_tm(wv, wkey, ncol_lo, n, per_bank, evac):
            for i0 in range(0, NT, per_bank):
                ps_, pk = bank()
                for ii in range(per_bank):
                    i = i0 + ii
                    for c in range(KC):
                        P.pe(lambda e, c=c, i=i, ii=ii, ps_=ps_: e.matmul(
                            ps_[:, ii * n:(ii + 1) * n], lhsT=hnT[:, c, til(i)], rhs=wv[:, c, ncol_lo:ncol_lo + n],
                            start=(c == 0), stop=(c == KC - 1)),
                            R=[wkey, ("hnT", c, i // 4)], W=[pk])
                evac(i0, ps_, pk)

        def head1(i, q, gate_t, gate_key):
            tt = tot2[:, q, :]
            sq_ = sml[:, 3 + q, :]
            for hh in range(2):
                P.act(lambda e: e.activation(out=junk2[:, q, hh, :],
                                             in_=tt[:, hh * 128:(hh + 1) * 128], func=AF.Square, accum_out=sq_[:, hh:hh + 1]),
                      R=[("acc", q)], W=[("junk", q, hh), ("sml0", q, hh)])
            P.act(lambda e: e.activation(out=sq_[:, 2:4], in_=sq_[:, 0:2], func=AF.Ln, scale=1.0 / 128, bias=EPS),
                  R=[("sml0", q, 0), ("sml0", q, 1)], W=[("sml0b", q)])
            P.act(lambda e: e.activation(out=sq_[:, 4:6], in_=sq_[:, 2:4], func=AF.Exp, scale=-0.5),
                  R=[("sml0b", q)], W=[("sml0c", q)])
            yt = ytile[:, q, :]
            for hh in range(2):
                P.dve(lambda e: e.scalar_tensor_tensor(
                    out=yt[:, hh * 128:(hh + 1) * 128], in0=tt[:, hh * 128:(hh + 1) * 128], scalar=sq_[:, 4 + hh:5 + hh],
                    in1=gate_t[:, i, hh * 128:(hh + 1) * 128], op0=ALU.mult, op1=ALU.mult),
                    R=[("acc", q), ("sml0c", q), gate_key], W=[("ytile", q)])

        def head2(i, q, p, grp):
            yt = ytile[:, q, :]
            pt_, ptk = tbank()
            for hh in range(2):
                P.pe(lambda e: e.transpose(pt_[:, hh * 128:(hh + 1) * 128], yt[:, hh * 128:(hh + 1) * 128], ident[:]),
                     R=[("ytile", q), "ident"], W=[ptk])
            ch = grp * 4 + 2 * p
            for hh in range(2):
                dstv = yview(ch + hh)[:, til(i)]
                P.act(lambda e: e.copy(out=dstv, in_=pt_[:, hh * 128:(hh + 1) * 128]), R=[ptk], W=[ykey(ch + hh, i)])

        def run_pipeline(core, tail, p, grp, gate_t, gate_key_fn, hd1, hd2, td=1):
            its = [(n, d) for n in range(NT) for d in range(2)]
            nit = len(its)
            seconds = []
            for it in range(nit + 2 + hd2):
                if it < nit:
                    core(*its[it])
                if 0 <= it - td < nit:
                    n, d = its[it - td]
                    q = tail(n, d)
                    if q is not None:
                        seconds.append((it - 1, orders_g[d][n], q))
                for (jt, ti, q) in seconds:
                    if jt == it - 1 - hd1:
                        head1(ti, q, gate_t, gate_key_fn(ti))
                for (jt, ti, q) in seconds:
                    if jt == it - 1 - hd2:
                        head2(ti, q, p, grp)

        def stage(name):
            if stop_at == name:
                raise _Stop()

        def layer(l):
            P.dma("sp", lambda e, l=l: e.dma_start(out=pcol[:], in_=pcol_d[:, l * PCOLS:(l + 1) * PCOLS]), W=["pcol"])
            P.dma("sp", lambda e, l=l: e.dma_start(out=pbc[:], in_=pbc_d[l:l + 1, :].partition_broadcast(128)), W=["pbc"])
            P.pool(lambda e: e.memset(w2bd[:], 0.0), W=["w2bd"])
            P.dma("pool", lambda e, l=l: e.dma_start(out=w2bd[0:16, 0:256], in_=w2_d[l, 0]), W=["w2bd"])
            P.dma("pool", lambda e, l=l: e.dma_start(out=w2bd[16:32, 256:512], in_=w2_d[l, 1]), W=["w2bd"])
            P.dma("pool", lambda e, l=l: e.dma_start(out=w2bd[32:33, :], in_=gb_d[l:l + 1, :]), W=["w2bd"])

            stage("params")
            pre_norm(l, 0)
            stage("prenorm")

            for p in range(2):
                h0 = 2 * p
                if p == 0:
                    (wv,), wk = load_w([wcols(w_in_d, l, 1424, 128)])
                    psg, pgk = bank()
                    for i in range(NT):
                        for c in range(KC):
                            P.pe(lambda e, c=c, i=i: e.matmul(psg[:, i * 16:(i + 1) * 16], lhsT=hnT[:, c, til(i)], rhs=wv[:, c, 112:128],
                                                              start=(c == 0), stop=(c == KC - 1)),
                                 R=[wk, ("hnT", c, i // 4)], W=[pgk])
                    P.dve(lambda e: e.tensor_tensor(out=gl[:], in0=psg[:, 0:256].rearrange("p (i g) -> p i g", i=NT),
                                                    in1=pbc[:, 0:16].unsqueeze(1).to_broadcast([128, NT, 16]), op=ALU.add),
                          R=[pgk, "pbc"], W=["gl"])
                    stage("g1")
                    for d in range(2):
                        fsl = gl[:, :, 4 + 8 * d:8 + 8 * d]
                        lfd = lf[:, d, :].rearrange("p (i h) -> p i h", i=NT)
                        P.act(lambda e, fsl=fsl, lfd=lfd: e.activation(out=lfd, in_=fsl, func=AF.Exp, scale=-1.0), R=["gl"], W=[("lf", d)])
                        P.act(lambda e, lfd=lfd: e.activation(out=lfd, in_=lfd, func=AF.Ln, bias=1.0), R=[("lf", d)], W=[("lf", d)])
                    stage("g2")
                    psb, pbk = bank()
                    for d in range(2):
                        P.pe(lambda e, d=d: e.matmul(psb[:, d * 64:(d + 1) * 64], lhsT=trif[:, d, :], rhs=lf[:, d, :], start=True, stop=True),
                             R=[("lf", d)] + CONSTS, W=[pbk])
                        P.pe(lambda e, d=d: e.matmul(psb[:, 128 + d * 64:128 + (d + 1) * 64], lhsT=negones_f[:], rhs=lf[:, d, :], start=True, stop=True),
                             R=[("lf", d)] + CONSTS, W=[pbk])
                    stage("g3")
                    P.act(lambda e: e.copy(out=bsb[:, :], in_=psb[:, 0:256]), R=[pbk], W=[("acc", 1)])
                    flat = lambda t_: t_[:].rearrange("p d i h -> p (d i h)")
                    P.act(lambda e: e.activation(out=flat(eb), in_=bsb[:, 0:128], func=AF.Exp), R=[("acc", 1)], W=["eb"])
                    P.act(lambda e: e.activation(out=flat(enb), in_=bsb[:, 0:128], func=AF.Exp, scale=-1.0), R=[("acc", 1)], W=["enb"])
                    P.act(lambda e: e.activation(out=flat(dec), in_=bsb[:, 128:256], func=AF.Exp), R=[("acc", 1)], W=["dec"])
                    for d in range(2):
                        isl = gl[:, :, 8 * d:8 * d + 4]
                        gt = gtmp[:, d, :].rearrange("p (i h) -> p i h", i=NT)
                        P.dve(lambda e, d=d, isl=isl, gt=gt: e.tensor_tensor(out=gt, in0=isl, in1=bsb[:, d * 64:(d + 1) * 64].rearrange("p (i h) -> p i h", i=NT),
                                                                             op=ALU.subtract), R=["gl", ("acc", 1)], W=[("gtmp", d)])
                        P.act(lambda e, d=d, gt=gt: e.activation(out=wsc[:, d].rearrange("p i h -> p (i h)"), in_=gtmp[:, d, :], func=AF.Exp), R=[("gtmp", d)], W=[("wsc", d)])
                        P.dve(lambda e, d=d: e.tensor_tensor(out=wdec[:, d], in0=wsc[:, d], in1=dec[:, d], op=ALU.mult),
                              R=[("wsc", d), "dec"], W=[("wdec", d)])
                stage("gates")
                for d in range(2):
                    for hh in range(2):
                        P.pool(lambda e, d=d, hh=hh: e.tensor_copy(out=decsel[hh * 64:(hh + 1) * 64, d, :], in_=dec[hh * 64:(hh + 1) * 64, d, :, h0 + hh]),
                               R=["dec"], W=[("decsel", hh)])
                (wq, wkk), wk = load_w([wcols(w_in_d, l, p * 128, 128), wcols(w_in_d, l, 256 + p * 128, 128)])
                for which, wv, dst in ((0, wq, qT), (1, wkk, kT)):
                    cj = which * 2 + p
                    cb = 32 + cj * 4
                    def qk_keys(j):
                        return [("qkraw", j)] + [("s5", ii) for ii in range(4 * j, min(NT, 4 * j + 5))]
                    P.pool(lambda e: e.memset(qkraw[:, 0:1], 0.0), W=qk_keys(0))
                    P.pool(lambda e: e.memset(qkraw[:, 2049:2050], 0.0), W=qk_keys(3))

                    def ev(j, ps_, pk):
                        P.act(lambda e, j=j, ps_=ps_: e.copy(out=qkraw[:, 1 + j * 512:1 + (j + 1) * 512], in_=ps_[:, :]),
                              R=[pk], W=qk_keys(j))
                    proj_fm(wv, wk, 0, 128, ev)
                    for j in range(NB):
                        a_ = acc[:, j % 2, :]
                        rk = []
                        for jj in range(max(0, j - 1), min(NB, j + 2)):
                            rk += qk_keys(jj)
                        P.dve(lambda e, j=j, a_=a_, cb=cb: e.tensor_scalar(out=a_, in0=qkraw[:, 1 + j * 512:1 + (j + 1) * 512],
                                                                           scalar1=pcol[:, cb + 1:cb + 2], scalar2=pcol[:, cb + 3:cb + 4],
                                                                           op0=ALU.mult, op1=ALU.add),
                              R=rk + ["pcol"], W=[("acc", j % 2)])
                        P.dve(lambda e, j=j, a_=a_, cb=cb: e.scalar_tensor_tensor(out=a_, in0=qkraw[:, j * 512:(j + 1) * 512],
                                                                                  scalar=pcol[:, cb:cb + 1], in1=a_, op0=ALU.mult, op1=ALU.add),
                              R=rk + ["pcol", ("acc", j % 2)], W=[("acc", j % 2)])
                        P.dve(lambda e, j=j, a_=a_, cb=cb: e.scalar_tensor_tensor(out=a_, in0=qkraw[:, 2 + j * 512:2 + (j + 1) * 512],
                                                                                  scalar=pcol[:, cb + 2:cb + 3], in1=a_, op0=ALU.mult, op1=ALU.add),
                              R=rk + ["pcol", ("acc", j % 2)], W=[("acc", j % 2)])
                        P.act(lambda e, j=j, a_=a_, dst=dst: e.activation(out=dst[:, blk(j)], in_=a_, func=AF.Silu),
                              R=[("acc", j % 2)], W=[("q" if which == 0 else "k", j)])
                stage("Aqk")
                for hh in range(2):
                    P.pool(lambda e, hh=hh: e.memset(vaug[:, :, hh, 128:130], 1.0), W=[("s3", i) for i in range(NT)])
                (wv,), wk = load_w([wcols(w_in_d, l, 512 + p * 256, 256)])

                def evv(i0, ps_, pk):
                    for ii in range(2):
                        for hh in range(2):
                            P.act(lambda e, i0=i0, ii=ii, hh=hh, ps_=ps_: e.copy(out=vaug[:, i0 + ii, hh, 0:128],
                                                                               in_=ps_[:, ii * 256 + hh * 128:ii * 256 + (hh + 1) * 128]),
                                  R=[pk], W=[("s3", i0 + ii)])
                proj_tm(wv, wk, 0, 256, 2, evv)
                (wv,), wk = load_w([wcols(w_in_d, l, 1024 + p * 256, 256)])

                def evo(i0, ps_, pk):
                    P.act(lambda e, i0=i0, ps_=ps_: e.activation(out=sigo[:, i0:i0 + 2, :].rearrange("p i e -> p (i e)"), in_=ps_[:, :],
                                                                func=AF.Sigmoid), R=[pk], W=[("s4", i0), ("s4", i0 + 1)])
                    P.pool(lambda e, i0=i0: e.tensor_tensor(out=sigo[:, i0:i0 + 2, :], in0=sigo[:, i0:i0 + 2, :],
                                                           in1=pbc[:, 16 + h0 * 128:16 + h0 * 128 + 256].unsqueeze(1).to_broadcast([128, 2, 256]),
                                                           op=ALU.mult), R=[("s4", i0), ("s4", i0 + 1), "pbc"], W=[("s4", i0), ("s4", i0 + 1)])
                proj_tm(wv, wk, 0, 256, 2, evo)

                for i in range(NT):
                    pt_, ptk = tbank()
                    P.pe(lambda e, i=i, pt_=pt_: e.transpose(pt_[:, 0:128], kT[:, til(i)], ident[:]), R=[("k", i // 4), "ident"], W=[ptk])
                    for d in range(2):
                        for hh in range(2):
                            P.dve(lambda e, i=i, d=d, hh=hh, pt_=pt_: e.tensor_scalar(
                                out=kw[:, d, i, hh, :], in0=pt_[:, hh * 64:(hh + 1) * 64], scalar1=wdec[:, d, i, h0 + hh:h0 + hh + 1], scalar2=0.125,
                                op0=ALU.mult, op1=ALU.mult), R=[ptk, ("wdec", d)], W=[("s2", i)])
                stage("Aproj")
                orders = [list(range(NT)), list(range(NT - 1, -1, -1))]
                for d in range(2):
                    P.pool(lambda e, d=d: e.memset(C32[:, d, :], 0.0), W=[("C32", d)])
                    P.pool(lambda e, d=d: e.memset(Cbf[:, d, 0, :], 0.0), W=[("Cbf", d, 0)])

                def a_early(d, n):
                    i = orders[d][n]
                    par = n % 2
                    psSs = [bank(), bank()]
                    for hh in range(2):
                        rs = slice(hh * 64, (hh + 1) * 64)
                        psS, psk = psSs[hh]
                        P.pe(lambda e: e.matmul(psS[:, 0:128], lhsT=kT[rs, til(i)], rhs=qT[rs, til(i)], start=True, stop=True),
                             R=[("k", i // 4), ("q", i // 4)], W=[psk])
                    for hh in range(2):
                        psS, psk = psSs[hh]
                        P.dve(lambda e: e.scalar_tensor_tensor(
                            out=Sm[:, d, par, hh, :], in0=psS[:, 0:128], scalar=wsc[:, d, i, h0 + hh:h0 + hh + 1],
                            in1=maskA[:, d, :], op0=ALU.mult, op1=ALU.mult),
                            R=[psk, ("wsc", d)] + CONSTS, W=[("Sm", d, par, hh)])

                def a_main(d, n):
                    i = orders[d][n]
                    par = n % 2
                    psU, puk = bank()
                    P.pe(lambda e: e.matmul(psU[0:64, 0:130], lhsT=kw[:, d, i, 0, :], rhs=vaug[:, i, 0, 0:130], start=True, stop=True),
                         R=[("s2", i), ("s3", i)], W=[puk])
                    P.pe(lambda e: e.matmul(psU[64:128, 0:130], lhsT=kw[:, d, i, 1, :], rhs=vaug[:, i, 1, 0:130], start=True, stop=True,
                                            tile_position=(0, 64)),
                         R=[("s2", i), ("s3", i)], W=[puk])
                    psO, pok = bank()
                    for hh in range(2):
                        rs = slice(hh * 64, (hh + 1) * 64)
                        P.pe(lambda e: e.matmul(psO[:, hh * 130:hh * 130 + 130], lhsT=Sm[:, d, par, hh, :],
                                                rhs=vaug[:, i, hh, 0:130], start=True, stop=False),
                             R=[("Sm", d, par, hh), ("s3", i)], W=[pok])
                        P.pe(lambda e: e.matmul(psO[:, hh * 130:hh * 130 + 130], lhsT=qT[rs, til(i)],
                                                rhs=Cbf[rs, d, par, 0:130], start=False, stop=True),
                             R=[("q", i // 4), ("Cbf", d, par)], W=[pok])
                    P.dve(lambda e: e.scalar_tensor_tensor(
                        out=C32[:, d, 0:130], in0=C32[:, d, 0:130], scalar=decsel[:, d, i:i + 1], in1=psU[:, 0:130], op0=ALU.mult, op1=ALU.add),
                        R=[("C32", d), ("decsel", 0), ("decsel", 1), puk], W=[("C32", d)])
                    P.pool(lambda e: e.tensor_copy(out=Cbf[:, d, 1 - par, 0:130], in_=C32[:, d, 0:130]),
                           R=[("C32", d)], W=[("Cbf", d, 1 - par)])
                    sm = sml[:, 1 + d, :]
                    P.act(lambda e: e.activation(out=sm[:, 0:2], in_=psO[:, 0:260].rearrange("p (h e) -> p h e", h=2)[:, :, 128], func=AF.Abs),
                          R=[pok], W=[("sml1a", d)])
                    aps[(n, d)] = (psO, pok)

                aps = {}

                def a_tail(n, d):
                    i = orders[d][n]
                    psO, pok = aps.pop((n, d))
                    sm = sml[:, 1 + d, :]
                    q = None
                    if n >= NT // 2:
                        q = acnt[0] % 2
                        acnt[0] += 1
                    P.dve(lambda e: e.tensor_tensor(out=sm[:, 2:4], in0=sm[:, 0:2], in1=enb[:, d, i, h0:h0 + 2], op=ALU.max),
                          R=[("sml1a", d), "enb"], W=[("sml1b", d)])
                    P.dve(lambda e: e.reciprocal(out=sm[:, 4:6], in_=sm[:, 2:4]), R=[("sml1b", d)], W=[("sml1c", d)])
                    for hh in range(2):
                        if n < NT // 2:
                            P.act(lambda e: e.activation(
                                out=hsum[:, i, hh * 128:(hh + 1) * 128], in_=psO[:, hh * 130:hh * 130 + 128], func=AF.Copy, scale=sm[:, 4 + hh:5 + hh]),
                                R=[pok, ("sml1c", d)], W=[("s5", i)])
                        else:
                            P.dve(lambda e: e.scalar_tensor_tensor(
                                out=tot2[:, q, hh * 128:(hh + 1) * 128], in0=psO[:, hh * 130:hh * 130 + 128], scalar=sm[:, 4 + hh:5 + hh],
                                in1=hsum[:, i, hh * 128:(hh + 1) * 128], op0=ALU.mult, op1=ALU.add),
                                R=[pok, ("sml1c", d), ("s5", i)], W=[("acc", q)])
                    return q

                acnt = [0]

                def a_core(n, d):
                    if n == 0:
                        a_early(d, 0)
                    if n + 1 < NT:
                        a_early(d, n + 1)
                    a_main(d, n)

                run_pipeline(a_core, a_tail, p, 0, sigo, lambda ti: ("s4", ti), hd1=1, hd2=2, td=0)

            stage("A")
            for p in range(2):
                h0 = 2 * p
                if p == 0:
                    (wv,), wk = load_w([wcols(w_in_d, l, 2992, 128)])

                    def evl(j, ps_, pk):
                        P.act(lambda e, j=j, ps_=ps_: e.copy(out=lrT[0:32, blk(j)], in_=ps_[0:32, :]), R=[pk], W=[("lrT", j)])
                    proj_fm(wv, wk, 96, 32, evl)
                (wq, wkk), wk = load_w([wcols(w_in_d, l, 1552 + p * 128, 128), wcols(w_in_d, l, 1808 + p * 128, 128)])
                for which, wv, dst in ((0, wq, qT), (1, wkk, kT)):
                    def ev(j, ps_, pk, dst=dst, which=which):
                        P.act(lambda e, j=j, ps_=ps_: e.copy(out=dst[:, blk(j)], in_=ps_[:, :]), R=[pk], W=[("q" if which == 0 else "k", j)])
                    proj_fm(wv, wk, 0, 128, ev)
                (wv,), wk = load_w([wcols(w_in_d, l, 2064 + p * 256, 256)])

                def evv(i0, ps_, pk):
                    P.act(lambda e, i0=i0, ps_=ps_: e.copy(out=bv[:, i0:i0 + 2, :].rearrange("p i e -> p (i e)"), in_=ps_[:, :]),
                          R=[pk], W=[("s2", i0), ("s2", i0 + 1)])
                proj_tm(wv, wk, 0, 256, 2, evv)
                (wv,), wk = load_w([wcols(w_in_d, l, 2576 + p * 256, 256)])

                def evr(i0, ps_, pk):
                    P.act(lambda e, i0=i0, ps_=ps_: e.activation(out=sr[:, i0:i0 + 2, :].rearrange("p i e -> p (i e)"), in_=ps_[:, :],
                                                                func=AF.Silu), R=[pk], W=[("s3", i0), ("s3", i0 + 1)])
                    P.pool(lambda e, i0=i0: e.tensor_tensor(out=sr[:, i0:i0 + 2, :], in0=sr[:, i0:i0 + 2, :],
                                                           in1=pbc[:, 528 + h0 * 128:528 + h0 * 128 + 256].unsqueeze(1).to_broadcast([128, 2, 256]),
                                                           op=ALU.mult), R=[("s3", i0), ("s3", i0 + 1), "pbc"], W=[("s3", i0), ("s3", i0 + 1)])
                proj_tm(wv, wk, 0, 256, 2, evr)
                for i0 in range(0, NT, 2):
                    ps_, pk = bank()
                    for ii in range(2):
                        i = i0 + ii
                        for d in range(2):
                            P.pe(lambda e, i=i, ii=ii, d=d, ps_=ps_: e.matmul(ps_[:, ii * 256 + d * 128:ii * 256 + (d + 1) * 128], lhsT=lrT[0:33, til(i)],
                                                                              rhs=w2bd[0:33, d * 256 + p * 128:d * 256 + (p + 1) * 128], start=True, stop=True),
                                 R=[("lrT", i // 4), "lrT1", "w2bd"], W=[pk])
                    g_ = acc[:, (i0 // 2) % 2, :]
                    P.act(lambda e, ps_=ps_, g_=g_: e.activation(out=g_, in_=ps_[:, :], func=AF.Exp, scale=-1.0), R=[pk], W=[("acc", (i0 // 2) % 2)])
                    P.act(lambda e, i0=i0, g_=g_: e.activation(out=la[:, i0:i0 + 2, :].rearrange("p i e -> p (i e)"), in_=g_, func=AF.Ln, bias=1.0),
                          R=[("acc", (i0 // 2) % 2)], W=[("s4", i0), ("s4", i0 + 1)])
                stage("Bproj")
                orders = [list(range(NT)), list(range(NT - 1, -1, -1))]
                for d in range(2):
                    P.pool(lambda e, d=d: e.memset(C32[:, d, :], 0.0), W=[("C32", d)])
                    P.pool(lambda e, d=d: e.memset(Cbf[:, d, 0, :], 0.0), W=[("Cbf", d, 0)])

                def b_early(d, n):
                    i = orders[d][n]
                    par = n % 2
                    psB, pbk2 = bank()
                    P.pe(lambda e: e.matmul(psB[:, 0:128], lhsT=la[:, i, d * 128:(d + 1) * 128], rhs=tri16[:, d, :], start=True, stop=True),
                         R=[("s4", i)] + CONSTS, W=[pbk2])
                    P.act(lambda e: e.activation(out=eBt[:, d, par, :], in_=psB[:, 0:128], func=AF.Exp), R=[pbk2], W=[("eBt", d, par)])
                    P.act(lambda e: e.activation(out=eNBt[:, d, :], in_=psB[:, 0:128], func=AF.Exp, scale=-1.0), R=[pbk2], W=[("eNBt", d)])
                    P.pool(lambda e: e.tensor_tensor(out=qs[:, d, par, :], in0=qT[:, til(i)], in1=eBt[:, d, par, :], op=ALU.mult),
                           R=[("q", i // 4), ("eBt", d, par)], W=[("qs", d, par)])
                    P.dve(lambda e: e.tensor_tensor(out=ks[:, d, par, :], in0=kT[:, til(i)], in1=eNBt[:, d, :], op=ALU.mult),
                          R=[("k", i // 4), ("eNBt", d)], W=[("ks", d, par)])
                    psSs = [bank(), bank()]
                    for hh in range(2):
                        rs = slice(hh * 64, (hh + 1) * 64)
                        psS, psk = psSs[hh]
                        P.pe(lambda e: e.matmul(psS[:, 0:128], lhsT=ks[rs, d, par, :], rhs=qs[rs, d, par, :], start=True, stop=True),
                             R=[("ks", d, par), ("qs", d, par)], W=[psk])
                    pt_, ptk = tbank()
                    P.pe(lambda e: e.transpose(pt_[:, 0:128], ks[:, d, par, :], ident[:]), R=[("ks", d, par), "ident"], W=[ptk])
                    for hh in range(2):
                        psS, psk = psSs[hh]
                        P.dve(lambda e: e.tensor_tensor(out=Sm[:, d, par, hh, :], in0=psS[:, 0:128], in1=maskB[:, d, :], op=ALU.mult),
                              R=[psk] + CONSTS, W=[("Sm", d, par, hh)])
                    P.act(lambda e: e.activation(out=ktok[:, d, par, :], in_=pt_[:, 0:128], func=AF.Copy, scale=0.125), R=[ptk], W=[("ktok", d, par)])

                def b_main(d, n):
                    i = orders[d][n]
                    par = n % 2
                    lastcol = 127 if d == 0 else 0
                    psU, puk = bank()
                    P.pe(lambda e: e.matmul(psU[0:64, 0:128], lhsT=ktok[:, d, par, 0:64], rhs=bv[:, i, 0:128], start=True, stop=True),
                         R=[("ktok", d, par), ("s2", i)], W=[puk])
                    P.pe(lambda e: e.matmul(psU[64:128, 0:128], lhsT=ktok[:, d, par, 64:128], rhs=bv[:, i, 128:256], start=True, stop=True,
                                            tile_position=(0, 64)), R=[("ktok", d, par), ("s2", i)], W=[puk])
                    psO, pok = bank()
                    for hh in range(2):
                        rs = slice(hh * 64, (hh + 1) * 64)
                        P.pe(lambda e: e.matmul(psO[:, hh * 128:(hh + 1) * 128], lhsT=Sm[:, d, par, hh, :],
                                                rhs=bv[:, i, hh * 128:(hh + 1) * 128], start=True, stop=False),
                             R=[("Sm", d, par, hh), ("s2", i)], W=[pok])
                        P.pe(lambda e: e.matmul(psO[:, hh * 128:(hh + 1) * 128], lhsT=qs[rs, d, par, :],
                                                rhs=Cbf[rs, d, par, 0:128], start=False, stop=True),
                             R=[("qs", d, par), ("Cbf", d, par)], W=[pok])
                    P.act(lambda e: e.activation(out=tmpS[:, d, :], in_=psU[:, 0:128], func=AF.Copy, scale=eBt[:, d, par, lastcol:lastcol + 1]),
                          R=[puk, ("eBt", d, par)], W=[("tmpS", d)])
                    P.dve(lambda e: e.scalar_tensor_tensor(out=C32[:, d, 0:128], in0=C32[:, d, 0:128], scalar=eBt[:, d, par, lastcol:lastcol + 1],
                                                           in1=tmpS[:, d, :], op0=ALU.mult, op1=ALU.add),
                          R=[("tmpS", d), ("eBt", d, par), ("C32", d)], W=[("C32", d)])
                    P.pool(lambda e: e.tensor_copy(out=Cbf[:, d, 1 - par, 0:128], in_=C32[:, d, 0:128]),
                           R=[("C32", d)], W=[("Cbf", d, 1 - par)])
                    if n < NT // 2:
                        P.act(lambda e: e.copy(out=hsum[:, i, :], in_=psO[:, 0:256]), R=[pok], W=[("s5", i)])
                    else:
                        q = bcnt[0] % 2
                        bcnt[0] += 1
                        bq[(n, d)] = q
                        P.dve(lambda e: e.tensor_tensor(out=tot2[:, q, :], in0=psO[:, 0:256], in1=hsum[:, i, :], op=ALU.add),
                              R=[pok, ("s5", i)], W=[("acc", q)])

                bcnt = [0]
                bq = {}

                def b_core(n, d):
                    if n == 0:
                        b_early(d, 0)
                    if n + 1 < NT:
                        b_early(d, n + 1)
                    b_main(d, n)

                run_pipeline(b_core, lambda n, d: bq.get((n, d)), p, 1, sr, lambda ti: ("s3", ti), hd1=1, hd2=2, td=0)

            stage("B")
            if dbg and "yT" in dbg and l == 0:
                P.barrier()
                for c in range(KC):
                    P.act(lambda e, c=c: e.copy(out=xT[:, c, :], in_=yT[:, c, :]), R=[], W=[("xT", c, j) for j in range(NB)])
                P.barrier()

            P.barrier()
            for q4 in range(4):
                P.dma("pool", lambda e, q4=q4, l=l: e.dma_start(out=wo[:, :, q4 * 256:(q4 + 1) * 256],
                                                                in_=w_out_d[l, :, q4 * 256:(q4 + 1) * 256].rearrange("(c p) n -> p c n", p=128)),
                      W=["wo"])
            for j in range(NB):
                for co in range(KC):
                    ps_, pk = bank()
                    for ch in range(KC):
                        P.pe(lambda e, co=co, ch=ch, j=j, ps_=ps_: e.matmul(ps_[:, :], lhsT=wo[:, ch, co * 128:(co + 1) * 128], rhs=yview(ch)[:, blk(j)],
                                                                            start=(ch == 0), stop=(ch == KC - 1)),
                             R=["wo"] + [ykey(ch, i) for i in range(4 * j, 4 * j + 4)], W=[pk])
                    P.act(lambda e, co=co, ps_=ps_: e.copy(out=mixT[:, co, :], in_=ps_[:, :]), R=[pk], W=[("mixT", co)])
                post_norm_residual(l, 8, mixT, lambda c: ("mixT", c), j * 512, j)
                if j >= 1:
                    pre_norm_block(l, 16, j - 1)
            pre_norm_block(l, 16, NB - 1)

            stage("wout")
            P.pool(lambda e: e.tensor_copy(out=hhalo[:, :, :], in_=hnT[:, :, 1023:1025]),
                   R=[("hnT", c, j) for c in range(KC) for j in (1, 2)], W=["hhalo"])
            P.barrier()
            ffT_bf = arena[:, NF * 1024:NF * 1024 + 2 * KC * 512]
            gu_pool = [(wbuf[0][:, :], [("wbuf", 0)]), (wbuf[1][:, :], [("wbuf", 1)])] + \
                      [(ffT_bf[:, j * 2048:(j + 1) * 2048], [("ffT", 2 * j), ("ffT", 2 * j + 1)]) for j in range(4)]
            gurot = [0]
            for H in range(2):
                t0 = H * 1024
                graws = [graw, pbc[:, 0:1026]]
                wst = {}

                def G(f):
                    gr = graws[f % 2]
                    (wgv, wuv), wks = load_into(gu_pool[gurot[0] % len(gu_pool)], [wcols(wg_d, l, f * 128, 128), wcols(wu_d, l, f * 128, 128)])
                    gurot[0] += 1
                    wst[f] = (wuv, wks)
                    if H == 0:
                        P.pool(lambda e: e.memset(gr[:, 0:1], 0.0), W=[("graw", f % 2, 0)])
                        halo_col, hsel = 1025, 1
                    else:
                        P.pool(lambda e: e.memset(gr[:, 1025:1026], 0.0), W=[("graw", f % 2, 1)])
                        halo_col, hsel = 0, 0
                    psh, phk = bank()
                    for c in range(KC):
                        P.pe(lambda e: e.matmul(psh[:, 0:1], lhsT=wgv[:, c, :], rhs=hhalo[:, c, hsel:hsel + 1],
                                                start=(c == 0), stop=(c == KC - 1)), R=wks + ["hhalo"], W=[phk])
                    P.act(lambda e: e.copy(out=gr[:, halo_col:halo_col + 1], in_=psh[:, 0:1]),
                          R=[phk], W=[("graw", f % 2, 0 if halo_col == 0 else 1)])
                    for b in range(2):
                        jj = 2 * H + b
                        ps_, pk = bank()
                        for c in range(KC):
                            P.pe(lambda e: e.matmul(ps_[:, :], lhsT=wgv[:, c, :], rhs=hnT[:, c, blk(jj)],
                                                    start=(c == 0), stop=(c == KC - 1)), R=wks + [("hnT", c, jj)], W=[pk])
                        P.act(lambda e: e.copy(out=gr[:, 1 + b * 512:1 + (b + 1) * 512], in_=ps_[:, :]), R=[pk], W=[("grawb", f % 2, b)])

                def C(f):
                    gr = graws[f % 2]
                    cb = 48 + f * 4
                    rk = [("graw", f % 2, 0), ("graw", f % 2, 1), ("grawb", f % 2, 0), ("grawb", f % 2, 1)]
                    for b in range(2):
                        a_ = acc[:, b, :]
                        P.dve(lambda e: e.tensor_scalar(out=a_, in0=gr[:, 1 + b * 512:1 + (b + 1) * 512],
                                                        scalar1=pcol[:, cb + 1:cb + 2], scalar2=pcol[:, cb + 3:cb + 4],
                                                        op0=ALU.mult, op1=ALU.add), R=rk + ["pcol"], W=[("acc", b)])
                        P.dve(lambda e: e.scalar_tensor_tensor(out=a_, in0=gr[:, b * 512:(b + 1) * 512],
                                                               scalar=pcol[:, cb:cb + 1], in1=a_, op0=ALU.mult, op1=ALU.add),
                              R=rk + ["pcol", ("acc", b)], W=[("acc", b)])
                        P.dve(lambda e: e.scalar_tensor_tensor(out=a_, in0=gr[:, 2 + b * 512:2 + (b + 1) * 512],
                                                               scalar=pcol[:, cb + 2:cb + 3], in1=a_, op0=ALU.mult, op1=ALU.add),
                              R=rk + ["pcol", ("acc", b)], W=[("acc", b)])
                        P.act(lambda e: e.activation(out=gel[:, b, :], in_=a_, func=AF.Gelu_apprx_tanh), R=[("acc", b)], W=[("gel", b)])

                def U(f):
                    wuv, wks = wst.pop(f)
                    for b in range(2):
                        jj = 2 * H + b
                        ps_, pk = bank()
                        for c in range(KC):
                            P.pe(lambda e: e.matmul(ps_[:, :], lhsT=wuv[:, c, :], rhs=hnT[:, c, blk(jj)],
                                                    start=(c == 0), stop=(c == KC - 1)), R=wks + [("hnT", c, jj)], W=[pk])
                        P.dve(lambda e: e.tensor_tensor(out=actT[:, f, b * 512:(b + 1) * 512], in0=gel[:, b, :], in1=ps_[:, :], op=ALU.mult),
                              R=[("gel", b), pk], W=[("actT", f, b)])

                G(0)
                for f in range(NF):
                    if f + 1 < NF:
                        G(f + 1)
                    C(f)
                    U(f)

                d_pool = [(hnT[:, c, t0:t0 + 1024], [("hnT", c, 2 * H), ("hnT", c, 2 * H + 1)]) for c in range(KC)]
                drot = 0
                for b in range(2):
                    for co in range(KC):
                        parts = []
                        for (fa, fb) in ((0, 8), (8, 16), (16, 22)):
                            (wv_,), wk_ = load_into(d_pool[drot % KC], [wd_d[l, fa * 128:fb * 128, co * 128:(co + 1) * 128].rearrange("(f p) n -> p f n", p=128)])
                            drot += 1
                            parts.append((fa, fb, wv_, wk_))
                        ps_, pk = bank()
                        for f in range(NF):
                            fa, fb, wdv, wk_ = parts[f // 8]
                            P.pe(lambda e: e.matmul(ps_[:, :], lhsT=wdv[:, f - fa, :], rhs=actT[:, f, b * 512:(b + 1) * 512],
                                                    start=(f == 0), stop=(f == NF - 1)), R=wk_ + [("actT", f, b)], W=[pk])
                        P.act(lambda e, co=co, ps_=ps_: e.copy(out=ffT[:, co, :], in_=ps_[:, :]), R=[pk], W=[("ffT", co)])
                    post_norm_residual(l, 24, ffT, lambda c: ("ffT", c), t0 + b * 512, 2 * H + b)
            P.barrier()

        try:
            stage("load")
            for l in range(nl):
                layer(l)
        except _Stop:
            pass
        for c in range(KC):
            P.dma("sp", lambda e, c=c: e.dma_start(out=outT_d[c * 128:(c + 1) * 128, :], in_=xT[:, c, :]),
                  R=[("xT", c, j) for j in range(NB)], W=[("out", c)])
        P.add("sp", None, R=[("out", c) for c in range(KC)])
        P.emit(nc)
    return nc


def _pack_params(ls, norm_mix_pre, norm_mix_post, norm_ffn_pre, norm_ffn_post, mlstm_conv_w, mlstm_conv_b,
                 ffn_conv_w, ffn_conv_b, mlstm_gate_b, mlstm_norm, gla_norm):
    nl = len(ls)
    pcol = np.zeros((128, nl * PCOLS), np.float32)
    pbc = np.zeros((nl, PBC), np.float32)
    for li, l in enumerate(ls):
        o = li * PCOLS
        for gi, g in enumerate((norm_mix_pre, norm_mix_post, norm_ffn_pre, norm_ffn_post)):
            pcol[:, o + gi * 8:o + gi * 8 + 8] = g[l].reshape(8, 128).T
        for cj in range(4):
            pcol[:, o + 32 + cj * 4:o + 32 + cj * 4 + 3] = mlstm_conv_w[l][:, cj * 128:(cj + 1) * 128].T
            pcol[:, o + 32 + cj * 4 + 3] = mlstm_conv_b[l][cj * 128:(cj + 1) * 128]
        for f in range(NF):
            pcol[:, o + 48 + f * 4:o + 48 + f * 4 + 3] = ffn_conv_w[l][:, f * 128:(f + 1) * 128].T
            pcol[:, o + 48 + f * 4 + 3] = ffn_conv_b[l][f * 128:(f + 1) * 128]
        pbc[li, 0:16] = mlstm_gate_b[l]
        pbc[li, 16:528] = mlstm_norm[l].reshape(-1)
        pbc[li, 528:1040] = gla_norm[l].reshape(-1)
    return pcol, pbc


_CACHE = {}


def _get_prog(nl):
    if nl not in _CACHE:
        _CACHE[nl] = build_program(nl)
    return _CACHE[nl]


FUSED = True


def kernel(x, norm_mix_pre, norm_mix_post, norm_ffn_pre, norm_ffn_post, w_in, mlstm_gate_b, mlstm_conv_w,
           mlstm_conv_b, mlstm_norm, gla_w2, gla_b, gla_norm, w_out, ffn_w_gate, ffn_w_up, ffn_conv_w,
           ffn_conv_b, ffn_w_down):
    f = lambda a: np.ascontiguousarray(np.asarray(a), dtype=np.float32)
    x = f(x)
    args = [f(a) for a in (norm_mix_pre, norm_mix_post, norm_ffn_pre, norm_ffn_post, mlstm_conv_w, mlstm_conv_b,
                           ffn_conv_w, ffn_conv_b, mlstm_gate_b, mlstm_norm, gla_norm)]
    w_in, w_out, wg, wu, wd = f(w_in), f(w_out), f(ffn_w_gate), f(ffn_w_up), f(ffn_w_down)
    w2, gb = f(gla_w2), f(gla_b)
    xTs = [np.ascontiguousarray(x[b].T) for b in range(NCORES)]
    groups = [list(range(DEPTH))] if FUSED else [[l] for l in range(DEPTH)]
    for ls in groups:
        nl = len(ls)
        nc = _get_prog(nl)
        pcol, pbc = _pack_params(ls, *args)
        sl = slice(ls[0], ls[-1] + 1)
        shared = {"w_in": w_in[sl], "w_out": w_out[sl], "wg": wg[sl], "wu": wu[sl], "wd": wd[sl], "pcol": pcol, "pbc": pbc,
                  "w2": w2[sl], "gb": np.ascontiguousarray(gb[sl].reshape(nl, 512))}
        in_maps = [dict(shared, xT=xTs[b]) for b in range(NCORES)]
        res = run_bass_kernel_spmd(nc, in_maps, core_ids=list(range(NCORES)))
        xTs = [np.asarray(r["outT"]) for r in res.results]
    return np.stack([np.ascontiguousarray(t.T) for t in xTs], axis=0).astype(np.float32)
```
